# Optimizing a Trainium2 kernel written in Bass

```python
import math
import jax
import jax.numpy as jnp
from jax import lax
import numpy as np

D_MODEL = 1024
BATCH = 8
SEQ = 2048
DEPTH = 4

N_MIXERS = 3
RMS_EPS = 1e-6
LRU_WIDTH = D_MODEL
LRU_BLOCKS = 8
LRU_BLOCK_W = LRU_WIDTH // LRU_BLOCKS
CONV_WIDTH = 4
LRU_C = 8.0
POOL_WINDOWS = (2, 4, 8, 16)
POOL_GROUPS = len(POOL_WINDOWS)
POOL_GROUP_W = D_MODEL // POOL_GROUPS
SB_HEADS = 16
SB_HEAD_DIM = D_MODEL // SB_HEADS
SB_BLOCK = 128
N_EXPERTS = 32
TOP_K = 4
D_EXPERT = D_MODEL
SWIGLU_LIMIT = 7.0
SWIGLU_ALPHA = 1.702
MOE_BLOCK = 256
N_LRU_LAYERS = len(range(0, DEPTH, N_MIXERS))
N_POOL_LAYERS = len(range(1, DEPTH, N_MIXERS))
N_SB_LAYERS = len(range(2, DEPTH, N_MIXERS))

kernel_name = 'hybrid_lru_pool_stickbreak_moe'


def rms_norm(x, g):
    xf = x.astype(jnp.float32)
    y = xf * lax.rsqrt(jnp.mean(xf * xf, axis=-1, keepdims=True) + RMS_EPS)
    return (y * g.astype(jnp.float32)).astype(x.dtype)


def causal_depthwise_conv(x, w, b):
    k_w = w.shape[0]
    s = x.shape[1]
    xp = jnp.pad(x, ((0, 0), (k_w - 1, 0), (0, 0)))
    y = b
    for k in range(k_w):
        y = y + xp[:, k:k + s] * w[k]
    return y


def block_diag_linear(x, w, b):
    g, cg, _ = w.shape
    xg = x.reshape(x.shape[:-1] + (g, cg))
    return jnp.einsum('bsgi,gio->bsgo', xg, w).reshape(x.shape) + b


def rg_lru(x, w_a, b_a, w_x, b_x, a_param):
    xf = x.astype(jnp.float32)
    r = jax.nn.sigmoid(block_diag_linear(x, w_a, b_a).astype(jnp.float32))
    i = jax.nn.sigmoid(block_diag_linear(x, w_x, b_x).astype(jnp.float32))
    log_a = -LRU_C * r * jax.nn.softplus(-a_param.astype(jnp.float32))
    a = jnp.exp(log_a)
    b_in = jnp.sqrt(-jnp.expm1(2.0 * log_a)) * (i * xf)

    def combine(left, right):
        a_l, b_l = left
        a_r, b_r = right
        return a_l * a_r, a_r * b_l + b_r

    _, h = lax.associative_scan(combine, (a, b_in), axis=1)
    return h.astype(x.dtype)


def lru_mixer(u, w_in, conv_w, conv_b, w_a, b_a, w_x, b_x, a_param, w_out):
    proj = u @ w_in
    gate, xb = proj[..., :LRU_WIDTH], proj[..., LRU_WIDTH:]
    xb = causal_depthwise_conv(xb, conv_w, conv_b)
    y = rg_lru(xb, w_a, b_a, w_x, b_x, a_param)
    y = y * jax.nn.gelu(gate)
    return y @ w_out


def pool_mixer(u, w_in, w_group, scale, w_out):
    v = u @ w_in
    vf = v.astype(jnp.float32)
    s = v.shape[1]
    csum = jnp.cumsum(vf, axis=1)
    n_avail = jnp.arange(1, s + 1, dtype=jnp.float32)[None, :, None]
    outs = []
    for g, win in enumerate(POOL_WINDOWS):
        lo, hi = g * POOL_GROUP_W, (g + 1) * POOL_GROUP_W
        c = csum[..., lo:hi]
        lag = jnp.pad(c, ((0, 0), (win, 0), (0, 0)))[:, :s]
        mean = (c - lag) / jnp.minimum(n_avail, float(win))
        outs.append(mean - vf[..., lo:hi])
    pooled = jnp.concatenate(outs, axis=-1).astype(v.dtype)
    pg = pooled.reshape(pooled.shape[:-1] + (POOL_GROUPS, POOL_GROUP_W))
    mixed = jnp.einsum('bsgi,gio->bsgo', pg, w_group).reshape(pooled.shape)
    return (mixed * scale) @ w_out


def stick_breaking_attention(q, k, v):
    s = q.shape[2]
    scale = 1.0 / math.sqrt(q.shape[-1])
    outs = []
    for blk in range(s // SB_BLOCK):
        q0 = blk * SB_BLOCK
        kv_len = q0 + SB_BLOCK
        q_b = q[:, :, q0:q0 + SB_BLOCK]
        k_b = k[:, :, :kv_len]
        v_b = v[:, :, :kv_len]
        z = jnp.einsum('bhqd,bhkd->bhqk', q_b, k_b).astype(jnp.float32) * scale
        t_pos = q0 + jnp.arange(SB_BLOCK)[:, None]
        s_pos = jnp.arange(kv_len)[None, :]
        mask = s_pos < t_pos
        log_1m = jnp.where(mask, jax.nn.log_sigmoid(-z), 0.0)
        suffix = lax.cumsum(log_1m, axis=3, reverse=True) - log_1m
        a = jnp.where(mask, jnp.exp(jax.nn.log_sigmoid(z) + suffix), 0.0)
        outs.append(jnp.einsum('bhqk,bhkd->bhqd', a.astype(v.dtype), v_b))
    return jnp.concatenate(outs, axis=2)


def sb_mixer(u, w_qkv, w_out):
    b, s, d = u.shape
    qkv = (u @ w_qkv).reshape(b, s, 3, SB_HEADS, SB_HEAD_DIM)
    qkv = qkv.transpose(2, 0, 3, 1, 4)
    o = stick_breaking_attention(qkv[0], qkv[1], qkv[2])
    o = o.transpose(0, 2, 1, 3).reshape(b, s, d)
    return o @ w_out


def moe(x2d, w_router, b_router, w_gate_up, b_gate_up, w_down, b_down):
    t, d = x2d.shape
    logits = (x2d @ w_router + b_router).astype(jnp.float32)
    top_vals, top_idx = lax.top_k(logits, TOP_K)
    gates = jax.nn.softmax(top_vals, axis=-1)
    tk = t * TOP_K
    flat_e = top_idx.reshape(-1).astype(jnp.int32)
    flat_tok = jnp.repeat(jnp.arange(t, dtype=jnp.int32), TOP_K)
    flat_gate = gates.reshape(-1)
    order = jnp.argsort(flat_e)
    sorted_e = flat_e[order]
    sorted_tok = flat_tok[order]
    sorted_gate = flat_gate[order]
    counts = jnp.bincount(flat_e, length=N_EXPERTS).astype(jnp.int32)
    starts = jnp.cumsum(counts) - counts
    padded_counts = (counts + MOE_BLOCK - 1) // MOE_BLOCK * MOE_BLOCK
    padded_ends = jnp.cumsum(padded_counts)
    padded_starts = padded_ends - padded_counts
    dest = padded_starts[sorted_e] + (jnp.arange(tk, dtype=jnp.int32) - starts[sorted_e])
    n_blocks = -(-tk // MOE_BLOCK) + N_EXPERTS
    n_slots = n_blocks * MOE_BLOCK
    slot_tok = jnp.zeros((n_slots,), jnp.int32).at[dest].set(sorted_tok)
    slot_gate = jnp.zeros((n_slots,), jnp.float32).at[dest].set(sorted_gate)
    block_starts = jnp.arange(n_blocks, dtype=jnp.int32) * MOE_BLOCK
    block_expert = jnp.minimum(jnp.searchsorted(padded_ends, block_starts, side='right'),
                               N_EXPERTS - 1).astype(jnp.int32)
    xs = x2d[slot_tok].reshape(n_blocks, MOE_BLOCK, d)

    def expert_block(args):
        xb, e = args
        gu = xb @ w_gate_up[e] + b_gate_up[e]
        gate = jnp.minimum(gu[..., :D_EXPERT], SWIGLU_LIMIT)
        up = jnp.clip(gu[..., D_EXPERT:], -SWIGLU_LIMIT, SWIGLU_LIMIT)
        glu = gate * jax.nn.sigmoid(gate * SWIGLU_ALPHA)
        return ((up + 1.0) * glu) @ w_down[e] + b_down[e]

    ys = lax.map(expert_block, (xs, block_expert)).reshape(n_slots, d)
    out = jnp.zeros((t, d), jnp.float32).at[slot_tok].add(ys.astype(jnp.float32) * slot_gate[:, None])
    return out.astype(x2d.dtype)


def setup_inputs(seed: int = 0) -> dict:
    key = jax.random.key(seed)
    ks = jax.random.split(key, 32)

    def nrm(i, shape, scale):
        return jax.random.normal(ks[i], shape, jnp.float32) * scale

    d, w, f, e = D_MODEL, LRU_WIDTH, D_EXPERT, N_EXPERTS
    out_scale = (2 * DEPTH) ** -0.5
    a0 = jax.random.uniform(ks[31], (N_LRU_LAYERS, w), jnp.float32, 0.9, 0.999)
    return {
        'x': nrm(0, (BATCH, SEQ, d), 1.0),
        'mix_norm': 1.0 + nrm(1, (DEPTH, d), 0.02),
        'ffn_norm': 1.0 + nrm(2, (DEPTH, d), 0.02),
        'final_norm': 1.0 + nrm(3, (d,), 0.02),
        'lru_w_in': nrm(4, (N_LRU_LAYERS, d, 2 * w), d ** -0.5),
        'lru_conv_w': nrm(5, (N_LRU_LAYERS, CONV_WIDTH, w), CONV_WIDTH ** -0.5),
        'lru_conv_b': nrm(6, (N_LRU_LAYERS, w), 0.01),
        'lru_w_a': nrm(7, (N_LRU_LAYERS, LRU_BLOCKS, LRU_BLOCK_W, LRU_BLOCK_W), LRU_BLOCK_W ** -0.5),
        'lru_b_a': nrm(8, (N_LRU_LAYERS, w), 0.01),
        'lru_w_x': nrm(9, (N_LRU_LAYERS, LRU_BLOCKS, LRU_BLOCK_W, LRU_BLOCK_W), LRU_BLOCK_W ** -0.5),
        'lru_b_x': nrm(10, (N_LRU_LAYERS, w), 0.01),
        'lru_a_param': jnp.log(a0) - jnp.log1p(-a0),
        'lru_w_out': nrm(11, (N_LRU_LAYERS, w, d), w ** -0.5 * out_scale),
        'pool_w_in': nrm(12, (N_POOL_LAYERS, d, d), d ** -0.5),
        'pool_w_group': nrm(13, (N_POOL_LAYERS, POOL_GROUPS, POOL_GROUP_W, POOL_GROUP_W), POOL_GROUP_W ** -0.5),
        'pool_scale': 1.0 + nrm(14, (N_POOL_LAYERS, d), 0.1),
        'pool_w_out': nrm(15, (N_POOL_LAYERS, d, d), d ** -0.5 * out_scale),
        'sb_w_qkv': nrm(16, (N_SB_LAYERS, d, 3 * d), d ** -0.5),
        'sb_w_out': nrm(17, (N_SB_LAYERS, d, d), d ** -0.5 * out_scale),
        'moe_w_router': nrm(18, (DEPTH, d, e), d ** -0.5),
        'moe_b_router': nrm(19, (DEPTH, e), 0.01),
        'moe_w_gate_up': nrm(20, (DEPTH, e, d, 2 * f), d ** -0.5),
        'moe_b_gate_up': nrm(21, (DEPTH, e, 2 * f), 0.01),
        'moe_w_down': nrm(22, (DEPTH, e, f, d), f ** -0.5 * out_scale),
        'moe_b_down': nrm(23, (DEPTH, e, d), 0.01),
    }


def reference(x, mix_norm, ffn_norm, final_norm,
              lru_w_in, lru_conv_w, lru_conv_b, lru_w_a, lru_b_a, lru_w_x, lru_b_x,
              lru_a_param, lru_w_out,
              pool_w_in, pool_w_group, pool_scale, pool_w_out,
              sb_w_qkv, sb_w_out,
              moe_w_router, moe_b_router, moe_w_gate_up, moe_b_gate_up, moe_w_down, moe_b_down):
    b, s, d = x.shape
    h = x
    for layer in range(DEPTH):
        kind = layer % N_MIXERS
        slot = layer // N_MIXERS
        u = rms_norm(h, mix_norm[layer])
        if kind == 0:
            m = lru_mixer(u, lru_w_in[slot], lru_conv_w[slot], lru_conv_b[slot],
                          lru_w_a[slot], lru_b_a[slot], lru_w_x[slot], lru_b_x[slot],
                          lru_a_param[slot], lru_w_out[slot])
        elif kind == 1:
            m = pool_mixer(u, pool_w_in[slot], pool_w_group[slot], pool_scale[slot], pool_w_out[slot])
        else:
            m = sb_mixer(u, sb_w_qkv[slot], sb_w_out[slot])
        h = h + m
        u = rms_norm(h, ffn_norm[layer]).reshape(b * s, d)
        h = h + moe(u, moe_w_router[layer], moe_b_router[layer], moe_w_gate_up[layer],
                    moe_b_gate_up[layer], moe_w_down[layer], moe_b_down[layer]).reshape(b, s, d)
    return rms_norm(h, final_norm)
```

```python
import numpy as np
from contextlib import ExitStack
import concourse.bass as bass
import concourse.mybir as mybir
from concourse.bass_utils import run_bass_kernel_spmd

F32 = mybir.dt.float32
BF16 = mybir.dt.bfloat16
I32 = mybir.dt.int32
U32 = mybir.dt.uint32
AF = mybir.ActivationFunctionType
ALU = mybir.AluOpType
AX = mybir.AxisListType

D = 1024
S = 2048
NCH = S // 128
NDC = D // 128
NE = 32
TOPK = 4
CAP = 384
NSLOT = NE * CAP
RMS_EPS = 1e-6
SAME_ENG_SYNC = True


class Buf:
    __slots__ = ("name", "w", "r")

    def __init__(self, name):
        self.name = name
        self.w = None
        self.r = {}


class Prog:
    def __init__(self, nc, stack, n_dsem=(20, 8, 20)):
        self.nc = nc
        self.eng = {"pe": nc.tensor, "dve": nc.vector, "act": nc.scalar,
                    "pool": nc.gpsimd, "sp": nc.sync}
        self.esem = {k: stack.enter_context(nc.semaphore("es_" + k)) for k in self.eng}
        self.ecnt = {k: 0 for k in self.eng}
        self.waited = {k: {} for k in self.eng}
        self.dq = {}
        for q, n in zip(("sp", "act", "pool"), n_dsem):
            sems = [stack.enter_context(nc.semaphore(f"ds_{q}{i}")) for i in range(n)]
            self.dq[q] = {"sems": sems, "cnt": [0] * n, "next": 0}
        self.n_instr = 0

    def _wait(self, e, evs):
        for ev in evs:
            if ev is None:
                continue
            key, sem, val = ev
            if not SAME_ENG_SYNC and key == e:
                continue
            if key == e and e == "pe":
                continue
            if self.waited[e].get(key, 0) >= val:
                continue
            self.eng[e].wait_ge(sem, val)
            self.waited[e][key] = val

    def _deps(self, reads, writes):
        evs = []
        for b in reads:
            evs.append(b.w)
        for b in writes:
            evs.append(b.w)
            evs.extend(b.r.values())
        return evs

    def _commit(self, ev, reads, writes):
        for b in reads:
            old = b.r.get(ev[0])
            if old is None or old[2] < ev[2]:
                b.r[ev[0]] = ev
        for b in writes:
            b.w = ev
            b.r = {}

    def op(self, e, fn, reads=(), writes=()):
        self._wait(e, self._deps(reads, writes))
        ins = fn(self.eng[e])
        self.ecnt[e] += 1
        ins.then_inc(self.esem[e], 1)
        ev = (e, self.esem[e], self.ecnt[e])
        self._commit(ev, reads, writes)
        self.n_instr += 1
        return ev

    def dma(self, q, fn, reads=(), writes=()):
        d = self.dq[q]
        i = d["next"]
        d["next"] = (i + 1) % len(d["sems"])
        key = f"d_{q}{i}"
        evs = self._deps(reads, writes)
        if d["cnt"][i] > 0:
            evs.append((key, d["sems"][i], d["cnt"][i]))
        self._wait(q, evs)
        ins = fn(self.eng[q])
        d["cnt"][i] += 16
        ins.then_inc(d["sems"][i], 16)
        ev = (key, d["sems"][i], d["cnt"][i])
        self._commit(ev, reads, writes)
        self.n_instr += 1
        return ev

    def barrier(self):
        evs = [(k, self.esem[k], self.ecnt[k]) for k in self.eng if self.ecnt[k] > 0]
        for q, d in self.dq.items():
            for i, sm_ in enumerate(d["sems"]):
                if d["cnt"][i] > 0:
                    evs.append((f"d_{q}{i}", sm_, d["cnt"][i]))
        for e in self.eng:
            self._wait(e, [ev for ev in evs if ev[0] != e])

    def wait_all(self, e, bufs):
        evs = []
        for b in bufs:
            evs.append(b.w)
            evs.extend(b.r.values())
        self._wait(e, evs)


class Ctx:
    def __init__(self, nc, stack):
        self.nc = nc
        self.stack = stack
        self.bufs = {}

    _uid = [0]

    def sb(self, name, shape, dt):
        Ctx._uid[0] += 1
        t = self.stack.enter_context(self.nc.sbuf_tensor(f"sb_{name}_{Ctx._uid[0]}", list(shape), dt))
        return t

    def ps(self, name, shape, dt=F32):
        Ctx._uid[0] += 1
        t = self.stack.enter_context(self.nc.psum_tensor(f"pp_{name}_{Ctx._uid[0]}", list(shape), dt))
        return t

    def B(self, name):
        if name not in self.bufs:
            self.bufs[name] = Buf(name)
        return self.bufs[name]


def build(nc, cfg):
    layers = cfg["layers"]
    stack = ExitStack()
    with stack:
        P = Prog(nc, stack)
        C = Ctx(nc, stack)
        B = C.B

        def din(name, shape, dt=F32):
            return nc.dram_tensor(name, list(shape), dt, kind="ExternalInput").ap()

        x_d = din("x", [S, D])
        out_d = nc.dram_tensor("out", [S, D], F32, kind="ExternalOutput").ap()
        cst_d = din("cst", [128, 4 * 128])
        ecap_d = din("ecap", [128, NCH * NE])
        nrm_d = din("norms", [128, 9 * D])
        L = {}
        for l in layers:
            if cfg["moe"][l]:
                L[l, "w_router"] = din(f"w_router{l}", [D, NE])
                L[l, "b_router"] = din(f"b_router{l}", [128, NE])
                L[l, "w_gu"] = din(f"w_gu{l}", [NE, D, 2 * D])
                L[l, "b_gu"] = din(f"b_gu{l}", [128, NE * 16])
                L[l, "w_dn"] = din(f"w_dn{l}", [NE, D, D])
                L[l, "b_dn"] = din(f"b_dn{l}", [NE, D])
        def sb_layer(l):
            w_qkv, negm_d = L[l, "w_qkv"], L[l, "negm"]
            NH = 16
            with ExitStack() as ls:
                LC = Ctx(nc, ls)
                QT = LC.sb("QT", [128, NDC, S], BF16)
                KT = LC.sb("KT", [128, NDC, S], BF16)
                BQ = [[B(f"QT{hd}_{qg}") for qg in range(4)] for hd in range(NH)]
                BK = [B(f"KT{g}") for g in range(NDC)]
                Bvd = [B(f"vst{c}") for c in range(NCH)]
                with ExitStack() as us:
                    UC = Ctx(nc, us)
                    uT = UC.sb("uT", [128, NDC, S], BF16)
                    BuT = [B(f"uTq{i}") for i in range(4)]
                    norm_to_uT(l, uT, BuT)
                    with ExitStack() as ms:
                        MC = Ctx(nc, ms)
                        wq = [MC.sb(f"wq{i}", [128, NDC, 128], BF16) for i in range(2)]
                        wv = MC.sb("wv", [128, NDC, 512], BF16)
                        vst = [MC.sb(f"vst{i}", [128, 512], BF16) for i in range(2)]
                        Bwq = [B(f"wq{i}") for i in range(2)]
                        Bwv = B("wv")
                        Bvs = [B(f"vstage{i}") for i in range(2)]

                        def load_wq(j):
                            i2 = j % 2
                            P.dma("pool", lambda e: e.dma_start(
                                out=wq[i2][:], in_=w_qkv[:, j * 128:(j + 1) * 128].rearrange("(c p) n -> p c n", p=128)),
                                writes=[Bwq[i2]])
                        load_wq(0)
                        for j in range(16):
                            i2 = j % 2
                            if j + 1 < 16:
                                load_wq(j + 1)
                            g = j % 8
                            for tq in range(4):
                                pb = (j % 2) * 4 + tq

                                def mm(e):
                                    r = None
                                    for dc in range(NDC):
                                        r = e.matmul(psum[pb][:], lhsT=wq[i2][:, dc, :],
                                                     rhs=uT[:, dc, tq * 512:(tq + 1) * 512],
                                                     start=(dc == 0), stop=(dc == NDC - 1))
                                    return r
                                P.op("pe", mm, reads=[Bwq[i2], BuT[tq]], writes=[Bps[pb]])
                                if j < 8:
                                    P.op("act", lambda e: e.activation(out=QT[:, g, tq * 512:(tq + 1) * 512],
                                                                       in_=psum[pb][:], func=AF.Copy, scale=0.125),
                                         reads=[Bps[pb]], writes=[BQ[2 * g][tq], BQ[2 * g + 1][tq]])
                                else:
                                    P.op("dve", lambda e: e.tensor_copy(out=KT[:, g, tq * 512:(tq + 1) * 512],
                                                                        in_=psum[pb][:]),
                                         reads=[Bps[pb]], writes=[BK[g]])
                        for half in range(2):
                            P.dma("pool", lambda e: e.dma_start(
                                out=wv[:], in_=w_qkv[:, 2 * D + half * 512:2 * D + (half + 1) * 512].rearrange(
                                    "(c p) n -> p c n", p=128)), writes=[Bwv])
                            for c in range(NCH):
                                pb = c % 8
                                i2 = c % 2

                                def mv(e):
                                    r = None
                                    for dc in range(NDC):
                                        r = e.matmul(psum[pb][:], lhsT=uT[:, dc, c * 128:(c + 1) * 128],
                                                     rhs=wv[:, dc, :], start=(dc == 0), stop=(dc == NDC - 1))
                                    return r
                                P.op("pe", mv, reads=[Bwv, BuT[c // 4]], writes=[Bps[pb]])
                                if c % 2 == 0:
                                    P.op("act", lambda e: e.copy(out=vst[i2][:], in_=psum[pb][:]),
                                         reads=[Bps[pb]], writes=[Bvs[i2]])
                                else:
                                    P.op("dve", lambda e: e.tensor_copy(out=vst[i2][:], in_=psum[pb][:]),
                                         reads=[Bps[pb]], writes=[Bvs[i2]])
                                P.dma("sp", lambda e: e.dma_start(
                                    out=ust_d[c * 128:(c + 1) * 128, half * 512:(half + 1) * 512], in_=vst[i2][:]),
                                    reads=[Bvs[i2]], writes=[Bvd[c]])
                        P.barrier()
                with ExitStack() as ms:
                    MC = Ctx(nc, ms)
                    Vt = MC.sb("Vt", [128, NCH, D], BF16)
                    BV = B("Vt")
                    for c in range(NCH):
                        P.dma("sp", lambda e: e.dma_start(out=Vt[:, c, :], in_=ust_d[c * 128:(c + 1) * 128, :]),
                              reads=[Bvd[c]], writes=[BV])
                    negm = MC.sb("negm", [128, 4, 512], BF16)
                    ntri = MC.sb("ntri", [128, 128], BF16)
                    nones = MC.sb("nones", [128, 128], BF16)
                    identb = MC.sb("identb2", [128, 128], BF16)
                    Bc2 = B("sb_consts")
                    P.dma("pool", lambda e: e.dma_start(out=negm[:].rearrange("p a b -> p (a b)"), in_=negm_d),
                          writes=[Bc2])
                    P.op("dve", lambda e: e.tensor_scalar(out=ntri[:], in0=cst[:, 384:512], scalar1=-1.0, scalar2=None,
                                                          op0=ALU.mult), reads=[Bcst], writes=[Bc2])
                    P.op("dve", lambda e: e.tensor_scalar(out=nones[:], in0=ones, scalar1=-1.0, scalar2=None,
                                                          op0=ALU.mult), reads=[Bcst], writes=[Bc2])
                    P.op("dve", lambda e: e.tensor_copy(out=identb[:], in_=ident), reads=[Bcst], writes=[Bc2])
                    NLB, NAT, NE_ = 3, 3, 2
                    Lb = [MC.sb(f"Lb{i}", [128, 512], BF16) for i in range(NLB)]
                    AT = [MC.sb(f"AT{i}", [128, 512], BF16) for i in range(NAT)]
                    Et = [MC.sb(f"Et{i}", [128, 512], F32) for i in range(NE_)]
                    Lacc = MC.sb("Lacc", [128, 512], F32)
                    Laccb = [MC.sb(f"Laccb{i}", [128, 512], BF16) for i in range(2)]
                    BLb = [B(f"Lb{i}") for i in range(NLB)]
                    BAT = [B(f"AT{i}") for i in range(NAT)]
                    BEt = [B(f"Et{i}") for i in range(NE_)]
                    BLacc = B("Lacc")
                    BLaccb = [B(f"Laccb{i}") for i in range(2)]
                    blocks = []
                    for hd in range(NH):
                        for qg in range(4):
                            nkb = 4 * qg + 4
                            for kb in reversed(range(nkb)):
                                blocks.append(dict(hd=hd, qg=qg, kb=kb, first=(kb == nkb - 1), last=(kb == 0),
                                                   grp=hd * 4 + qg))
                    nacc = [0]

                    def stage_a(i, b):
                        hd, qg, kb = b["hd"], b["qg"], b["kb"]
                        g, pbase = hd // 2, (hd % 2) * 64
                        xb = i % 4
                        diag = kb >= 4 * qg
                        m = kb - 4 * qg

                        def mz(e):
                            r = e.matmul(psum[xb][:], lhsT=KT[pbase:pbase + 64, g, kb * 128:(kb + 1) * 128],
                                         rhs=QT[pbase:pbase + 64, g, qg * 512:(qg + 1) * 512], start=True,
                                         stop=True, skip_group_check=True)
                            if diag:
                                r = e.matmul(psum[xb][:], lhsT=identb[:], rhs=negm[:, m, :], start=False, stop=True,
                                             skip_group_check=True)
                            return r
                        P.op("pe", mz, reads=[BK[g], BQ[hd][qg], Bc2], writes=[Bps[xb]])
                        P.op("act", lambda e: e.activation(out=Et[i % NE_][:], in_=psum[xb][:], func=AF.Exp),
                             reads=[Bps[xb]], writes=[BEt[i % NE_]])
                        P.op("act", lambda e: e.activation(out=Lb[i % NLB][:], in_=Et[i % NE_][:], func=AF.Ln,
                                                           bias=1.0, scale=1.0),
                             reads=[BEt[i % NE_]], writes=[BLb[i % NLB]])

                    def stage_b(i, b):
                        hd, qg, kb = b["hd"], b["qg"], b["kb"]
                        g, pbase = hd // 2, (hd % 2) * 64
                        xb = i % 4
                        ob = 4 + b["grp"] % 2
                        first, last = b["first"], b["last"]
                        la = nacc[0] % 2

                        def m2(e):
                            r = e.matmul(psum[xb][:], lhsT=ntri[:], rhs=Lb[i % NLB][:], start=False, stop=True,
                                         skip_group_check=True)
                            if not first:
                                r = e.matmul(psum[xb][:], lhsT=nones[:], rhs=Laccb[la][:], start=False, stop=True,
                                             skip_group_check=True)
                            return r
                        P.op("pe", m2, reads=[BLb[i % NLB], Bc2] + ([] if first else [BLaccb[la]]), writes=[Bps[xb]])
                        P.op("act", lambda e: e.activation(out=AT[i % NAT][:], in_=psum[xb][:], func=AF.Exp),
                             reads=[Bps[xb]], writes=[BAT[i % NAT]])
                        P.op("pe", lambda e: e.matmul(psum[ob][:], lhsT=Vt[:, kb, g * 128:(g + 1) * 128],
                                                      rhs=AT[i % NAT][:], start=first, stop=last,
                                                      skip_group_check=True),
                             reads=[BV, BAT[i % NAT]], writes=[Bps[ob]])
                        if not last:
                            if first:
                                P.op("dve", lambda e: e.tensor_copy(out=Lacc[:], in_=Lb[i % NLB][:]),
                                     reads=[BLb[i % NLB]], writes=[BLacc])
                            else:
                                P.op("dve", lambda e: e.tensor_tensor(out=Lacc[:], in0=Lacc[:], in1=Lb[i % NLB][:],
                                                                      op=ALU.add),
                                     reads=[BLb[i % NLB], BLacc], writes=[BLacc])
                            nacc[0] += 1
                            lb2 = nacc[0] % 2
                            P.op("dve", lambda e: e.tensor_copy(out=Laccb[lb2][:], in_=Lacc[:]),
                                 reads=[BLacc], writes=[BLaccb[lb2]])
                        else:
                            P.op("act", lambda e: e.copy(out=QT[pbase:pbase + 64, g, qg * 512:(qg + 1) * 512],
                                                         in_=psum[ob][pbase:pbase + 64, :]),
                                 reads=[Bps[ob]], writes=[BQ[hd][qg]])

                    nb = len(blocks)
                    for i in range(nb + 1):
                        if i < nb:
                            stage_a(i, blocks[i])
                        if i >= 1:
                            stage_b(i - 1, blocks[i - 1])
                    P.barrier()
                out_proj(l, QT, [BQ[hd][qg] for hd in range(NH) for qg in range(4)])


        for l in layers:
            if cfg["mix"][l]:
                kind = l % 3
                if kind == 0:
                    L[l, "w_in"] = din(f"lru_w_in{l}", [D, 2 * D])
                    L[l, "vec"] = din(f"lru_vec{l}", [128, 8 * 8])
                    L[l, "w_a"] = din(f"lru_w_a{l}", [8, 128, 128])
                    L[l, "w_x"] = din(f"lru_w_x{l}", [8, 128, 128])
                    L[l, "w_out"] = din(f"mix_w_out{l}", [D, D])
                elif kind == 1:
                    L[l, "w_in"] = din(f"pool_w_in{l}", [D, D])
                    L[l, "w_grp"] = din(f"pool_w_grp{l}", [4, 256, 256])
                    L[l, "vec"] = din(f"pool_vec{l}", [128, 8 + 64])
                    L[l, "w_out"] = din(f"mix_w_out{l}", [D, D])
                else:
                    L[l, "w_qkv"] = din(f"sb_w_qkv{l}", [D, 3 * D])
                    L[l, "w_out"] = din(f"mix_w_out{l}", [D, D])
                    L[l, "negm"] = din(f"sb_negmask{l}", [128, 4 * 512])
        dbg = cfg.get("debug", False)
        skind = "ExternalOutput" if dbg else "Internal"
        xs_d = nc.dram_tensor("xs_scr", [NSLOT, D], BF16, kind=skind).ap()
        ys_d = nc.dram_tensor("ys_scr", [NSLOT, D], F32, kind=skind).ap()
        ust_d = nc.dram_tensor("ust_scr", [S, D], BF16, kind=skind).ap()
        if dbg:
            dbg_d = nc.dram_tensor("dbg", [128, 8 * 512], F32, kind="ExternalOutput").ap()
            dbgx_d = nc.dram_tensor("dbgx", [128, NDC * CAP], BF16, kind="ExternalOutput").ap()
            dbgw_d = nc.dram_tensor("dbgw", [128, NDC * 512], BF16, kind="ExternalOutput").ap()
            dbgh_d = nc.dram_tensor("dbgh", [128, NDC * CAP], BF16, kind="ExternalOutput").ap()
        B_xs, B_ys = B("xs_d"), B("ys_d")

        h = C.sb("h", [128, NCH, D], F32)
        Bh = [B(f"h{c}") for c in range(NCH)]
        cst = C.sb("cst", [128, 4 * 128], F32)
        ident = cst[:, 0:128]
        tri = cst[:, 128:256]
        ones = cst[:, 256:384]
        ecap = C.sb("ecap", [128, NCH * NE], F32)
        ssq = C.sb("ssq", [128, 2], F32)
        rstd = C.sb("rstd", [128, 2], F32)
        Bcst = B("cst")

        psum = [C.ps(f"ps{i}", [128, 512]) for i in range(8)]
        Bps = [B(f"ps{i}") for i in range(8)]

        bound_reg = nc.gpsimd.to_reg(NSLOT - 1)
        P.dma("sp", lambda e: e.dma_start(out=cst[:], in_=cst_d), writes=[Bcst])
        P.dma("pool", lambda e: e.dma_start(out=ecap[:], in_=ecap_d), writes=[Bcst])
        for c in range(NCH):
            P.dma("sp", lambda e: e.dma_start(out=h[:, c, :], in_=x_d[c * 128:(c + 1) * 128, :]),
                  writes=[Bh[c]])

        def rms_rstd(c, par, junk, Bj):
            Bs, Br = B(f"ssq{par}"), B(f"rstd{par}")
            P.op("act", lambda e: e.activation(out=junk[:], in_=h[:, c, :], func=AF.Square,
                                               accum_out=ssq[:, par:par + 1]),
                 reads=[Bh[c]], writes=[Bj, Bs])
            P.op("dve", lambda e: e.tensor_scalar(out=rstd[:, par:par + 1], in0=ssq[:, par:par + 1],
                                                  scalar1=1.0 / D, scalar2=RMS_EPS, op0=ALU.mult, op1=ALU.add),
                 reads=[Bs], writes=[Br])
            P.op("act", lambda e: e.activation(out=rstd[:, par:par + 1], in_=rstd[:, par:par + 1], func=AF.Sqrt),
                 reads=[Br], writes=[Br])
            P.op("dve", lambda e: e.reciprocal(out=rstd[:, par:par + 1], in_=rstd[:, par:par + 1]),
                 reads=[Br], writes=[Br])
            return Br

        def moe_layer(l):
            gcol = (4 + l) * D
            w_router, b_router = L[l, "w_router"], L[l, "b_router"]
            w_gu, b_gu, w_dn, b_dn = L[l, "w_gu"], L[l, "b_gu"], L[l, "w_dn"], L[l, "b_dn"]
            with ExitStack() as ls:
                LC = Ctx(nc, ls)
                gat = LC.sb("gat", [128, NCH, NE], F32)
                dki = LC.sb("dki", [128, NCH * TOPK], I32)
                gk = LC.sb("gk", [128, NCH * TOPK], F32)
                bgu = LC.sb("bgu", [128, NE * 16], F32)
                bdn = LC.sb("bdn", [NE, D], F32)
                Bw = B("moe_small")
                Ball = B("route")
                P.dma("sp", lambda e: e.dma_start(out=bgu[:], in_=b_gu), writes=[Bw])
                P.dma("sp", lambda e: e.dma_start(out=bdn[:], in_=b_dn), writes=[Bw])

                with ExitStack() as ms:
                    MC = Ctx(nc, ms)
                    wr = MC.sb("wr", [128, NDC, NE], F32)
                    br = MC.sb("br", [128, NE], F32)
                    gam = MC.sb("gam", [128, D], F32)
                    junk = MC.sb("junk", [128, D], F32)
                    Bj = B("junk")
                    P.dma("sp", lambda e: e.dma_start(out=wr[:], in_=w_router.rearrange("(c p) n -> p c n", p=128)),
                          writes=[Bw])
                    P.dma("sp", lambda e: e.dma_start(out=br[:], in_=b_router), writes=[Bw])
                    P.dma("sp", lambda e: e.dma_start(out=gam[:], in_=nrm_d[:, gcol:gcol + D]), writes=[Bw])
                    uf = [MC.sb(f"uf{i}", [128, D], F32) for i in range(2)]
                    ub = [MC.sb(f"ub{i}", [128, D], BF16) for i in range(2)]
                    uT = [MC.sb(f"uT{i}", [128, NDC, 128], F32) for i in range(2)]
                    logit = MC.sb("logit", [128, NCH, NE], F32)
                    top8 = MC.sb("top8", [128, NCH, 8], F32)
                    mask = MC.sb("mask", [128, NCH, NE], F32)
                    sm = MC.sb("sm", [128, NCH, 4], F32)
                    dest = MC.sb("dest", [128, NCH, NE], F32)
                    tmp32 = MC.sb("tmp32", [128, NCH, NE], F32)
                    off = MC.sb("off", [128, NCH, NE], F32)
                    dk = MC.sb("dk", [128, NCH * TOPK], F32)
                    jk = MC.sb("jk", [128, NE], F32)
                    Blog = [B(f"logit{c}") for c in range(NCH)]

                    for c in range(NCH):
                        par = c % 2
                        Br = rms_rstd(c, par, junk, Bj)
                        Buf_, Bub, BuT = B(f"uf{par}"), B(f"ub{par}"), B(f"uT{par}")
                        P.op("dve", lambda e: e.scalar_tensor_tensor(
                            out=uf[par][:], in0=h[:, c, :], scalar=rstd[:, par:par + 1],
                            in1=gam[:], op0=ALU.mult, op1=ALU.mult),
                            reads=[Bh[c], Br, Bw], writes=[Buf_])
                        P.op("act", lambda e: e.copy(out=ub[par][:], in_=uf[par][:]), reads=[Buf_], writes=[Bub])
                        P.dma("sp", lambda e: e.dma_start(out=ust_d[c * 128:(c + 1) * 128, :], in_=ub[par][:]),
                              reads=[Bub], writes=[B(f"ust{c}")])
                        for g in range(2):
                            pb = 2 * par + g

                            def tr(e):
                                r = None
                                for j in range(4):
                                    dc = g * 4 + j
                                    r = e.transpose(out=psum[pb][:, j * 128:(j + 1) * 128],
                                                    in_=uf[par][:, dc * 128:(dc + 1) * 128], identity=ident)
                                return r
                            P.op("pe", tr, reads=[Buf_, Bcst], writes=[Bps[pb]])
                            src = psum[pb][:].rearrange("p (a b) -> p a b", a=4)
                            if g == 0:
                                P.op("act", lambda e: e.copy(out=uT[par][:, 0:4, :], in_=src),
                                     reads=[Bps[pb]], writes=[BuT])
                            else:
                                P.op("dve", lambda e: e.tensor_copy(out=uT[par][:, 4:8, :], in_=src),
                                     reads=[Bps[pb]], writes=[BuT])
                        pl = 4 + par

                        def rt(e):
                            r = None
                            for dc in range(NDC):
                                r = e.matmul(psum[pl][:, 0:NE], lhsT=uT[par][:, dc, :], rhs=wr[:, dc, :],
                                             start=(dc == 0), stop=(dc == NDC - 1))
                            return r
                        P.op("pe", rt, reads=[BuT, Bw], writes=[Bps[pl]])
                        Bl = Blog[c]
                        P.op("dve", lambda e: e.tensor_tensor(out=logit[:, c, :], in0=psum[pl][:, 0:NE], in1=br[:],
                                                              op=ALU.add),
                             reads=[Bps[pl], Bw], writes=[Bl])
                        P.op("dve", lambda e: e.max(out=top8[:, c, :], in_=logit[:, c, :]), reads=[Bl], writes=[Bl])
                        P.op("dve", lambda e: e.tensor_scalar(out=mask[:, c, :], in0=logit[:, c, :],
                                                              scalar1=top8[:, c, 3:4], scalar2=None, op0=ALU.is_ge),
                             reads=[Bl], writes=[Bl])
                        P.op("dve", lambda e: e.tensor_scalar(out=sm[:, c, 0:1], in0=top8[:, c, 0:1], scalar1=-1.0,
                                                              scalar2=None, op0=ALU.mult),
                             reads=[Bl], writes=[Bl])
                        P.op("act", lambda e: e.activation(out=gat[:, c, :], in_=logit[:, c, :], func=AF.Exp,
                                                           bias=sm[:, c, 0:1], scale=1.0),
                             reads=[Bl], writes=[Bl])
                        P.op("dve", lambda e: e.scalar_tensor_tensor(
                            out=gat[:, c, :], in0=gat[:, c, :], scalar=1.0, in1=mask[:, c, :], op0=ALU.mult,
                            op1=ALU.mult, accum_out=sm[:, c, 1:2]),
                            reads=[Bl], writes=[Bl])
                        P.op("dve", lambda e: e.reciprocal(out=sm[:, c, 2:3], in_=sm[:, c, 1:2]), reads=[Bl],
                             writes=[Bl])
                        P.op("dve", lambda e: e.tensor_scalar(out=gat[:, c, :], in0=gat[:, c, :],
                                                              scalar1=sm[:, c, 2:3], scalar2=None, op0=ALU.mult),
                             reads=[Bl], writes=[Bl])

                    mflat = mask[:].rearrange("p c e -> p (c e)")
                    P.op("pe", lambda e: e.matmul(psum[6][:], lhsT=tri, rhs=mflat, start=True, stop=True),
                         reads=Blog + [Bcst], writes=[Bps[6]])
                    P.op("pe", lambda e: e.matmul(psum[7][:], lhsT=ones, rhs=mflat, start=True, stop=True),
                         reads=Blog + [Bcst], writes=[Bps[7]])
                    P.op("dve", lambda e: e.tensor_copy(out=tmp32[:].rearrange("p c e -> p (c e)"), in_=psum[7][:]),
                         reads=[Bps[7]], writes=[Ball])
                    P.op("dve", lambda e: e.memset(off[:, 0, :], 0.0), writes=[Ball])
                    for c in range(1, NCH):
                        P.op("dve", lambda e: e.tensor_tensor(out=off[:, c, :], in0=off[:, c - 1, :],
                                                              in1=tmp32[:, c - 1, :], op=ALU.add),
                             reads=[Ball], writes=[Ball])
                    offf = off[:].rearrange("p c e -> p (c e)")
                    destf = dest[:].rearrange("p c e -> p (c e)")
                    tmpf = tmp32[:].rearrange("p c e -> p (c e)")
                    gatf = gat[:].rearrange("p c e -> p (c e)")
                    P.op("dve", lambda e: e.tensor_tensor(out=offf, in0=offf, in1=psum[6][:], op=ALU.add),
                         reads=[Ball, Bps[6]], writes=[Ball])
                    P.op("dve", lambda e: e.tensor_scalar(out=tmpf, in0=offf, scalar1=float(CAP), scalar2=None,
                                                          op0=ALU.is_lt), reads=[Ball], writes=[Ball])
                    P.op("dve", lambda e: e.tensor_tensor(out=gatf, in0=gatf, in1=tmpf, op=ALU.mult),
                         reads=[Ball] + Blog, writes=[Ball] + Blog)
                    P.op("dve", lambda e: e.tensor_tensor(out=destf, in0=offf, in1=ecap[:], op=ALU.add),
                         reads=[Ball, Bcst], writes=[Ball])
                    P.op("dve", lambda e: e.tensor_scalar(out=tmpf, in0=tmpf, scalar1=-1.0e6, scalar2=1.0e6,
                                                          op0=ALU.mult, op1=ALU.add), reads=[Ball], writes=[Ball])
                    P.op("dve", lambda e: e.tensor_tensor(out=destf, in0=destf, in1=tmpf, op=ALU.add),
                         reads=[Ball], writes=[Ball])
                    for c in range(NCH):
                        for k in range(TOPK):
                            col = c * TOPK + k
                            P.op("dve", lambda e: e.scalar_tensor_tensor(
                                out=jk[:], in0=logit[:, c, :], scalar=top8[:, c, k:k + 1], in1=dest[:, c, :],
                                op0=ALU.is_equal, op1=ALU.mult, accum_out=dk[:, col:col + 1]),
                                reads=[Ball], writes=[Ball])
                            P.op("dve", lambda e: e.scalar_tensor_tensor(
                                out=jk[:], in0=logit[:, c, :], scalar=top8[:, c, k:k + 1], in1=gat[:, c, :],
                                op0=ALU.is_equal, op1=ALU.mult, accum_out=gk[:, col:col + 1]),
                                reads=[Ball], writes=[Ball])
                    P.op("dve", lambda e: e.tensor_copy(out=dki[:], in_=dk[:]), reads=[Ball], writes=[Ball])
                    if dbg:
                        for i, (t_, n_) in enumerate([(logit, 512), (mask, 512), (gat, 512), (dest, 512), (off, 512)]):
                            P.dma("sp", lambda e: e.dma_start(out=dbg_d[:, i * 512:i * 512 + n_],
                                                              in_=t_[:].rearrange("p c e -> p (c e)")),
                                  reads=[Ball] + Blog, writes=[B("dbg")])
                        P.dma("sp", lambda e: e.dma_start(out=dbg_d[:, 5 * 512:5 * 512 + 64], in_=dk[:]),
                              reads=[Ball], writes=[B("dbg")])
                        P.dma("sp", lambda e: e.dma_start(out=dbg_d[:, 6 * 512:6 * 512 + 64], in_=gk[:]),
                              reads=[Ball], writes=[B("dbg")])
                        P.dma("sp", lambda e: e.dma_start(out=dbg_d[:, 7 * 512:7 * 512 + 128],
                                                          in_=top8[:].rearrange("p c e -> p (c e)")),
                              reads=[Ball] + Blog, writes=[B("dbg")])

                    for c in range(NCH):
                        par = c % 2
                        Bub = B(f"ub{par}")
                        P.dma("sp", lambda e: e.dma_start(out=ub[par][:], in_=ust_d[c * 128:(c + 1) * 128, :]),
                              reads=[B(f"ust{c}")], writes=[Bub])
                        for k in range(TOPK):
                            col = c * TOPK + k
                            P.dma("pool", lambda e: e.indirect_dma_start(
                                out=xs_d, out_offset=bass.IndirectOffsetOnAxis(ap=dki[:, col:col + 1], axis=0),
                                in_=ub[par][:], in_offset=None, bounds_check=bound_reg, oob_is_err=False),
                                reads=[Bub, Ball], writes=[B_xs])
                    P.barrier()

                with ExitStack() as ms:
                    MC = Ctx(nc, ms)
                    NWG, NWD = 6, 3
                    wg = [MC.sb(f"wg{i}", [128, NDC, 512], BF16) for i in range(NWG)]
                    wd = [MC.sb(f"wd{i}", [128, NDC, 512], BF16) for i in range(NWD)]
                    Bwg = [B(f"wg{i}") for i in range(NWG)]
                    Bwd = [B(f"wd{i}") for i in range(NWD)]
                    xT = [MC.sb(f"xT{i}", [128, NDC, CAP], BF16) for i in range(2)]
                    hT = [MC.sb(f"hT{i}", [128, NDC, CAP], BF16) for i in range(2)]
                    ysb = [MC.sb(f"ysb{i}", [128, 512], F32) for i in range(4)]
                    t_g = [MC.sb(f"t_g{i}", [128, CAP], F32) for i in range(2)]
                    t_s = [MC.sb(f"t_s{i}", [128, CAP], F32) for i in range(2)]
                    t_u = [MC.sb(f"t_u{i}", [128, CAP], F32) for i in range(2)]
                    BxT = [B(f"xT{i}") for i in range(2)]
                    BhT = [B(f"hT{i}") for i in range(2)]
                    Bys = [B(f"ysb{i}") for i in range(4)]
                    Btg = [B(f"t_g{i}") for i in range(2)]
                    Bts = [B(f"t_s{i}") for i in range(2)]
                    Btu = [B(f"t_u{i}") for i in range(2)]

                    def load_x(e_):
                        i2 = e_ % 2
                        for dc in range(NDC):
                            P.dma("sp", lambda e: e.dma_start_transpose(
                                out=xT[i2][:, dc, :], in_=xs_d[e_ * CAP:(e_ + 1) * CAP, dc * 128:(dc + 1) * 128]),
                                reads=[B_xs], writes=[BxT[i2]])

                    def load_wg(e_, q):
                        slot = (e_ * 4 + q) % NWG
                        half, is_up = q // 2, q % 2
                        c0 = is_up * D + half * 512
                        P.dma("pool", lambda e: e.dma_start(
                            out=wg[slot][:], in_=w_gu[e_, :, c0:c0 + 512].rearrange("(c p) n -> p c n", p=128)),
                            writes=[Bwg[slot]])

                    def load_wd(e_, half):
                        slot = (e_ * 2 + half) % NWD
                        P.dma("pool", lambda e: e.dma_start(
                            out=wd[slot][:],
                            in_=w_dn[e_, :, half * 512:(half + 1) * 512].rearrange("(c p) n -> p c n", p=128)),
                            writes=[Bwd[slot]])

                    def load_stage(e_, st):
                        if e_ >= NE:
                            return
                        if st == 0:
                            load_x(e_)
                            load_wg(e_, 0)
                            load_wg(e_, 1)
                            load_wd(e_, 0)
                        elif st == 1:
                            load_wg(e_, 2)
                            load_wg(e_, 3)
                        else:
                            load_wd(e_, 1)

                    def compute_expert(e_):
                        i2 = e_ % 2
                        for fcp in range(8):
                            if fcp == 4:
                                load_stage(e_ + 1, 1)
                            half, j = fcp // 4, fcp % 4
                            sg = (e_ * 4 + half * 2 + 0) % NWG
                            su = (e_ * 4 + half * 2 + 1) % NWG
                            pg, pu = (fcp % 2) * 2, (fcp % 2) * 2 + 1
                            ti = fcp % 2

                            def mm(e, slot, pb):
                                r = None
                                for dc in range(NDC):
                                    r = e.matmul(psum[pb][:, 0:CAP], lhsT=wg[slot][:, dc, j * 128:(j + 1) * 128],
                                                 rhs=xT[i2][:, dc, :], start=(dc == 0), stop=(dc == NDC - 1))
                                return r
                            P.op("pe", lambda e: mm(e, sg, pg), reads=[Bwg[sg], BxT[i2]], writes=[Bps[pg]])
                            P.op("pe", lambda e: mm(e, su, pu), reads=[Bwg[su], BxT[i2]], writes=[Bps[pu]])
                            bg = bgu[:, e_ * 16 + fcp:e_ * 16 + fcp + 1]
                            bu = bgu[:, e_ * 16 + 8 + fcp:e_ * 16 + 8 + fcp + 1]
                            P.op("dve", lambda e: e.tensor_scalar(out=t_g[ti][:], in0=psum[pg][:, 0:CAP], scalar1=bg,
                                                                  scalar2=7.0, op0=ALU.add, op1=ALU.min),
                                 reads=[Bps[pg], Bw], writes=[Btg[ti]])
                            P.op("act", lambda e: e.activation(out=t_s[ti][:], in_=t_g[ti][:], func=AF.Sigmoid,
                                                               scale=1.702),
                                 reads=[Btg[ti]], writes=[Bts[ti]])
                            P.op("dve", lambda e: e.tensor_scalar(out=t_u[ti][:], in0=psum[pu][:, 0:CAP], scalar1=bu,
                                                                  scalar2=7.0, op0=ALU.add, op1=ALU.min),
                                 reads=[Bps[pu], Bw], writes=[Btu[ti]])
                            P.op("dve", lambda e: e.tensor_scalar(out=t_u[ti][:], in0=t_u[ti][:], scalar1=-7.0,
                                                                  scalar2=1.0, op0=ALU.max, op1=ALU.add),
                                 reads=[Btu[ti]], writes=[Btu[ti]])
                            P.op("dve", lambda e: e.tensor_tensor(out=t_g[ti][:], in0=t_g[ti][:], in1=t_s[ti][:],
                                                                  op=ALU.mult),
                                 reads=[Btg[ti], Bts[ti]], writes=[Btg[ti]])
                            P.op("dve", lambda e: e.tensor_tensor(out=hT[i2][:, fcp, :], in0=t_u[ti][:],
                                                                  in1=t_g[ti][:], op=ALU.mult),
                                 reads=[Btu[ti], Btg[ti]], writes=[BhT[i2]])
                        nsc = CAP // 128
                        for half in range(2):
                            if half == 1:
                                load_stage(e_ + 1, 2)
                            sd = (e_ * 2 + half) % NWD
                            for sc in range(nsc):
                                idx = half * nsc + sc
                                pb = 4 + idx % 4
                                yi = idx % 4

                                def md(e):
                                    r = None
                                    for fc in range(NDC):
                                        r = e.matmul(psum[pb][:], lhsT=hT[i2][:, fc, sc * 128:(sc + 1) * 128],
                                                     rhs=wd[sd][:, fc, :], start=(fc == 0), stop=(fc == NDC - 1))
                                    return r
                                P.op("pe", md, reads=[BhT[i2], Bwd[sd]], writes=[Bps[pb]])
                                P.op("act", lambda e: e.copy(out=ysb[yi][:], in_=psum[pb][:]),
                                     reads=[Bps[pb]], writes=[Bys[yi]])
                                r0 = e_ * CAP + sc * 128
                                P.dma("sp", lambda e: e.dma_start(
                                    out=ys_d[r0:r0 + 128, half * 512:(half + 1) * 512], in_=ysb[yi][:]),
                                    reads=[Bys[yi]], writes=[B_ys])

                    for st in range(3):
                        load_stage(0, st)
                    if dbg:
                        P.dma("sp", lambda e: e.dma_start(out=dbgx_d, in_=xT[0][:].rearrange("p c n -> p (c n)")),
                              reads=[BxT[0]], writes=[B("dbgx")])
                    if dbg:
                        P.dma("sp", lambda e: e.dma_start(out=dbgw_d, in_=wg[0][:].rearrange("p c n -> p (c n)")),
                              reads=[Bwg[0]], writes=[B("dbgw")])
                    for e_ in range(NE):
                        load_stage(e_ + 1, 0)
                        compute_expert(e_)
                        if dbg and e_ == 0:
                            P.dma("sp", lambda e: e.dma_start(out=dbgh_d, in_=hT[0][:].rearrange("p c n -> p (c n)")),
                                  reads=[BhT[0]], writes=[B("dbgh")])
                    P.barrier()

                with ExitStack() as ms:
                    MC = Ctx(nc, ms)
                    yg = [MC.sb(f"yg{i}", [128, D], F32) for i in range(8)]
                    Byg = [B(f"yg{i}") for i in range(8)]
                    gT = MC.sb("gT", [NE, 128], F32)
                    for i in range(8):
                        P.op("dve", lambda e: e.memset(yg[i][:], 0.0), writes=[Byg[i]])
                    for c in range(NCH):
                        P.op("pe", lambda e: e.transpose(out=psum[0][0:NE, 0:128], in_=gat[:, c, :], identity=ident),
                             reads=[Ball, Bcst], writes=[Bps[0]])
                        P.op("act", lambda e: e.copy(out=gT[:], in_=psum[0][0:NE, 0:128]), reads=[Bps[0]],
                             writes=[B("gT")])
                        for half in range(2):
                            P.op("pe", lambda e: e.matmul(psum[1 + half][:], lhsT=gT[:],
                                                          rhs=bdn[:, half * 512:(half + 1) * 512],
                                                          start=True, stop=True),
                                 reads=[B("gT"), Bw], writes=[Bps[1 + half]])
                            P.op("dve", lambda e: e.tensor_tensor(out=h[:, c, half * 512:(half + 1) * 512],
                                                                  in0=h[:, c, half * 512:(half + 1) * 512],
                                                                  in1=psum[1 + half][:], op=ALU.add),
                                 reads=[Bps[1 + half], Bh[c]], writes=[Bh[c]])
                        for k in range(TOPK):
                            col = c * TOPK + k
                            yi = (c % 2) * 4 + k
                            P.dma("pool", lambda e: e.indirect_dma_start(
                                out=yg[yi][:], out_offset=None, in_=ys_d,
                                in_offset=bass.IndirectOffsetOnAxis(ap=dki[:, col:col + 1], axis=0),
                                bounds_check=bound_reg, oob_is_err=False),
                                reads=[B_ys, Ball], writes=[Byg[yi]])
                            P.op("dve", lambda e: e.scalar_tensor_tensor(
                                out=h[:, c, :], in0=yg[yi][:], scalar=gk[:, col:col + 1], in1=h[:, c, :],
                                op0=ALU.mult, op1=ALU.add),
                                reads=[Byg[yi], Ball, Bh[c]], writes=[Bh[c]])
                    P.barrier()


        def norm_to_uT(l, uT, BuT):
            gcol = l * D
            with ExitStack() as ms:
                MC = Ctx(nc, ms)
                gam = MC.sb("gam", [128, D], F32)
                junk = MC.sb("junk", [128, D], F32)
                ubf = [MC.sb(f"ubf{i}", [128, D], BF16) for i in range(2)]
                identb = MC.sb("identb", [128, 128], BF16)
                Bg, Bj = B("gam_m"), B("junk_m")
                P.dma("sp", lambda e: e.dma_start(out=gam[:], in_=nrm_d[:, gcol:gcol + D]), writes=[Bg])
                P.op("dve", lambda e: e.tensor_copy(out=identb[:], in_=ident), reads=[Bcst], writes=[Bg])
                for c in range(NCH):
                    par = c % 2
                    Br = rms_rstd(c, par, junk, Bj)
                    Bu = B(f"ubf{par}")
                    P.op("dve", lambda e: e.scalar_tensor_tensor(
                        out=ubf[par][:], in0=h[:, c, :], scalar=rstd[:, par:par + 1], in1=gam[:],
                        op0=ALU.mult, op1=ALU.mult), reads=[Bh[c], Br, Bg], writes=[Bu])
                    pb = par
                    pv = psum[pb][:].bitcast(BF16)

                    def tr(e):
                        r = None
                        for dc in range(NDC):
                            r = e.transpose(out=pv[:, dc * 128:(dc + 1) * 128],
                                            in_=ubf[par][:, dc * 128:(dc + 1) * 128], identity=identb[:])
                        return r
                    P.op("pe", tr, reads=[Bu, Bg], writes=[Bps[pb]])
                    src = pv.rearrange("p (a b) -> p a b", a=NDC)
                    if par == 0:
                        P.op("act", lambda e: e.copy(out=uT[:, :, c * 128:(c + 1) * 128], in_=src),
                             reads=[Bps[pb]], writes=[BuT[c // 4]])
                    else:
                        P.op("dve", lambda e: e.tensor_copy(out=uT[:, :, c * 128:(c + 1) * 128], in_=src),
                             reads=[Bps[pb]], writes=[BuT[c // 4]])
                P.barrier()

        def out_proj(l, yT, ByT):
            w_out = L[l, "w_out"]
            with ExitStack() as ms:
                MC = Ctx(nc, ms)
                wo = MC.sb("wo", [128, NDC, D], BF16)
                Bwo = B("wo")
                for half in range(2):
                    P.dma("pool", lambda e: e.dma_start(
                        out=wo[:, :, half * 512:(half + 1) * 512],
                        in_=w_out[:, half * 512:(half + 1) * 512].rearrange("(c p) n -> p c n", p=128)),
                        writes=[Bwo])
                for c in range(NCH):
                    for half in range(2):
                        pb = (c * 2 + half) % 8

                        def mm(e):
                            r = None
                            for g in range(NDC):
                                r = e.matmul(psum[pb][:], lhsT=yT[:, g, c * 128:(c + 1) * 128],
                                             rhs=wo[:, g, half * 512:(half + 1) * 512],
                                             start=(g == 0), stop=(g == NDC - 1))
                            return r
                        P.op("pe", mm, reads=list(ByT) + [Bwo], writes=[Bps[pb]])
                        P.op("dve", lambda e: e.tensor_tensor(out=h[:, c, half * 512:(half + 1) * 512],
                                                              in0=h[:, c, half * 512:(half + 1) * 512],
                                                              in1=psum[pb][:], op=ALU.add),
                             reads=[Bps[pb], Bh[c]], writes=[Bh[c]])
                P.barrier()

        def lru_layer(l):
            w_in, vec_d, w_a, w_x = L[l, "w_in"], L[l, "vec"], L[l, "w_a"], L[l, "w_x"]
            PAD = 4
            with ExitStack() as ls:
                LC = Ctx(nc, ls)
                yT = LC.sb("yT", [128, NDC, S], BF16)
                ByT = [B(f"yT{g}") for g in range(NDC)]
                with ExitStack() as us:
                    UC = Ctx(nc, us)
                    uT = UC.sb("uT", [128, NDC, S], BF16)
                    BuT = [B(f"uTq{i}") for i in range(4)]
                    norm_to_uT(l, uT, BuT)
                    with ExitStack() as ms:
                        MC = Ctx(nc, ms)
                        vec = MC.sb("vec", [128, 8, 8], F32)
                        sc = MC.sb("sc", [128, 8, 2], F32)
                        wa = MC.sb("wa", [128, 8, 128], BF16)
                        wx = MC.sb("wx", [128, 8, 128], BF16)
                        wig = [MC.sb(f"wig{i}", [128, NDC, 128], BF16) for i in range(2)]
                        wix = [MC.sb(f"wix{i}", [128, NDC, 128], BF16) for i in range(2)]
                        XB = MC.sb("XB", [128, PAD + S], F32)
                        XC = MC.sb("XC", [128, S], F32)
                        XCB = MC.sb("XCB", [128, S], BF16)
                        R = MC.sb("R", [128, S], F32)
                        I_ = MC.sb("I", [128, S], F32)
                        A = MC.sb("A", [128, S], F32)
                        Bv = B("lru_small")
                        BXB, BXC, BXCB, BR, BI, BA = B("XB"), B("XC"), B("XCB"), B("R"), B("I"), B("A")
                        Bwig = [B(f"wig{i}") for i in range(2)]
                        Bwix = [B(f"wix{i}") for i in range(2)]
                        P.dma("sp", lambda e: e.dma_start(out=vec[:].rearrange("p g k -> p (g k)"), in_=vec_d),
                              writes=[Bv])
                        P.dma("pool", lambda e: e.dma_start(out=wa[:], in_=w_a.rearrange("g i o -> i g o")),
                              writes=[Bv])
                        P.dma("pool", lambda e: e.dma_start(out=wx[:], in_=w_x.rearrange("g i o -> i g o")),
                              writes=[Bv])
                        P.op("act", lambda e: e.activation(out=sc[:, :, 0], in_=vec[:, :, 7], func=AF.Exp, scale=-1.0),
                             reads=[Bv], writes=[Bv])
                        P.op("act", lambda e: e.activation(out=sc[:, :, 0], in_=sc[:, :, 0], func=AF.Ln, bias=1.0,
                                                           scale=1.0), reads=[Bv], writes=[Bv])
                        P.op("dve", lambda e: e.tensor_scalar(out=sc[:, :, 1], in0=sc[:, :, 0], scalar1=-16.0,
                                                              scalar2=None, op0=ALU.mult), reads=[Bv], writes=[Bv])
                        P.op("dve", lambda e: e.tensor_scalar(out=sc[:, :, 0], in0=sc[:, :, 0], scalar1=-8.0,
                                                              scalar2=None, op0=ALU.mult), reads=[Bv], writes=[Bv])
                        P.op("dve", lambda e: e.memset(XB[:, 0:PAD], 0.0), writes=[BXB])

                        def load_w(g):
                            i2 = g % 2
                            P.dma("pool", lambda e: e.dma_start(
                                out=wig[i2][:], in_=w_in[:, g * 128:(g + 1) * 128].rearrange("(c p) n -> p c n", p=128)),
                                writes=[Bwig[i2]])
                            P.dma("pool", lambda e: e.dma_start(
                                out=wix[i2][:],
                                in_=w_in[:, D + g * 128:D + (g + 1) * 128].rearrange("(c p) n -> p c n", p=128)),
                                writes=[Bwix[i2]])

                        def proj(wt, Bwt, tq, pb):
                            def mm(e):
                                r = None
                                for dc in range(NDC):
                                    r = e.matmul(psum[pb][:], lhsT=wt[:, dc, :], rhs=uT[:, dc, tq * 512:(tq + 1) * 512],
                                                 start=(dc == 0), stop=(dc == NDC - 1))
                                return r
                            P.op("pe", mm, reads=[Bwt, BuT[tq]], writes=[Bps[pb]])

                        load_w(0)
                        for g in range(NDC):
                            i2 = g % 2
                            if g + 1 < NDC:
                                load_w(g + 1)
                            for tq in range(4):
                                proj(wix[i2], Bwix[i2], tq, tq)
                                P.op("act", lambda e: e.copy(out=XB[:, PAD + tq * 512:PAD + (tq + 1) * 512],
                                                             in_=psum[tq][:]), reads=[Bps[tq]], writes=[BXB])
                            P.op("dve", lambda e: e.tensor_scalar(out=XC[:], in0=XB[:, PAD:PAD + S],
                                                                  scalar1=vec[:, g, 3:4], scalar2=vec[:, g, 4:5],
                                                                  op0=ALU.mult, op1=ALU.add),
                                 reads=[BXB, Bv], writes=[BXC])
                            for j in range(1, 4):
                                P.op("dve", lambda e: e.scalar_tensor_tensor(
                                    out=XC[:], in0=XB[:, PAD - j:PAD - j + S], scalar=vec[:, g, 3 - j:4 - j], in1=XC[:],
                                    op0=ALU.mult, op1=ALU.add), reads=[BXB, Bv, BXC], writes=[BXC])
                            P.op("act", lambda e: e.copy(out=XCB[:], in_=XC[:]), reads=[BXC], writes=[BXCB])
                            for tq in range(4):
                                P.op("pe", lambda e: e.matmul(psum[tq][:], lhsT=wa[:, g, :],
                                                              rhs=XCB[:, tq * 512:(tq + 1) * 512], start=True, stop=True),
                                     reads=[BXCB, Bv], writes=[Bps[tq]])
                                P.op("act", lambda e: e.activation(out=R[:, tq * 512:(tq + 1) * 512], in_=psum[tq][:],
                                                                   func=AF.Sigmoid, bias=vec[:, g, 5:6], scale=1.0),
                                     reads=[Bps[tq], Bv], writes=[BR])
                            for tq in range(4):
                                P.op("pe", lambda e: e.matmul(psum[4 + tq][:], lhsT=wx[:, g, :],
                                                              rhs=XCB[:, tq * 512:(tq + 1) * 512], start=True, stop=True),
                                     reads=[BXCB, Bv], writes=[Bps[4 + tq]])
                                P.op("act", lambda e: e.activation(out=I_[:, tq * 512:(tq + 1) * 512],
                                                                   in_=psum[4 + tq][:], func=AF.Sigmoid,
                                                                   bias=vec[:, g, 6:7], scale=1.0),
                                     reads=[Bps[4 + tq], Bv], writes=[BI])
                            P.op("act", lambda e: e.activation(out=A[:], in_=R[:], func=AF.Exp, scale=sc[:, g, 0:1]),
                                 reads=[BR, Bv], writes=[BA])
                            P.op("act", lambda e: e.activation(out=R[:], in_=R[:], func=AF.Exp, scale=sc[:, g, 1:2]),
                                 reads=[BR, Bv], writes=[BR])
                            P.op("dve", lambda e: e.tensor_scalar(out=R[:], in0=R[:], scalar1=-1.0, scalar2=1.0,
                                                                  op0=ALU.mult, op1=ALU.add), reads=[BR], writes=[BR])
                            P.op("act", lambda e: e.activation(out=R[:], in_=R[:], func=AF.Sqrt), reads=[BR],
                                 writes=[BR])
                            P.op("dve", lambda e: e.tensor_tensor(out=I_[:], in0=I_[:], in1=XC[:], op=ALU.mult),
                                 reads=[BI, BXC], writes=[BI])
                            P.op("dve", lambda e: e.tensor_tensor(out=I_[:], in0=I_[:], in1=R[:], op=ALU.mult),
                                 reads=[BI, BR], writes=[BI])
                            Y = XB[:, PAD:PAD + S]
                            P.op("dve", lambda e: e.tensor_tensor_scan(out=Y, data0=A[:], data1=I_[:], initial=0.0,
                                                                       op0=ALU.mult, op1=ALU.add),
                                 reads=[BA, BI], writes=[BXB])
                            for tq in range(4):
                                pb = 4 + tq
                                sl = slice(tq * 512, (tq + 1) * 512)
                                proj(wig[i2], Bwig[i2], tq, pb)
                                P.op("act", lambda e: e.activation(out=R[:, sl], in_=psum[pb][:], func=AF.Square),
                                     reads=[Bps[pb]], writes=[BR])
                                P.op("dve", lambda e: e.tensor_scalar(out=R[:, sl], in0=R[:, sl], scalar1=0.044715,
                                                                      scalar2=1.0, op0=ALU.mult, op1=ALU.add),
                                     reads=[BR], writes=[BR])
                                P.op("dve", lambda e: e.tensor_tensor(out=R[:, sl], in0=R[:, sl], in1=psum[pb][:],
                                                                      op=ALU.mult), reads=[BR, Bps[pb]], writes=[BR])
                                P.op("act", lambda e: e.activation(out=R[:, sl], in_=R[:, sl], func=AF.Sigmoid,
                                                                   scale=1.5957691216057308), reads=[BR], writes=[BR])
                                P.op("dve", lambda e: e.tensor_tensor(out=R[:, sl], in0=R[:, sl], in1=psum[pb][:],
                                                                      op=ALU.mult), reads=[BR, Bps[pb]], writes=[BR])
                                P.op("dve", lambda e: e.tensor_tensor(out=yT[:, g, sl], in0=R[:, sl], in1=Y[:, sl],
                                                                      op=ALU.mult), reads=[BR, BXB], writes=[ByT[g]])
                        P.barrier()
                out_proj(l, yT, ByT)

        def pool_layer(l):
            w_in, w_grp, vec_d = L[l, "w_in"], L[l, "w_grp"], L[l, "vec"]
            PAD = 16
            WINS = (2, 4, 8, 16)
            with ExitStack() as ls:
                LC = Ctx(nc, ls)
                yT = LC.sb("yT", [128, NDC, S], BF16)
                ByT = [B(f"yT{g}") for g in range(NDC)]
                with ExitStack() as us:
                    UC = Ctx(nc, us)
                    uT = UC.sb("uT", [128, NDC, S], BF16)
                    BuT = [B(f"uTq{i}") for i in range(4)]
                    norm_to_uT(l, uT, BuT)
                    with ExitStack() as ms:
                        MC = Ctx(nc, ms)
                        vec = MC.sb("pvec", [128, 8 + 64], F32)
                        wgr = MC.sb("wgr", [128, 4, 2, 256], BF16)
                        wi = [MC.sb(f"wi{i}", [128, NDC, 128], BF16) for i in range(2)]
                        V_ = [MC.sb(f"V{i}", [128, PAD + S], F32) for i in range(2)]
                        S1 = [MC.sb(f"S1{i}", [128, PAD + S], F32) for i in range(2)]
                        S2 = [MC.sb(f"S2{i}", [128, PAD + S], F32) for i in range(2)]
                        PT = [MC.sb(f"PT{i}", [128, S], BF16) for i in range(2)]
                        Bv = B("pool_small")
                        Bwi = [B(f"pwi{i}") for i in range(2)]
                        BV = [B(f"pV{i}") for i in range(2)]
                        BS1 = [B(f"pS1{i}") for i in range(2)]
                        BS2 = [B(f"pS2{i}") for i in range(2)]
                        BPT = [B(f"pPT{i}") for i in range(2)]
                        P.dma("sp", lambda e: e.dma_start(out=vec[:], in_=vec_d), writes=[Bv])
                        P.dma("pool", lambda e: e.dma_start(
                            out=wgr[:].rearrange("p a b o -> p (a b) o"),
                            in_=w_grp.rearrange("a (b p) o -> p (a b) o", p=128)), writes=[Bv])
                        for i in range(2):
                            P.op("dve", lambda e: e.memset(V_[i][:, 0:PAD], 0.0), writes=[BV[i]])
                            P.op("dve", lambda e: e.memset(S1[i][:, 0:PAD], 0.0), writes=[BS1[i]])
                            P.op("dve", lambda e: e.memset(S2[i][:, 0:PAD], 0.0), writes=[BS2[i]])

                        def load_w(g):
                            i2 = g % 2
                            P.dma("pool", lambda e: e.dma_start(
                                out=wi[i2][:], in_=w_in[:, g * 128:(g + 1) * 128].rearrange("(c p) n -> p c n", p=128)),
                                writes=[Bwi[i2]])

                        load_w(0)
                        for g in range(NDC):
                            i2 = g % 2
                            grp = g // 2
                            win = WINS[grp]
                            if g + 1 < NDC:
                                load_w(g + 1)
                            for tq in range(4):
                                pb = (g % 2) * 4 + tq

                                def mm(e):
                                    r = None
                                    for dc in range(NDC):
                                        r = e.matmul(psum[pb][:], lhsT=wi[i2][:, dc, :],
                                                     rhs=uT[:, dc, tq * 512:(tq + 1) * 512],
                                                     start=(dc == 0), stop=(dc == NDC - 1))
                                    return r
                                P.op("pe", mm, reads=[Bwi[i2], BuT[tq]], writes=[Bps[pb]])
                                P.op("act", lambda e: e.copy(out=V_[i2][:, PAD + tq * 512:PAD + (tq + 1) * 512],
                                                             in_=psum[pb][:]), reads=[Bps[pb]], writes=[BV[i2]])
                            cur, Bcur = V_[i2], BV[i2]
                            w = 1
                            nxt = [(S1[i2], BS1[i2]), (S2[i2], BS2[i2])]
                            k = 0
                            while w < win:
                                dst, Bdst = nxt[k % 2]
                                P.op("dve", lambda e: e.tensor_tensor(out=dst[:, PAD:PAD + S], in0=cur[:, PAD:PAD + S],
                                                                      in1=cur[:, PAD - w:PAD - w + S], op=ALU.add),
                                     reads=[Bcur], writes=[Bdst])
                                cur, Bcur = dst, Bdst
                                w *= 2
                                k += 1
                            P.op("dve", lambda e: e.scalar_tensor_tensor(
                                out=PT[i2][:], in0=cur[:, PAD:PAD + S], scalar=1.0 / win, in1=V_[i2][:, PAD:PAD + S],
                                op0=ALU.mult, op1=ALU.subtract), reads=[Bcur, BV[i2]], writes=[BPT[i2]])
                            P.op("dve", lambda e: e.tensor_tensor(out=cur[:, PAD:PAD + 16], in0=cur[:, PAD:PAD + 16],
                                                                  in1=vec[:, 8 + grp * 16:8 + (grp + 1) * 16],
                                                                  op=ALU.mult), reads=[Bcur, Bv], writes=[Bcur])
                            P.op("dve", lambda e: e.tensor_tensor(out=PT[i2][:, 0:16], in0=cur[:, PAD:PAD + 16],
                                                                  in1=V_[i2][:, PAD:PAD + 16], op=ALU.subtract),
                                 reads=[Bcur, BV[i2]], writes=[BPT[i2]])
                            if g % 2 == 1:
                                for oc in range(2):
                                    go = grp * 2 + oc
                                    for tq in range(4):
                                        pb = oc * 4 + tq

                                        def mg(e):
                                            r = None
                                            for ic in range(2):
                                                r = e.matmul(psum[pb][:], lhsT=wgr[:, grp, ic, oc * 128:(oc + 1) * 128],
                                                             rhs=PT[ic][:, tq * 512:(tq + 1) * 512],
                                                             start=(ic == 0), stop=(ic == 1))
                                            return r
                                        P.op("pe", mg, reads=[BPT[0], BPT[1], Bv], writes=[Bps[pb]])
                                        P.op("act", lambda e: e.activation(
                                            out=yT[:, go, tq * 512:(tq + 1) * 512], in_=psum[pb][:], func=AF.Copy,
                                            scale=vec[:, go:go + 1]), reads=[Bps[pb], Bv], writes=[ByT[go]])
                        P.barrier()
                out_proj(l, yT, ByT)


        def sb_layer(l):
            w_qkv, negm_d = L[l, "w_qkv"], L[l, "negm"]
            NH = 16
            with ExitStack() as ls:
                LC = Ctx(nc, ls)
                QT = LC.sb("QT", [128, NDC, S], BF16)
                KT = LC.sb("KT", [128, NDC, S], BF16)
                BQ = [[B(f"QT{hd}_{qg}") for qg in range(4)] for hd in range(NH)]
                BK = [B(f"KT{g}") for g in range(NDC)]
                Bvd = [B(f"vst{c}") for c in range(NCH)]
                with ExitStack() as us:
                    UC = Ctx(nc, us)
                    uT = UC.sb("uT", [128, NDC, S], BF16)
                    BuT = [B(f"uTq{i}") for i in range(4)]
                    norm_to_uT(l, uT, BuT)
                    with ExitStack() as ms:
                        MC = Ctx(nc, ms)
                        wq = [MC.sb(f"wq{i}", [128, NDC, 128], BF16) for i in range(2)]
                        wv = MC.sb("wv", [128, NDC, 512], BF16)
                        vst = [MC.sb(f"vst{i}", [128, 512], BF16) for i in range(2)]
                        Bwq = [B(f"wq{i}") for i in range(2)]
                        Bwv = B("wv")
                        Bvs = [B(f"vstage{i}") for i in range(2)]

                        def load_wq(j):
                            i2 = j % 2
                            P.dma("pool", lambda e: e.dma_start(
                                out=wq[i2][:], in_=w_qkv[:, j * 128:(j + 1) * 128].rearrange("(c p) n -> p c n", p=128)),
                                writes=[Bwq[i2]])
                        load_wq(0)
                        for j in range(16):
                            i2 = j % 2
                            if j + 1 < 16:
                                load_wq(j + 1)
                            g = j % 8
                            for tq in range(4):
                                pb = (j % 2) * 4 + tq

                                def mm(e):
                                    r = None
                                    for dc in range(NDC):
                                        r = e.matmul(psum[pb][:], lhsT=wq[i2][:, dc, :],
                                                     rhs=uT[:, dc, tq * 512:(tq + 1) * 512],
                                                     start=(dc == 0), stop=(dc == NDC - 1))
                                    return r
                                P.op("pe", mm, reads=[Bwq[i2], BuT[tq]], writes=[Bps[pb]])
                                if j < 8:
                                    P.op("act", lambda e: e.activation(out=QT[:, g, tq * 512:(tq + 1) * 512],
                                                                       in_=psum[pb][:], func=AF.Copy, scale=0.125),
                                         reads=[Bps[pb]], writes=[BQ[2 * g][tq], BQ[2 * g + 1][tq]])
                                else:
                                    P.op("dve", lambda e: e.tensor_copy(out=KT[:, g, tq * 512:(tq + 1) * 512],
                                                                        in_=psum[pb][:]),
                                         reads=[Bps[pb]], writes=[BK[g]])
                        for half in range(2):
                            P.dma("pool", lambda e: e.dma_start(
                                out=wv[:], in_=w_qkv[:, 2 * D + half * 512:2 * D + (half + 1) * 512].rearrange(
                                    "(c p) n -> p c n", p=128)), writes=[Bwv])
                            for c in range(NCH):
                                pb = c % 8
                                i2 = c % 2

                                def mv(e):
                                    r = None
                                    for dc in range(NDC):
                                        r = e.matmul(psum[pb][:], lhsT=uT[:, dc, c * 128:(c + 1) * 128],
                                                     rhs=wv[:, dc, :], start=(dc == 0), stop=(dc == NDC - 1))
                                    return r
                                P.op("pe", mv, reads=[Bwv, BuT[c // 4]], writes=[Bps[pb]])
                                if c % 2 == 0:
                                    P.op("act", lambda e: e.copy(out=vst[i2][:], in_=psum[pb][:]),
                                         reads=[Bps[pb]], writes=[Bvs[i2]])
                                else:
                                    P.op("dve", lambda e: e.tensor_copy(out=vst[i2][:], in_=psum[pb][:]),
                                         reads=[Bps[pb]], writes=[Bvs[i2]])
                                P.dma("sp", lambda e: e.dma_start(
                                    out=ust_d[c * 128:(c + 1) * 128, half * 512:(half + 1) * 512], in_=vst[i2][:]),
                                    reads=[Bvs[i2]], writes=[Bvd[c]])
                        P.barrier()
                with ExitStack() as ms:
                    MC = Ctx(nc, ms)
                    Vt = MC.sb("Vt", [128, NCH, D], BF16)
                    BV = B("Vt")
                    for c in range(NCH):
                        P.dma("sp", lambda e: e.dma_start(out=Vt[:, c, :], in_=ust_d[c * 128:(c + 1) * 128, :]),
                              reads=[Bvd[c]], writes=[BV])
                    negm = MC.sb("negm", [128, 4, 512], BF16)
                    ntri = MC.sb("ntri", [128, 128], BF16)
                    nones = MC.sb("nones", [128, 128], BF16)
                    identb = MC.sb("identb2", [128, 128], BF16)
                    Bc2 = B("sb_consts")
                    P.dma("pool", lambda e: e.dma_start(out=negm[:].rearrange("p a b -> p (a b)"), in_=negm_d),
                          writes=[Bc2])
                    P.op("dve", lambda e: e.tensor_scalar(out=ntri[:], in0=cst[:, 384:512], scalar1=-1.0, scalar2=None,
                                                          op0=ALU.mult), reads=[Bcst], writes=[Bc2])
                    P.op("dve", lambda e: e.tensor_scalar(out=nones[:], in0=ones, scalar1=-1.0, scalar2=None,
                                                          op0=ALU.mult), reads=[Bcst], writes=[Bc2])
                    P.op("dve", lambda e: e.tensor_copy(out=identb[:], in_=ident), reads=[Bcst], writes=[Bc2])
                    NLB, NAT, NE_ = 3, 3, 2
                    Lb = [MC.sb(f"Lb{i}", [128, 512], BF16) for i in range(NLB)]
                    AT = [MC.sb(f"AT{i}", [128, 512], BF16) for i in range(NAT)]
                    Et = [MC.sb(f"Et{i}", [128, 512], F32) for i in range(NE_)]
                    Lacc = MC.sb("Lacc", [128, 512], F32)
                    Laccb = [MC.sb(f"Laccb{i}", [128, 512], BF16) for i in range(2)]
                    BLb = [B(f"Lb{i}") for i in range(NLB)]
                    BAT = [B(f"AT{i}") for i in range(NAT)]
                    BEt = [B(f"Et{i}") for i in range(NE_)]
                    BLacc = B("Lacc")
                    BLaccb = [B(f"Laccb{i}") for i in range(2)]
                    blocks = []
                    for hd in range(NH):
                        for qg in range(4):
                            nkb = 4 * qg + 4
                            for kb in reversed(range(nkb)):
                                blocks.append(dict(hd=hd, qg=qg, kb=kb, first=(kb == nkb - 1), last=(kb == 0),
                                                   grp=hd * 4 + qg))
                    nacc = [0]

                    def stage_a(i, b):
                        hd, qg, kb = b["hd"], b["qg"], b["kb"]
                        g, pbase = hd // 2, (hd % 2) * 64
                        xb = i % 4
                        diag = kb >= 4 * qg
                        m = kb - 4 * qg

                        def mz(e):
                            r = e.matmul(psum[xb][:], lhsT=KT[pbase:pbase + 64, g, kb * 128:(kb + 1) * 128],
                                         rhs=QT[pbase:pbase + 64, g, qg * 512:(qg + 1) * 512], start=True,
                                         stop=True, skip_group_check=True)
                            if diag:
                                r = e.matmul(psum[xb][:], lhsT=identb[:], rhs=negm[:, m, :], start=False, stop=True,
                                             skip_group_check=True)
                            return r
                        P.op("pe", mz, reads=[BK[g], BQ[hd][qg], Bc2], writes=[Bps[xb]])
                        P.op("act", lambda e: e.activation(out=Et[i % NE_][:], in_=psum[xb][:], func=AF.Exp),
                             reads=[Bps[xb]], writes=[BEt[i % NE_]])
                        P.op("act", lambda e: e.activation(out=Lb[i % NLB][:], in_=Et[i % NE_][:], func=AF.Ln,
                                                           bias=1.0, scale=1.0),
                             reads=[BEt[i % NE_]], writes=[BLb[i % NLB]])

                    def stage_b(i, b):
                        hd, qg, kb = b["hd"], b["qg"], b["kb"]
                        g, pbase = hd // 2, (hd % 2) * 64
                        xb = i % 4
                        ob = 4 + b["grp"] % 2
                        first, last = b["first"], b["last"]
                        la = nacc[0] % 2

                        def m2(e):
                            r = e.matmul(psum[xb][:], lhsT=ntri[:], rhs=Lb[i % NLB][:], start=False, stop=True,
                                         skip_group_check=True)
                            if not first:
                                r = e.matmul(psum[xb][:], lhsT=nones[:], rhs=Laccb[la][:], start=False, stop=True,
                                             skip_group_check=True)
                            return r
                        P.op("pe", m2, reads=[BLb[i % NLB], Bc2] + ([] if first else [BLaccb[la]]), writes=[Bps[xb]])
                        P.op("act", lambda e: e.activation(out=AT[i % NAT][:], in_=psum[xb][:], func=AF.Exp),
                             reads=[Bps[xb]], writes=[BAT[i % NAT]])
                        P.op("pe", lambda e: e.matmul(psum[ob][:], lhsT=Vt[:, kb, g * 128:(g + 1) * 128],
                                                      rhs=AT[i % NAT][:], start=first, stop=last,
                                                      skip_group_check=True),
                             reads=[BV, BAT[i % NAT]], writes=[Bps[ob]])
                        if not last:
                            if first:
                                P.op("dve", lambda e: e.tensor_copy(out=Lacc[:], in_=Lb[i % NLB][:]),
                                     reads=[BLb[i % NLB]], writes=[BLacc])
                            else:
                                P.op("dve", lambda e: e.tensor_tensor(out=Lacc[:], in0=Lacc[:], in1=Lb[i % NLB][:],
                                                                      op=ALU.add),
                                     reads=[BLb[i % NLB], BLacc], writes=[BLacc])
                            nacc[0] += 1
                            lb2 = nacc[0] % 2
                            P.op("dve", lambda e: e.tensor_copy(out=Laccb[lb2][:], in_=Lacc[:]),
                                 reads=[BLacc], writes=[BLaccb[lb2]])
                        else:
                            P.op("act", lambda e: e.copy(out=QT[pbase:pbase + 64, g, qg * 512:(qg + 1) * 512],
                                                         in_=psum[ob][pbase:pbase + 64, :]),
                                 reads=[Bps[ob]], writes=[BQ[hd][qg]])

                    nb = len(blocks)
                    for i in range(nb + 1):
                        if i < nb:
                            stage_a(i, blocks[i])
                        if i >= 1:
                            stage_b(i - 1, blocks[i - 1])
                    P.barrier()
                out_proj(l, QT, [BQ[hd][qg] for hd in range(NH) for qg in range(4)])


        for l in layers:
            if cfg["mix"][l]:
                [lru_layer, pool_layer, sb_layer][l % 3](l)
            if cfg["moe"][l]:
                moe_layer(l)

        if cfg.get("final", False):
            fcol = 8 * D
            with ExitStack() as ms:
                MC = Ctx(nc, ms)
                gam = MC.sb("gamf", [128, D], F32)
                junk = MC.sb("junkf", [128, D], F32)
                P.dma("sp", lambda e: e.dma_start(out=gam[:], in_=nrm_d[:, fcol:fcol + D]), writes=[B("gamf")])
                for c in range(NCH):
                    par = c % 2
                    Br = rms_rstd(c, par, junk, B("junkf"))
                    P.op("dve", lambda e: e.scalar_tensor_tensor(
                        out=h[:, c, :], in0=h[:, c, :], scalar=rstd[:, par:par + 1],
                        in1=gam[:], op0=ALU.mult, op1=ALU.mult),
                        reads=[Bh[c], Br, B("gamf")], writes=[Bh[c]])
                P.barrier()
        Bout = B("out_d")
        for c in range(NCH):
            P.dma("sp", lambda e: e.dma_start(out=out_d[c * 128:(c + 1) * 128, :], in_=h[:, c, :]),
                  reads=[Bh[c]], writes=[Bout])
        P.barrier()
        print("instructions:", P.n_instr, {k: v for k, v in P.ecnt.items()})
    return nc


def host_consts():
    ident = np.eye(128, dtype=np.float32)
    tri = np.triu(np.ones((128, 128), np.float32), 1)
    ones = np.ones((128, 128), np.float32)
    lowi = np.tril(np.ones((128, 128), np.float32))
    cst = np.concatenate([ident, tri, ones, lowi], axis=1)
    ecap = np.tile((np.arange(NE, dtype=np.float32) * CAP)[None, None, :], (128, NCH, 1)).reshape(128, NCH * NE)
    return np.ascontiguousarray(cst), np.ascontiguousarray(ecap)


def layer_inputs(inputs, l):
    m = {}
    m[f"w_router{l}"] = np.ascontiguousarray(inputs["moe_w_router"][l])
    m[f"b_router{l}"] = np.ascontiguousarray(np.broadcast_to(inputs["moe_b_router"][l][None, :], (128, NE)))
    m[f"w_gu{l}"] = np.ascontiguousarray(inputs["moe_w_gate_up"][l])
    bgu = inputs["moe_b_gate_up"][l].reshape(NE, 16, 128).transpose(2, 0, 1).reshape(128, NE * 16)
    m[f"b_gu{l}"] = np.ascontiguousarray(bgu)
    m[f"w_dn{l}"] = np.ascontiguousarray(inputs["moe_w_down"][l])
    m[f"b_dn{l}"] = np.ascontiguousarray(inputs["moe_b_down"][l])
    return m


def mixer_inputs(inputs, l):
    kind, slot = l % 3, l // 3
    m = {}
    if kind == 0:
        m[f"lru_w_in{l}"] = np.ascontiguousarray(inputs["lru_w_in"][slot])
        rows = [inputs["lru_conv_w"][slot][k] for k in range(4)] + [
            inputs["lru_conv_b"][slot], inputs["lru_b_a"][slot], inputs["lru_b_x"][slot], inputs["lru_a_param"][slot]]
        v = np.stack(rows, axis=-1).reshape(8, 128, 8).transpose(1, 0, 2).reshape(128, 64)
        m[f"lru_vec{l}"] = np.ascontiguousarray(v.astype(np.float32))
        m[f"lru_w_a{l}"] = np.ascontiguousarray(inputs["lru_w_a"][slot])
        m[f"lru_w_x{l}"] = np.ascontiguousarray(inputs["lru_w_x"][slot])
        m[f"mix_w_out{l}"] = np.ascontiguousarray(inputs["lru_w_out"][slot])
    elif kind == 1:
        m[f"pool_w_in{l}"] = np.ascontiguousarray(inputs["pool_w_in"][slot])
        m[f"pool_w_grp{l}"] = np.ascontiguousarray(inputs["pool_w_group"][slot])
        sc = inputs["pool_scale"][slot].reshape(8, 128).T
        corr = np.ones((4, 16), np.float32)
        for gi, w in enumerate((2, 4, 8, 16)):
            t = np.arange(16)
            corr[gi] = 1.0 / np.minimum(t + 1, w)
        v = np.concatenate([sc, np.broadcast_to(corr.reshape(1, 64), (128, 64))], axis=1)
        m[f"pool_vec{l}"] = np.ascontiguousarray(v.astype(np.float32))
        m[f"mix_w_out{l}"] = np.ascontiguousarray(inputs["pool_w_out"][slot])
    else:
        m[f"sb_w_qkv{l}"] = np.ascontiguousarray(inputs["sb_w_qkv"][slot])
        m[f"mix_w_out{l}"] = np.ascontiguousarray(inputs["sb_w_out"][slot])
        nm = np.zeros((128, 4, 512), np.float32)
        sl = np.arange(128)[:, None]
        tl = np.arange(512)[None, :]
        for mi in range(4):
            nm[:, mi, :] = np.where(mi * 128 + sl < tl, 0.0, -30000.0)
        m[f"sb_negmask{l}"] = np.ascontiguousarray(nm.reshape(128, 2048))
    return m


def norms_input(inputs):
    rows = [inputs["mix_norm"][i] for i in range(4)] + [inputs["ffn_norm"][i] for i in range(4)] + [inputs["final_norm"]]
    v = np.concatenate(rows).astype(np.float32)
    return np.ascontiguousarray(np.broadcast_to(v[None, :], (128, 9 * D)))


N_CORES = 8


def kernel(**inputs):
    inputs = {k: np.asarray(v) for k, v in inputs.items()}
    cfg = dict(layers=[0, 1, 2, 3], moe=[True] * 4, mix=[True] * 4, final=True)
    nc = bass.Bass("TRN2", target_bir_lowering=False)
    build(nc, cfg)
    cst, ecap = host_consts()
    shared = {"cst": cst, "ecap": ecap, "norms": norms_input(inputs)}
    for l in range(4):
        shared.update(layer_inputs(inputs, l))
        shared.update(mixer_inputs(inputs, l))
    x = np.ascontiguousarray(inputs["x"].astype(np.float32))
    in_maps = []
    for c in range(N_CORES):
        m = dict(shared)
        m["x"] = x[c]
        in_maps.append(m)
    res = run_bass_kernel_spmd(nc, in_maps, core_ids=list(range(N_CORES)))
    out = np.stack([np.asarray(r["out"]) for r in res.results], axis=0)
    return out.astype(np.float32)
```

```python
import numpy as np
from contextlib import ExitStack
import concourse.bass as bass
import concourse.mybir as mybir
from concourse.bass_utils import run_bass_kernel_spmd

F32 = mybir.dt.float32
BF16 = mybir.dt.bfloat16
I32 = mybir.dt.int32
U32 = mybir.dt.uint32
AF = mybir.ActivationFunctionType
ALU = mybir.AluOpType
AX = mybir.AxisListType

D = 1024
S = 2048
NCH = S // 128
NDC = D // 128
NE = 32
TOPK = 4
CAP = 384
NSLOT = NE * CAP
RMS_EPS = 1e-6
SAME_ENG_SYNC = True


class Buf:
    __slots__ = ("name", "w", "r")

    def __init__(self, name):
        self.name = name
        self.w = None
        self.r = {}


class Prog:
    def __init__(self, nc, stack, n_dsem=(20, 8, 20)):
        self.nc = nc
        self.eng = {"pe": nc.tensor, "dve": nc.vector, "act": nc.scalar,
                    "pool": nc.gpsimd, "sp": nc.sync}
        self.esem = {k: stack.enter_context(nc.semaphore("es_" + k)) for k in self.eng}
        self.ecnt = {k: 0 for k in self.eng}
        self.waited = {k: {} for k in self.eng}
        self.dq = {}
        for q, n in zip(("sp", "act", "pool"), n_dsem):
            sems = [stack.enter_context(nc.semaphore(f"ds_{q}{i}")) for i in range(n)]
            self.dq[q] = {"sems": sems, "cnt": [0] * n, "next": 0}
        self.n_instr = 0

    def _wait(self, e, evs):
        for ev in evs:
            if ev is None:
                continue
            key, sem, val = ev
            if not SAME_ENG_SYNC and key == e:
                continue
            if key == e and e == "pe":
                continue
            if self.waited[e].get(key, 0) >= val:
                continue
            self.eng[e].wait_ge(sem, val)
            self.waited[e][key] = val

    def _deps(self, reads, writes):
        evs = []
        for b in reads:
            evs.append(b.w)
        for b in writes:
            evs.append(b.w)
            evs.extend(b.r.values())
        return evs

    def _commit(self, ev, reads, writes):
        for b in reads:
            old = b.r.get(ev[0])
            if old is None or old[2] < ev[2]:
                b.r[ev[0]] = ev
        for b in writes:
            b.w = ev
            b.r = {}

    def op(self, e, fn, reads=(), writes=()):
        self._wait(e, self._deps(reads, writes))
        ins = fn(self.eng[e])
        self.ecnt[e] += 1
        ins.then_inc(self.esem[e], 1)
        ev = (e, self.esem[e], self.ecnt[e])
        self._commit(ev, reads, writes)
        self.n_instr += 1
        return ev

    def dma(self, q, fn, reads=(), writes=()):
        d = self.dq[q]
        i = d["next"]
        d["next"] = (i + 1) % len(d["sems"])
        key = f"d_{q}{i}"
        evs = self._deps(reads, writes)
        if d["cnt"][i] > 0:
            evs.append((key, d["sems"][i], d["cnt"][i]))
        self._wait(q, evs)
        ins = fn(self.eng[q])
        d["cnt"][i] += 16
        ins.then_inc(d["sems"][i], 16)
        ev = (key, d["sems"][i], d["cnt"][i])
        self._commit(ev, reads, writes)
        self.n_instr += 1
        return ev

    def barrier(self):
        evs = [(k, self.esem[k], self.ecnt[k]) for k in self.eng if self.ecnt[k] > 0]
        for q, d in self.dq.items():
            for i, sm_ in enumerate(d["sems"]):
                if d["cnt"][i] > 0:
                    evs.append((f"d_{q}{i}", sm_, d["cnt"][i]))
        for e in self.eng:
            self._wait(e, [ev for ev in evs if ev[0] != e])

    def wait_all(self, e, bufs):
        evs = []
        for b in bufs:
            evs.append(b.w)
            evs.extend(b.r.values())
        self._wait(e, evs)


class Ctx:
    def __init__(self, nc, stack):
        self.nc = nc
        self.stack = stack
        self.bufs = {}

    _uid = [0]

    def sb(self, name, shape, dt):
        Ctx._uid[0] += 1
        t = self.stack.enter_context(self.nc.sbuf_tensor(f"sb_{name}_{Ctx._uid[0]}", list(shape), dt))
        return t

    def ps(self, name, shape, dt=F32):
        Ctx._uid[0] += 1
        t = self.stack.enter_context(self.nc.psum_tensor(f"pp_{name}_{Ctx._uid[0]}", list(shape), dt))
        return t

    def B(self, name):
        if name not in self.bufs:
            self.bufs[name] = Buf(name)
        return self.bufs[name]


def build(nc, cfg):
    layers = cfg["layers"]
    stack = ExitStack()
    with stack:
        P = Prog(nc, stack)
        C = Ctx(nc, stack)
        B = C.B

        def din(name, shape, dt=F32):
            return nc.dram_tensor(name, list(shape), dt, kind="ExternalInput").ap()

        x_d = din("x", [S, D])
        out_d = nc.dram_tensor("out", [S, D], F32, kind="ExternalOutput").ap()
        cst_d = din("cst", [128, 4 * 128])
        ecap_d = din("ecap", [128, NCH * NE])
        nrm_d = din("norms", [128, 9 * D])
        L = {}
        for l in layers:
            if cfg["moe"][l]:
                L[l, "w_router"] = din(f"w_router{l}", [D, NE])
                L[l, "b_router"] = din(f"b_router{l}", [128, NE])
                L[l, "w_gu"] = din(f"w_gu{l}", [NE, D, 2 * D])
                L[l, "b_gu"] = din(f"b_gu{l}", [128, NE * 16])
                L[l, "w_dn"] = din(f"w_dn{l}", [NE, D, D])
                L[l, "b_dn"] = din(f"b_dn{l}", [NE, D])
        def sb_layer(l):
            w_qkv, negm_d = L[l, "w_qkv"], L[l, "negm"]
            NH = 16
            with ExitStack() as ls:
                LC = Ctx(nc, ls)
                QT = LC.sb("QT", [128, NDC, S], BF16)
                KT = LC.sb("KT", [128, NDC, S], BF16)
                BQ = [[B(f"QT{hd}_{qg}") for qg in range(4)] for hd in range(NH)]
                BK = [B(f"KT{g}") for g in range(NDC)]
                Bvd = [B(f"vst{c}") for c in range(NCH)]
                with ExitStack() as us:
                    UC = Ctx(nc, us)
                    uT = UC.sb("uT", [128, NDC, S], BF16)
                    BuT = [B(f"uTq{i}") for i in range(4)]
                    norm_to_uT(l, uT, BuT)
                    with ExitStack() as ms:
                        MC = Ctx(nc, ms)
                        wq = [MC.sb(f"wq{i}", [128, NDC, 128], BF16) for i in range(2)]
                        wv = MC.sb("wv", [128, NDC, 512], BF16)
                        vst = [MC.sb(f"vst{i}", [128, 512], BF16) for i in range(2)]
                        Bwq = [B(f"wq{i}") for i in range(2)]
                        Bwv = B("wv")
                        Bvs = [B(f"vstage{i}") for i in range(2)]

                        def load_wq(j):
                            i2 = j % 2
                            P.dma("pool", lambda e: e.dma_start(
                                out=wq[i2][:], in_=w_qkv[:, j * 128:(j + 1) * 128].rearrange("(c p) n -> p c n", p=128)),
                                writes=[Bwq[i2]])
                        load_wq(0)
                        for j in range(16):
                            i2 = j % 2
                            if j + 1 < 16:
                                load_wq(j + 1)
                            g = j % 8
                            for tq in range(4):
                                pb = (j % 2) * 4 + tq

                                def mm(e):
                                    r = None
                                    for dc in range(NDC):
                                        r = e.matmul(psum[pb][:], lhsT=wq[i2][:, dc, :],
                                                     rhs=uT[:, dc, tq * 512:(tq + 1) * 512],
                                                     start=(dc == 0), stop=(dc == NDC - 1))
                                    return r
                                P.op("pe", mm, reads=[Bwq[i2], BuT[tq]], writes=[Bps[pb]])
                                if j < 8:
                                    P.op("act", lambda e: e.activation(out=QT[:, g, tq * 512:(tq + 1) * 512],
                                                                       in_=psum[pb][:], func=AF.Copy, scale=0.125),
                                         reads=[Bps[pb]], writes=[BQ[2 * g][tq], BQ[2 * g + 1][tq]])
                                else:
                                    P.op("dve", lambda e: e.tensor_copy(out=KT[:, g, tq * 512:(tq + 1) * 512],
                                                                        in_=psum[pb][:]),
                                         reads=[Bps[pb]], writes=[BK[g]])
                        for half in range(2):
                            P.dma("pool", lambda e: e.dma_start(
                                out=wv[:], in_=w_qkv[:, 2 * D + half * 512:2 * D + (half + 1) * 512].rearrange(
                                    "(c p) n -> p c n", p=128)), writes=[Bwv])
                            for c in range(NCH):
                                pb = c % 8
                                i2 = c % 2

                                def mv(e):
                                    r = None
                                    for dc in range(NDC):
                                        r = e.matmul(psum[pb][:], lhsT=uT[:, dc, c * 128:(c + 1) * 128],
                                                     rhs=wv[:, dc, :], start=(dc == 0), stop=(dc == NDC - 1))
                                    return r
                                P.op("pe", mv, reads=[Bwv, BuT[c // 4]], writes=[Bps[pb]])
                                if c % 2 == 0:
                                    P.op("act", lambda e: e.copy(out=vst[i2][:], in_=psum[pb][:]),
                                         reads=[Bps[pb]], writes=[Bvs[i2]])
                                else:
                                    P.op("dve", lambda e: e.tensor_copy(out=vst[i2][:], in_=psum[pb][:]),
                                         reads=[Bps[pb]], writes=[Bvs[i2]])
                                P.dma("sp", lambda e: e.dma_start(
                                    out=ust_d[c * 128:(c + 1) * 128, half * 512:(half + 1) * 512], in_=vst[i2][:]),
                                    reads=[Bvs[i2]], writes=[Bvd[c]])
                        P.barrier()
                with ExitStack() as ms:
                    MC = Ctx(nc, ms)
                    Vt = MC.sb("Vt", [128, NCH, D], BF16)
                    BV = B("Vt")
                    for c in range(NCH):
                        P.dma("sp", lambda e: e.dma_start(out=Vt[:, c, :], in_=ust_d[c * 128:(c + 1) * 128, :]),
                              reads=[Bvd[c]], writes=[BV])
                    negm = MC.sb("negm", [128, 4, 512], BF16)
                    ntri = MC.sb("ntri", [128, 128], BF16)
                    nones = MC.sb("nones", [128, 128], BF16)
                    identb = MC.sb("identb2", [128, 128], BF16)
                    Bc2 = B("sb_consts")
                    P.dma("pool", lambda e: e.dma_start(out=negm[:].rearrange("p a b -> p (a b)"), in_=negm_d),
                          writes=[Bc2])
                    P.op("dve", lambda e: e.tensor_scalar(out=ntri[:], in0=cst[:, 384:512], scalar1=-1.0, scalar2=None,
                                                          op0=ALU.mult), reads=[Bcst], writes=[Bc2])
                    P.op("dve", lambda e: e.tensor_scalar(out=nones[:], in0=ones, scalar1=-1.0, scalar2=None,
                                                          op0=ALU.mult), reads=[Bcst], writes=[Bc2])
                    P.op("dve", lambda e: e.tensor_copy(out=identb[:], in_=ident), reads=[Bcst], writes=[Bc2])
                    NLB, NAT, NE_ = 4, 4, 3
                    Lb = [MC.sb(f"Lb{i}", [128, 512], BF16) for i in range(NLB)]
                    AT = [MC.sb(f"AT{i}", [128, 512], BF16) for i in range(NAT)]
                    Et = [MC.sb(f"Et{i}", [128, 512], F32) for i in range(NE_)]
                    Lacc = MC.sb("Lacc", [128, 512], F32)
                    Laccb = [MC.sb(f"Laccb{i}", [128, 512], BF16) for i in range(2)]
                    BLb = [B(f"Lb{i}") for i in range(NLB)]
                    BAT = [B(f"AT{i}") for i in range(NAT)]
                    BEt = [B(f"Et{i}") for i in range(NE_)]
                    BLacc = B("Lacc")
                    BLaccb = [B(f"Laccb{i}") for i in range(2)]
                    blocks = []
                    for hd in range(NH):
                        for qg in range(4):
                            nkb = 4 * qg + 4
                            for kb in reversed(range(nkb)):
                                blocks.append(dict(hd=hd, qg=qg, kb=kb, first=(kb == nkb - 1), last=(kb == 0),
                                                   grp=hd * 4 + qg))
                    nacc = [0]

                    def stage_a(i, b):
                        hd, qg, kb = b["hd"], b["qg"], b["kb"]
                        g, pbase = hd // 2, (hd % 2) * 64
                        xb = i % 4
                        diag = kb >= 4 * qg
                        m = kb - 4 * qg

                        def mz(e):
                            r = e.matmul(psum[xb][:], lhsT=KT[pbase:pbase + 64, g, kb * 128:(kb + 1) * 128],
                                         rhs=QT[pbase:pbase + 64, g, qg * 512:(qg + 1) * 512], start=True,
                                         stop=True, skip_group_check=True)
                            if diag:
                                r = e.matmul(psum[xb][:], lhsT=identb[:], rhs=negm[:, m, :], start=False, stop=True,
                                             skip_group_check=True)
                            return r
                        P.op("pe", mz, reads=[BK[g], BQ[hd][qg], Bc2], writes=[Bps[xb]])
                        P.op("act", lambda e: e.activation(out=Et[i % NE_][:], in_=psum[xb][:], func=AF.Exp),
                             reads=[Bps[xb]], writes=[BEt[i % NE_]])
                        P.op("act", lambda e: e.activation(out=Lb[i % NLB][:], in_=Et[i % NE_][:], func=AF.Ln,
                                                           bias=1.0, scale=1.0),
                             reads=[BEt[i % NE_]], writes=[BLb[i % NLB]])

                    def stage_b(i, b):
                        hd, qg, kb = b["hd"], b["qg"], b["kb"]
                        g, pbase = hd // 2, (hd % 2) * 64
                        xb = i % 4
                        ob = 4 + b["grp"] % 2
                        first, last = b["first"], b["last"]
                        la = nacc[0] % 2

                        def m2(e):
                            r = e.matmul(psum[xb][:], lhsT=ntri[:], rhs=Lb[i % NLB][:], start=False, stop=True,
                                         skip_group_check=True)
                            if not first:
                                r = e.matmul(psum[xb][:], lhsT=nones[:], rhs=Laccb[la][:], start=False, stop=True,
                                             skip_group_check=True)
                            return r
                        P.op("pe", m2, reads=[BLb[i % NLB], Bc2] + ([] if first else [BLaccb[la]]), writes=[Bps[xb]])
                        P.op("act", lambda e: e.activation(out=AT[i % NAT][:], in_=psum[xb][:], func=AF.Exp),
                             reads=[Bps[xb]], writes=[BAT[i % NAT]])
                        if not last:
                            if first:
                                P.op("dve", lambda e: e.tensor_copy(out=Lacc[:], in_=Lb[i % NLB][:]),
                                     reads=[BLb[i % NLB]], writes=[BLacc])
                            else:
                                P.op("dve", lambda e: e.tensor_tensor(out=Lacc[:], in0=Lacc[:], in1=Lb[i % NLB][:],
                                                                      op=ALU.add),
                                     reads=[BLb[i % NLB], BLacc], writes=[BLacc])
                            nacc[0] += 1
                            lb2 = nacc[0] % 2
                            P.op("dve", lambda e: e.tensor_copy(out=Laccb[lb2][:], in_=Lacc[:]),
                                 reads=[BLacc], writes=[BLaccb[lb2]])

                    def stage_c(i, b):
                        hd, qg, kb = b["hd"], b["qg"], b["kb"]
                        g, pbase = hd // 2, (hd % 2) * 64
                        ob = 4 + b["grp"] % 2
                        first, last = b["first"], b["last"]
                        P.op("pe", lambda e: e.matmul(psum[ob][:], lhsT=Vt[:, kb, g * 128:(g + 1) * 128],
                                                      rhs=AT[i % NAT][:], start=first, stop=last,
                                                      skip_group_check=True),
                             reads=[BV, BAT[i % NAT]], writes=[Bps[ob]])
                        if last:
                            P.op("act", lambda e: e.copy(out=QT[pbase:pbase + 64, g, qg * 512:(qg + 1) * 512],
                                                         in_=psum[ob][pbase:pbase + 64, :]),
                                 reads=[Bps[ob]], writes=[BQ[hd][qg]])

                    nb = len(blocks)
                    for i in range(nb + 2):
                        if i < nb:
                            stage_a(i, blocks[i])
                        if 1 <= i <= nb:
                            stage_b(i - 1, blocks[i - 1])
                        if i >= 2:
                            stage_c(i - 2, blocks[i - 2])
                    P.barrier()
                out_proj(l, QT, [BQ[hd][qg] for hd in range(NH) for qg in range(4)])


        for l in layers:
            if cfg["mix"][l]:
                kind = l % 3
                if kind == 0:
                    L[l, "w_in"] = din(f"lru_w_in{l}", [D, 2 * D])
                    L[l, "vec"] = din(f"lru_vec{l}", [128, 8 * 8])
                    L[l, "w_a"] = din(f"lru_w_a{l}", [8, 128, 128])
                    L[l, "w_x"] = din(f"lru_w_x{l}", [8, 128, 128])
                    L[l, "w_out"] = din(f"mix_w_out{l}", [D, D])
                elif kind == 1:
                    L[l, "w_in"] = din(f"pool_w_in{l}", [D, D])
                    L[l, "w_grp"] = din(f"pool_w_grp{l}", [4, 256, 256])
                    L[l, "vec"] = din(f"pool_vec{l}", [128, 8 + 64])
                    L[l, "w_out"] = din(f"mix_w_out{l}", [D, D])
                else:
                    L[l, "w_qkv"] = din(f"sb_w_qkv{l}", [D, 3 * D])
                    L[l, "w_out"] = din(f"mix_w_out{l}", [D, D])
                    L[l, "negm"] = din(f"sb_negmask{l}", [128, 4 * 512])
        dbg = cfg.get("debug", False)
        skind = "ExternalOutput" if dbg else "Internal"
        xs_d = nc.dram_tensor("xs_scr", [NSLOT, D], BF16, kind=skind).ap()
        ys_d = nc.dram_tensor("ys_scr", [NSLOT, D], F32, kind=skind).ap()
        ust_d = nc.dram_tensor("ust_scr", [S, D], BF16, kind=skind).ap()
        if dbg:
            dbg_d = nc.dram_tensor("dbg", [128, 8 * 512], F32, kind="ExternalOutput").ap()
            dbgx_d = nc.dram_tensor("dbgx", [128, NDC * CAP], BF16, kind="ExternalOutput").ap()
            dbgw_d = nc.dram_tensor("dbgw", [128, NDC * 512], BF16, kind="ExternalOutput").ap()
            dbgh_d = nc.dram_tensor("dbgh", [128, NDC * CAP], BF16, kind="ExternalOutput").ap()
        B_xs, B_ys = B("xs_d"), B("ys_d")

        cst = C.sb("cst", [128, 4 * 128], F32)
        ident = cst[:, 0:128]
        tri = cst[:, 128:256]
        ones = cst[:, 256:384]
        ecap = C.sb("ecap", [128, NCH * NE], F32)
        ssq = C.sb("ssq", [128, 2], F32)
        rstd = C.sb("rstd", [128, 2], F32)
        Bcst = B("cst")
        gat = C.sb("gat", [128, NCH, NE], F32)
        dki = C.sb("dki", [128, NCH * TOPK], I32)
        gk = C.sb("gk", [128, NCH * TOPK], F32)
        bgu = C.sb("bgu", [128, NE * 16], F32)
        bdn = C.sb("bdn", [NE, D], F32)
        hsp_d = nc.dram_tensor("hsp_scr", [S, D], F32, kind="Internal").ap()
        Bhsp = [B(f"hsp{c}") for c in range(NCH)]

        psum = [C.ps(f"ps{i}", [128, 512]) for i in range(8)]
        Bps = [B(f"ps{i}") for i in range(8)]

        bound_reg = nc.gpsimd.to_reg(NSLOT - 1)
        hst = {"stack": ExitStack(), "n": 0}
        h = hst["stack"].enter_context(nc.sbuf_tensor("h_res0", [128, NCH, D], F32))
        Bh = [B(f"h{c}") for c in range(NCH)]

        P.dma("sp", lambda e: e.dma_start(out=cst[:], in_=cst_d), writes=[Bcst])
        P.dma("pool", lambda e: e.dma_start(out=ecap[:], in_=ecap_d), writes=[Bcst])
        for c in range(NCH):
            P.dma("sp", lambda e: e.dma_start(out=h[:, c, :], in_=x_d[c * 128:(c + 1) * 128, :]),
                  writes=[Bh[c]])

        def rms_rstd(c, par, junk, Bj):
            Bs, Br = B(f"ssq{par}"), B(f"rstd{par}")
            P.op("act", lambda e: e.activation(out=junk[:], in_=h[:, c, :], func=AF.Square,
                                               accum_out=ssq[:, par:par + 1]),
                 reads=[Bh[c]], writes=[Bj, Bs])
            P.op("dve", lambda e: e.tensor_scalar(out=rstd[:, par:par + 1], in0=ssq[:, par:par + 1],
                                                  scalar1=1.0 / D, scalar2=RMS_EPS, op0=ALU.mult, op1=ALU.add),
                 reads=[Bs], writes=[Br])
            P.op("act", lambda e: e.activation(out=rstd[:, par:par + 1], in_=rstd[:, par:par + 1], func=AF.Sqrt),
                 reads=[Br], writes=[Br])
            P.op("dve", lambda e: e.reciprocal(out=rstd[:, par:par + 1], in_=rstd[:, par:par + 1]),
                 reads=[Br], writes=[Br])
            return Br

        MS = {}

        def moe_common(l):
            Bw = B("moe_small")
            Ball = B("route")
            return (4 + l) * D, L[l, "w_router"], L[l, "b_router"], L[l, "w_gu"], L[l, "b_gu"], L[l, "w_dn"], L[l, "b_dn"], Bw, Ball

        def moe_A(l):
            gcol, w_router, b_router, w_gu, b_gu, w_dn, b_dn, Bw, Ball = moe_common(l)
            P.dma("sp", lambda e: e.dma_start(out=bgu[:], in_=b_gu), writes=[Bw])
            P.dma("sp", lambda e: e.dma_start(out=bdn[:], in_=b_dn), writes=[Bw])
            with ExitStack() as ms:
                MC = Ctx(nc, ms)
                wr = MC.sb("wr", [128, NDC, NE], F32)
                br = MC.sb("br", [128, NE], F32)
                gam = MC.sb("gam", [128, D], F32)
                junk = MC.sb("junk", [128, D], F32)
                Bj = B("junk")
                P.dma("sp", lambda e: e.dma_start(out=wr[:], in_=w_router.rearrange("(c p) n -> p c n", p=128)),
                      writes=[Bw])
                P.dma("sp", lambda e: e.dma_start(out=br[:], in_=b_router), writes=[Bw])
                P.dma("sp", lambda e: e.dma_start(out=gam[:], in_=nrm_d[:, gcol:gcol + D]), writes=[Bw])
                NUF = 3
                uf = [MC.sb(f"uf{i}", [128, D], F32) for i in range(NUF)]
                ubig = MC.sb("ubig", [128, NCH, D], BF16)
                uT = [MC.sb(f"uT{i}", [128, NDC, 128], F32) for i in range(2)]
                ssq16 = MC.sb("ssq16", [128, NCH], F32)
                rs16 = MC.sb("rs16", [128, NCH], F32)
                logit = MC.sb("logit", [128, NCH, NE], F32)
                top8 = MC.sb("top8", [128, NCH, 8], F32)
                mask = MC.sb("mask", [128, NCH, NE], F32)
                sm = MC.sb("sm", [128, NCH, 2], F32)
                dest = MC.sb("dest", [128, NCH, NE], F32)
                tmp32 = MC.sb("tmp32", [128, NCH, NE], F32)
                off = MC.sb("off", [128, NCH, NE], F32)
                dk = MC.sb("dk", [128, NCH, TOPK], F32)
                Blog = [B(f"logit{c}") for c in range(NCH)]
                Bub = [B(f"ubig{c}") for c in range(NCH)]
                Bss = B("ssq16")
                for c in range(NCH):
                    P.op("act", lambda e: e.activation(out=junk[:], in_=h[:, c, :], func=AF.Square,
                                                       accum_out=ssq16[:, c:c + 1]),
                         reads=[Bh[c]], writes=[Bj, Bss])
                P.op("dve", lambda e: e.tensor_scalar(out=rs16[:], in0=ssq16[:], scalar1=1.0 / D, scalar2=RMS_EPS,
                                                      op0=ALU.mult, op1=ALU.add), reads=[Bss], writes=[Bss])
                P.op("act", lambda e: e.activation(out=rs16[:], in_=rs16[:], func=AF.Sqrt), reads=[Bss],
                     writes=[Bss])
                P.op("dve", lambda e: e.reciprocal(out=rs16[:], in_=rs16[:]), reads=[Bss], writes=[Bss])

                def s1(c):
                    i3 = c % NUF
                    P.op("dve", lambda e: e.scalar_tensor_tensor(
                        out=uf[i3][:], in0=h[:, c, :], scalar=rs16[:, c:c + 1], in1=gam[:],
                        op0=ALU.mult, op1=ALU.mult), reads=[Bh[c], Bss, Bw], writes=[B(f"uf{i3}")])
                    P.op("act", lambda e: e.copy(out=ubig[:, c, :], in_=uf[i3][:]), reads=[B(f"uf{i3}")],
                         writes=[Bub[c]])

                def s2(c):
                    i3, par = c % NUF, c % 2
                    for g in range(2):
                        pb = 2 * par + g

                        def tr(e):
                            r = None
                            for j in range(4):
                                dc = g * 4 + j
                                r = e.transpose(out=psum[pb][:, j * 128:(j + 1) * 128],
                                                in_=uf[i3][:, dc * 128:(dc + 1) * 128], identity=ident)
                            return r
                        P.op("pe", tr, reads=[B(f"uf{i3}"), Bcst], writes=[Bps[pb]])
                        src = psum[pb][:].rearrange("p (a b) -> p a b", a=4)
                        if g == 0:
                            P.op("act", lambda e: e.copy(out=uT[par][:, 0:4, :], in_=src),
                                 reads=[Bps[pb]], writes=[B(f"uT{par}")])
                        else:
                            P.op("dve", lambda e: e.tensor_copy(out=uT[par][:, 4:8, :], in_=src),
                                 reads=[Bps[pb]], writes=[B(f"uT{par}")])

                def s3(c):
                    par = c % 2
                    pl = 4 + par

                    def rt(e):
                        r = None
                        for dc in range(NDC):
                            r = e.matmul(psum[pl][:, 0:NE], lhsT=uT[par][:, dc, :], rhs=wr[:, dc, :],
                                         start=(dc == 0), stop=(dc == NDC - 1))
                        return r
                    P.op("pe", rt, reads=[B(f"uT{par}"), Bw], writes=[Bps[pl]])
                    P.op("dve", lambda e: e.tensor_tensor(out=logit[:, c, :], in0=psum[pl][:, 0:NE], in1=br[:],
                                                          op=ALU.add),
                         reads=[Bps[pl], Bw], writes=[Blog[c]])
                    P.op("dve", lambda e: e.max(out=top8[:, c, :], in_=logit[:, c, :]), reads=[Blog[c]],
                         writes=[Blog[c]])

                for it in range(NCH + 2):
                    if it < NCH:
                        s1(it)
                    if 1 <= it <= NCH:
                        s2(it - 1)
                    if it >= 2:
                        s3(it - 2)

                L3 = [128, NCH, NE]
                P.op("dve", lambda e: e.tensor_tensor(out=mask[:], in0=logit[:], in1=top8[:, :, 3:4].broadcast_to(L3),
                                                      op=ALU.is_ge), reads=Blog, writes=[Ball])
                P.op("dve", lambda e: e.tensor_tensor(out=gat[:], in0=logit[:], in1=top8[:, :, 0:1].broadcast_to(L3),
                                                      op=ALU.subtract), reads=Blog, writes=[Ball])
                P.op("act", lambda e: e.activation(out=gat[:], in_=gat[:], func=AF.Exp), reads=[Ball],
                     writes=[Ball])
                P.op("dve", lambda e: e.tensor_tensor(out=gat[:], in0=gat[:], in1=mask[:], op=ALU.mult),
                     reads=[Ball], writes=[Ball])
                P.op("dve", lambda e: e.tensor_reduce(out=sm[:, :, 0], in_=gat[:], axis=AX.X, op=ALU.add),
                     reads=[Ball], writes=[Ball])
                P.op("dve", lambda e: e.reciprocal(out=sm[:, :, 1], in_=sm[:, :, 0]), reads=[Ball], writes=[Ball])
                P.op("dve", lambda e: e.tensor_tensor(out=gat[:], in0=gat[:],
                                                      in1=sm[:, :, 1:2].broadcast_to(L3), op=ALU.mult),
                     reads=[Ball], writes=[Ball])

                mflat = mask[:].rearrange("p c e -> p (c e)")
                P.op("pe", lambda e: e.matmul(psum[6][:], lhsT=tri, rhs=mflat, start=True, stop=True),
                     reads=[Ball, Bcst], writes=[Bps[6]])
                P.op("pe", lambda e: e.matmul(psum[7][:], lhsT=ones, rhs=mflat, start=True, stop=True),
                     reads=[Ball, Bcst], writes=[Bps[7]])
                P.op("dve", lambda e: e.tensor_copy(out=tmp32[:].rearrange("p c e -> p (c e)"), in_=psum[7][:]),
                     reads=[Bps[7]], writes=[Ball])
                P.op("dve", lambda e: e.memset(off[:, 0, :], 0.0), writes=[Ball])
                for c in range(1, NCH):
                    P.op("dve", lambda e: e.tensor_tensor(out=off[:, c, :], in0=off[:, c - 1, :],
                                                          in1=tmp32[:, c - 1, :], op=ALU.add),
                         reads=[Ball], writes=[Ball])
                offf = off[:].rearrange("p c e -> p (c e)")
                destf = dest[:].rearrange("p c e -> p (c e)")
                tmpf = tmp32[:].rearrange("p c e -> p (c e)")
                gatf = gat[:].rearrange("p c e -> p (c e)")
                P.op("dve", lambda e: e.tensor_tensor(out=offf, in0=offf, in1=psum[6][:], op=ALU.add),
                     reads=[Ball, Bps[6]], writes=[Ball])
                P.op("dve", lambda e: e.tensor_scalar(out=tmpf, in0=offf, scalar1=float(CAP), scalar2=None,
                                                      op0=ALU.is_lt), reads=[Ball], writes=[Ball])
                P.op("dve", lambda e: e.tensor_tensor(out=gatf, in0=gatf, in1=tmpf, op=ALU.mult),
                     reads=[Ball], writes=[Ball])
                P.op("dve", lambda e: e.tensor_tensor(out=destf, in0=offf, in1=ecap[:], op=ALU.add),
                     reads=[Ball, Bcst], writes=[Ball])
                P.op("dve", lambda e: e.tensor_scalar(out=tmpf, in0=tmpf, scalar1=-1.0e6, scalar2=1.0e6,
                                                      op0=ALU.mult, op1=ALU.add), reads=[Ball], writes=[Ball])
                P.op("dve", lambda e: e.tensor_tensor(out=destf, in0=destf, in1=tmpf, op=ALU.add),
                     reads=[Ball], writes=[Ball])
                gk3 = gk[:].rearrange("p (c k) -> p c k", k=TOPK)
                for k in range(TOPK):
                    P.op("dve", lambda e: e.tensor_tensor(out=mask[:], in0=logit[:],
                                                          in1=top8[:, :, k:k + 1].broadcast_to(L3), op=ALU.is_equal),
                         reads=[Ball], writes=[Ball])
                    P.op("dve", lambda e: e.tensor_tensor(out=tmp32[:], in0=mask[:], in1=dest[:], op=ALU.mult),
                         reads=[Ball], writes=[Ball])
                    P.op("dve", lambda e: e.tensor_reduce(out=dk[:, :, k], in_=tmp32[:], axis=AX.X, op=ALU.add),
                         reads=[Ball], writes=[Ball])
                    P.op("dve", lambda e: e.tensor_tensor(out=tmp32[:], in0=mask[:], in1=gat[:], op=ALU.mult),
                         reads=[Ball], writes=[Ball])
                    P.op("dve", lambda e: e.tensor_reduce(out=gk3[:, :, k], in_=tmp32[:], axis=AX.X, op=ALU.add),
                         reads=[Ball], writes=[Ball])
                P.op("dve", lambda e: e.tensor_copy(out=dki[:], in_=dk[:].rearrange("p c k -> p (c k)")),
                     reads=[Ball], writes=[Ball])

                xs_bufs = []
                MS['xs_bufs'] = xs_bufs
                for c in range(NCH):
                    for k in range(TOPK):
                        col = c * TOPK + k
                        bx = Buf(f"xs{col}")
                        xs_bufs.append(bx)
                        P.dma("pool", lambda e: e.indirect_dma_start(
                            out=xs_d, out_offset=bass.IndirectOffsetOnAxis(ap=dki[:, col:col + 1], axis=0),
                            in_=ubig[:, c, :], in_offset=None, bounds_check=bound_reg, oob_is_err=False),
                            reads=[Bub[c], Ball], writes=[bx])
                P.barrier()


        def moe_B(l):
            gcol, w_router, b_router, w_gu, b_gu, w_dn, b_dn, Bw, Ball = moe_common(l)
            xs_bufs = MS["xs_bufs"]
            with ExitStack() as ms:
                MC = Ctx(nc, ms)
                PW = 256
                NWF, NWG, NWD = 6, 16, 8
                wf = [MC.sb(f"wf{i}", [128, NDC, PW], F32) for i in range(NWF)]
                wg = [MC.sb(f"wg{i}", [128, NDC, PW], BF16) for i in range(NWG)]
                wd = [MC.sb(f"wd{i}", [128, NDC, PW], BF16) for i in range(NWD)]
                Bwf = [B(f"wf{i}") for i in range(NWF)]
                Bwg = [B(f"wg{i}") for i in range(NWG)]
                Bwd = [B(f"wd{i}") for i in range(NWD)]
                xT = [MC.sb(f"xT{i}", [128, NDC, CAP], BF16) for i in range(2)]
                hT = [MC.sb(f"hT{i}", [128, NDC, CAP], BF16) for i in range(2)]
                ysb = [MC.sb(f"ysb{i}", [128, 512], F32) for i in range(4)]
                t_g = [MC.sb(f"t_g{i}", [128, CAP], F32) for i in range(2)]
                t_s = [MC.sb(f"t_s{i}", [128, CAP], F32) for i in range(2)]
                t_u = [MC.sb(f"t_u{i}", [128, CAP], F32) for i in range(2)]
                BxT = [B(f"xT{i}") for i in range(2)]
                BhT = [B(f"hT{i}") for i in range(2)]
                Bys = [B(f"ysb{i}") for i in range(4)]
                Btg = [B(f"t_g{i}") for i in range(2)]
                Bts = [B(f"t_s{i}") for i in range(2)]
                Btu = [B(f"t_u{i}") for i in range(2)]
                NPC = 12
                NG = NE * NPC

                def piece_src(G):
                    e_, p = G // NPC, G % NPC
                    if p < 8:
                        q, is_up = p // 2, p % 2
                        c0 = is_up * D + q * PW
                        return w_gu[e_, :, c0:c0 + PW].rearrange("(c p) n -> p c n", p=128)
                    q = p - 8
                    return w_dn[e_, :, q * PW:(q + 1) * PW].rearrange("(c p) n -> p c n", p=128)

                def piece_dst(G):
                    e_, p = G // NPC, G % NPC
                    if p < 8:
                        i = (e_ * 8 + p) % NWG
                        return wg[i], Bwg[i]
                    i = (e_ * 4 + (p - 8)) % NWD
                    return wd[i], Bwd[i]

                def issue_dma(G):
                    if G >= NG:
                        return
                    i = G % NWF
                    P.dma("sp", lambda e: e.dma_start(out=wf[i][:], in_=piece_src(G)), writes=[Bwf[i]])

                def issue_cast(G):
                    if G >= NG:
                        return
                    i = G % NWF
                    dst, Bdst = piece_dst(G)
                    if (G % NPC) < 8:
                        P.op("act", lambda e: e.copy(out=dst[:], in_=wf[i][:]), reads=[Bwf[i]], writes=[Bdst])
                    else:
                        P.op("dve", lambda e: e.tensor_copy(out=dst[:], in_=wf[i][:]), reads=[Bwf[i]], writes=[Bdst])

                def tick(G):
                    issue_cast(G)
                    issue_dma(G + NWF)

                xr = MC.sb("xr", [128, CAP // 128, D], BF16)
                identb = MC.sb("identb3", [128, 128], BF16)
                Bxr = B("xr")
                P.op("dve", lambda e: e.tensor_copy(out=identb[:], in_=ident), reads=[Bcst], writes=[B("identb3")])

                def load_x(e_):
                    if e_ >= NE:
                        return
                    P.dma("sp", lambda e: e.dma_start(
                        out=xr[:], in_=xs_d[e_ * CAP:(e_ + 1) * CAP, :].rearrange("(c p) n -> p c n", p=128)),
                        reads=xs_bufs, writes=[Bxr])

                def transpose_x(e_):
                    if e_ >= NE:
                        return
                    i2 = e_ % 2
                    pv = psum[7][:].bitcast(BF16)
                    for sc in range(CAP // 128):
                        def tr(e):
                            r = None
                            for dc in range(NDC):
                                r = e.transpose(out=pv[:, dc * 128:(dc + 1) * 128],
                                                in_=xr[:, sc, dc * 128:(dc + 1) * 128], identity=identb[:])
                            return r
                        P.op("pe", tr, reads=[Bxr, B("identb3")], writes=[Bps[7]])
                        P.op("dve", lambda e: e.tensor_copy(out=xT[i2][:, :, sc * 128:(sc + 1) * 128],
                                                            in_=pv.rearrange("p (a b) -> p a b", a=NDC)),
                             reads=[Bps[7]], writes=[BxT[i2]])

                def compute_expert(e_):
                    i2 = e_ % 2
                    Gn = (e_ + 1) * NPC
                    transpose_x(e_ + 1)
                    load_x(e_ + 2)
                    for fcp in range(8):
                        q, j = fcp // 2, fcp % 2
                        sg = (e_ * 8 + 2 * q) % NWG
                        su = (e_ * 8 + 2 * q + 1) % NWG
                        pg, pu = (fcp % 2) * 2, (fcp % 2) * 2 + 1
                        ti = fcp % 2

                        def mm(e, slot, pb):
                            r = None
                            for dc in range(NDC):
                                r = e.matmul(psum[pb][:, 0:CAP], lhsT=wg[slot][:, dc, j * 128:(j + 1) * 128],
                                             rhs=xT[i2][:, dc, :], start=(dc == 0), stop=(dc == NDC - 1))
                            return r
                        P.op("pe", lambda e: mm(e, sg, pg), reads=[Bwg[sg], BxT[i2]], writes=[Bps[pg]])
                        P.op("pe", lambda e: mm(e, su, pu), reads=[Bwg[su], BxT[i2]], writes=[Bps[pu]])
                        bg = bgu[:, e_ * 16 + fcp:e_ * 16 + fcp + 1]
                        bu = bgu[:, e_ * 16 + 8 + fcp:e_ * 16 + 8 + fcp + 1]
                        P.op("dve", lambda e: e.tensor_scalar(out=t_g[ti][:], in0=psum[pg][:, 0:CAP], scalar1=bg,
                                                              scalar2=7.0, op0=ALU.add, op1=ALU.min),
                             reads=[Bps[pg], Bw], writes=[Btg[ti]])
                        P.op("act", lambda e: e.activation(out=t_s[ti][:], in_=t_g[ti][:], func=AF.Sigmoid,
                                                           scale=1.702),
                             reads=[Btg[ti]], writes=[Bts[ti]])
                        P.op("dve", lambda e: e.tensor_scalar(out=t_u[ti][:], in0=psum[pu][:, 0:CAP], scalar1=bu,
                                                              scalar2=7.0, op0=ALU.add, op1=ALU.min),
                             reads=[Bps[pu], Bw], writes=[Btu[ti]])
                        P.op("dve", lambda e: e.tensor_scalar(out=t_u[ti][:], in0=t_u[ti][:], scalar1=-7.0,
                                                               scalar2=1.0, op0=ALU.max, op1=ALU.add),
                             reads=[Btu[ti]], writes=[Btu[ti]])
                        P.op("dve", lambda e: e.tensor_tensor(out=t_g[ti][:], in0=t_g[ti][:], in1=t_s[ti][:],
                                                               op=ALU.mult),
                             reads=[Btg[ti], Bts[ti]], writes=[Btg[ti]])
                        P.op("dve", lambda e: e.tensor_tensor(out=hT[i2][:, fcp, :], in0=t_u[ti][:],
                                                              in1=t_g[ti][:], op=ALU.mult),
                             reads=[Btu[ti], Btg[ti]], writes=[BhT[i2]])
                        tick(Gn + fcp)
                    chunks = [(s0, min(128, CAP - s0)) for s0 in range(0, CAP, 128)]
                    step = 0
                    for half in range(2):
                        for (s0, sn) in chunks:
                            idx = step
                            pb = 4 + idx % 3
                            yi = idx % 4

                            def md(e):
                                r = None
                                for qq in range(2):
                                    sd = (e_ * 4 + half * 2 + qq) % NWD
                                    for fc in range(NDC):
                                        r = e.matmul(psum[pb][0:sn, qq * PW:(qq + 1) * PW],
                                                     lhsT=hT[i2][:, fc, s0:s0 + sn], rhs=wd[sd][:, fc, :],
                                                     start=(fc == 0), stop=(fc == NDC - 1))
                                return r
                            sds = [(e_ * 4 + half * 2 + qq) % NWD for qq in range(2)]
                            P.op("pe", md, reads=[BhT[i2]] + [Bwd[x] for x in sds], writes=[Bps[pb]])
                            P.op("act", lambda e: e.copy(out=ysb[yi][0:sn, :], in_=psum[pb][0:sn, :]),
                                 reads=[Bps[pb]], writes=[Bys[yi]])
                            r0 = e_ * CAP + s0
                            P.dma("act", lambda e: e.dma_start(
                                out=ys_d[r0:r0 + sn, half * 512:(half + 1) * 512], in_=ysb[yi][0:sn, :]),
                                reads=[Bys[yi]], writes=[Buf("ys")])
                            if step < 4:
                                tick(Gn + 8 + step)
                            step += 1
                    for st in range(step, 4):
                        tick(Gn + 8 + st)

                for G in range(NWF):
                    issue_dma(G)
                load_x(0)
                transpose_x(0)
                load_x(1)
                for G in range(NPC):
                    tick(G)
                for e_ in range(NE):
                    compute_expert(e_)
                P.barrier()

        def moe_C(l):
            gcol, w_router, b_router, w_gu, b_gu, w_dn, b_dn, Bw, Ball = moe_common(l)
            with ExitStack() as ms:
                MC = Ctx(nc, ms)
                yg = [MC.sb(f"yg{i}", [128, D], F32) for i in range(8)]
                Byg = [B(f"yg{i}") for i in range(8)]
                gT = MC.sb("gT", [NE, 128], F32)
                for i in range(8):
                    P.op("dve", lambda e: e.memset(yg[i][:], 0.0), writes=[Byg[i]])
                for c in range(NCH):
                    P.op("pe", lambda e: e.transpose(out=psum[0][0:NE, 0:128], in_=gat[:, c, :], identity=ident),
                         reads=[Ball, Bcst], writes=[Bps[0]])
                    P.op("act", lambda e: e.copy(out=gT[:], in_=psum[0][0:NE, 0:128]), reads=[Bps[0]],
                         writes=[B("gT")])
                    for half in range(2):
                        P.op("pe", lambda e: e.matmul(psum[1 + half][:], lhsT=gT[:],
                                                      rhs=bdn[:, half * 512:(half + 1) * 512],
                                                      start=True, stop=True),
                             reads=[B("gT"), Bw], writes=[Bps[1 + half]])
                        P.op("dve", lambda e: e.tensor_tensor(out=h[:, c, half * 512:(half + 1) * 512],
                                                              in0=h[:, c, half * 512:(half + 1) * 512],
                                                              in1=psum[1 + half][:], op=ALU.add),
                             reads=[Bps[1 + half], Bh[c]], writes=[Bh[c]])
                    for k in range(TOPK):
                        col = c * TOPK + k
                        yi = (c % 2) * 4 + k
                        P.dma("pool", lambda e: e.indirect_dma_start(
                            out=yg[yi][:], out_offset=None, in_=ys_d,
                            in_offset=bass.IndirectOffsetOnAxis(ap=dki[:, col:col + 1], axis=0),
                            bounds_check=bound_reg, oob_is_err=False),
                            reads=[Ball], writes=[Byg[yi]])
                        P.op("dve", lambda e: e.scalar_tensor_tensor(
                            out=h[:, c, :], in0=yg[yi][:], scalar=gk[:, col:col + 1], in1=h[:, c, :],
                            op0=ALU.mult, op1=ALU.add),
                            reads=[Byg[yi], Ball, Bh[c]], writes=[Bh[c]])
                P.barrier()


        def norm_to_uT(l, uT, BuT):
            gcol = l * D
            with ExitStack() as ms:
                MC = Ctx(nc, ms)
                gam = MC.sb("gam", [128, D], F32)
                junk = MC.sb("junk", [128, D], F32)
                ubf = [MC.sb(f"ubf{i}", [128, D], BF16) for i in range(2)]
                identb = MC.sb("identb", [128, 128], BF16)
                Bg, Bj = B("gam_m"), B("junk_m")
                P.dma("sp", lambda e: e.dma_start(out=gam[:], in_=nrm_d[:, gcol:gcol + D]), writes=[Bg])
                P.op("dve", lambda e: e.tensor_copy(out=identb[:], in_=ident), reads=[Bcst], writes=[Bg])
                for c in range(NCH):
                    par = c % 2
                    Br = rms_rstd(c, par, junk, Bj)
                    Bu = B(f"ubf{par}")
                    P.op("dve", lambda e: e.scalar_tensor_tensor(
                        out=ubf[par][:], in0=h[:, c, :], scalar=rstd[:, par:par + 1], in1=gam[:],
                        op0=ALU.mult, op1=ALU.mult), reads=[Bh[c], Br, Bg], writes=[Bu])
                    pb = par
                    pv = psum[pb][:].bitcast(BF16)

                    def tr(e):
                        r = None
                        for dc in range(NDC):
                            r = e.transpose(out=pv[:, dc * 128:(dc + 1) * 128],
                                            in_=ubf[par][:, dc * 128:(dc + 1) * 128], identity=identb[:])
                        return r
                    P.op("pe", tr, reads=[Bu, Bg], writes=[Bps[pb]])
                    src = pv.rearrange("p (a b) -> p a b", a=NDC)
                    if par == 0:
                        P.op("act", lambda e: e.copy(out=uT[:, :, c * 128:(c + 1) * 128], in_=src),
                             reads=[Bps[pb]], writes=[BuT[c // 4]])
                    else:
                        P.op("dve", lambda e: e.tensor_copy(out=uT[:, :, c * 128:(c + 1) * 128], in_=src),
                             reads=[Bps[pb]], writes=[BuT[c // 4]])
                P.barrier()

        def out_proj(l, yT, ByT):
            w_out = L[l, "w_out"]
            with ExitStack() as ms:
                MC = Ctx(nc, ms)
                wo = MC.sb("wo", [128, NDC, D], BF16)
                Bwo = B("wo")
                for half in range(2):
                    P.dma("pool", lambda e: e.dma_start(
                        out=wo[:, :, half * 512:(half + 1) * 512],
                        in_=w_out[:, half * 512:(half + 1) * 512].rearrange("(c p) n -> p c n", p=128)),
                        writes=[Bwo])
                for c in range(NCH):
                    for half in range(2):
                        pb = (c * 2 + half) % 8

                        def mm(e):
                            r = None
                            for g in range(NDC):
                                r = e.matmul(psum[pb][:], lhsT=yT[:, g, c * 128:(c + 1) * 128],
                                             rhs=wo[:, g, half * 512:(half + 1) * 512],
                                             start=(g == 0), stop=(g == NDC - 1))
                            return r
                        P.op("pe", mm, reads=list(ByT) + [Bwo], writes=[Bps[pb]])
                        P.op("dve", lambda e: e.tensor_tensor(out=h[:, c, half * 512:(half + 1) * 512],
                                                              in0=h[:, c, half * 512:(half + 1) * 512],
                                                              in1=psum[pb][:], op=ALU.add),
                             reads=[Bps[pb], Bh[c]], writes=[Bh[c]])
                P.barrier()

        def lru_layer(l):
            w_in, vec_d, w_a, w_x = L[l, "w_in"], L[l, "vec"], L[l, "w_a"], L[l, "w_x"]
            PAD = 4
            with ExitStack() as ls:
                LC = Ctx(nc, ls)
                yT = LC.sb("yT", [128, NDC, S], BF16)
                ByT = [B(f"yT{g}") for g in range(NDC)]
                with ExitStack() as us:
                    UC = Ctx(nc, us)
                    uT = UC.sb("uT", [128, NDC, S], BF16)
                    BuT = [B(f"uTq{i}") for i in range(4)]
                    norm_to_uT(l, uT, BuT)
                    with ExitStack() as ms:
                        MC = Ctx(nc, ms)
                        vec = MC.sb("vec", [128, 8, 8], F32)
                        sc = MC.sb("sc", [128, 8, 2], F32)
                        wa = MC.sb("wa", [128, 8, 128], BF16)
                        wx = MC.sb("wx", [128, 8, 128], BF16)
                        wig = [MC.sb(f"wig{i}", [128, NDC, 128], BF16) for i in range(2)]
                        wix = [MC.sb(f"wix{i}", [128, NDC, 128], BF16) for i in range(2)]
                        XB = MC.sb("XB", [128, PAD + S], F32)
                        XC = MC.sb("XC", [128, S], F32)
                        XCB = MC.sb("XCB", [128, S], BF16)
                        R = MC.sb("R", [128, S], F32)
                        I_ = MC.sb("I", [128, S], F32)
                        A = MC.sb("A", [128, S], F32)
                        Bv = B("lru_small")
                        BXB, BXC, BXCB, BR, BI, BA = B("XB"), B("XC"), B("XCB"), B("R"), B("I"), B("A")
                        Bwig = [B(f"wig{i}") for i in range(2)]
                        Bwix = [B(f"wix{i}") for i in range(2)]
                        P.dma("sp", lambda e: e.dma_start(out=vec[:].rearrange("p g k -> p (g k)"), in_=vec_d),
                              writes=[Bv])
                        P.dma("pool", lambda e: e.dma_start(out=wa[:], in_=w_a.rearrange("g i o -> i g o")),
                              writes=[Bv])
                        P.dma("pool", lambda e: e.dma_start(out=wx[:], in_=w_x.rearrange("g i o -> i g o")),
                              writes=[Bv])
                        P.op("act", lambda e: e.activation(out=sc[:, :, 0], in_=vec[:, :, 7], func=AF.Exp, scale=-1.0),
                             reads=[Bv], writes=[Bv])
                        P.op("act", lambda e: e.activation(out=sc[:, :, 0], in_=sc[:, :, 0], func=AF.Ln, bias=1.0,
                                                           scale=1.0), reads=[Bv], writes=[Bv])
                        P.op("dve", lambda e: e.tensor_scalar(out=sc[:, :, 1], in0=sc[:, :, 0], scalar1=-16.0,
                                                              scalar2=None, op0=ALU.mult), reads=[Bv], writes=[Bv])
                        P.op("dve", lambda e: e.tensor_scalar(out=sc[:, :, 0], in0=sc[:, :, 0], scalar1=-8.0,
                                                              scalar2=None, op0=ALU.mult), reads=[Bv], writes=[Bv])
                        P.op("dve", lambda e: e.memset(XB[:, 0:PAD], 0.0), writes=[BXB])

                        def load_w(g):
                            i2 = g % 2
                            P.dma("pool", lambda e: e.dma_start(
                                out=wig[i2][:], in_=w_in[:, g * 128:(g + 1) * 128].rearrange("(c p) n -> p c n", p=128)),
                                writes=[Bwig[i2]])
                            P.dma("pool", lambda e: e.dma_start(
                                out=wix[i2][:],
                                in_=w_in[:, D + g * 128:D + (g + 1) * 128].rearrange("(c p) n -> p c n", p=128)),
                                writes=[Bwix[i2]])

                        def proj(wt, Bwt, tq, pb):
                            def mm(e):
                                r = None
                                for dc in range(NDC):
                                    r = e.matmul(psum[pb][:], lhsT=wt[:, dc, :], rhs=uT[:, dc, tq * 512:(tq + 1) * 512],
                                                 start=(dc == 0), stop=(dc == NDC - 1))
                                return r
                            P.op("pe", mm, reads=[Bwt, BuT[tq]], writes=[Bps[pb]])

                        load_w(0)
                        for g in range(NDC):
                            i2 = g % 2
                            if g + 1 < NDC:
                                load_w(g + 1)
                            for tq in range(4):
                                proj(wix[i2], Bwix[i2], tq, tq)
                                P.op("act", lambda e: e.copy(out=XB[:, PAD + tq * 512:PAD + (tq + 1) * 512],
                                                             in_=psum[tq][:]), reads=[Bps[tq]], writes=[BXB])
                            P.op("dve", lambda e: e.tensor_scalar(out=XC[:], in0=XB[:, PAD:PAD + S],
                                                                  scalar1=vec[:, g, 3:4], scalar2=vec[:, g, 4:5],
                                                                  op0=ALU.mult, op1=ALU.add),
                                 reads=[BXB, Bv], writes=[BXC])
                            for j in range(1, 4):
                                P.op("dve", lambda e: e.scalar_tensor_tensor(
                                    out=XC[:], in0=XB[:, PAD - j:PAD - j + S], scalar=vec[:, g, 3 - j:4 - j], in1=XC[:],
                                    op0=ALU.mult, op1=ALU.add), reads=[BXB, Bv, BXC], writes=[BXC])
                            P.op("act", lambda e: e.copy(out=XCB[:], in_=XC[:]), reads=[BXC], writes=[BXCB])
                            for tq in range(4):
                                P.op("pe", lambda e: e.matmul(psum[tq][:], lhsT=wa[:, g, :],
                                                              rhs=XCB[:, tq * 512:(tq + 1) * 512], start=True, stop=True),
                                     reads=[BXCB, Bv], writes=[Bps[tq]])
                                P.op("act", lambda e: e.activation(out=R[:, tq * 512:(tq + 1) * 512], in_=psum[tq][:],
                                                                   func=AF.Sigmoid, bias=vec[:, g, 5:6], scale=1.0),
                                     reads=[Bps[tq], Bv], writes=[BR])
                            for tq in range(4):
                                P.op("pe", lambda e: e.matmul(psum[4 + tq][:], lhsT=wx[:, g, :],
                                                              rhs=XCB[:, tq * 512:(tq + 1) * 512], start=True, stop=True),
                                     reads=[BXCB, Bv], writes=[Bps[4 + tq]])
                                P.op("act", lambda e: e.activation(out=I_[:, tq * 512:(tq + 1) * 512],
                                                                   in_=psum[4 + tq][:], func=AF.Sigmoid,
                                                                   bias=vec[:, g, 6:7], scale=1.0),
                                     reads=[Bps[4 + tq], Bv], writes=[BI])
                            P.op("act", lambda e: e.activation(out=A[:], in_=R[:], func=AF.Exp, scale=sc[:, g, 0:1]),
                                 reads=[BR, Bv], writes=[BA])
                            P.op("act", lambda e: e.activation(out=R[:], in_=R[:], func=AF.Exp, scale=sc[:, g, 1:2]),
                                 reads=[BR, Bv], writes=[BR])
                            P.op("dve", lambda e: e.tensor_scalar(out=R[:], in0=R[:], scalar1=-1.0, scalar2=1.0,
                                                                  op0=ALU.mult, op1=ALU.add), reads=[BR], writes=[BR])
                            P.op("act", lambda e: e.activation(out=R[:], in_=R[:], func=AF.Sqrt), reads=[BR],
                                 writes=[BR])
                            P.op("dve", lambda e: e.tensor_tensor(out=I_[:], in0=I_[:], in1=XC[:], op=ALU.mult),
                                 reads=[BI, BXC], writes=[BI])
                            P.op("dve", lambda e: e.tensor_tensor(out=I_[:], in0=I_[:], in1=R[:], op=ALU.mult),
                                 reads=[BI, BR], writes=[BI])
                            Y = XB[:, PAD:PAD + S]
                            P.op("dve", lambda e: e.tensor_tensor_scan(out=Y, data0=A[:], data1=I_[:], initial=0.0,
                                                                       op0=ALU.mult, op1=ALU.add),
                                 reads=[BA, BI], writes=[BXB])
                            for tq in range(4):
                                pb = 4 + tq
                                sl = slice(tq * 512, (tq + 1) * 512)
                                proj(wig[i2], Bwig[i2], tq, pb)
                                P.op("act", lambda e: e.activation(out=R[:, sl], in_=psum[pb][:], func=AF.Square),
                                     reads=[Bps[pb]], writes=[BR])
                                P.op("dve", lambda e: e.tensor_scalar(out=R[:, sl], in0=R[:, sl], scalar1=0.044715,
                                                                      scalar2=1.0, op0=ALU.mult, op1=ALU.add),
                                     reads=[BR], writes=[BR])
                                P.op("dve", lambda e: e.tensor_tensor(out=R[:, sl], in0=R[:, sl], in1=psum[pb][:],
                                                                      op=ALU.mult), reads=[BR, Bps[pb]], writes=[BR])
                                P.op("act", lambda e: e.activation(out=R[:, sl], in_=R[:, sl], func=AF.Sigmoid,
                                                                   scale=1.5957691216057308), reads=[BR], writes=[BR])
                                P.op("dve", lambda e: e.tensor_tensor(out=R[:, sl], in0=R[:, sl], in1=psum[pb][:],
                                                                      op=ALU.mult), reads=[BR, Bps[pb]], writes=[BR])
                                P.op("dve", lambda e: e.tensor_tensor(out=yT[:, g, sl], in0=R[:, sl], in1=Y[:, sl],
                                                                      op=ALU.mult), reads=[BR, BXB], writes=[ByT[g]])
                        P.barrier()
                out_proj(l, yT, ByT)

        def pool_layer(l):
            w_in, w_grp, vec_d = L[l, "w_in"], L[l, "w_grp"], L[l, "vec"]
            PAD = 16
            WINS = (2, 4, 8, 16)
            with ExitStack() as ls:
                LC = Ctx(nc, ls)
                yT = LC.sb("yT", [128, NDC, S], BF16)
                ByT = [B(f"yT{g}") for g in range(NDC)]
                with ExitStack() as us:
                    UC = Ctx(nc, us)
                    uT = UC.sb("uT", [128, NDC, S], BF16)
                    BuT = [B(f"uTq{i}") for i in range(4)]
                    norm_to_uT(l, uT, BuT)
                    with ExitStack() as ms:
                        MC = Ctx(nc, ms)
                        vec = MC.sb("pvec", [128, 8 + 64], F32)
                        wgr = MC.sb("wgr", [128, 4, 2, 256], BF16)
                        wi = [MC.sb(f"wi{i}", [128, NDC, 128], BF16) for i in range(2)]
                        V_ = [MC.sb(f"V{i}", [128, PAD + S], F32) for i in range(2)]
                        S1 = [MC.sb(f"S1{i}", [128, PAD + S], F32) for i in range(2)]
                        S2 = [MC.sb(f"S2{i}", [128, PAD + S], F32) for i in range(2)]
                        PT = [MC.sb(f"PT{i}", [128, S], BF16) for i in range(2)]
                        Bv = B("pool_small")
                        Bwi = [B(f"pwi{i}") for i in range(2)]
                        BV = [B(f"pV{i}") for i in range(2)]
                        BS1 = [B(f"pS1{i}") for i in range(2)]
                        BS2 = [B(f"pS2{i}") for i in range(2)]
                        BPT = [B(f"pPT{i}") for i in range(2)]
                        P.dma("sp", lambda e: e.dma_start(out=vec[:], in_=vec_d), writes=[Bv])
                        P.dma("pool", lambda e: e.dma_start(
                            out=wgr[:].rearrange("p a b o -> p (a b) o"),
                            in_=w_grp.rearrange("a (b p) o -> p (a b) o", p=128)), writes=[Bv])
                        for i in range(2):
                            P.op("dve", lambda e: e.memset(V_[i][:, 0:PAD], 0.0), writes=[BV[i]])
                            P.op("dve", lambda e: e.memset(S1[i][:, 0:PAD], 0.0), writes=[BS1[i]])
                            P.op("dve", lambda e: e.memset(S2[i][:, 0:PAD], 0.0), writes=[BS2[i]])

                        def load_w(g):
                            i2 = g % 2
                            P.dma("pool", lambda e: e.dma_start(
                                out=wi[i2][:], in_=w_in[:, g * 128:(g + 1) * 128].rearrange("(c p) n -> p c n", p=128)),
                                writes=[Bwi[i2]])

                        load_w(0)
                        for g in range(NDC):
                            i2 = g % 2
                            grp = g // 2
                            win = WINS[grp]
                            if g + 1 < NDC:
                                load_w(g + 1)
                            for tq in range(4):
                                pb = (g % 2) * 4 + tq

                                def mm(e):
                                    r = None
                                    for dc in range(NDC):
                                        r = e.matmul(psum[pb][:], lhsT=wi[i2][:, dc, :],
                                                     rhs=uT[:, dc, tq * 512:(tq + 1) * 512],
                                                     start=(dc == 0), stop=(dc == NDC - 1))
                                    return r
                                P.op("pe", mm, reads=[Bwi[i2], BuT[tq]], writes=[Bps[pb]])
                                P.op("act", lambda e: e.copy(out=V_[i2][:, PAD + tq * 512:PAD + (tq + 1) * 512],
                                                             in_=psum[pb][:]), reads=[Bps[pb]], writes=[BV[i2]])
                            cur, Bcur = V_[i2], BV[i2]
                            w = 1
                            nxt = [(S1[i2], BS1[i2]), (S2[i2], BS2[i2])]
                            k = 0
                            while w < win:
                                dst, Bdst = nxt[k % 2]
                                P.op("dve", lambda e: e.tensor_tensor(out=dst[:, PAD:PAD + S], in0=cur[:, PAD:PAD + S],
                                                                      in1=cur[:, PAD - w:PAD - w + S], op=ALU.add),
                                     reads=[Bcur], writes=[Bdst])
                                cur, Bcur = dst, Bdst
                                w *= 2
                                k += 1
                            P.op("dve", lambda e: e.scalar_tensor_tensor(
                                out=PT[i2][:], in0=cur[:, PAD:PAD + S], scalar=1.0 / win, in1=V_[i2][:, PAD:PAD + S],
                                op0=ALU.mult, op1=ALU.subtract), reads=[Bcur, BV[i2]], writes=[BPT[i2]])
                            P.op("dve", lambda e: e.tensor_tensor(out=cur[:, PAD:PAD + 16], in0=cur[:, PAD:PAD + 16],
                                                                  in1=vec[:, 8 + grp * 16:8 + (grp + 1) * 16],
                                                                  op=ALU.mult), reads=[Bcur, Bv], writes=[Bcur])
                            P.op("dve", lambda e: e.tensor_tensor(out=PT[i2][:, 0:16], in0=cur[:, PAD:PAD + 16],
                                                                  in1=V_[i2][:, PAD:PAD + 16], op=ALU.subtract),
                                 reads=[Bcur, BV[i2]], writes=[BPT[i2]])
                            if g % 2 == 1:
                                for oc in range(2):
                                    go = grp * 2 + oc
                                    for tq in range(4):
                                        pb = oc * 4 + tq

                                        def mg(e):
                                            r = None
                                            for ic in range(2):
                                                r = e.matmul(psum[pb][:], lhsT=wgr[:, grp, ic, oc * 128:(oc + 1) * 128],
                                                             rhs=PT[ic][:, tq * 512:(tq + 1) * 512],
                                                             start=(ic == 0), stop=(ic == 1))
                                            return r
                                        P.op("pe", mg, reads=[BPT[0], BPT[1], Bv], writes=[Bps[pb]])
                                        P.op("act", lambda e: e.activation(
                                            out=yT[:, go, tq * 512:(tq + 1) * 512], in_=psum[pb][:], func=AF.Copy,
                                            scale=vec[:, go:go + 1]), reads=[Bps[pb], Bv], writes=[ByT[go]])
                        P.barrier()
                out_proj(l, yT, ByT)


        def sb_layer(l):
            w_qkv, negm_d = L[l, "w_qkv"], L[l, "negm"]
            NH = 16
            with ExitStack() as ls:
                LC = Ctx(nc, ls)
                QT = LC.sb("QT", [128, NDC, S], BF16)
                KT = LC.sb("KT", [128, NDC, S], BF16)
                BQ = [[B(f"QT{hd}_{qg}") for qg in range(4)] for hd in range(NH)]
                BK = [B(f"KT{g}") for g in range(NDC)]
                Bvd = [B(f"vst{c}") for c in range(NCH)]
                with ExitStack() as us:
                    UC = Ctx(nc, us)
                    uT = UC.sb("uT", [128, NDC, S], BF16)
                    BuT = [B(f"uTq{i}") for i in range(4)]
                    norm_to_uT(l, uT, BuT)
                    with ExitStack() as ms:
                        MC = Ctx(nc, ms)
                        wq = [MC.sb(f"wq{i}", [128, NDC, 128], BF16) for i in range(2)]
                        wv = MC.sb("wv", [128, NDC, 512], BF16)
                        vst = [MC.sb(f"vst{i}", [128, 512], BF16) for i in range(2)]
                        Bwq = [B(f"wq{i}") for i in range(2)]
                        Bwv = B("wv")
                        Bvs = [B(f"vstage{i}") for i in range(2)]

                        def load_wq(j):
                            i2 = j % 2
                            P.dma("pool", lambda e: e.dma_start(
                                out=wq[i2][:], in_=w_qkv[:, j * 128:(j + 1) * 128].rearrange("(c p) n -> p c n", p=128)),
                                writes=[Bwq[i2]])
                        load_wq(0)
                        for j in range(16):
                            i2 = j % 2
                            if j + 1 < 16:
                                load_wq(j + 1)
                            g = j % 8
                            for tq in range(4):
                                pb = (j % 2) * 4 + tq

                                def mm(e):
                                    r = None
                                    for dc in range(NDC):
                                        r = e.matmul(psum[pb][:], lhsT=wq[i2][:, dc, :],
                                                     rhs=uT[:, dc, tq * 512:(tq + 1) * 512],
                                                     start=(dc == 0), stop=(dc == NDC - 1))
                                    return r
                                P.op("pe", mm, reads=[Bwq[i2], BuT[tq]], writes=[Bps[pb]])
                                if j < 8:
                                    P.op("act", lambda e: e.activation(out=QT[:, g, tq * 512:(tq + 1) * 512],
                                                                       in_=psum[pb][:], func=AF.Copy, scale=0.125),
                                         reads=[Bps[pb]], writes=[BQ[2 * g][tq], BQ[2 * g + 1][tq]])
                                else:
                                    P.op("dve", lambda e: e.tensor_copy(out=KT[:, g, tq * 512:(tq + 1) * 512],
                                                                        in_=psum[pb][:]),
                                         reads=[Bps[pb]], writes=[BK[g]])
                        for half in range(2):
                            P.dma("pool", lambda e: e.dma_start(
                                out=wv[:], in_=w_qkv[:, 2 * D + half * 512:2 * D + (half + 1) * 512].rearrange(
                                    "(c p) n -> p c n", p=128)), writes=[Bwv])
                            for c in range(NCH):
                                pb = c % 8
                                i2 = c % 2

                                def mv(e):
                                    r = None
                                    for dc in range(NDC):
                                        r = e.matmul(psum[pb][:], lhsT=uT[:, dc, c * 128:(c + 1) * 128],
                                                     rhs=wv[:, dc, :], start=(dc == 0), stop=(dc == NDC - 1))
                                    return r
                                P.op("pe", mv, reads=[Bwv, BuT[c // 4]], writes=[Bps[pb]])
                                if c % 2 == 0:
                                    P.op("act", lambda e: e.copy(out=vst[i2][:], in_=psum[pb][:]),
                                         reads=[Bps[pb]], writes=[Bvs[i2]])
                                else:
                                    P.op("dve", lambda e: e.tensor_copy(out=vst[i2][:], in_=psum[pb][:]),
                                         reads=[Bps[pb]], writes=[Bvs[i2]])
                                P.dma("sp", lambda e: e.dma_start(
                                    out=ust_d[c * 128:(c + 1) * 128, half * 512:(half + 1) * 512], in_=vst[i2][:]),
                                    reads=[Bvs[i2]], writes=[Bvd[c]])
                        P.barrier()
                with ExitStack() as ms:
                    MC = Ctx(nc, ms)
                    Vt = MC.sb("Vt", [128, NCH, D], BF16)
                    BV = B("Vt")
                    for c in range(NCH):
                        P.dma("sp", lambda e: e.dma_start(out=Vt[:, c, :], in_=ust_d[c * 128:(c + 1) * 128, :]),
                              reads=[Bvd[c]], writes=[BV])
                    negm = MC.sb("negm", [128, 4, 512], BF16)
                    ntri = MC.sb("ntri", [128, 128], BF16)
                    nones = MC.sb("nones", [128, 128], BF16)
                    identb = MC.sb("identb2", [128, 128], BF16)
                    Bc2 = B("sb_consts")
                    P.dma("pool", lambda e: e.dma_start(out=negm[:].rearrange("p a b -> p (a b)"), in_=negm_d),
                          writes=[Bc2])
                    P.op("dve", lambda e: e.tensor_scalar(out=ntri[:], in0=cst[:, 384:512], scalar1=-1.0, scalar2=None,
                                                          op0=ALU.mult), reads=[Bcst], writes=[Bc2])
                    P.op("dve", lambda e: e.tensor_scalar(out=nones[:], in0=ones, scalar1=-1.0, scalar2=None,
                                                          op0=ALU.mult), reads=[Bcst], writes=[Bc2])
                    P.op("dve", lambda e: e.tensor_copy(out=identb[:], in_=ident), reads=[Bcst], writes=[Bc2])
                    NLB, NAT, NE_ = 4, 4, 3
                    Lb = [MC.sb(f"Lb{i}", [128, 512], BF16) for i in range(NLB)]
                    AT = [MC.sb(f"AT{i}", [128, 512], BF16) for i in range(NAT)]
                    Et = [MC.sb(f"Et{i}", [128, 512], F32) for i in range(NE_)]
                    Lacc = MC.sb("Lacc", [128, 512], F32)
                    Laccb = [MC.sb(f"Laccb{i}", [128, 512], BF16) for i in range(2)]
                    BLb = [B(f"Lb{i}") for i in range(NLB)]
                    BAT = [B(f"AT{i}") for i in range(NAT)]
                    BEt = [B(f"Et{i}") for i in range(NE_)]
                    BLacc = B("Lacc")
                    BLaccb = [B(f"Laccb{i}") for i in range(2)]
                    blocks = []
                    for hd in range(NH):
                        for qg in range(4):
                            nkb = 4 * qg + 4
                            for kb in reversed(range(nkb)):
                                blocks.append(dict(hd=hd, qg=qg, kb=kb, first=(kb == nkb - 1), last=(kb == 0),
                                                   grp=hd * 4 + qg))
                    nacc = [0]

                    def stage_a(i, b):
                        hd, qg, kb = b["hd"], b["qg"], b["kb"]
                        g, pbase = hd // 2, (hd % 2) * 64
                        xb = i % 4
                        diag = kb >= 4 * qg
                        m = kb - 4 * qg

                        def mz(e):
                            r = e.matmul(psum[xb][:], lhsT=KT[pbase:pbase + 64, g, kb * 128:(kb + 1) * 128],
                                         rhs=QT[pbase:pbase + 64, g, qg * 512:(qg + 1) * 512], start=True,
                                         stop=True, skip_group_check=True)
                            if diag:
                                r = e.matmul(psum[xb][:], lhsT=identb[:], rhs=negm[:, m, :], start=False, stop=True,
                                             skip_group_check=True)
                            return r
                        P.op("pe", mz, reads=[BK[g], BQ[hd][qg], Bc2], writes=[Bps[xb]])
                        P.op("act", lambda e: e.activation(out=Et[i % NE_][:], in_=psum[xb][:], func=AF.Exp),
                             reads=[Bps[xb]], writes=[BEt[i % NE_]])
                        P.op("act", lambda e: e.activation(out=Lb[i % NLB][:], in_=Et[i % NE_][:], func=AF.Ln,
                                                           bias=1.0, scale=1.0),
                             reads=[BEt[i % NE_]], writes=[BLb[i % NLB]])

                    def stage_b(i, b):
                        hd, qg, kb = b["hd"], b["qg"], b["kb"]
                        g, pbase = hd // 2, (hd % 2) * 64
                        xb = i % 4
                        ob = 4 + b["grp"] % 2
                        first, last = b["first"], b["last"]
                        la = nacc[0] % 2

                        def m2(e):
                            r = e.matmul(psum[xb][:], lhsT=ntri[:], rhs=Lb[i % NLB][:], start=False, stop=True,
                                         skip_group_check=True)
                            if not first:
                                r = e.matmul(psum[xb][:], lhsT=nones[:], rhs=Laccb[la][:], start=False, stop=True,
                                             skip_group_check=True)
                            return r
                        P.op("pe", m2, reads=[BLb[i % NLB], Bc2] + ([] if first else [BLaccb[la]]), writes=[Bps[xb]])
                        P.op("act", lambda e: e.activation(out=AT[i % NAT][:], in_=psum[xb][:], func=AF.Exp),
                             reads=[Bps[xb]], writes=[BAT[i % NAT]])
                        if not last:
                            if first:
                                P.op("dve", lambda e: e.tensor_copy(out=Lacc[:], in_=Lb[i % NLB][:]),
                                     reads=[BLb[i % NLB]], writes=[BLacc])
                            else:
                                P.op("dve", lambda e: e.tensor_tensor(out=Lacc[:], in0=Lacc[:], in1=Lb[i % NLB][:],
                                                                      op=ALU.add),
                                     reads=[BLb[i % NLB], BLacc], writes=[BLacc])
                            nacc[0] += 1
                            lb2 = nacc[0] % 2
                            P.op("dve", lambda e: e.tensor_copy(out=Laccb[lb2][:], in_=Lacc[:]),
                                 reads=[BLacc], writes=[BLaccb[lb2]])

                    def stage_c(i, b):
                        hd, qg, kb = b["hd"], b["qg"], b["kb"]
                        g, pbase = hd // 2, (hd % 2) * 64
                        ob = 4 + b["grp"] % 2
                        first, last = b["first"], b["last"]
                        P.op("pe", lambda e: e.matmul(psum[ob][:], lhsT=Vt[:, kb, g * 128:(g + 1) * 128],
                                                      rhs=AT[i % NAT][:], start=first, stop=last,
                                                      skip_group_check=True),
                             reads=[BV, BAT[i % NAT]], writes=[Bps[ob]])
                        if last:
                            P.op("act", lambda e: e.copy(out=QT[pbase:pbase + 64, g, qg * 512:(qg + 1) * 512],
                                                         in_=psum[ob][pbase:pbase + 64, :]),
                                 reads=[Bps[ob]], writes=[BQ[hd][qg]])

                    nb = len(blocks)
                    for i in range(nb + 2):
                        if i < nb:
                            stage_a(i, blocks[i])
                        if 1 <= i <= nb:
                            stage_b(i - 1, blocks[i - 1])
                        if i >= 2:
                            stage_c(i - 2, blocks[i - 2])
                    P.barrier()
                out_proj(l, QT, [BQ[hd][qg] for hd in range(NH) for qg in range(4)])


        for l in layers:
            if cfg["mix"][l]:
                [lru_layer, pool_layer, sb_layer][l % 3](l)
            if cfg["moe"][l]:
                moe_A(l)
                for c in range(NCH):
                    P.dma("sp", lambda e: e.dma_start(out=hsp_d[c * 128:(c + 1) * 128, :], in_=h[:, c, :]),
                          reads=[Bh[c]], writes=[Bhsp[c]])
                P.barrier()
                hst["stack"].close()
                moe_B(l)
                hst["stack"] = ExitStack()
                hst["n"] += 1
                h = hst["stack"].enter_context(nc.sbuf_tensor(f"h_res{hst['n']}", [128, NCH, D], F32))
                for c in range(NCH):
                    P.dma("sp", lambda e: e.dma_start(out=h[:, c, :], in_=hsp_d[c * 128:(c + 1) * 128, :]),
                          reads=[Bhsp[c]], writes=[Bh[c]])
                moe_C(l)

        if cfg.get("final", False):
            fcol = 8 * D
            with ExitStack() as ms:
                MC = Ctx(nc, ms)
                gam = MC.sb("gamf", [128, D], F32)
                junk = MC.sb("junkf", [128, D], F32)
                P.dma("sp", lambda e: e.dma_start(out=gam[:], in_=nrm_d[:, fcol:fcol + D]), writes=[B("gamf")])
                for c in range(NCH):
                    par = c % 2
                    Br = rms_rstd(c, par, junk, B("junkf"))
                    P.op("dve", lambda e: e.scalar_tensor_tensor(
                        out=h[:, c, :], in0=h[:, c, :], scalar=rstd[:, par:par + 1],
                        in1=gam[:], op0=ALU.mult, op1=ALU.mult),
                        reads=[Bh[c], Br, B("gamf")], writes=[Bh[c]])
                P.barrier()
        Bout = B("out_d")
        for c in range(NCH):
            P.dma("sp", lambda e: e.dma_start(out=out_d[c * 128:(c + 1) * 128, :], in_=h[:, c, :]),
                  reads=[Bh[c]], writes=[Bout])
        P.barrier()
        hst["stack"].close()
        print("instructions:", P.n_instr, {k: v for k, v in P.ecnt.items()})
    return nc


def host_consts():
    ident = np.eye(128, dtype=np.float32)
    tri = np.triu(np.ones((128, 128), np.float32), 1)
    ones = np.ones((128, 128), np.float32)
    lowi = np.tril(np.ones((128, 128), np.float32))
    cst = np.concatenate([ident, tri, ones, lowi], axis=1)
    ecap = np.tile((np.arange(NE, dtype=np.float32) * CAP)[None, None, :], (128, NCH, 1)).reshape(128, NCH * NE)
    return np.ascontiguousarray(cst), np.ascontiguousarray(ecap)


def layer_inputs(inputs, l):
    m = {}
    m[f"w_router{l}"] = np.ascontiguousarray(inputs["moe_w_router"][l])
    m[f"b_router{l}"] = np.ascontiguousarray(np.broadcast_to(inputs["moe_b_router"][l][None, :], (128, NE)))
    m[f"w_gu{l}"] = np.ascontiguousarray(inputs["moe_w_gate_up"][l])
    bgu = inputs["moe_b_gate_up"][l].reshape(NE, 16, 128).transpose(2, 0, 1).reshape(128, NE * 16)
    m[f"b_gu{l}"] = np.ascontiguousarray(bgu)
    m[f"w_dn{l}"] = np.ascontiguousarray(inputs["moe_w_down"][l])
    m[f"b_dn{l}"] = np.ascontiguousarray(inputs["moe_b_down"][l])
    return m


def mixer_inputs(inputs, l):
    kind, slot = l % 3, l // 3
    m = {}
    if kind == 0:
        m[f"lru_w_in{l}"] = np.ascontiguousarray(inputs["lru_w_in"][slot])
        rows = [inputs["lru_conv_w"][slot][k] for k in range(4)] + [
            inputs["lru_conv_b"][slot], inputs["lru_b_a"][slot], inputs["lru_b_x"][slot], inputs["lru_a_param"][slot]]
        v = np.stack(rows, axis=-1).reshape(8, 128, 8).transpose(1, 0, 2).reshape(128, 64)
        m[f"lru_vec{l}"] = np.ascontiguousarray(v.astype(np.float32))
        m[f"lru_w_a{l}"] = np.ascontiguousarray(inputs["lru_w_a"][slot])
        m[f"lru_w_x{l}"] = np.ascontiguousarray(inputs["lru_w_x"][slot])
        m[f"mix_w_out{l}"] = np.ascontiguousarray(inputs["lru_w_out"][slot])
    elif kind == 1:
        m[f"pool_w_in{l}"] = np.ascontiguousarray(inputs["pool_w_in"][slot])
        m[f"pool_w_grp{l}"] = np.ascontiguousarray(inputs["pool_w_group"][slot])
        sc = inputs["pool_scale"][slot].reshape(8, 128).T
        corr = np.ones((4, 16), np.float32)
        for gi, w in enumerate((2, 4, 8, 16)):
            t = np.arange(16)
            corr[gi] = 1.0 / np.minimum(t + 1, w)
        v = np.concatenate([sc, np.broadcast_to(corr.reshape(1, 64), (128, 64))], axis=1)
        m[f"pool_vec{l}"] = np.ascontiguousarray(v.astype(np.float32))
        m[f"mix_w_out{l}"] = np.ascontiguousarray(inputs["pool_w_out"][slot])
    else:
        m[f"sb_w_qkv{l}"] = np.ascontiguousarray(inputs["sb_w_qkv"][slot])
        m[f"mix_w_out{l}"] = np.ascontiguousarray(inputs["sb_w_out"][slot])
        nm = np.zeros((128, 4, 512), np.float32)
        sl = np.arange(128)[:, None]
        tl = np.arange(512)[None, :]
        for mi in range(4):
            nm[:, mi, :] = np.where(mi * 128 + sl < tl, 0.0, -30000.0)
        m[f"sb_negmask{l}"] = np.ascontiguousarray(nm.reshape(128, 2048))
    return m


def norms_input(inputs):
    rows = [inputs["mix_norm"][i] for i in range(4)] + [inputs["ffn_norm"][i] for i in range(4)] + [inputs["final_norm"]]
    v = np.concatenate(rows).astype(np.float32)
    return np.ascontiguousarray(np.broadcast_to(v[None, :], (128, 9 * D)))


N_CORES = 8


def kernel(**inputs):
    inputs = {k: np.asarray(v) for k, v in inputs.items()}
    cfg = dict(layers=[0, 1, 2, 3], moe=[True] * 4, mix=[True] * 4, final=True)
    nc = bass.Bass("TRN2", target_bir_lowering=False)
    build(nc, cfg)
    cst, ecap = host_consts()
    shared = {"cst": cst, "ecap": ecap, "norms": norms_input(inputs)}
    for l in range(4):
        shared.update(layer_inputs(inputs, l))
        shared.update(mixer_inputs(inputs, l))
    x = np.ascontiguousarray(inputs["x"].astype(np.float32))
    in_maps = []
    for c in range(N_CORES):
        m = dict(shared)
        m["x"] = x[c]
        in_maps.append(m)
    res = run_bass_kernel_spmd(nc, in_maps, core_ids=list(range(N_CORES)))
    out = np.stack([np.asarray(r["out"]) for r in res.results], axis=0)
    return out.astype(np.float32)
```

```python
import numpy as np
from contextlib import ExitStack
import concourse.bass as bass
import concourse.mybir as mybir
from concourse.bass_utils import run_bass_kernel_spmd

F32 = mybir.dt.float32
BF16 = mybir.dt.bfloat16
I32 = mybir.dt.int32
U32 = mybir.dt.uint32
AF = mybir.ActivationFunctionType
ALU = mybir.AluOpType
AX = mybir.AxisListType

D = 1024
S = 2048
NCH = S // 128
NDC = D // 128
NE = 32
TOPK = 4
CAP = 384
NSLOT = NE * CAP
RMS_EPS = 1e-6
SAME_ENG_SYNC = True


class Buf:
    __slots__ = ("name", "w", "r")

    def __init__(self, name):
        self.name = name
        self.w = None
        self.r = {}


class Prog:
    def __init__(self, nc, stack, n_dsem=(20, 8, 20)):
        self.nc = nc
        self.eng = {"pe": nc.tensor, "dve": nc.vector, "act": nc.scalar,
                    "pool": nc.gpsimd, "sp": nc.sync}
        self.esem = {k: stack.enter_context(nc.semaphore("es_" + k)) for k in self.eng}
        self.ecnt = {k: 0 for k in self.eng}
        self.waited = {k: {} for k in self.eng}
        self.dq = {}
        for q, n in zip(("sp", "act", "pool"), n_dsem):
            sems = [stack.enter_context(nc.semaphore(f"ds_{q}{i}")) for i in range(n)]
            self.dq[q] = {"sems": sems, "cnt": [0] * n, "next": 0}
        self.n_instr = 0

    def _wait(self, e, evs):
        for ev in evs:
            if ev is None:
                continue
            key, sem, val = ev
            if not SAME_ENG_SYNC and key == e:
                continue
            if key == e and e == "pe":
                continue
            if self.waited[e].get(key, 0) >= val:
                continue
            self.eng[e].wait_ge(sem, val)
            self.waited[e][key] = val

    def _deps(self, reads, writes):
        evs = []
        for b in reads:
            evs.append(b.w)
        for b in writes:
            evs.append(b.w)
            evs.extend(b.r.values())
        return evs

    def _commit(self, ev, reads, writes):
        for b in reads:
            old = b.r.get(ev[0])
            if old is None or old[2] < ev[2]:
                b.r[ev[0]] = ev
        for b in writes:
            b.w = ev
            b.r = {}

    def op(self, e, fn, reads=(), writes=()):
        self._wait(e, self._deps(reads, writes))
        ins = fn(self.eng[e])
        self.ecnt[e] += 1
        ins.then_inc(self.esem[e], 1)
        ev = (e, self.esem[e], self.ecnt[e])
        self._commit(ev, reads, writes)
        self.n_instr += 1
        return ev

    def dma(self, q, fn, reads=(), writes=()):
        d = self.dq[q]
        i = d["next"]
        d["next"] = (i + 1) % len(d["sems"])
        key = f"d_{q}{i}"
        evs = self._deps(reads, writes)
        if d["cnt"][i] > 0:
            evs.append((key, d["sems"][i], d["cnt"][i]))
        self._wait(q, evs)
        ins = fn(self.eng[q])
        d["cnt"][i] += 16
        ins.then_inc(d["sems"][i], 16)
        ev = (key, d["sems"][i], d["cnt"][i])
        self._commit(ev, reads, writes)
        self.n_instr += 1
        return ev

    def barrier(self):
        evs = [(k, self.esem[k], self.ecnt[k]) for k in self.eng if self.ecnt[k] > 0]
        for q, d in self.dq.items():
            for i, sm_ in enumerate(d["sems"]):
                if d["cnt"][i] > 0:
                    evs.append((f"d_{q}{i}", sm_, d["cnt"][i]))
        for e in self.eng:
            self._wait(e, [ev for ev in evs if ev[0] != e])

    def wait_all(self, e, bufs):
        evs = []
        for b in bufs:
            evs.append(b.w)
            evs.extend(b.r.values())
        self._wait(e, evs)


class Ctx:
    def __init__(self, nc, stack):
        self.nc = nc
        self.stack = stack
        self.bufs = {}

    _uid = [0]

    def sb(self, name, shape, dt):
        Ctx._uid[0] += 1
        t = self.stack.enter_context(self.nc.sbuf_tensor(f"sb_{name}_{Ctx._uid[0]}", list(shape), dt))
        return t

    def ps(self, name, shape, dt=F32):
        Ctx._uid[0] += 1
        t = self.stack.enter_context(self.nc.psum_tensor(f"pp_{name}_{Ctx._uid[0]}", list(shape), dt))
        return t

    def B(self, name):
        if name not in self.bufs:
            self.bufs[name] = Buf(name)
        return self.bufs[name]


def build(nc, cfg):
    layers = cfg["layers"]
    stack = ExitStack()
    with stack:
        P = Prog(nc, stack)
        C = Ctx(nc, stack)
        B = C.B

        def din(name, shape, dt=F32):
            return nc.dram_tensor(name, list(shape), dt, kind="ExternalInput").ap()

        x_d = din("x", [S, D])
        out_d = nc.dram_tensor("out", [S, D], F32, kind="ExternalOutput").ap()
        cst_d = din("cst", [128, 4 * 128])
        ecap_d = din("ecap", [128, NCH * NE])
        nrm_d = din("norms", [128, 9 * D])
        L = {}
        for l in layers:
            if cfg["moe"][l]:
                L[l, "w_router"] = din(f"w_router{l}", [D, NE])
                L[l, "b_router"] = din(f"b_router{l}", [128, NE])
                L[l, "w_gu"] = din(f"w_gu{l}", [NE, D, 2 * D])
                L[l, "b_gu"] = din(f"b_gu{l}", [128, NE * 16])
                L[l, "w_dn"] = din(f"w_dn{l}", [NE, D, D])
                L[l, "b_dn"] = din(f"b_dn{l}", [NE, D])

        for l in layers:
            if cfg["mix"][l]:
                kind = l % 3
                if kind == 0:
                    L[l, "w_in"] = din(f"lru_w_in{l}", [D, 2 * D])
                    L[l, "vec"] = din(f"lru_vec{l}", [128, 8 * 8])
                    L[l, "w_a"] = din(f"lru_w_a{l}", [8, 128, 128])
                    L[l, "w_x"] = din(f"lru_w_x{l}", [8, 128, 128])
                    L[l, "w_out"] = din(f"mix_w_out{l}", [D, D])
                elif kind == 1:
                    L[l, "w_in"] = din(f"pool_w_in{l}", [D, D])
                    L[l, "w_grp"] = din(f"pool_w_grp{l}", [4, 256, 256])
                    L[l, "vec"] = din(f"pool_vec{l}", [128, 8 + 64])
                    L[l, "w_out"] = din(f"mix_w_out{l}", [D, D])
                else:
                    L[l, "w_qkv"] = din(f"sb_w_qkv{l}", [D, 3 * D])
                    L[l, "w_out"] = din(f"mix_w_out{l}", [D, D])
                    L[l, "negm"] = din(f"sb_negmask{l}", [128, 4 * 512])
        dbg = cfg.get("debug", False)
        skind = "ExternalOutput" if dbg else "Internal"
        xs_d = nc.dram_tensor("xs_scr", [NSLOT, D], BF16, kind=skind).ap()
        ys_d = nc.dram_tensor("ys_scr", [NSLOT, D], F32, kind=skind).ap()
        ust_d = nc.dram_tensor("ust_scr", [S, D], BF16, kind=skind).ap()
        if dbg:
            dbg_d = nc.dram_tensor("dbg", [128, 8 * 512], F32, kind="ExternalOutput").ap()
            dbgx_d = nc.dram_tensor("dbgx", [128, NDC * CAP], BF16, kind="ExternalOutput").ap()
            dbgw_d = nc.dram_tensor("dbgw", [128, NDC * 512], BF16, kind="ExternalOutput").ap()
            dbgh_d = nc.dram_tensor("dbgh", [128, NDC * CAP], BF16, kind="ExternalOutput").ap()
        B_xs, B_ys = B("xs_d"), B("ys_d")

        cst = C.sb("cst", [128, 4 * 128], F32)
        ident = cst[:, 0:128]
        tri = cst[:, 128:256]
        ones = cst[:, 256:384]
        ecap = C.sb("ecap", [128, NCH * NE], F32)
        ssq = C.sb("ssq", [128, 2], F32)
        rstd = C.sb("rstd", [128, 2], F32)
        Bcst = B("cst")
        gat = C.sb("gat", [128, NCH, NE], F32)
        dki = C.sb("dki", [128, NCH * TOPK], I32)
        gk = C.sb("gk", [128, NCH * TOPK], F32)
        bgu = C.sb("bgu", [128, NE * 16], F32)
        bdn = C.sb("bdn", [NE, D], F32)
        hsp_d = nc.dram_tensor("hsp_scr", [S, D], F32, kind="Internal").ap()
        Bhsp = [B(f"hsp{c}") for c in range(NCH)]

        psum_all = C.ps("psall", [128, 8 * 512])
        psum = [psum_all[:, i * 512:(i + 1) * 512] for i in range(8)]
        Bps = [B(f"ps{i}") for i in range(8)]

        bound_reg = nc.gpsimd.to_reg(NSLOT - 1)
        hst = {"stack": ExitStack(), "n": 0}
        h = hst["stack"].enter_context(nc.sbuf_tensor("h_res0", [128, NCH, D], F32))
        Bh = [B(f"h{c}") for c in range(NCH)]

        P.dma("sp", lambda e: e.dma_start(out=cst[:], in_=cst_d), writes=[Bcst])
        P.dma("pool", lambda e: e.dma_start(out=ecap[:], in_=ecap_d), writes=[Bcst])
        for c in range(NCH):
            P.dma("sp", lambda e: e.dma_start(out=h[:, c, :], in_=x_d[c * 128:(c + 1) * 128, :]),
                  writes=[Bh[c]])

        def rms_rstd(c, par, junk, Bj):
            Bs, Br = B(f"ssq{par}"), B(f"rstd{par}")
            P.op("act", lambda e: e.activation(out=junk[:], in_=h[:, c, :], func=AF.Square,
                                               accum_out=ssq[:, par:par + 1]),
                 reads=[Bh[c]], writes=[Bj, Bs])
            P.op("dve", lambda e: e.tensor_scalar(out=rstd[:, par:par + 1], in0=ssq[:, par:par + 1],
                                                  scalar1=1.0 / D, scalar2=RMS_EPS, op0=ALU.mult, op1=ALU.add),
                 reads=[Bs], writes=[Br])
            P.op("act", lambda e: e.activation(out=rstd[:, par:par + 1], in_=rstd[:, par:par + 1], func=AF.Sqrt),
                 reads=[Br], writes=[Br])
            P.op("dve", lambda e: e.reciprocal(out=rstd[:, par:par + 1], in_=rstd[:, par:par + 1]),
                 reads=[Br], writes=[Br])
            return Br

        MS = {}
        RB = []

        def moe_common(l):
            Bw = B("moe_small")
            Ball = B("route")
            RB[:] = [B("routeA"), B("routeB")]
            return (4 + l) * D, L[l, "w_router"], L[l, "b_router"], L[l, "w_gu"], L[l, "b_gu"], L[l, "w_dn"], L[l, "b_dn"], Bw, Ball

        def moe_A(l):
            gcol, w_router, b_router, w_gu, b_gu, w_dn, b_dn, Bw, Ball = moe_common(l)
            P.dma("sp", lambda e: e.dma_start(out=bgu[:], in_=b_gu), writes=[Bw])
            P.dma("sp", lambda e: e.dma_start(out=bdn[:], in_=b_dn), writes=[Bw])
            with ExitStack() as ms:
                MC = Ctx(nc, ms)
                wr = MC.sb("wr", [128, NDC, NE], F32)
                br = MC.sb("br", [128, NE], F32)
                gam = MC.sb("gam", [128, D], F32)
                junk = MC.sb("junk", [128, D], F32)
                Bj = B("junk")
                P.dma("sp", lambda e: e.dma_start(out=wr[:], in_=w_router.rearrange("(c p) n -> p c n", p=128)),
                      writes=[Bw])
                P.dma("sp", lambda e: e.dma_start(out=br[:], in_=b_router), writes=[Bw])
                P.dma("sp", lambda e: e.dma_start(out=gam[:], in_=nrm_d[:, gcol:gcol + D]), writes=[Bw])
                NUF = 3
                uf = [MC.sb(f"uf{i}", [128, D], F32) for i in range(NUF)]
                ubig = MC.sb("ubig", [128, NCH, D], BF16)
                uT = [MC.sb(f"uT{i}", [128, NDC, 128], F32) for i in range(2)]
                ssq16 = MC.sb("ssq16", [128, NCH], F32)
                rs16 = MC.sb("rs16", [128, NCH], F32)
                logit = MC.sb("logit", [128, NCH, NE], F32)
                top8 = MC.sb("top8", [128, NCH, 8], F32)
                mask = MC.sb("mask", [128, NCH, NE], F32)
                sm = MC.sb("sm", [128, NCH, 2], F32)
                dest = MC.sb("dest", [128, NCH, NE], F32)
                tmp32 = MC.sb("tmp32", [128, NCH, NE], F32)
                off = MC.sb("off", [128, NCH, NE], F32)
                dk = MC.sb("dk", [128, NCH, TOPK], F32)
                Blog = [B(f"logit{c}") for c in range(NCH)]
                Bub = [B(f"ubig{c}") for c in range(NCH)]
                Bss = B("ssq16")
                for c in range(NCH):
                    P.op("act", lambda e: e.activation(out=junk[:], in_=h[:, c, :], func=AF.Square,
                                                       accum_out=ssq16[:, c:c + 1]),
                         reads=[Bh[c]], writes=[Bj, Bss])
                P.op("dve", lambda e: e.tensor_scalar(out=rs16[:], in0=ssq16[:], scalar1=1.0 / D, scalar2=RMS_EPS,
                                                      op0=ALU.mult, op1=ALU.add), reads=[Bss], writes=[Bss])
                P.op("act", lambda e: e.activation(out=rs16[:], in_=rs16[:], func=AF.Sqrt), reads=[Bss],
                     writes=[Bss])
                P.op("dve", lambda e: e.reciprocal(out=rs16[:], in_=rs16[:]), reads=[Bss], writes=[Bss])

                def s1(c):
                    i3 = c % NUF
                    P.op("dve", lambda e: e.scalar_tensor_tensor(
                        out=uf[i3][:], in0=h[:, c, :], scalar=rs16[:, c:c + 1], in1=gam[:],
                        op0=ALU.mult, op1=ALU.mult), reads=[Bh[c], Bss, Bw], writes=[B(f"uf{i3}")])
                    P.op("act", lambda e: e.copy(out=ubig[:, c, :], in_=uf[i3][:]), reads=[B(f"uf{i3}")],
                         writes=[Bub[c]])

                def s2(c):
                    i3, par = c % NUF, c % 2
                    for g in range(2):
                        pb = 2 * par + g

                        def tr(e):
                            r = None
                            for j in range(4):
                                dc = g * 4 + j
                                r = e.transpose(out=psum[pb][:, j * 128:(j + 1) * 128],
                                                in_=uf[i3][:, dc * 128:(dc + 1) * 128], identity=ident)
                            return r
                        P.op("pe", tr, reads=[B(f"uf{i3}"), Bcst], writes=[Bps[pb]])
                        src = psum[pb][:].rearrange("p (a b) -> p a b", a=4)
                        if g == 0:
                            P.op("act", lambda e: e.copy(out=uT[par][:, 0:4, :], in_=src),
                                 reads=[Bps[pb]], writes=[B(f"uT{par}")])
                        else:
                            P.op("dve", lambda e: e.tensor_copy(out=uT[par][:, 4:8, :], in_=src),
                                 reads=[Bps[pb]], writes=[B(f"uT{par}")])

                def s3(c):
                    par = c % 2
                    pl = 4 + par

                    def rt(e):
                        r = None
                        for dc in range(NDC):
                            r = e.matmul(psum[pl][:, 0:NE], lhsT=uT[par][:, dc, :], rhs=wr[:, dc, :],
                                         start=(dc == 0), stop=(dc == NDC - 1))
                        return r
                    P.op("pe", rt, reads=[B(f"uT{par}"), Bw], writes=[Bps[pl]])
                    P.op("dve", lambda e: e.tensor_tensor(out=logit[:, c, :], in0=psum[pl][:, 0:NE], in1=br[:],
                                                          op=ALU.add),
                         reads=[Bps[pl], Bw], writes=[Blog[c]])
                    P.op("dve", lambda e: e.max(out=top8[:, c, :], in_=logit[:, c, :]), reads=[Blog[c]],
                         writes=[Blog[c]])

                carry = MC.sb("carry", [128, NE], F32)
                xs_bufs = []
                MS['xs_bufs'] = xs_bufs
                HC = NCH // 2

                def route_batch(c0, c1, hi):
                    nch = c1 - c0
                    Bl = Blog[c0:c1]
                    Br_ = RB[hi]
                    L3 = [128, nch, NE]
                    sl3 = lambda t: t[:, c0:c1, :]
                    fl = lambda t: t[:, c0:c1, :].rearrange("p c e -> p (c e)")
                    P.op("dve", lambda e: e.tensor_tensor(out=sl3(mask), in0=sl3(logit),
                                                          in1=top8[:, c0:c1, 3:4].broadcast_to(L3), op=ALU.is_ge),
                         reads=Bl, writes=[Br_])
                    P.op("dve", lambda e: e.tensor_tensor(out=sl3(gat), in0=sl3(logit),
                                                          in1=top8[:, c0:c1, 0:1].broadcast_to(L3), op=ALU.subtract),
                         reads=Bl, writes=[Br_])
                    P.op("act", lambda e: e.activation(out=sl3(gat), in_=sl3(gat), func=AF.Exp), reads=[Br_],
                         writes=[Br_])
                    P.op("dve", lambda e: e.tensor_tensor(out=sl3(gat), in0=sl3(gat), in1=sl3(mask), op=ALU.mult),
                         reads=[Br_], writes=[Br_])
                    P.op("dve", lambda e: e.tensor_reduce(out=sm[:, c0:c1, 0], in_=sl3(gat), axis=AX.X, op=ALU.add),
                         reads=[Br_], writes=[Br_])
                    P.op("dve", lambda e: e.reciprocal(out=sm[:, c0:c1, 1], in_=sm[:, c0:c1, 0]), reads=[Br_],
                         writes=[Br_])
                    P.op("dve", lambda e: e.tensor_tensor(out=sl3(gat), in0=sl3(gat),
                                                          in1=sm[:, c0:c1, 1:2].broadcast_to(L3), op=ALU.mult),
                         reads=[Br_], writes=[Br_])
                    W = nch * NE
                    P.op("pe", lambda e: e.matmul(psum[6][:, 0:W], lhsT=tri, rhs=fl(mask), start=True, stop=True),
                         reads=[Br_, Bcst], writes=[Bps[6]])
                    P.op("pe", lambda e: e.matmul(psum[7][:, 0:W], lhsT=ones, rhs=fl(mask), start=True, stop=True),
                         reads=[Br_, Bcst], writes=[Bps[7]])
                    P.op("dve", lambda e: e.tensor_copy(out=fl(tmp32), in_=psum[7][:, 0:W]),
                         reads=[Bps[7]], writes=[Br_])
                    if hi == 0:
                        P.op("dve", lambda e: e.memset(off[:, c0, :], 0.0), writes=[Br_])
                    else:
                        P.op("dve", lambda e: e.tensor_copy(out=off[:, c0, :], in_=carry[:]),
                             reads=[B("carry")], writes=[Br_])
                    for c in range(c0 + 1, c1):
                        P.op("dve", lambda e: e.tensor_tensor(out=off[:, c, :], in0=off[:, c - 1, :],
                                                              in1=tmp32[:, c - 1, :], op=ALU.add),
                             reads=[Br_], writes=[Br_])
                    if hi == 0:
                        P.op("dve", lambda e: e.tensor_tensor(out=carry[:], in0=off[:, c1 - 1, :],
                                                              in1=tmp32[:, c1 - 1, :], op=ALU.add),
                             reads=[Br_], writes=[B("carry")])
                    P.op("dve", lambda e: e.tensor_tensor(out=fl(off), in0=fl(off), in1=psum[6][:, 0:W], op=ALU.add),
                         reads=[Br_, Bps[6]], writes=[Br_])
                    P.op("dve", lambda e: e.tensor_scalar(out=fl(tmp32), in0=fl(off), scalar1=float(CAP), scalar2=None,
                                                          op0=ALU.is_lt), reads=[Br_], writes=[Br_])
                    P.op("dve", lambda e: e.tensor_tensor(out=fl(gat), in0=fl(gat), in1=fl(tmp32), op=ALU.mult),
                         reads=[Br_], writes=[Br_])
                    P.op("dve", lambda e: e.tensor_tensor(out=fl(dest), in0=fl(off), in1=ecap[:, c0 * NE:c1 * NE],
                                                          op=ALU.add), reads=[Br_, Bcst], writes=[Br_])
                    P.op("dve", lambda e: e.tensor_scalar(out=fl(tmp32), in0=fl(tmp32), scalar1=-1.0e6, scalar2=1.0e6,
                                                          op0=ALU.mult, op1=ALU.add), reads=[Br_], writes=[Br_])
                    P.op("dve", lambda e: e.tensor_tensor(out=fl(dest), in0=fl(dest), in1=fl(tmp32), op=ALU.add),
                         reads=[Br_], writes=[Br_])
                    gk3 = gk[:].rearrange("p (c k) -> p c k", k=TOPK)
                    for k in range(TOPK):
                        P.op("dve", lambda e: e.tensor_tensor(out=sl3(mask), in0=sl3(logit),
                                                              in1=top8[:, c0:c1, k:k + 1].broadcast_to(L3),
                                                              op=ALU.is_equal), reads=[Br_], writes=[Br_])
                        P.op("dve", lambda e: e.tensor_tensor(out=sl3(tmp32), in0=sl3(mask), in1=sl3(dest),
                                                              op=ALU.mult), reads=[Br_], writes=[Br_])
                        P.op("dve", lambda e: e.tensor_reduce(out=dk[:, c0:c1, k], in_=sl3(tmp32), axis=AX.X,
                                                              op=ALU.add), reads=[Br_], writes=[Br_])
                        P.op("dve", lambda e: e.tensor_tensor(out=sl3(tmp32), in0=sl3(mask), in1=sl3(gat),
                                                              op=ALU.mult), reads=[Br_], writes=[Br_])
                        P.op("dve", lambda e: e.tensor_reduce(out=gk3[:, c0:c1, k], in_=sl3(tmp32), axis=AX.X,
                                                              op=ALU.add), reads=[Br_], writes=[Br_])
                    P.op("dve", lambda e: e.tensor_copy(out=dki[:, c0 * TOPK:c1 * TOPK],
                                                        in_=dk[:, c0:c1, :].rearrange("p c k -> p (c k)")),
                         reads=[Br_], writes=[Br_])
                    for c in range(c0, c1):
                        for k in range(TOPK):
                            col = c * TOPK + k
                            bx = Buf(f"xs{col}")
                            xs_bufs.append(bx)
                            P.dma("pool", lambda e: e.indirect_dma_start(
                                out=xs_d, out_offset=bass.IndirectOffsetOnAxis(ap=dki[:, col:col + 1], axis=0),
                                in_=ubig[:, c, :], in_offset=None, bounds_check=bound_reg, oob_is_err=False),
                                reads=[Bub[c], Br_], writes=[bx])

                for it in range(NCH + 2):
                    if it < NCH:
                        s1(it)
                    if 1 <= it <= NCH:
                        s2(it - 1)
                    if it >= 2:
                        s3(it - 2)
                    if it == HC + 1:
                        route_batch(0, HC, 0)
                route_batch(HC, NCH, 1)
                P.barrier()


        def moe_B(l):
            gcol, w_router, b_router, w_gu, b_gu, w_dn, b_dn, Bw, Ball = moe_common(l)
            xs_bufs = MS["xs_bufs"]
            with ExitStack() as ms:
                MC = Ctx(nc, ms)
                PW = 256
                NWF, NWG, NWD = 6, 16, 8
                wf = [MC.sb(f"wf{i}", [128, NDC, PW], F32) for i in range(NWF)]
                wg = [MC.sb(f"wg{i}", [128, NDC, PW], BF16) for i in range(NWG)]
                wd = [MC.sb(f"wd{i}", [128, NDC, PW], BF16) for i in range(NWD)]
                Bwf = [B(f"wf{i}") for i in range(NWF)]
                Bwg = [B(f"wg{i}") for i in range(NWG)]
                Bwd = [B(f"wd{i}") for i in range(NWD)]
                xT = [MC.sb(f"xT{i}", [128, NDC, CAP], BF16) for i in range(2)]
                hT = [MC.sb(f"hT{i}", [128, NDC, CAP], BF16) for i in range(2)]
                ysb = [MC.sb(f"ysb{i}", [128, 512], F32) for i in range(4)]
                t_g = [MC.sb(f"t_g{i}", [128, CAP], F32) for i in range(2)]
                t_s = [MC.sb(f"t_s{i}", [128, CAP], F32) for i in range(2)]
                t_u = [MC.sb(f"t_u{i}", [128, CAP], F32) for i in range(2)]
                BxT = [B(f"xT{i}") for i in range(2)]
                BhT = [B(f"hT{i}") for i in range(2)]
                Bys = [B(f"ysb{i}") for i in range(4)]
                Btg = [B(f"t_g{i}") for i in range(2)]
                Bts = [B(f"t_s{i}") for i in range(2)]
                Btu = [B(f"t_u{i}") for i in range(2)]
                NPC = 12
                NG = NE * NPC

                def piece_src(G):
                    e_, p = G // NPC, G % NPC
                    if p < 8:
                        q, is_up = p // 2, p % 2
                        c0 = is_up * D + q * PW
                        return w_gu[e_, :, c0:c0 + PW].rearrange("(c p) n -> p c n", p=128)
                    q = p - 8
                    return w_dn[e_, :, q * PW:(q + 1) * PW].rearrange("(c p) n -> p c n", p=128)

                def piece_dst(G):
                    e_, p = G // NPC, G % NPC
                    if p < 8:
                        i = (e_ * 8 + p) % NWG
                        return wg[i], Bwg[i]
                    i = (e_ * 4 + (p - 8)) % NWD
                    return wd[i], Bwd[i]

                def issue_dma(G):
                    if G >= NG:
                        return
                    i = G % NWF
                    P.dma("sp", lambda e: e.dma_start(out=wf[i][:], in_=piece_src(G)), writes=[Bwf[i]])

                def issue_cast(G):
                    if G >= NG:
                        return
                    i = G % NWF
                    dst, Bdst = piece_dst(G)
                    if (G % NPC) < 8:
                        P.op("act", lambda e: e.copy(out=dst[:], in_=wf[i][:]), reads=[Bwf[i]], writes=[Bdst])
                    else:
                        P.op("dve", lambda e: e.tensor_copy(out=dst[:], in_=wf[i][:]), reads=[Bwf[i]], writes=[Bdst])

                def tick(G):
                    issue_cast(G)
                    issue_dma(G + NWF)

                xr = MC.sb("xr", [128, CAP // 128, D], BF16)
                identb = MC.sb("identb3", [128, 128], BF16)
                Bxr = B("xr")
                P.op("dve", lambda e: e.tensor_copy(out=identb[:], in_=ident), reads=[Bcst], writes=[B("identb3")])

                def load_x(e_):
                    if e_ >= NE:
                        return
                    P.dma("sp", lambda e: e.dma_start(
                        out=xr[:], in_=xs_d[e_ * CAP:(e_ + 1) * CAP, :].rearrange("(c p) n -> p c n", p=128)),
                        reads=xs_bufs, writes=[Bxr])

                def transpose_x(e_):
                    if e_ >= NE:
                        return
                    i2 = e_ % 2
                    pv = psum[7][:].bitcast(BF16)
                    for sc in range(CAP // 128):
                        def tr(e):
                            r = None
                            for dc in range(NDC):
                                r = e.transpose(out=pv[:, dc * 128:(dc + 1) * 128],
                                                in_=xr[:, sc, dc * 128:(dc + 1) * 128], identity=identb[:])
                            return r
                        P.op("pe", tr, reads=[Bxr, B("identb3")], writes=[Bps[7]])
                        P.op("dve", lambda e: e.tensor_copy(out=xT[i2][:, :, sc * 128:(sc + 1) * 128],
                                                            in_=pv.rearrange("p (a b) -> p a b", a=NDC)),
                             reads=[Bps[7]], writes=[BxT[i2]])

                def compute_expert(e_):
                    i2 = e_ % 2
                    Gn = (e_ + 1) * NPC
                    transpose_x(e_ + 1)
                    load_x(e_ + 2)
                    for fcp in range(8):
                        q, j = fcp // 2, fcp % 2
                        sg = (e_ * 8 + 2 * q) % NWG
                        su = (e_ * 8 + 2 * q + 1) % NWG
                        pg, pu = (fcp % 2) * 2, (fcp % 2) * 2 + 1
                        ti = fcp % 2

                        def mm(e, slot, pb):
                            r = None
                            for dc in range(NDC):
                                r = e.matmul(psum[pb][:, 0:CAP], lhsT=wg[slot][:, dc, j * 128:(j + 1) * 128],
                                             rhs=xT[i2][:, dc, :], start=(dc == 0), stop=(dc == NDC - 1))
                            return r
                        P.op("pe", lambda e: mm(e, sg, pg), reads=[Bwg[sg], BxT[i2]], writes=[Bps[pg]])
                        P.op("pe", lambda e: mm(e, su, pu), reads=[Bwg[su], BxT[i2]], writes=[Bps[pu]])
                        bg = bgu[:, e_ * 16 + fcp:e_ * 16 + fcp + 1]
                        bu = bgu[:, e_ * 16 + 8 + fcp:e_ * 16 + 8 + fcp + 1]
                        P.op("dve", lambda e: e.tensor_scalar(out=t_g[ti][:], in0=psum[pg][:, 0:CAP], scalar1=bg,
                                                              scalar2=7.0, op0=ALU.add, op1=ALU.min),
                             reads=[Bps[pg], Bw], writes=[Btg[ti]])
                        P.op("act", lambda e: e.activation(out=t_s[ti][:], in_=t_g[ti][:], func=AF.Sigmoid,
                                                           scale=1.702),
                             reads=[Btg[ti]], writes=[Bts[ti]])
                        P.op("dve", lambda e: e.tensor_scalar(out=t_u[ti][:], in0=psum[pu][:, 0:CAP], scalar1=bu,
                                                              scalar2=7.0, op0=ALU.add, op1=ALU.min),
                             reads=[Bps[pu], Bw], writes=[Btu[ti]])
                        P.op("dve", lambda e: e.tensor_scalar(out=t_u[ti][:], in0=t_u[ti][:], scalar1=-7.0,
                                                               scalar2=1.0, op0=ALU.max, op1=ALU.add),
                             reads=[Btu[ti]], writes=[Btu[ti]])
                        P.op("dve", lambda e: e.tensor_tensor(out=t_g[ti][:], in0=t_g[ti][:], in1=t_s[ti][:],
                                                               op=ALU.mult),
                             reads=[Btg[ti], Bts[ti]], writes=[Btg[ti]])
                        P.op("dve", lambda e: e.tensor_tensor(out=hT[i2][:, fcp, :], in0=t_u[ti][:],
                                                              in1=t_g[ti][:], op=ALU.mult),
                             reads=[Btu[ti], Btg[ti]], writes=[BhT[i2]])
                        tick(Gn + fcp)
                    chunks = [(s0, min(128, CAP - s0)) for s0 in range(0, CAP, 128)]
                    step = 0
                    for half in range(2):
                        for (s0, sn) in chunks:
                            idx = step
                            pb = 4 + idx % 3
                            yi = idx % 4

                            def md(e):
                                r = None
                                for qq in range(2):
                                    sd = (e_ * 4 + half * 2 + qq) % NWD
                                    for fc in range(NDC):
                                        r = e.matmul(psum[pb][0:sn, qq * PW:(qq + 1) * PW],
                                                     lhsT=hT[i2][:, fc, s0:s0 + sn], rhs=wd[sd][:, fc, :],
                                                     start=(fc == 0), stop=(fc == NDC - 1))
                                return r
                            sds = [(e_ * 4 + half * 2 + qq) % NWD for qq in range(2)]
                            P.op("pe", md, reads=[BhT[i2]] + [Bwd[x] for x in sds], writes=[Bps[pb]])
                            P.op("act", lambda e: e.copy(out=ysb[yi][0:sn, :], in_=psum[pb][0:sn, :]),
                                 reads=[Bps[pb]], writes=[Bys[yi]])
                            r0 = e_ * CAP + s0
                            P.dma("act", lambda e: e.dma_start(
                                out=ys_d[r0:r0 + sn, half * 512:(half + 1) * 512], in_=ysb[yi][0:sn, :]),
                                reads=[Bys[yi]], writes=[Buf("ys")])
                            if step < 4:
                                tick(Gn + 8 + step)
                            step += 1
                    for st in range(step, 4):
                        tick(Gn + 8 + st)

                for G in range(NWF):
                    issue_dma(G)
                load_x(0)
                transpose_x(0)
                load_x(1)
                for G in range(NPC):
                    tick(G)
                for e_ in range(NE):
                    compute_expert(e_)
                P.barrier()

        def moe_C(l):
            gcol, w_router, b_router, w_gu, b_gu, w_dn, b_dn, Bw, Ball = moe_common(l)
            with ExitStack() as ms:
                MC = Ctx(nc, ms)
                yg = [MC.sb(f"yg{i}", [128, D], F32) for i in range(8)]
                Byg = [B(f"yg{i}") for i in range(8)]
                gT = MC.sb("gT", [NE, 128], F32)
                for i in range(8):
                    P.op("dve", lambda e: e.memset(yg[i][:], 0.0), writes=[Byg[i]])
                for c in range(NCH):
                    P.op("pe", lambda e: e.transpose(out=psum[0][0:NE, 0:128], in_=gat[:, c, :], identity=ident),
                         reads=RB + [Bcst], writes=[Bps[0]])
                    P.op("act", lambda e: e.copy(out=gT[:], in_=psum[0][0:NE, 0:128]), reads=[Bps[0]],
                         writes=[B("gT")])
                    for half in range(2):
                        P.op("pe", lambda e: e.matmul(psum[1 + half][:], lhsT=gT[:],
                                                      rhs=bdn[:, half * 512:(half + 1) * 512],
                                                      start=True, stop=True),
                             reads=[B("gT"), Bw], writes=[Bps[1 + half]])
                        P.op("dve", lambda e: e.tensor_tensor(out=h[:, c, half * 512:(half + 1) * 512],
                                                              in0=h[:, c, half * 512:(half + 1) * 512],
                                                              in1=psum[1 + half][:], op=ALU.add),
                             reads=[Bps[1 + half], Bh[c]], writes=[Bh[c]])
                    for k in range(TOPK):
                        col = c * TOPK + k
                        yi = (c % 2) * 4 + k
                        P.dma("pool", lambda e: e.indirect_dma_start(
                            out=yg[yi][:], out_offset=None, in_=ys_d,
                            in_offset=bass.IndirectOffsetOnAxis(ap=dki[:, col:col + 1], axis=0),
                            bounds_check=bound_reg, oob_is_err=False),
                            reads=list(RB), writes=[Byg[yi]])
                        P.op("dve", lambda e: e.scalar_tensor_tensor(
                            out=h[:, c, :], in0=yg[yi][:], scalar=gk[:, col:col + 1], in1=h[:, c, :],
                            op0=ALU.mult, op1=ALU.add),
                            reads=[Byg[yi], Bh[c]] + RB, writes=[Bh[c]])
                P.barrier()


        def norm_to_uT(l, uT, BuT):
            gcol = l * D
            with ExitStack() as ms:
                MC = Ctx(nc, ms)
                gam = MC.sb("gam", [128, D], F32)
                junk = MC.sb("junk", [128, D], F32)
                NU = 3
                ubf = [MC.sb(f"ubf{i}", [128, D], BF16) for i in range(NU)]
                identb = MC.sb("identb", [128, 128], BF16)
                ssq16 = MC.sb("nssq16", [128, NCH], F32)
                rs16 = MC.sb("nrs16", [128, NCH], F32)
                Bg, Bj, Bss = B("gam_m"), B("junk_m"), B("nssq16")
                P.dma("sp", lambda e: e.dma_start(out=gam[:], in_=nrm_d[:, gcol:gcol + D]), writes=[Bg])
                P.op("dve", lambda e: e.tensor_copy(out=identb[:], in_=ident), reads=[Bcst], writes=[Bg])
                for c in range(NCH):
                    P.op("act", lambda e: e.activation(out=junk[:], in_=h[:, c, :], func=AF.Square,
                                                       accum_out=ssq16[:, c:c + 1]),
                         reads=[Bh[c]], writes=[Bj, Bss])
                P.op("dve", lambda e: e.tensor_scalar(out=rs16[:], in0=ssq16[:], scalar1=1.0 / D, scalar2=RMS_EPS,
                                                      op0=ALU.mult, op1=ALU.add), reads=[Bss], writes=[Bss])
                P.op("act", lambda e: e.activation(out=rs16[:], in_=rs16[:], func=AF.Sqrt), reads=[Bss], writes=[Bss])
                P.op("dve", lambda e: e.reciprocal(out=rs16[:], in_=rs16[:]), reads=[Bss], writes=[Bss])

                def s1(c):
                    i3 = c % NU
                    P.op("dve", lambda e: e.scalar_tensor_tensor(
                        out=ubf[i3][:], in0=h[:, c, :], scalar=rs16[:, c:c + 1], in1=gam[:],
                        op0=ALU.mult, op1=ALU.mult), reads=[Bh[c], Bss, Bg], writes=[B(f"ubf{i3}")])

                def s2(c):
                    i3, pb = c % NU, c % 2
                    pv = psum[pb][:].bitcast(BF16)

                    def tr(e):
                        r = None
                        for dc in range(NDC):
                            r = e.transpose(out=pv[:, dc * 128:(dc + 1) * 128],
                                            in_=ubf[i3][:, dc * 128:(dc + 1) * 128], identity=identb[:])
                        return r
                    P.op("pe", tr, reads=[B(f"ubf{i3}"), Bg], writes=[Bps[pb]])
                    src = pv.rearrange("p (a b) -> p a b", a=NDC)
                    P.op("act", lambda e: e.copy(out=uT[:, :, c * 128:(c + 1) * 128], in_=src),
                         reads=[Bps[pb]], writes=[BuT[c // 4]])

                for it in range(NCH + 1):
                    if it < NCH:
                        s1(it)
                    if it >= 1:
                        s2(it - 1)
                P.barrier()

        def out_proj(l, yT, ByT):
            w_out = L[l, "w_out"]
            with ExitStack() as ms:
                MC = Ctx(nc, ms)
                wo = MC.sb("wo", [128, NDC, D], BF16)
                Bwo = B("wo")
                for half in range(2):
                    P.dma("pool", lambda e: e.dma_start(
                        out=wo[:, :, half * 512:(half + 1) * 512],
                        in_=w_out[:, half * 512:(half + 1) * 512].rearrange("(c p) n -> p c n", p=128)),
                        writes=[Bwo])
                for c in range(NCH):
                    for half in range(2):
                        pb = (c * 2 + half) % 8

                        def mm(e):
                            r = None
                            for g in range(NDC):
                                r = e.matmul(psum[pb][:], lhsT=yT[:, g, c * 128:(c + 1) * 128],
                                             rhs=wo[:, g, half * 512:(half + 1) * 512],
                                             start=(g == 0), stop=(g == NDC - 1))
                            return r
                        P.op("pe", mm, reads=list(ByT) + [Bwo], writes=[Bps[pb]])
                        P.op("dve", lambda e: e.tensor_tensor(out=h[:, c, half * 512:(half + 1) * 512],
                                                              in0=h[:, c, half * 512:(half + 1) * 512],
                                                              in1=psum[pb][:], op=ALU.add),
                             reads=[Bps[pb], Bh[c]], writes=[Bh[c]])
                P.barrier()

        def lru_layer(l):
            w_in, vec_d, w_a, w_x = L[l, "w_in"], L[l, "vec"], L[l, "w_a"], L[l, "w_x"]
            PAD = 4
            with ExitStack() as ls:
                LC = Ctx(nc, ls)
                yT = LC.sb("yT", [128, NDC, S], BF16)
                ByT = [B(f"yT{g}") for g in range(NDC)]
                with ExitStack() as us:
                    UC = Ctx(nc, us)
                    uT = UC.sb("uT", [128, NDC, S], BF16)
                    BuT = [B(f"uTq{i}") for i in range(4)]
                    norm_to_uT(l, uT, BuT)
                    with ExitStack() as ms:
                        MC = Ctx(nc, ms)
                        vec = MC.sb("vec", [128, 8, 8], F32)
                        sc = MC.sb("sc", [128, 8, 2], F32)
                        wa = MC.sb("wa", [128, 8, 128], BF16)
                        wx = MC.sb("wx", [128, 8, 128], BF16)
                        wig = [MC.sb(f"wig{i}", [128, NDC, 128], BF16) for i in range(2)]
                        wix = [MC.sb(f"wix{i}", [128, NDC, 128], BF16) for i in range(2)]
                        XB = MC.sb("XB", [128, PAD + S], F32)
                        XC = MC.sb("XC", [128, S], F32)
                        XCB = MC.sb("XCB", [128, S], BF16)
                        R = MC.sb("R", [128, S], F32)
                        I_ = MC.sb("I", [128, S], F32)
                        A = MC.sb("A", [128, S], F32)
                        Bv = B("lru_small")
                        BXB, BXC, BXCB, BR, BI, BA = B("XB"), B("XC"), B("XCB"), B("R"), B("I"), B("A")
                        Bwig = [B(f"wig{i}") for i in range(2)]
                        Bwix = [B(f"wix{i}") for i in range(2)]
                        P.dma("sp", lambda e: e.dma_start(out=vec[:].rearrange("p g k -> p (g k)"), in_=vec_d),
                              writes=[Bv])
                        P.dma("pool", lambda e: e.dma_start(out=wa[:], in_=w_a.rearrange("g i o -> i g o")),
                              writes=[Bv])
                        P.dma("pool", lambda e: e.dma_start(out=wx[:], in_=w_x.rearrange("g i o -> i g o")),
                              writes=[Bv])
                        P.op("act", lambda e: e.activation(out=sc[:, :, 0], in_=vec[:, :, 7], func=AF.Exp, scale=-1.0),
                             reads=[Bv], writes=[Bv])
                        P.op("act", lambda e: e.activation(out=sc[:, :, 0], in_=sc[:, :, 0], func=AF.Ln, bias=1.0,
                                                           scale=1.0), reads=[Bv], writes=[Bv])
                        P.op("dve", lambda e: e.tensor_scalar(out=sc[:, :, 1], in0=sc[:, :, 0], scalar1=-16.0,
                                                              scalar2=None, op0=ALU.mult), reads=[Bv], writes=[Bv])
                        P.op("dve", lambda e: e.tensor_scalar(out=sc[:, :, 0], in0=sc[:, :, 0], scalar1=-8.0,
                                                              scalar2=None, op0=ALU.mult), reads=[Bv], writes=[Bv])
                        P.op("dve", lambda e: e.memset(XB[:, 0:PAD], 0.0), writes=[BXB])

                        def load_w(g):
                            i2 = g % 2
                            P.dma("pool", lambda e: e.dma_start(
                                out=wig[i2][:], in_=w_in[:, g * 128:(g + 1) * 128].rearrange("(c p) n -> p c n", p=128)),
                                writes=[Bwig[i2]])
                            P.dma("pool", lambda e: e.dma_start(
                                out=wix[i2][:],
                                in_=w_in[:, D + g * 128:D + (g + 1) * 128].rearrange("(c p) n -> p c n", p=128)),
                                writes=[Bwix[i2]])

                        def proj(wt, Bwt, tq, pb):
                            def mm(e):
                                r = None
                                for dc in range(NDC):
                                    r = e.matmul(psum[pb][:], lhsT=wt[:, dc, :], rhs=uT[:, dc, tq * 512:(tq + 1) * 512],
                                                 start=(dc == 0), stop=(dc == NDC - 1))
                                return r
                            P.op("pe", mm, reads=[Bwt, BuT[tq]], writes=[Bps[pb]])

                        T_ = MC.sb("T", [128, S], F32)
                        BT = [B(f"lruT{i}") for i in range(4)]
                        load_w(0)
                        for g in range(NDC):
                            i2 = g % 2
                            if g + 1 < NDC:
                                load_w(g + 1)
                            for tq in range(4):
                                proj(wix[i2], Bwix[i2], tq, tq)
                                P.op("act", lambda e: e.copy(out=XB[:, PAD + tq * 512:PAD + (tq + 1) * 512],
                                                             in_=psum[tq][:]), reads=[Bps[tq]], writes=[BXB])
                            for tq in range(4):
                                proj(wig[i2], Bwig[i2], tq, 4 + tq)
                            P.op("dve", lambda e: e.tensor_scalar(out=XC[:], in0=XB[:, PAD:PAD + S],
                                                                  scalar1=vec[:, g, 3:4], scalar2=vec[:, g, 4:5],
                                                                  op0=ALU.mult, op1=ALU.add),
                                 reads=[BXB, Bv], writes=[BXC])
                            for tq in range(4):
                                sl = slice(tq * 512, (tq + 1) * 512)
                                P.op("act", lambda e: e.activation(out=T_[:, sl], in_=psum[4 + tq][:], func=AF.Square),
                                     reads=[Bps[4 + tq]], writes=[BT[tq]])
                            for j in range(1, 4):
                                P.op("dve", lambda e: e.scalar_tensor_tensor(
                                    out=XC[:], in0=XB[:, PAD - j:PAD - j + S], scalar=vec[:, g, 3 - j:4 - j], in1=XC[:],
                                    op0=ALU.mult, op1=ALU.add), reads=[BXB, Bv, BXC], writes=[BXC])
                            P.op("act", lambda e: e.copy(out=XCB[:], in_=XC[:]), reads=[BXC], writes=[BXCB])
                            for tq in range(4):
                                sl = slice(tq * 512, (tq + 1) * 512)
                                P.op("dve", lambda e: e.tensor_scalar(out=T_[:, sl], in0=T_[:, sl], scalar1=0.044715,
                                                                      scalar2=1.0, op0=ALU.mult, op1=ALU.add),
                                     reads=[BT[tq]], writes=[BT[tq]])
                                P.op("dve", lambda e: e.tensor_tensor(out=T_[:, sl], in0=T_[:, sl], in1=psum[4 + tq][:],
                                                                      op=ALU.mult), reads=[BT[tq], Bps[4 + tq]],
                                     writes=[BT[tq]])
                            for tq in range(4):
                                P.op("pe", lambda e: e.matmul(psum[tq][:], lhsT=wa[:, g, :],
                                                              rhs=XCB[:, tq * 512:(tq + 1) * 512], start=True, stop=True),
                                     reads=[BXCB, Bv], writes=[Bps[tq]])
                                P.op("act", lambda e: e.activation(out=R[:, tq * 512:(tq + 1) * 512], in_=psum[tq][:],
                                                                   func=AF.Sigmoid, bias=vec[:, g, 5:6], scale=1.0),
                                     reads=[Bps[tq], Bv], writes=[BR])
                            for tq in range(4):
                                P.op("pe", lambda e: e.matmul(psum[tq][:], lhsT=wx[:, g, :],
                                                              rhs=XCB[:, tq * 512:(tq + 1) * 512], start=True, stop=True),
                                     reads=[BXCB, Bv], writes=[Bps[tq]])
                                P.op("act", lambda e: e.activation(out=I_[:, tq * 512:(tq + 1) * 512],
                                                                   in_=psum[tq][:], func=AF.Sigmoid,
                                                                   bias=vec[:, g, 6:7], scale=1.0),
                                     reads=[Bps[tq], Bv], writes=[BI])
                            for tq in range(4):
                                sl = slice(tq * 512, (tq + 1) * 512)
                                P.op("act", lambda e: e.activation(out=T_[:, sl], in_=T_[:, sl], func=AF.Sigmoid,
                                                                   scale=1.5957691216057308), reads=[BT[tq]],
                                     writes=[BT[tq]])
                            P.op("act", lambda e: e.activation(out=A[:], in_=R[:], func=AF.Exp, scale=sc[:, g, 0:1]),
                                 reads=[BR, Bv], writes=[BA])
                            P.op("act", lambda e: e.activation(out=R[:], in_=R[:], func=AF.Exp, scale=sc[:, g, 1:2]),
                                 reads=[BR, Bv], writes=[BR])
                            for tq in range(4):
                                sl = slice(tq * 512, (tq + 1) * 512)
                                P.op("dve", lambda e: e.tensor_tensor(out=T_[:, sl], in0=T_[:, sl], in1=psum[4 + tq][:],
                                                                      op=ALU.mult), reads=[BT[tq], Bps[4 + tq]],
                                     writes=[BT[tq]])
                            P.op("dve", lambda e: e.tensor_tensor(out=I_[:], in0=I_[:], in1=XC[:], op=ALU.mult),
                                 reads=[BI, BXC], writes=[BI])
                            P.op("dve", lambda e: e.tensor_scalar(out=R[:], in0=R[:], scalar1=-1.0, scalar2=1.0,
                                                                  op0=ALU.mult, op1=ALU.add), reads=[BR], writes=[BR])
                            P.op("act", lambda e: e.activation(out=R[:], in_=R[:], func=AF.Sqrt), reads=[BR],
                                 writes=[BR])
                            P.op("dve", lambda e: e.tensor_tensor(out=I_[:], in0=I_[:], in1=R[:], op=ALU.mult),
                                 reads=[BI, BR], writes=[BI])
                            Y = XB[:, PAD:PAD + S]
                            P.op("dve", lambda e: e.tensor_tensor_scan(out=Y, data0=A[:], data1=I_[:], initial=0.0,
                                                                       op0=ALU.mult, op1=ALU.add),
                                 reads=[BA, BI], writes=[BXB])
                            for tq in range(4):
                                sl = slice(tq * 512, (tq + 1) * 512)
                                P.op("dve", lambda e: e.tensor_tensor(out=yT[:, g, sl], in0=T_[:, sl], in1=Y[:, sl],
                                                                      op=ALU.mult), reads=[BT[tq], BXB], writes=[ByT[g]])
                        P.barrier()
                out_proj(l, yT, ByT)

        def pool_layer(l):
            w_in, w_grp, vec_d = L[l, "w_in"], L[l, "w_grp"], L[l, "vec"]
            PAD = 16
            WINS = (2, 4, 8, 16)
            with ExitStack() as ls:
                LC = Ctx(nc, ls)
                yT = LC.sb("yT", [128, NDC, S], BF16)
                ByT = [B(f"yT{g}") for g in range(NDC)]
                with ExitStack() as us:
                    UC = Ctx(nc, us)
                    uT = UC.sb("uT", [128, NDC, S], BF16)
                    BuT = [B(f"uTq{i}") for i in range(4)]
                    norm_to_uT(l, uT, BuT)
                    with ExitStack() as ms:
                        MC = Ctx(nc, ms)
                        vec = MC.sb("pvec", [128, 8 + 64], F32)
                        wgr = MC.sb("wgr", [128, 4, 2, 256], BF16)
                        wi = [MC.sb(f"wi{i}", [128, NDC, 128], BF16) for i in range(2)]
                        V_ = [MC.sb(f"V{i}", [128, PAD + S], F32) for i in range(2)]
                        S1 = [MC.sb(f"S1{i}", [128, PAD + S], F32) for i in range(2)]
                        S2 = [MC.sb(f"S2{i}", [128, PAD + S], F32) for i in range(2)]
                        PT = [MC.sb(f"PT{i}", [128, S], BF16) for i in range(2)]
                        Bv = B("pool_small")
                        Bwi = [B(f"pwi{i}") for i in range(2)]
                        BV = [B(f"pV{i}") for i in range(2)]
                        BS1 = [B(f"pS1{i}") for i in range(2)]
                        BS2 = [B(f"pS2{i}") for i in range(2)]
                        BPT = [B(f"pPT{i}") for i in range(2)]
                        P.dma("sp", lambda e: e.dma_start(out=vec[:], in_=vec_d), writes=[Bv])
                        P.dma("pool", lambda e: e.dma_start(
                            out=wgr[:].rearrange("p a b o -> p (a b) o"),
                            in_=w_grp.rearrange("a (b p) o -> p (a b) o", p=128)), writes=[Bv])
                        for i in range(2):
                            P.op("dve", lambda e: e.memset(V_[i][:, 0:PAD], 0.0), writes=[BV[i]])
                            P.op("dve", lambda e: e.memset(S1[i][:, 0:PAD], 0.0), writes=[BS1[i]])
                            P.op("dve", lambda e: e.memset(S2[i][:, 0:PAD], 0.0), writes=[BS2[i]])

                        def load_w(g):
                            i2 = g % 2
                            P.dma("pool", lambda e: e.dma_start(
                                out=wi[i2][:], in_=w_in[:, g * 128:(g + 1) * 128].rearrange("(c p) n -> p c n", p=128)),
                                writes=[Bwi[i2]])

                        load_w(0)
                        for g in range(NDC):
                            i2 = g % 2
                            grp = g // 2
                            win = WINS[grp]
                            if g + 1 < NDC:
                                load_w(g + 1)
                            for tq in range(4):
                                pb = (g % 2) * 4 + tq

                                def mm(e):
                                    r = None
                                    for dc in range(NDC):
                                        r = e.matmul(psum[pb][:], lhsT=wi[i2][:, dc, :],
                                                     rhs=uT[:, dc, tq * 512:(tq + 1) * 512],
                                                     start=(dc == 0), stop=(dc == NDC - 1))
                                    return r
                                P.op("pe", mm, reads=[Bwi[i2], BuT[tq]], writes=[Bps[pb]])
                                P.op("act", lambda e: e.copy(out=V_[i2][:, PAD + tq * 512:PAD + (tq + 1) * 512],
                                                             in_=psum[pb][:]), reads=[Bps[pb]], writes=[BV[i2]])
                            cur, Bcur = V_[i2], BV[i2]
                            w = 1
                            nxt = [(S1[i2], BS1[i2]), (S2[i2], BS2[i2])]
                            k = 0
                            while w < win:
                                dst, Bdst = nxt[k % 2]
                                P.op("dve", lambda e: e.tensor_tensor(out=dst[:, PAD:PAD + S], in0=cur[:, PAD:PAD + S],
                                                                      in1=cur[:, PAD - w:PAD - w + S], op=ALU.add),
                                     reads=[Bcur], writes=[Bdst])
                                cur, Bcur = dst, Bdst
                                w *= 2
                                k += 1
                            P.op("dve", lambda e: e.scalar_tensor_tensor(
                                out=PT[i2][:], in0=cur[:, PAD:PAD + S], scalar=1.0 / win, in1=V_[i2][:, PAD:PAD + S],
                                op0=ALU.mult, op1=ALU.subtract), reads=[Bcur, BV[i2]], writes=[BPT[i2]])
                            P.op("dve", lambda e: e.tensor_tensor(out=cur[:, PAD:PAD + 16], in0=cur[:, PAD:PAD + 16],
                                                                  in1=vec[:, 8 + grp * 16:8 + (grp + 1) * 16],
                                                                  op=ALU.mult), reads=[Bcur, Bv], writes=[Bcur])
                            P.op("dve", lambda e: e.tensor_tensor(out=PT[i2][:, 0:16], in0=cur[:, PAD:PAD + 16],
                                                                  in1=V_[i2][:, PAD:PAD + 16], op=ALU.subtract),
                                 reads=[Bcur, BV[i2]], writes=[BPT[i2]])
                            if g % 2 == 1:
                                for oc in range(2):
                                    go = grp * 2 + oc
                                    for tq in range(4):
                                        pb = oc * 4 + tq

                                        def mg(e):
                                            r = None
                                            for ic in range(2):
                                                r = e.matmul(psum[pb][:], lhsT=wgr[:, grp, ic, oc * 128:(oc + 1) * 128],
                                                             rhs=PT[ic][:, tq * 512:(tq + 1) * 512],
                                                             start=(ic == 0), stop=(ic == 1))
                                            return r
                                        P.op("pe", mg, reads=[BPT[0], BPT[1], Bv], writes=[Bps[pb]])
                                        P.op("act", lambda e: e.activation(
                                            out=yT[:, go, tq * 512:(tq + 1) * 512], in_=psum[pb][:], func=AF.Copy,
                                            scale=vec[:, go:go + 1]), reads=[Bps[pb], Bv], writes=[ByT[go]])
                        P.barrier()
                out_proj(l, yT, ByT)


        def sb_layer(l):
            w_qkv, negm_d = L[l, "w_qkv"], L[l, "negm"]
            NH = 16
            with ExitStack() as ls:
                LC = Ctx(nc, ls)
                QT = LC.sb("QT", [128, NDC, S], BF16)
                KT = LC.sb("KT", [128, NDC, S], BF16)
                BQ = [[B(f"QT{hd}_{qg}") for qg in range(4)] for hd in range(NH)]
                BK = [B(f"KT{g}") for g in range(NDC)]
                Bvd = [B(f"vst{c}") for c in range(NCH)]
                with ExitStack() as us:
                    UC = Ctx(nc, us)
                    uT = UC.sb("uT", [128, NDC, S], BF16)
                    BuT = [B(f"uTq{i}") for i in range(4)]
                    norm_to_uT(l, uT, BuT)
                    with ExitStack() as ms:
                        MC = Ctx(nc, ms)
                        wq = [MC.sb(f"wq{i}", [128, NDC, 128], BF16) for i in range(2)]
                        wv = MC.sb("wv", [128, NDC, 512], BF16)
                        vst = [MC.sb(f"vst{i}", [128, 512], BF16) for i in range(2)]
                        Bwq = [B(f"wq{i}") for i in range(2)]
                        Bwv = B("wv")
                        Bvs = [B(f"vstage{i}") for i in range(2)]

                        def load_wq(j):
                            i2 = j % 2
                            P.dma("pool", lambda e: e.dma_start(
                                out=wq[i2][:], in_=w_qkv[:, j * 128:(j + 1) * 128].rearrange("(c p) n -> p c n", p=128)),
                                writes=[Bwq[i2]])
                        load_wq(0)
                        for j in range(16):
                            i2 = j % 2
                            if j + 1 < 16:
                                load_wq(j + 1)
                            g = j % 8
                            for tq in range(4):
                                pb = (j % 2) * 4 + tq

                                def mm(e):
                                    r = None
                                    for dc in range(NDC):
                                        r = e.matmul(psum[pb][:], lhsT=wq[i2][:, dc, :],
                                                     rhs=uT[:, dc, tq * 512:(tq + 1) * 512],
                                                     start=(dc == 0), stop=(dc == NDC - 1))
                                    return r
                                P.op("pe", mm, reads=[Bwq[i2], BuT[tq]], writes=[Bps[pb]])
                                if j < 8:
                                    P.op("act", lambda e: e.activation(out=QT[:, g, tq * 512:(tq + 1) * 512],
                                                                       in_=psum[pb][:], func=AF.Copy, scale=0.125),
                                         reads=[Bps[pb]], writes=[BQ[2 * g][tq], BQ[2 * g + 1][tq]])
                                else:
                                    P.op("dve", lambda e: e.tensor_copy(out=KT[:, g, tq * 512:(tq + 1) * 512],
                                                                        in_=psum[pb][:]),
                                         reads=[Bps[pb]], writes=[BK[g]])
                        for half in range(2):
                            P.dma("pool", lambda e: e.dma_start(
                                out=wv[:], in_=w_qkv[:, 2 * D + half * 512:2 * D + (half + 1) * 512].rearrange(
                                    "(c p) n -> p c n", p=128)), writes=[Bwv])
                            for c in range(NCH):
                                pb = c % 8
                                i2 = c % 2

                                def mv(e):
                                    r = None
                                    for dc in range(NDC):
                                        r = e.matmul(psum[pb][:], lhsT=uT[:, dc, c * 128:(c + 1) * 128],
                                                     rhs=wv[:, dc, :], start=(dc == 0), stop=(dc == NDC - 1))
                                    return r
                                P.op("pe", mv, reads=[Bwv, BuT[c // 4]], writes=[Bps[pb]])
                                if c % 2 == 0:
                                    P.op("act", lambda e: e.copy(out=vst[i2][:], in_=psum[pb][:]),
                                         reads=[Bps[pb]], writes=[Bvs[i2]])
                                else:
                                    P.op("dve", lambda e: e.tensor_copy(out=vst[i2][:], in_=psum[pb][:]),
                                         reads=[Bps[pb]], writes=[Bvs[i2]])
                                P.dma("sp", lambda e: e.dma_start(
                                    out=ust_d[c * 128:(c + 1) * 128, half * 512:(half + 1) * 512], in_=vst[i2][:]),
                                    reads=[Bvs[i2]], writes=[Bvd[c]])
                        P.barrier()
                with ExitStack() as ms:
                    MC = Ctx(nc, ms)
                    Vt = MC.sb("Vt", [128, NCH, D], BF16)
                    BV = B("Vt")
                    for c in range(NCH):
                        P.dma("sp", lambda e: e.dma_start(out=Vt[:, c, :], in_=ust_d[c * 128:(c + 1) * 128, :]),
                              reads=[Bvd[c]], writes=[BV])
                    negm = MC.sb("negm", [128, 4, 512], BF16)
                    ntri = MC.sb("ntri", [128, 128], BF16)
                    nones = MC.sb("nones", [128, 128], BF16)
                    identb = MC.sb("identb2", [128, 128], BF16)
                    Bc2 = B("sb_consts")
                    P.dma("pool", lambda e: e.dma_start(out=negm[:].rearrange("p a b -> p (a b)"), in_=negm_d),
                          writes=[Bc2])
                    P.op("dve", lambda e: e.tensor_scalar(out=ntri[:], in0=cst[:, 384:512], scalar1=-1.0, scalar2=None,
                                                          op0=ALU.mult), reads=[Bcst], writes=[Bc2])
                    P.op("dve", lambda e: e.tensor_scalar(out=nones[:], in0=ones, scalar1=-1.0, scalar2=None,
                                                          op0=ALU.mult), reads=[Bcst], writes=[Bc2])
                    P.op("dve", lambda e: e.tensor_copy(out=identb[:], in_=ident), reads=[Bcst], writes=[Bc2])
                    NR = 3
                    Lb = [MC.sb(f"Lb{i}", [128, 1024], BF16) for i in range(NR)]
                    AT = [MC.sb(f"AT{i}", [128, 1024], BF16) for i in range(NR)]
                    Et = [MC.sb(f"Et{i}", [128, 1024], F32) for i in range(2)]
                    Lacc = MC.sb("Lacc", [128, 512], F32)
                    Laccb = [MC.sb(f"Laccb{i}", [128, 512], BF16) for i in range(2)]
                    BLb = [B(f"Lb{i}") for i in range(NR)]
                    BAT = [B(f"AT{i}") for i in range(NR)]
                    BEt = [B(f"Et{i}") for i in range(2)]
                    BLacc = B("Lacc")
                    BLaccb = [B(f"Laccb{i}") for i in range(2)]
                    pairs = []
                    for hd in range(NH):
                        for qg in range(4):
                            nkb = 4 * qg + 4
                            for kb in range(nkb - 1, 0, -2):
                                pairs.append(dict(hd=hd, qg=qg, kb=kb, first=(kb == nkb - 1), last=(kb == 1),
                                                  grp=hd * 4 + qg))
                    nacc = [0]

                    def xpair(i):
                        x0 = (i % 3) * 2
                        return x0, psum_all[:, x0 * 512:(x0 + 2) * 512]

                    def stage_a(i, b):
                        hd, qg, kb = b["hd"], b["qg"], b["kb"]
                        g, pbase = hd // 2, (hd % 2) * 64
                        x0, xp = xpair(i)

                        def mz(e):
                            r = None
                            for t in range(2):
                                k_ = kb - t
                                r = e.matmul(psum[x0 + t][:], lhsT=KT[pbase:pbase + 64, g, k_ * 128:(k_ + 1) * 128],
                                             rhs=QT[pbase:pbase + 64, g, qg * 512:(qg + 1) * 512], start=True,
                                             stop=True, skip_group_check=True)
                                if k_ >= 4 * qg:
                                    r = e.matmul(psum[x0 + t][:], lhsT=identb[:], rhs=negm[:, k_ - 4 * qg, :],
                                                 start=False, stop=True, skip_group_check=True)
                            return r
                        P.op("pe", mz, reads=[BK[g], BQ[hd][qg], Bc2], writes=[Bps[x0], Bps[x0 + 1]])
                        P.op("act", lambda e: e.activation(out=Et[i % 2][:], in_=xp, func=AF.Exp),
                             reads=[Bps[x0], Bps[x0 + 1]], writes=[BEt[i % 2]])
                        P.op("act", lambda e: e.activation(out=Lb[i % NR][:], in_=Et[i % 2][:], func=AF.Ln,
                                                           bias=1.0, scale=1.0),
                             reads=[BEt[i % 2]], writes=[BLb[i % NR]])

                    def stage_b(i, b):
                        hd, qg, kb = b["hd"], b["qg"], b["kb"]
                        x0, xp = xpair(i)
                        first, last = b["first"], b["last"]
                        la = nacc[0] % 2
                        L0, L1 = Lb[i % NR][:, 0:512], Lb[i % NR][:, 512:1024]

                        def m2(e):
                            r = e.matmul(psum[x0][:], lhsT=ntri[:], rhs=L0, start=False, stop=True,
                                         skip_group_check=True)
                            if not first:
                                r = e.matmul(psum[x0][:], lhsT=nones[:], rhs=Laccb[la][:], start=False, stop=True,
                                             skip_group_check=True)
                            r = e.matmul(psum[x0 + 1][:], lhsT=ntri[:], rhs=L1, start=False, stop=True,
                                         skip_group_check=True)
                            r = e.matmul(psum[x0 + 1][:], lhsT=nones[:], rhs=L0, start=False, stop=True,
                                         skip_group_check=True)
                            if not first:
                                r = e.matmul(psum[x0 + 1][:], lhsT=nones[:], rhs=Laccb[la][:], start=False, stop=True,
                                             skip_group_check=True)
                            return r
                        P.op("pe", m2, reads=[BLb[i % NR], Bc2] + ([] if first else [BLaccb[la]]),
                             writes=[Bps[x0], Bps[x0 + 1]])
                        P.op("act", lambda e: e.activation(out=AT[i % NR][:], in_=xp, func=AF.Exp),
                             reads=[Bps[x0], Bps[x0 + 1]], writes=[BAT[i % NR]])
                        if not last:
                            if first:
                                P.op("dve", lambda e: e.tensor_tensor(out=Lacc[:], in0=L0, in1=L1, op=ALU.add),
                                     reads=[BLb[i % NR]], writes=[BLacc])
                            else:
                                P.op("dve", lambda e: e.tensor_tensor(out=Lacc[:], in0=Lacc[:], in1=L0, op=ALU.add),
                                     reads=[BLb[i % NR], BLacc], writes=[BLacc])
                                P.op("dve", lambda e: e.tensor_tensor(out=Lacc[:], in0=Lacc[:], in1=L1, op=ALU.add),
                                     reads=[BLb[i % NR], BLacc], writes=[BLacc])
                            nacc[0] += 1
                            lb2 = nacc[0] % 2
                            P.op("dve", lambda e: e.tensor_copy(out=Laccb[lb2][:], in_=Lacc[:]),
                                 reads=[BLacc], writes=[BLaccb[lb2]])

                    def stage_c(i, b):
                        hd, qg, kb = b["hd"], b["qg"], b["kb"]
                        g, pbase = hd // 2, (hd % 2) * 64
                        ob = 6 + b["grp"] % 2
                        first, last = b["first"], b["last"]

                        def mo(e):
                            r = e.matmul(psum[ob][:], lhsT=Vt[:, kb, g * 128:(g + 1) * 128],
                                         rhs=AT[i % NR][:, 0:512], start=first, stop=False, skip_group_check=True)
                            r = e.matmul(psum[ob][:], lhsT=Vt[:, kb - 1, g * 128:(g + 1) * 128],
                                         rhs=AT[i % NR][:, 512:1024], start=False, stop=last, skip_group_check=True)
                            return r
                        P.op("pe", mo, reads=[BV, BAT[i % NR]], writes=[Bps[ob]])
                        if last:
                            P.op("act", lambda e: e.copy(out=QT[pbase:pbase + 64, g, qg * 512:(qg + 1) * 512],
                                                         in_=psum[ob][pbase:pbase + 64, :]),
                                 reads=[Bps[ob]], writes=[BQ[hd][qg]])

                    nb = len(pairs)
                    for i in range(nb + 2):
                        if i < nb:
                            stage_a(i, pairs[i])
                        if 1 <= i <= nb:
                            stage_b(i - 1, pairs[i - 1])
                        if i >= 2:
                            stage_c(i - 2, pairs[i - 2])
                    P.barrier()
                out_proj(l, QT, [BQ[hd][qg] for hd in range(NH) for qg in range(4)])


        for l in layers:
            if cfg["mix"][l]:
                [lru_layer, pool_layer, sb_layer][l % 3](l)
            if cfg["moe"][l]:
                for c in range(NCH):
                    P.dma("sp", lambda e: e.dma_start(out=hsp_d[c * 128:(c + 1) * 128, :], in_=h[:, c, :]),
                          reads=[Bh[c]], writes=[Bhsp[c]])
                moe_A(l)
                hst["stack"].close()
                moe_B(l)
                hst["stack"] = ExitStack()
                hst["n"] += 1
                h = hst["stack"].enter_context(nc.sbuf_tensor(f"h_res{hst['n']}", [128, NCH, D], F32))
                for c in range(NCH):
                    P.dma("sp", lambda e: e.dma_start(out=h[:, c, :], in_=hsp_d[c * 128:(c + 1) * 128, :]),
                          reads=[Bhsp[c]], writes=[Bh[c]])
                moe_C(l)

        if cfg.get("final", False):
            fcol = 8 * D
            with ExitStack() as ms:
                MC = Ctx(nc, ms)
                gam = MC.sb("gamf", [128, D], F32)
                junk = MC.sb("junkf", [128, D], F32)
                P.dma("sp", lambda e: e.dma_start(out=gam[:], in_=nrm_d[:, fcol:fcol + D]), writes=[B("gamf")])
                for c in range(NCH):
                    par = c % 2
                    Br = rms_rstd(c, par, junk, B("junkf"))
                    P.op("dve", lambda e: e.scalar_tensor_tensor(
                        out=h[:, c, :], in0=h[:, c, :], scalar=rstd[:, par:par + 1],
                        in1=gam[:], op0=ALU.mult, op1=ALU.mult),
                        reads=[Bh[c], Br, B("gamf")], writes=[Bh[c]])
                P.barrier()
        Bout = B("out_d")
        for c in range(NCH):
            P.dma("sp", lambda e: e.dma_start(out=out_d[c * 128:(c + 1) * 128, :], in_=h[:, c, :]),
                  reads=[Bh[c]], writes=[Bout])
        P.barrier()
        hst["stack"].close()
        print("instructions:", P.n_instr, {k: v for k, v in P.ecnt.items()})
    return nc


def host_consts():
    ident = np.eye(128, dtype=np.float32)
    tri = np.triu(np.ones((128, 128), np.float32), 1)
    ones = np.ones((128, 128), np.float32)
    lowi = np.tril(np.ones((128, 128), np.float32))
    cst = np.concatenate([ident, tri, ones, lowi], axis=1)
    ecap = np.tile((np.arange(NE, dtype=np.float32) * CAP)[None, None, :], (128, NCH, 1)).reshape(128, NCH * NE)
    return np.ascontiguousarray(cst), np.ascontiguousarray(ecap)


def layer_inputs(inputs, l):
    m = {}
    m[f"w_router{l}"] = np.ascontiguousarray(inputs["moe_w_router"][l])
    m[f"b_router{l}"] = np.ascontiguousarray(np.broadcast_to(inputs["moe_b_router"][l][None, :], (128, NE)))
    m[f"w_gu{l}"] = np.ascontiguousarray(inputs["moe_w_gate_up"][l])
    bgu = inputs["moe_b_gate_up"][l].reshape(NE, 16, 128).transpose(2, 0, 1).reshape(128, NE * 16)
    m[f"b_gu{l}"] = np.ascontiguousarray(bgu)
    m[f"w_dn{l}"] = np.ascontiguousarray(inputs["moe_w_down"][l])
    m[f"b_dn{l}"] = np.ascontiguousarray(inputs["moe_b_down"][l])
    return m


def mixer_inputs(inputs, l):
    kind, slot = l % 3, l // 3
    m = {}
    if kind == 0:
        m[f"lru_w_in{l}"] = np.ascontiguousarray(inputs["lru_w_in"][slot])
        rows = [inputs["lru_conv_w"][slot][k] for k in range(4)] + [
            inputs["lru_conv_b"][slot], inputs["lru_b_a"][slot], inputs["lru_b_x"][slot], inputs["lru_a_param"][slot]]
        v = np.stack(rows, axis=-1).reshape(8, 128, 8).transpose(1, 0, 2).reshape(128, 64)
        m[f"lru_vec{l}"] = np.ascontiguousarray(v.astype(np.float32))
        m[f"lru_w_a{l}"] = np.ascontiguousarray(inputs["lru_w_a"][slot])
        m[f"lru_w_x{l}"] = np.ascontiguousarray(inputs["lru_w_x"][slot])
        m[f"mix_w_out{l}"] = np.ascontiguousarray(inputs["lru_w_out"][slot])
    elif kind == 1:
        m[f"pool_w_in{l}"] = np.ascontiguousarray(inputs["pool_w_in"][slot])
        m[f"pool_w_grp{l}"] = np.ascontiguousarray(inputs["pool_w_group"][slot])
        sc = inputs["pool_scale"][slot].reshape(8, 128).T
        corr = np.ones((4, 16), np.float32)
        for gi, w in enumerate((2, 4, 8, 16)):
            t = np.arange(16)
            corr[gi] = 1.0 / np.minimum(t + 1, w)
        v = np.concatenate([sc, np.broadcast_to(corr.reshape(1, 64), (128, 64))], axis=1)
        m[f"pool_vec{l}"] = np.ascontiguousarray(v.astype(np.float32))
        m[f"mix_w_out{l}"] = np.ascontiguousarray(inputs["pool_w_out"][slot])
    else:
        m[f"sb_w_qkv{l}"] = np.ascontiguousarray(inputs["sb_w_qkv"][slot])
        m[f"mix_w_out{l}"] = np.ascontiguousarray(inputs["sb_w_out"][slot])
        nm = np.zeros((128, 4, 512), np.float32)
        sl = np.arange(128)[:, None]
        tl = np.arange(512)[None, :]
        for mi in range(4):
            nm[:, mi, :] = np.where(mi * 128 + sl < tl, 0.0, -30000.0)
        m[f"sb_negmask{l}"] = np.ascontiguousarray(nm.reshape(128, 2048))
    return m


def norms_input(inputs):
    rows = [inputs["mix_norm"][i] for i in range(4)] + [inputs["ffn_norm"][i] for i in range(4)] + [inputs["final_norm"]]
    v = np.concatenate(rows).astype(np.float32)
    return np.ascontiguousarray(np.broadcast_to(v[None, :], (128, 9 * D)))


N_CORES = 8


def kernel(**inputs):
    inputs = {k: np.asarray(v) for k, v in inputs.items()}
    cfg = dict(layers=[0, 1, 2, 3], moe=[True] * 4, mix=[True] * 4, final=True)
    nc = bass.Bass("TRN2", target_bir_lowering=False)
    build(nc, cfg)
    cst, ecap = host_consts()
    shared = {"cst": cst, "ecap": ecap, "norms": norms_input(inputs)}
    for l in range(4):
        shared.update(layer_inputs(inputs, l))
        shared.update(mixer_inputs(inputs, l))
    x = np.ascontiguousarray(inputs["x"].astype(np.float32))
    in_maps = []
    for c in range(N_CORES):
        m = dict(shared)
        m["x"] = x[c]
        in_maps.append(m)
    res = run_bass_kernel_spmd(nc, in_maps, core_ids=list(range(N_CORES)))
    out = np.stack([np.asarray(r["out"]) for r in res.results], axis=0)
    return out.astype(np.float32)
```

```python
import numpy as np
from contextlib import ExitStack
import concourse.bass as bass
import concourse.mybir as mybir
from concourse.bass_utils import run_bass_kernel_spmd

F32 = mybir.dt.float32
BF16 = mybir.dt.bfloat16
I32 = mybir.dt.int32
U32 = mybir.dt.uint32
AF = mybir.ActivationFunctionType
ALU = mybir.AluOpType
AX = mybir.AxisListType

D = 1024
S = 2048
NCH = S // 128
NDC = D // 128
NE = 32
TOPK = 4
CAP = 384
NSLOT = NE * CAP
RMS_EPS = 1e-6
SAME_ENG_SYNC = True


class Buf:
    __slots__ = ("name", "w", "r")

    def __init__(self, name):
        self.name = name
        self.w = None
        self.r = {}


class Prog:
    def __init__(self, nc, stack, n_dsem=(20, 8, 20)):
        self.nc = nc
        self.eng = {"pe": nc.tensor, "dve": nc.vector, "act": nc.scalar,
                    "pool": nc.gpsimd, "sp": nc.sync}
        self.esem = {k: stack.enter_context(nc.semaphore("es_" + k)) for k in self.eng}
        self.ecnt = {k: 0 for k in self.eng}
        self.waited = {k: {} for k in self.eng}
        self.dq = {}
        for q, n in zip(("sp", "act", "pool"), n_dsem):
            sems = [stack.enter_context(nc.semaphore(f"ds_{q}{i}")) for i in range(n)]
            self.dq[q] = {"sems": sems, "cnt": [0] * n, "next": 0}
        self.n_instr = 0

    def _wait(self, e, evs):
        for ev in evs:
            if ev is None:
                continue
            key, sem, val = ev
            if not SAME_ENG_SYNC and key == e:
                continue
            if key == e and e == "pe":
                continue
            if self.waited[e].get(key, 0) >= val:
                continue
            self.eng[e].wait_ge(sem, val)
            self.waited[e][key] = val

    def _deps(self, reads, writes):
        evs = []
        for b in reads:
            evs.append(b.w)
        for b in writes:
            evs.append(b.w)
            evs.extend(b.r.values())
        return evs

    def _commit(self, ev, reads, writes):
        for b in reads:
            old = b.r.get(ev[0])
            if old is None or old[2] < ev[2]:
                b.r[ev[0]] = ev
        for b in writes:
            b.w = ev
            b.r = {}

    def op(self, e, fn, reads=(), writes=()):
        self._wait(e, self._deps(reads, writes))
        ins = fn(self.eng[e])
        self.ecnt[e] += 1
        ins.then_inc(self.esem[e], 1)
        ev = (e, self.esem[e], self.ecnt[e])
        self._commit(ev, reads, writes)
        self.n_instr += 1
        return ev

    def dma(self, q, fn, reads=(), writes=()):
        d = self.dq[q]
        i = d["next"]
        d["next"] = (i + 1) % len(d["sems"])
        key = f"d_{q}{i}"
        evs = self._deps(reads, writes)
        if d["cnt"][i] > 0:
            evs.append((key, d["sems"][i], d["cnt"][i]))
        self._wait(q, evs)
        ins = fn(self.eng[q])
        d["cnt"][i] += 16
        ins.then_inc(d["sems"][i], 16)
        ev = (key, d["sems"][i], d["cnt"][i])
        self._commit(ev, reads, writes)
        self.n_instr += 1
        return ev

    def barrier(self):
        evs = [(k, self.esem[k], self.ecnt[k]) for k in self.eng if self.ecnt[k] > 0]
        for q, d in self.dq.items():
            for i, sm_ in enumerate(d["sems"]):
                if d["cnt"][i] > 0:
                    evs.append((f"d_{q}{i}", sm_, d["cnt"][i]))
        for e in self.eng:
            self._wait(e, [ev for ev in evs if ev[0] != e])

    def wait_all(self, e, bufs):
        evs = []
        for b in bufs:
            evs.append(b.w)
            evs.extend(b.r.values())
        self._wait(e, evs)


class Ctx:
    def __init__(self, nc, stack):
        self.nc = nc
        self.stack = stack
        self.bufs = {}

    _uid = [0]

    def sb(self, name, shape, dt):
        Ctx._uid[0] += 1
        t = self.stack.enter_context(self.nc.sbuf_tensor(f"sb_{name}_{Ctx._uid[0]}", list(shape), dt))
        return t

    def ps(self, name, shape, dt=F32):
        Ctx._uid[0] += 1
        t = self.stack.enter_context(self.nc.psum_tensor(f"pp_{name}_{Ctx._uid[0]}", list(shape), dt))
        return t

    def B(self, name):
        if name not in self.bufs:
            self.bufs[name] = Buf(name)
        return self.bufs[name]


def build(nc, cfg):
    layers = cfg["layers"]
    stack = ExitStack()
    with stack:
        P = Prog(nc, stack)
        C = Ctx(nc, stack)
        B = C.B

        def din(name, shape, dt=F32):
            return nc.dram_tensor(name, list(shape), dt, kind="ExternalInput").ap()

        x_d = din("x", [S, D])
        out_d = nc.dram_tensor("out", [S, D], F32, kind="ExternalOutput").ap()
        cst_d = din("cst", [128, 4 * 128])
        ecap_d = din("ecap", [128, NCH * NE])
        nrm_d = din("norms", [128, 9 * D])
        L = {}
        for l in layers:
            if cfg["moe"][l]:
                L[l, "w_router"] = din(f"w_router{l}", [D, NE])
                L[l, "b_router"] = din(f"b_router{l}", [128, NE])
                L[l, "w_gu"] = din(f"w_gu{l}", [NE, D, 2 * D])
                L[l, "b_gu"] = din(f"b_gu{l}", [128, NE * 16])
                L[l, "w_dn"] = din(f"w_dn{l}", [NE, D, D])
                L[l, "b_dn"] = din(f"b_dn{l}", [NE, D])

        for l in layers:
            if cfg["mix"][l]:
                kind = l % 3
                if kind == 0:
                    L[l, "w_in"] = din(f"lru_w_in{l}", [D, 2 * D])
                    L[l, "vec"] = din(f"lru_vec{l}", [128, 8 * 8])
                    L[l, "w_a"] = din(f"lru_w_a{l}", [8, 128, 128])
                    L[l, "w_x"] = din(f"lru_w_x{l}", [8, 128, 128])
                    L[l, "w_out"] = din(f"mix_w_out{l}", [D, D])
                elif kind == 1:
                    L[l, "w_in"] = din(f"pool_w_in{l}", [D, D])
                    L[l, "w_grp"] = din(f"pool_w_grp{l}", [4, 256, 256])
                    L[l, "vec"] = din(f"pool_vec{l}", [128, 8 + 64])
                    L[l, "w_out"] = din(f"mix_w_out{l}", [D, D])
                else:
                    L[l, "w_qkv"] = din(f"sb_w_qkv{l}", [D, 3 * D])
                    L[l, "w_out"] = din(f"mix_w_out{l}", [D, D])
                    L[l, "negm"] = din(f"sb_negmask{l}", [128, 4 * 512])
        dbg = cfg.get("debug", False)
        skind = "ExternalOutput" if dbg else "Internal"
        xs_d = nc.dram_tensor("xs_scr", [NSLOT, D], BF16, kind=skind).ap()
        ys_d = nc.dram_tensor("ys_scr", [NSLOT, D], F32, kind=skind).ap()
        ust_d = nc.dram_tensor("ust_scr", [S, D], BF16, kind=skind).ap()
        if dbg:
            dbg_d = nc.dram_tensor("dbg", [128, 8 * 512], F32, kind="ExternalOutput").ap()
            dbgx_d = nc.dram_tensor("dbgx", [128, NDC * CAP], BF16, kind="ExternalOutput").ap()
            dbgw_d = nc.dram_tensor("dbgw", [128, NDC * 512], BF16, kind="ExternalOutput").ap()
            dbgh_d = nc.dram_tensor("dbgh", [128, NDC * CAP], BF16, kind="ExternalOutput").ap()
        B_xs, B_ys = B("xs_d"), B("ys_d")

        cst = C.sb("cst", [128, 4 * 128], F32)
        ident = cst[:, 0:128]
        tri = cst[:, 128:256]
        ones = cst[:, 256:384]
        ecap = C.sb("ecap", [128, NCH * NE], F32)
        ssq = C.sb("ssq", [128, 2], F32)
        rstd = C.sb("rstd", [128, 2], F32)
        Bcst = B("cst")
        gat = C.sb("gat", [128, NCH, NE], F32)
        dki = C.sb("dki", [128, NCH * TOPK], I32)
        gk = C.sb("gk", [128, NCH * TOPK], F32)
        bgu = C.sb("bgu", [128, NE * 16], F32)
        bdn = C.sb("bdn", [NE, D], F32)
        hsp_d = nc.dram_tensor("hsp_scr", [S, D], F32, kind="Internal").ap()
        Bhsp = [B(f"hsp{c}") for c in range(NCH)]

        psum_all = C.ps("psall", [128, 8 * 512])
        psum = [psum_all[:, i * 512:(i + 1) * 512] for i in range(8)]
        Bps = [B(f"ps{i}") for i in range(8)]

        bound_reg = nc.gpsimd.to_reg(NSLOT - 1)
        hst = {"stack": ExitStack(), "n": 0}
        h = hst["stack"].enter_context(nc.sbuf_tensor("h_res0", [128, NCH, D], F32))
        Bh = [B(f"h{c}") for c in range(NCH)]

        P.dma("sp", lambda e: e.dma_start(out=cst[:], in_=cst_d), writes=[Bcst])
        P.dma("pool", lambda e: e.dma_start(out=ecap[:], in_=ecap_d), writes=[Bcst])
        for c in range(NCH):
            P.dma("sp", lambda e: e.dma_start(out=h[:, c, :], in_=x_d[c * 128:(c + 1) * 128, :]),
                  writes=[Bh[c]])

        def rms_rstd(c, par, junk, Bj):
            Bs, Br = B(f"ssq{par}"), B(f"rstd{par}")
            P.op("act", lambda e: e.activation(out=junk[:], in_=h[:, c, :], func=AF.Square,
                                               accum_out=ssq[:, par:par + 1]),
                 reads=[Bh[c]], writes=[Bj, Bs])
            P.op("dve", lambda e: e.tensor_scalar(out=rstd[:, par:par + 1], in0=ssq[:, par:par + 1],
                                                  scalar1=1.0 / D, scalar2=RMS_EPS, op0=ALU.mult, op1=ALU.add),
                 reads=[Bs], writes=[Br])
            P.op("act", lambda e: e.activation(out=rstd[:, par:par + 1], in_=rstd[:, par:par + 1], func=AF.Sqrt),
                 reads=[Br], writes=[Br])
            P.op("dve", lambda e: e.reciprocal(out=rstd[:, par:par + 1], in_=rstd[:, par:par + 1]),
                 reads=[Br], writes=[Br])
            return Br

        MS = {}
        RB = []

        def moe_common(l):
            Bw = B("moe_small")
            Ball = B("route")
            RB[:] = [B(f"route{i}") for i in range(4)]
            return (4 + l) * D, L[l, "w_router"], L[l, "b_router"], L[l, "w_gu"], L[l, "b_gu"], L[l, "w_dn"], L[l, "b_dn"], Bw, Ball

        def moe_A(l):
            gcol, w_router, b_router, w_gu, b_gu, w_dn, b_dn, Bw, Ball = moe_common(l)
            P.dma("sp", lambda e: e.dma_start(out=bgu[:], in_=b_gu), writes=[Bw])
            P.dma("sp", lambda e: e.dma_start(out=bdn[:], in_=b_dn), writes=[Bw])
            with ExitStack() as ms:
                MC = Ctx(nc, ms)
                wr = MC.sb("wr", [128, NDC, NE], F32)
                br = MC.sb("br", [128, NE], F32)
                gam = MC.sb("gam", [128, D], F32)
                junk = MC.sb("junk", [128, D], F32)
                Bj = B("junk")
                P.dma("sp", lambda e: e.dma_start(out=wr[:], in_=w_router.rearrange("(c p) n -> p c n", p=128)),
                      writes=[Bw])
                P.dma("sp", lambda e: e.dma_start(out=br[:], in_=b_router), writes=[Bw])
                P.dma("sp", lambda e: e.dma_start(out=gam[:], in_=nrm_d[:, gcol:gcol + D]), writes=[Bw])
                NUF = 3
                uf = [MC.sb(f"uf{i}", [128, D], F32) for i in range(NUF)]
                ubig = MC.sb("ubig", [128, NCH, D], BF16)
                uT = [MC.sb(f"uT{i}", [128, NDC, 128], F32) for i in range(2)]
                ssq16 = MC.sb("ssq16", [128, NCH], F32)
                rs16 = MC.sb("rs16", [128, NCH], F32)
                logit = MC.sb("logit", [128, NCH, NE], F32)
                top8 = MC.sb("top8", [128, NCH, 8], F32)
                mask = MC.sb("mask", [128, NCH, NE], F32)
                sm = MC.sb("sm", [128, NCH, 2], F32)
                dest = MC.sb("dest", [128, NCH, NE], F32)
                tmp32 = MC.sb("tmp32", [128, NCH, NE], F32)
                off = MC.sb("off", [128, NCH, NE], F32)
                dk = MC.sb("dk", [128, NCH, TOPK], F32)
                Blog = [B(f"logit{c}") for c in range(NCH)]
                Bub = [B(f"ubig{c}") for c in range(NCH)]
                Bss = B("ssq16")
                for c in range(NCH):
                    P.op("act", lambda e: e.activation(out=junk[:], in_=h[:, c, :], func=AF.Square,
                                                       accum_out=ssq16[:, c:c + 1]),
                         reads=[Bh[c]], writes=[Bj, Bss])
                P.op("dve", lambda e: e.tensor_scalar(out=rs16[:], in0=ssq16[:], scalar1=1.0 / D, scalar2=RMS_EPS,
                                                      op0=ALU.mult, op1=ALU.add), reads=[Bss], writes=[Bss])
                P.op("act", lambda e: e.activation(out=rs16[:], in_=rs16[:], func=AF.Sqrt), reads=[Bss],
                     writes=[Bss])
                P.op("dve", lambda e: e.reciprocal(out=rs16[:], in_=rs16[:]), reads=[Bss], writes=[Bss])

                def s1(c):
                    i3 = c % NUF
                    P.op("dve", lambda e: e.scalar_tensor_tensor(
                        out=uf[i3][:], in0=h[:, c, :], scalar=rs16[:, c:c + 1], in1=gam[:],
                        op0=ALU.mult, op1=ALU.mult), reads=[Bh[c], Bss, Bw], writes=[B(f"uf{i3}")])
                    P.op("act", lambda e: e.copy(out=ubig[:, c, :], in_=uf[i3][:]), reads=[B(f"uf{i3}")],
                         writes=[Bub[c]])

                def s2(c):
                    i3, par = c % NUF, c % 2
                    for g in range(2):
                        pb = 2 * par + g

                        def tr(e):
                            r = None
                            for j in range(4):
                                dc = g * 4 + j
                                r = e.transpose(out=psum[pb][:, j * 128:(j + 1) * 128],
                                                in_=uf[i3][:, dc * 128:(dc + 1) * 128], identity=ident)
                            return r
                        P.op("pe", tr, reads=[B(f"uf{i3}"), Bcst], writes=[Bps[pb]])
                        src = psum[pb][:].rearrange("p (a b) -> p a b", a=4)
                        if g == 0:
                            P.op("act", lambda e: e.copy(out=uT[par][:, 0:4, :], in_=src),
                                 reads=[Bps[pb]], writes=[B(f"uT{par}")])
                        else:
                            P.op("dve", lambda e: e.tensor_copy(out=uT[par][:, 4:8, :], in_=src),
                                 reads=[Bps[pb]], writes=[B(f"uT{par}")])

                def s3(c):
                    par = c % 2
                    pl = 4 + par

                    def rt(e):
                        r = None
                        for dc in range(NDC):
                            r = e.matmul(psum[pl][:, 0:NE], lhsT=uT[par][:, dc, :], rhs=wr[:, dc, :],
                                         start=(dc == 0), stop=(dc == NDC - 1))
                        return r
                    P.op("pe", rt, reads=[B(f"uT{par}"), Bw], writes=[Bps[pl]])
                    P.op("dve", lambda e: e.tensor_tensor(out=logit[:, c, :], in0=psum[pl][:, 0:NE], in1=br[:],
                                                          op=ALU.add),
                         reads=[Bps[pl], Bw], writes=[Blog[c]])
                    P.op("dve", lambda e: e.max(out=top8[:, c, :], in_=logit[:, c, :]), reads=[Blog[c]],
                         writes=[Blog[c]])

                carry = MC.sb("carry", [128, NE], F32)
                xs_bufs = []
                MS['xs_bufs'] = xs_bufs
                NRG = 4
                GC = NCH // NRG

                def route_batch(c0, c1, hi):
                    nch = c1 - c0
                    Bl = Blog[c0:c1]
                    Br_ = RB[hi]
                    L3 = [128, nch, NE]
                    sl3 = lambda t: t[:, c0:c1, :]
                    fl = lambda t: t[:, c0:c1, :].rearrange("p c e -> p (c e)")
                    P.op("dve", lambda e: e.tensor_tensor(out=sl3(mask), in0=sl3(logit),
                                                          in1=top8[:, c0:c1, 3:4].broadcast_to(L3), op=ALU.is_ge),
                         reads=Bl, writes=[Br_])
                    P.op("dve", lambda e: e.tensor_tensor(out=sl3(gat), in0=sl3(logit),
                                                          in1=top8[:, c0:c1, 0:1].broadcast_to(L3), op=ALU.subtract),
                         reads=Bl, writes=[Br_])
                    P.op("act", lambda e: e.activation(out=sl3(gat), in_=sl3(gat), func=AF.Exp), reads=[Br_],
                         writes=[Br_])
                    P.op("dve", lambda e: e.tensor_tensor(out=sl3(gat), in0=sl3(gat), in1=sl3(mask), op=ALU.mult),
                         reads=[Br_], writes=[Br_])
                    P.op("dve", lambda e: e.tensor_reduce(out=sm[:, c0:c1, 0], in_=sl3(gat), axis=AX.X, op=ALU.add),
                         reads=[Br_], writes=[Br_])
                    P.op("dve", lambda e: e.reciprocal(out=sm[:, c0:c1, 1], in_=sm[:, c0:c1, 0]), reads=[Br_],
                         writes=[Br_])
                    P.op("dve", lambda e: e.tensor_tensor(out=sl3(gat), in0=sl3(gat),
                                                          in1=sm[:, c0:c1, 1:2].broadcast_to(L3), op=ALU.mult),
                         reads=[Br_], writes=[Br_])
                    W = nch * NE
                    P.op("pe", lambda e: e.matmul(psum[6][:, 0:W], lhsT=tri, rhs=fl(mask), start=True, stop=True),
                         reads=[Br_, Bcst], writes=[Bps[6]])
                    P.op("pe", lambda e: e.matmul(psum[7][:, 0:W], lhsT=ones, rhs=fl(mask), start=True, stop=True),
                         reads=[Br_, Bcst], writes=[Bps[7]])
                    P.op("dve", lambda e: e.tensor_copy(out=fl(tmp32), in_=psum[7][:, 0:W]),
                         reads=[Bps[7]], writes=[Br_])
                    if c0 == 0:
                        P.op("dve", lambda e: e.memset(off[:, c0, :], 0.0), writes=[Br_])
                    else:
                        P.op("dve", lambda e: e.tensor_copy(out=off[:, c0, :], in_=carry[:]),
                             reads=[B("carry")], writes=[Br_])
                    for c in range(c0 + 1, c1):
                        P.op("dve", lambda e: e.tensor_tensor(out=off[:, c, :], in0=off[:, c - 1, :],
                                                              in1=tmp32[:, c - 1, :], op=ALU.add),
                             reads=[Br_], writes=[Br_])
                    if c1 < NCH:
                        P.op("dve", lambda e: e.tensor_tensor(out=carry[:], in0=off[:, c1 - 1, :],
                                                              in1=tmp32[:, c1 - 1, :], op=ALU.add),
                             reads=[Br_], writes=[B("carry")])
                    P.op("dve", lambda e: e.tensor_tensor(out=fl(off), in0=fl(off), in1=psum[6][:, 0:W], op=ALU.add),
                         reads=[Br_, Bps[6]], writes=[Br_])
                    P.op("dve", lambda e: e.tensor_scalar(out=fl(tmp32), in0=fl(off), scalar1=float(CAP), scalar2=None,
                                                          op0=ALU.is_lt), reads=[Br_], writes=[Br_])
                    P.op("dve", lambda e: e.tensor_tensor(out=fl(gat), in0=fl(gat), in1=fl(tmp32), op=ALU.mult),
                         reads=[Br_], writes=[Br_])
                    P.op("dve", lambda e: e.tensor_tensor(out=fl(dest), in0=fl(off), in1=ecap[:, c0 * NE:c1 * NE],
                                                          op=ALU.add), reads=[Br_, Bcst], writes=[Br_])
                    P.op("dve", lambda e: e.tensor_scalar(out=fl(tmp32), in0=fl(tmp32), scalar1=-1.0e6, scalar2=1.0e6,
                                                          op0=ALU.mult, op1=ALU.add), reads=[Br_], writes=[Br_])
                    P.op("dve", lambda e: e.tensor_tensor(out=fl(dest), in0=fl(dest), in1=fl(tmp32), op=ALU.add),
                         reads=[Br_], writes=[Br_])
                    gk3 = gk[:].rearrange("p (c k) -> p c k", k=TOPK)
                    for k in range(TOPK):
                        P.op("dve", lambda e: e.tensor_tensor(out=sl3(mask), in0=sl3(logit),
                                                              in1=top8[:, c0:c1, k:k + 1].broadcast_to(L3),
                                                              op=ALU.is_equal), reads=[Br_], writes=[Br_])
                        P.op("dve", lambda e: e.tensor_tensor(out=sl3(tmp32), in0=sl3(mask), in1=sl3(dest),
                                                              op=ALU.mult), reads=[Br_], writes=[Br_])
                        P.op("dve", lambda e: e.tensor_reduce(out=dk[:, c0:c1, k], in_=sl3(tmp32), axis=AX.X,
                                                              op=ALU.add), reads=[Br_], writes=[Br_])
                        P.op("dve", lambda e: e.tensor_tensor(out=sl3(tmp32), in0=sl3(mask), in1=sl3(gat),
                                                              op=ALU.mult), reads=[Br_], writes=[Br_])
                        P.op("dve", lambda e: e.tensor_reduce(out=gk3[:, c0:c1, k], in_=sl3(tmp32), axis=AX.X,
                                                              op=ALU.add), reads=[Br_], writes=[Br_])
                    P.op("dve", lambda e: e.tensor_copy(out=dki[:, c0 * TOPK:c1 * TOPK],
                                                        in_=dk[:, c0:c1, :].rearrange("p c k -> p (c k)")),
                         reads=[Br_], writes=[Br_])
                    for c in range(c0, c1):
                        for k in range(TOPK):
                            col = c * TOPK + k
                            bx = Buf(f"xs{col}")
                            xs_bufs.append(bx)
                            P.dma("pool", lambda e: e.indirect_dma_start(
                                out=xs_d, out_offset=bass.IndirectOffsetOnAxis(ap=dki[:, col:col + 1], axis=0),
                                in_=ubig[:, c, :], in_offset=None, bounds_check=bound_reg, oob_is_err=False),
                                reads=[Bub[c], Br_], writes=[bx])

                for it in range(NCH + 2):
                    if it < NCH:
                        s1(it)
                    if 1 <= it <= NCH:
                        s2(it - 1)
                    if it >= 2:
                        s3(it - 2)
                    for gi in range(NRG - 1):
                        if it == (gi + 1) * GC + 1:
                            route_batch(gi * GC, (gi + 1) * GC, gi)
                route_batch((NRG - 1) * GC, NCH, NRG - 1)
                P.barrier()


        def moe_B(l):
            gcol, w_router, b_router, w_gu, b_gu, w_dn, b_dn, Bw, Ball = moe_common(l)
            xs_bufs = MS["xs_bufs"]
            with ExitStack() as ms:
                MC = Ctx(nc, ms)
                PW = 256
                NWF, NWG, NWD = 6, 16, 8
                wf = [MC.sb(f"wf{i}", [128, NDC, PW], F32) for i in range(NWF)]
                wg = [MC.sb(f"wg{i}", [128, NDC, PW], BF16) for i in range(NWG)]
                wd = [MC.sb(f"wd{i}", [128, NDC, PW], BF16) for i in range(NWD)]
                Bwf = [B(f"wf{i}") for i in range(NWF)]
                Bwg = [B(f"wg{i}") for i in range(NWG)]
                Bwd = [B(f"wd{i}") for i in range(NWD)]
                xT = [MC.sb(f"xT{i}", [128, NDC, CAP], BF16) for i in range(2)]
                hT = [MC.sb(f"hT{i}", [128, NDC, CAP], BF16) for i in range(2)]
                ysb = [MC.sb(f"ysb{i}", [128, 512], F32) for i in range(4)]
                t_g = [MC.sb(f"t_g{i}", [128, CAP], F32) for i in range(2)]
                t_s = [MC.sb(f"t_s{i}", [128, CAP], F32) for i in range(2)]
                t_u = [MC.sb(f"t_u{i}", [128, CAP], F32) for i in range(2)]
                BxT = [B(f"xT{i}") for i in range(2)]
                BhT = [B(f"hT{i}") for i in range(2)]
                Bys = [B(f"ysb{i}") for i in range(4)]
                Btg = [B(f"t_g{i}") for i in range(2)]
                Bts = [B(f"t_s{i}") for i in range(2)]
                Btu = [B(f"t_u{i}") for i in range(2)]
                NPC = 12
                NG = NE * NPC

                def piece_src(G):
                    e_, p = G // NPC, G % NPC
                    if p < 8:
                        q, is_up = p // 2, p % 2
                        c0 = is_up * D + q * PW
                        return w_gu[e_, :, c0:c0 + PW].rearrange("(c p) n -> p c n", p=128)
                    q = p - 8
                    return w_dn[e_, :, q * PW:(q + 1) * PW].rearrange("(c p) n -> p c n", p=128)

                def piece_dst(G):
                    e_, p = G // NPC, G % NPC
                    if p < 8:
                        i = (e_ * 8 + p) % NWG
                        return wg[i], Bwg[i]
                    i = (e_ * 4 + (p - 8)) % NWD
                    return wd[i], Bwd[i]

                def issue_dma(G):
                    if G >= NG:
                        return
                    i = G % NWF
                    P.dma("sp", lambda e: e.dma_start(out=wf[i][:], in_=piece_src(G)), writes=[Bwf[i]])

                def issue_cast(G):
                    if G >= NG:
                        return
                    i = G % NWF
                    dst, Bdst = piece_dst(G)
                    if (G % NPC) < 8:
                        P.op("act", lambda e: e.copy(out=dst[:], in_=wf[i][:]), reads=[Bwf[i]], writes=[Bdst])
                    else:
                        P.op("dve", lambda e: e.tensor_copy(out=dst[:], in_=wf[i][:]), reads=[Bwf[i]], writes=[Bdst])

                def tick(G):
                    issue_cast(G)
                    issue_dma(G + NWF)

                xr = MC.sb("xr", [128, CAP // 128, D], BF16)
                identb = MC.sb("identb3", [128, 128], BF16)
                Bxr = B("xr")
                P.op("dve", lambda e: e.tensor_copy(out=identb[:], in_=ident), reads=[Bcst], writes=[B("identb3")])

                def load_x(e_):
                    if e_ >= NE:
                        return
                    P.dma("sp", lambda e: e.dma_start(
                        out=xr[:], in_=xs_d[e_ * CAP:(e_ + 1) * CAP, :].rearrange("(c p) n -> p c n", p=128)),
                        reads=xs_bufs, writes=[Bxr])

                def transpose_x(e_):
                    if e_ >= NE:
                        return
                    i2 = e_ % 2
                    pv = psum[7][:].bitcast(BF16)
                    for sc in range(CAP // 128):
                        def tr(e):
                            r = None
                            for dc in range(NDC):
                                r = e.transpose(out=pv[:, dc * 128:(dc + 1) * 128],
                                                in_=xr[:, sc, dc * 128:(dc + 1) * 128], identity=identb[:])
                            return r
                        P.op("pe", tr, reads=[Bxr, B("identb3")], writes=[Bps[7]])
                        P.op("dve", lambda e: e.tensor_copy(out=xT[i2][:, :, sc * 128:(sc + 1) * 128],
                                                            in_=pv.rearrange("p (a b) -> p a b", a=NDC)),
                             reads=[Bps[7]], writes=[BxT[i2]])

                def compute_expert(e_):
                    i2 = e_ % 2
                    Gn = (e_ + 1) * NPC
                    transpose_x(e_ + 1)
                    load_x(e_ + 2)
                    for fcp in range(8):
                        q, j = fcp // 2, fcp % 2
                        sg = (e_ * 8 + 2 * q) % NWG
                        su = (e_ * 8 + 2 * q + 1) % NWG
                        pg, pu = (fcp % 2) * 2, (fcp % 2) * 2 + 1
                        ti = fcp % 2

                        def mm(e, slot, pb):
                            r = None
                            for dc in range(NDC):
                                r = e.matmul(psum[pb][:, 0:CAP], lhsT=wg[slot][:, dc, j * 128:(j + 1) * 128],
                                             rhs=xT[i2][:, dc, :], start=(dc == 0), stop=(dc == NDC - 1))
                            return r
                        P.op("pe", lambda e: mm(e, sg, pg), reads=[Bwg[sg], BxT[i2]], writes=[Bps[pg]])
                        P.op("pe", lambda e: mm(e, su, pu), reads=[Bwg[su], BxT[i2]], writes=[Bps[pu]])
                        bg = bgu[:, e_ * 16 + fcp:e_ * 16 + fcp + 1]
                        bu = bgu[:, e_ * 16 + 8 + fcp:e_ * 16 + 8 + fcp + 1]
                        P.op("dve", lambda e: e.tensor_scalar(out=t_g[ti][:], in0=psum[pg][:, 0:CAP], scalar1=bg,
                                                              scalar2=7.0, op0=ALU.add, op1=ALU.min),
                             reads=[Bps[pg], Bw], writes=[Btg[ti]])
                        P.op("act", lambda e: e.activation(out=t_s[ti][:], in_=t_g[ti][:], func=AF.Sigmoid,
                                                           scale=1.702),
                             reads=[Btg[ti]], writes=[Bts[ti]])
                        P.op("dve", lambda e: e.tensor_scalar(out=t_u[ti][:], in0=psum[pu][:, 0:CAP], scalar1=bu,
                                                              scalar2=7.0, op0=ALU.add, op1=ALU.min),
                             reads=[Bps[pu], Bw], writes=[Btu[ti]])
                        P.op("dve", lambda e: e.tensor_scalar(out=t_u[ti][:], in0=t_u[ti][:], scalar1=-7.0,
                                                               scalar2=1.0, op0=ALU.max, op1=ALU.add),
                             reads=[Btu[ti]], writes=[Btu[ti]])
                        P.op("dve", lambda e: e.tensor_tensor(out=t_g[ti][:], in0=t_g[ti][:], in1=t_s[ti][:],
                                                               op=ALU.mult),
                             reads=[Btg[ti], Bts[ti]], writes=[Btg[ti]])
                        P.op("dve", lambda e: e.tensor_tensor(out=hT[i2][:, fcp, :], in0=t_u[ti][:],
                                                              in1=t_g[ti][:], op=ALU.mult),
                             reads=[Btu[ti], Btg[ti]], writes=[BhT[i2]])
                        tick(Gn + fcp)
                    chunks = [(s0, min(128, CAP - s0)) for s0 in range(0, CAP, 128)]
                    step = 0
                    for half in range(2):
                        for (s0, sn) in chunks:
                            idx = step
                            pb = 4 + idx % 3
                            yi = idx % 4

                            def md(e):
                                r = None
                                for qq in range(2):
                                    sd = (e_ * 4 + half * 2 + qq) % NWD
                                    for fc in range(NDC):
                                        r = e.matmul(psum[pb][0:sn, qq * PW:(qq + 1) * PW],
                                                     lhsT=hT[i2][:, fc, s0:s0 + sn], rhs=wd[sd][:, fc, :],
                                                     start=(fc == 0), stop=(fc == NDC - 1))
                                return r
                            sds = [(e_ * 4 + half * 2 + qq) % NWD for qq in range(2)]
                            P.op("pe", md, reads=[BhT[i2]] + [Bwd[x] for x in sds], writes=[Bps[pb]])
                            P.op("act", lambda e: e.copy(out=ysb[yi][0:sn, :], in_=psum[pb][0:sn, :]),
                                 reads=[Bps[pb]], writes=[Bys[yi]])
                            r0 = e_ * CAP + s0
                            P.dma("act", lambda e: e.dma_start(
                                out=ys_d[r0:r0 + sn, half * 512:(half + 1) * 512], in_=ysb[yi][0:sn, :]),
                                reads=[Bys[yi]], writes=[Buf("ys")])
                            if step < 4:
                                tick(Gn + 8 + step)
                            step += 1
                    for st in range(step, 4):
                        tick(Gn + 8 + st)

                for G in range(NWF):
                    issue_dma(G)
                load_x(0)
                transpose_x(0)
                load_x(1)
                for G in range(NPC):
                    tick(G)
                for e_ in range(NE):
                    compute_expert(e_)
                P.barrier()

        def moe_C(l):
            gcol, w_router, b_router, w_gu, b_gu, w_dn, b_dn, Bw, Ball = moe_common(l)
            with ExitStack() as ms:
                MC = Ctx(nc, ms)
                yg = [MC.sb(f"yg{i}", [128, D], F32) for i in range(8)]
                Byg = [B(f"yg{i}") for i in range(8)]
                gT = MC.sb("gT", [NE, 128], F32)
                for i in range(8):
                    P.op("dve", lambda e: e.memset(yg[i][:], 0.0), writes=[Byg[i]])
                for c in range(NCH):
                    P.op("pe", lambda e: e.transpose(out=psum[0][0:NE, 0:128], in_=gat[:, c, :], identity=ident),
                         reads=RB + [Bcst], writes=[Bps[0]])
                    P.op("act", lambda e: e.copy(out=gT[:], in_=psum[0][0:NE, 0:128]), reads=[Bps[0]],
                         writes=[B("gT")])
                    for half in range(2):
                        P.op("pe", lambda e: e.matmul(psum[1 + half][:], lhsT=gT[:],
                                                      rhs=bdn[:, half * 512:(half + 1) * 512],
                                                      start=True, stop=True),
                             reads=[B("gT"), Bw], writes=[Bps[1 + half]])
                        P.op("dve", lambda e: e.tensor_tensor(out=h[:, c, half * 512:(half + 1) * 512],
                                                              in0=h[:, c, half * 512:(half + 1) * 512],
                                                              in1=psum[1 + half][:], op=ALU.add),
                             reads=[Bps[1 + half], Bh[c]], writes=[Bh[c]])
                    for k in range(TOPK):
                        col = c * TOPK + k
                        yi = (c % 2) * 4 + k
                        P.dma("pool", lambda e: e.indirect_dma_start(
                            out=yg[yi][:], out_offset=None, in_=ys_d,
                            in_offset=bass.IndirectOffsetOnAxis(ap=dki[:, col:col + 1], axis=0),
                            bounds_check=bound_reg, oob_is_err=False),
                            reads=list(RB), writes=[Byg[yi]])
                        P.op("dve", lambda e: e.scalar_tensor_tensor(
                            out=h[:, c, :], in0=yg[yi][:], scalar=gk[:, col:col + 1], in1=h[:, c, :],
                            op0=ALU.mult, op1=ALU.add),
                            reads=[Byg[yi], Bh[c]] + RB, writes=[Bh[c]])
                P.barrier()


        def norm_to_uT(l, uT, BuT):
            gcol = l * D
            with ExitStack() as ms:
                MC = Ctx(nc, ms)
                gam = MC.sb("gam", [128, D], F32)
                junk = MC.sb("junk", [128, D], F32)
                NU = 3
                ubf = [MC.sb(f"ubf{i}", [128, D], BF16) for i in range(NU)]
                identb = MC.sb("identb", [128, 128], BF16)
                ssq16 = MC.sb("nssq16", [128, NCH], F32)
                rs16 = MC.sb("nrs16", [128, NCH], F32)
                Bg, Bj, Bss = B("gam_m"), B("junk_m"), B("nssq16")
                P.dma("sp", lambda e: e.dma_start(out=gam[:], in_=nrm_d[:, gcol:gcol + D]), writes=[Bg])
                P.op("dve", lambda e: e.tensor_copy(out=identb[:], in_=ident), reads=[Bcst], writes=[Bg])
                for c in range(NCH):
                    P.op("act", lambda e: e.activation(out=junk[:], in_=h[:, c, :], func=AF.Square,
                                                       accum_out=ssq16[:, c:c + 1]),
                         reads=[Bh[c]], writes=[Bj, Bss])
                P.op("dve", lambda e: e.tensor_scalar(out=rs16[:], in0=ssq16[:], scalar1=1.0 / D, scalar2=RMS_EPS,
                                                      op0=ALU.mult, op1=ALU.add), reads=[Bss], writes=[Bss])
                P.op("act", lambda e: e.activation(out=rs16[:], in_=rs16[:], func=AF.Sqrt), reads=[Bss], writes=[Bss])
                P.op("dve", lambda e: e.reciprocal(out=rs16[:], in_=rs16[:]), reads=[Bss], writes=[Bss])

                def s1(c):
                    i3 = c % NU
                    P.op("dve", lambda e: e.scalar_tensor_tensor(
                        out=ubf[i3][:], in0=h[:, c, :], scalar=rs16[:, c:c + 1], in1=gam[:],
                        op0=ALU.mult, op1=ALU.mult), reads=[Bh[c], Bss, Bg], writes=[B(f"ubf{i3}")])

                def s2(c):
                    i3, pb = c % NU, c % 2
                    pv = psum[pb][:].bitcast(BF16)

                    def tr(e):
                        r = None
                        for dc in range(NDC):
                            r = e.transpose(out=pv[:, dc * 128:(dc + 1) * 128],
                                            in_=ubf[i3][:, dc * 128:(dc + 1) * 128], identity=identb[:])
                        return r
                    P.op("pe", tr, reads=[B(f"ubf{i3}"), Bg], writes=[Bps[pb]])
                    src = pv.rearrange("p (a b) -> p a b", a=NDC)
                    P.op("act", lambda e: e.copy(out=uT[:, :, c * 128:(c + 1) * 128], in_=src),
                         reads=[Bps[pb]], writes=[BuT[c // 4]])

                for it in range(NCH + 1):
                    if it < NCH:
                        s1(it)
                    if it >= 1:
                        s2(it - 1)
                P.barrier()

        def out_proj(l, yT, ByT):
            w_out = L[l, "w_out"]
            with ExitStack() as ms:
                MC = Ctx(nc, ms)
                wo = MC.sb("wo", [128, NDC, D], BF16)
                Bwo = B("wo")
                for half in range(2):
                    P.dma("pool", lambda e: e.dma_start(
                        out=wo[:, :, half * 512:(half + 1) * 512],
                        in_=w_out[:, half * 512:(half + 1) * 512].rearrange("(c p) n -> p c n", p=128)),
                        writes=[Bwo])
                for c in range(NCH):
                    for half in range(2):
                        pb = (c * 2 + half) % 8

                        def mm(e):
                            r = None
                            for g in range(NDC):
                                r = e.matmul(psum[pb][:], lhsT=yT[:, g, c * 128:(c + 1) * 128],
                                             rhs=wo[:, g, half * 512:(half + 1) * 512],
                                             start=(g == 0), stop=(g == NDC - 1))
                            return r
                        P.op("pe", mm, reads=list(ByT) + [Bwo], writes=[Bps[pb]])
                        P.op("dve", lambda e: e.tensor_tensor(out=h[:, c, half * 512:(half + 1) * 512],
                                                              in0=h[:, c, half * 512:(half + 1) * 512],
                                                              in1=psum[pb][:], op=ALU.add),
                             reads=[Bps[pb], Bh[c]], writes=[Bh[c]])
                P.barrier()

        def lru_layer(l):
            w_in, vec_d, w_a, w_x = L[l, "w_in"], L[l, "vec"], L[l, "w_a"], L[l, "w_x"]
            PAD = 4
            with ExitStack() as ls:
                LC = Ctx(nc, ls)
                yT = LC.sb("yT", [128, NDC, S], BF16)
                ByT = [B(f"yT{g}") for g in range(NDC)]
                with ExitStack() as us:
                    UC = Ctx(nc, us)
                    uT = UC.sb("uT", [128, NDC, S], BF16)
                    BuT = [B(f"uTq{i}") for i in range(4)]
                    norm_to_uT(l, uT, BuT)
                    with ExitStack() as ms:
                        MC = Ctx(nc, ms)
                        vec = MC.sb("vec", [128, 8, 8], F32)
                        sc = MC.sb("sc", [128, 8, 2], F32)
                        wa = MC.sb("wa", [128, 8, 128], BF16)
                        wx = MC.sb("wx", [128, 8, 128], BF16)
                        wig = [MC.sb(f"wig{i}", [128, NDC, 128], BF16) for i in range(2)]
                        wix = [MC.sb(f"wix{i}", [128, NDC, 128], BF16) for i in range(2)]
                        XB = MC.sb("XB", [128, PAD + S], F32)
                        XC = MC.sb("XC", [128, S], F32)
                        XCB = MC.sb("XCB", [128, S], BF16)
                        R = MC.sb("R", [128, S], F32)
                        I_ = MC.sb("I", [128, S], F32)
                        A = MC.sb("A", [128, S], F32)
                        Bv = B("lru_small")
                        BXB, BXC, BXCB, BR, BI, BA = B("XB"), B("XC"), B("XCB"), B("R"), B("I"), B("A")
                        Bwig = [B(f"wig{i}") for i in range(2)]
                        Bwix = [B(f"wix{i}") for i in range(2)]
                        P.dma("sp", lambda e: e.dma_start(out=vec[:].rearrange("p g k -> p (g k)"), in_=vec_d),
                              writes=[Bv])
                        P.dma("pool", lambda e: e.dma_start(out=wa[:], in_=w_a.rearrange("g i o -> i g o")),
                              writes=[Bv])
                        P.dma("pool", lambda e: e.dma_start(out=wx[:], in_=w_x.rearrange("g i o -> i g o")),
                              writes=[Bv])
                        P.op("act", lambda e: e.activation(out=sc[:, :, 0], in_=vec[:, :, 7], func=AF.Exp, scale=-1.0),
                             reads=[Bv], writes=[Bv])
                        P.op("act", lambda e: e.activation(out=sc[:, :, 0], in_=sc[:, :, 0], func=AF.Ln, bias=1.0,
                                                           scale=1.0), reads=[Bv], writes=[Bv])
                        P.op("dve", lambda e: e.tensor_scalar(out=sc[:, :, 1], in0=sc[:, :, 0], scalar1=-16.0,
                                                              scalar2=None, op0=ALU.mult), reads=[Bv], writes=[Bv])
                        P.op("dve", lambda e: e.tensor_scalar(out=sc[:, :, 0], in0=sc[:, :, 0], scalar1=-8.0,
                                                              scalar2=None, op0=ALU.mult), reads=[Bv], writes=[Bv])
                        P.op("dve", lambda e: e.memset(XB[:, 0:PAD], 0.0), writes=[BXB])

                        def load_w(g):
                            i2 = g % 2
                            P.dma("pool", lambda e: e.dma_start(
                                out=wig[i2][:], in_=w_in[:, g * 128:(g + 1) * 128].rearrange("(c p) n -> p c n", p=128)),
                                writes=[Bwig[i2]])
                            P.dma("pool", lambda e: e.dma_start(
                                out=wix[i2][:],
                                in_=w_in[:, D + g * 128:D + (g + 1) * 128].rearrange("(c p) n -> p c n", p=128)),
                                writes=[Bwix[i2]])

                        def proj(wt, Bwt, tq, pb):
                            def mm(e):
                                r = None
                                for dc in range(NDC):
                                    r = e.matmul(psum[pb][:], lhsT=wt[:, dc, :], rhs=uT[:, dc, tq * 512:(tq + 1) * 512],
                                                 start=(dc == 0), stop=(dc == NDC - 1))
                                return r
                            P.op("pe", mm, reads=[Bwt, BuT[tq]], writes=[Bps[pb]])

                        T_ = MC.sb("T", [128, S], F32)
                        BT = [B(f"lruT{i}") for i in range(4)]
                        load_w(0)
                        for g in range(NDC):
                            i2 = g % 2
                            if g + 1 < NDC:
                                load_w(g + 1)
                            for tq in range(4):
                                proj(wix[i2], Bwix[i2], tq, tq)
                                P.op("act", lambda e: e.copy(out=XB[:, PAD + tq * 512:PAD + (tq + 1) * 512],
                                                             in_=psum[tq][:]), reads=[Bps[tq]], writes=[BXB])
                            for tq in range(4):
                                proj(wig[i2], Bwig[i2], tq, 4 + tq)
                            P.op("dve", lambda e: e.tensor_scalar(out=XC[:], in0=XB[:, PAD:PAD + S],
                                                                  scalar1=vec[:, g, 3:4], scalar2=vec[:, g, 4:5],
                                                                  op0=ALU.mult, op1=ALU.add),
                                 reads=[BXB, Bv], writes=[BXC])
                            for tq in range(4):
                                sl = slice(tq * 512, (tq + 1) * 512)
                                P.op("act", lambda e: e.activation(out=T_[:, sl], in_=psum[4 + tq][:], func=AF.Square),
                                     reads=[Bps[4 + tq]], writes=[BT[tq]])
                            for j in range(1, 4):
                                P.op("dve", lambda e: e.scalar_tensor_tensor(
                                    out=XC[:], in0=XB[:, PAD - j:PAD - j + S], scalar=vec[:, g, 3 - j:4 - j], in1=XC[:],
                                    op0=ALU.mult, op1=ALU.add), reads=[BXB, Bv, BXC], writes=[BXC])
                            P.op("act", lambda e: e.copy(out=XCB[:], in_=XC[:]), reads=[BXC], writes=[BXCB])
                            for tq in range(4):
                                sl = slice(tq * 512, (tq + 1) * 512)
                                P.op("dve", lambda e: e.tensor_scalar(out=T_[:, sl], in0=T_[:, sl], scalar1=0.044715,
                                                                      scalar2=1.0, op0=ALU.mult, op1=ALU.add),
                                     reads=[BT[tq]], writes=[BT[tq]])
                                P.op("dve", lambda e: e.tensor_tensor(out=T_[:, sl], in0=T_[:, sl], in1=psum[4 + tq][:],
                                                                      op=ALU.mult), reads=[BT[tq], Bps[4 + tq]],
                                     writes=[BT[tq]])
                            for tq in range(4):
                                P.op("pe", lambda e: e.matmul(psum[tq][:], lhsT=wa[:, g, :],
                                                              rhs=XCB[:, tq * 512:(tq + 1) * 512], start=True, stop=True),
                                     reads=[BXCB, Bv], writes=[Bps[tq]])
                                P.op("act", lambda e: e.activation(out=R[:, tq * 512:(tq + 1) * 512], in_=psum[tq][:],
                                                                   func=AF.Sigmoid, bias=vec[:, g, 5:6], scale=1.0),
                                     reads=[Bps[tq], Bv], writes=[BR])
                            for tq in range(4):
                                P.op("pe", lambda e: e.matmul(psum[tq][:], lhsT=wx[:, g, :],
                                                              rhs=XCB[:, tq * 512:(tq + 1) * 512], start=True, stop=True),
                                     reads=[BXCB, Bv], writes=[Bps[tq]])
                                P.op("act", lambda e: e.activation(out=I_[:, tq * 512:(tq + 1) * 512],
                                                                   in_=psum[tq][:], func=AF.Sigmoid,
                                                                   bias=vec[:, g, 6:7], scale=1.0),
                                     reads=[Bps[tq], Bv], writes=[BI])
                            for tq in range(4):
                                sl = slice(tq * 512, (tq + 1) * 512)
                                P.op("act", lambda e: e.activation(out=T_[:, sl], in_=T_[:, sl], func=AF.Sigmoid,
                                                                   scale=1.5957691216057308), reads=[BT[tq]],
                                     writes=[BT[tq]])
                            P.op("act", lambda e: e.activation(out=A[:], in_=R[:], func=AF.Exp, scale=sc[:, g, 0:1]),
                                 reads=[BR, Bv], writes=[BA])
                            P.op("act", lambda e: e.activation(out=R[:], in_=R[:], func=AF.Exp, scale=sc[:, g, 1:2]),
                                 reads=[BR, Bv], writes=[BR])
                            for tq in range(4):
                                sl = slice(tq * 512, (tq + 1) * 512)
                                P.op("dve", lambda e: e.tensor_tensor(out=T_[:, sl], in0=T_[:, sl], in1=psum[4 + tq][:],
                                                                      op=ALU.mult), reads=[BT[tq], Bps[4 + tq]],
                                     writes=[BT[tq]])
                            P.op("dve", lambda e: e.tensor_tensor(out=I_[:], in0=I_[:], in1=XC[:], op=ALU.mult),
                                 reads=[BI, BXC], writes=[BI])
                            P.op("dve", lambda e: e.tensor_scalar(out=R[:], in0=R[:], scalar1=-1.0, scalar2=1.0,
                                                                  op0=ALU.mult, op1=ALU.add), reads=[BR], writes=[BR])
                            P.op("act", lambda e: e.activation(out=R[:], in_=R[:], func=AF.Sqrt), reads=[BR],
                                 writes=[BR])
                            P.op("dve", lambda e: e.tensor_tensor(out=I_[:], in0=I_[:], in1=R[:], op=ALU.mult),
                                 reads=[BI, BR], writes=[BI])
                            Y = XB[:, PAD:PAD + S]
                            P.op("dve", lambda e: e.tensor_tensor_scan(out=Y, data0=A[:], data1=I_[:], initial=0.0,
                                                                       op0=ALU.mult, op1=ALU.add),
                                 reads=[BA, BI], writes=[BXB])
                            for tq in range(4):
                                sl = slice(tq * 512, (tq + 1) * 512)
                                P.op("dve", lambda e: e.tensor_tensor(out=yT[:, g, sl], in0=T_[:, sl], in1=Y[:, sl],
                                                                      op=ALU.mult), reads=[BT[tq], BXB], writes=[ByT[g]])
                        P.barrier()
                out_proj(l, yT, ByT)

        def pool_layer(l):
            w_in, w_grp, vec_d = L[l, "w_in"], L[l, "w_grp"], L[l, "vec"]
            PAD = 16
            WINS = (2, 4, 8, 16)
            with ExitStack() as ls:
                LC = Ctx(nc, ls)
                yT = LC.sb("yT", [128, NDC, S], BF16)
                ByT = [B(f"yT{g}") for g in range(NDC)]
                with ExitStack() as us:
                    UC = Ctx(nc, us)
                    uT = UC.sb("uT", [128, NDC, S], BF16)
                    BuT = [B(f"uTq{i}") for i in range(4)]
                    norm_to_uT(l, uT, BuT)
                    with ExitStack() as ms:
                        MC = Ctx(nc, ms)
                        vec = MC.sb("pvec", [128, 8 + 64], F32)
                        wgr = MC.sb("wgr", [128, 4, 2, 256], BF16)
                        wi = [MC.sb(f"wi{i}", [128, NDC, 128], BF16) for i in range(2)]
                        V_ = [MC.sb(f"V{i}", [128, PAD + S], F32) for i in range(2)]
                        S1 = [MC.sb(f"S1{i}", [128, PAD + S], F32) for i in range(2)]
                        S2 = [MC.sb(f"S2{i}", [128, PAD + S], F32) for i in range(2)]
                        PT = [MC.sb(f"PT{i}", [128, S], BF16) for i in range(2)]
                        Bv = B("pool_small")
                        Bwi = [B(f"pwi{i}") for i in range(2)]
                        BV = [B(f"pV{i}") for i in range(2)]
                        BS1 = [B(f"pS1{i}") for i in range(2)]
                        BS2 = [B(f"pS2{i}") for i in range(2)]
                        BPT = [B(f"pPT{i}") for i in range(2)]
                        P.dma("sp", lambda e: e.dma_start(out=vec[:], in_=vec_d), writes=[Bv])
                        P.dma("pool", lambda e: e.dma_start(
                            out=wgr[:].rearrange("p a b o -> p (a b) o"),
                            in_=w_grp.rearrange("a (b p) o -> p (a b) o", p=128)), writes=[Bv])
                        for i in range(2):
                            P.op("dve", lambda e: e.memset(V_[i][:, 0:PAD], 0.0), writes=[BV[i]])
                            P.op("dve", lambda e: e.memset(S1[i][:, 0:PAD], 0.0), writes=[BS1[i]])
                            P.op("dve", lambda e: e.memset(S2[i][:, 0:PAD], 0.0), writes=[BS2[i]])

                        def load_w(g):
                            i2 = g % 2
                            P.dma("pool", lambda e: e.dma_start(
                                out=wi[i2][:], in_=w_in[:, g * 128:(g + 1) * 128].rearrange("(c p) n -> p c n", p=128)),
                                writes=[Bwi[i2]])

                        load_w(0)
                        for g in range(NDC):
                            i2 = g % 2
                            grp = g // 2
                            win = WINS[grp]
                            if g + 1 < NDC:
                                load_w(g + 1)
                            for tq in range(4):
                                pb = (g % 2) * 4 + tq

                                def mm(e):
                                    r = None
                                    for dc in range(NDC):
                                        r = e.matmul(psum[pb][:], lhsT=wi[i2][:, dc, :],
                                                     rhs=uT[:, dc, tq * 512:(tq + 1) * 512],
                                                     start=(dc == 0), stop=(dc == NDC - 1))
                                    return r
                                P.op("pe", mm, reads=[Bwi[i2], BuT[tq]], writes=[Bps[pb]])
                                P.op("act", lambda e: e.copy(out=V_[i2][:, PAD + tq * 512:PAD + (tq + 1) * 512],
                                                             in_=psum[pb][:]), reads=[Bps[pb]], writes=[BV[i2]])
                            cur, Bcur = V_[i2], BV[i2]
                            w = 1
                            nxt = [(S1[i2], BS1[i2]), (S2[i2], BS2[i2])]
                            k = 0
                            while w < win:
                                dst, Bdst = nxt[k % 2]
                                P.op("dve", lambda e: e.tensor_tensor(out=dst[:, PAD:PAD + S], in0=cur[:, PAD:PAD + S],
                                                                      in1=cur[:, PAD - w:PAD - w + S], op=ALU.add),
                                     reads=[Bcur], writes=[Bdst])
                                cur, Bcur = dst, Bdst
                                w *= 2
                                k += 1
                            P.op("dve", lambda e: e.scalar_tensor_tensor(
                                out=PT[i2][:], in0=cur[:, PAD:PAD + S], scalar=1.0 / win, in1=V_[i2][:, PAD:PAD + S],
                                op0=ALU.mult, op1=ALU.subtract), reads=[Bcur, BV[i2]], writes=[BPT[i2]])
                            P.op("dve", lambda e: e.tensor_tensor(out=cur[:, PAD:PAD + 16], in0=cur[:, PAD:PAD + 16],
                                                                  in1=vec[:, 8 + grp * 16:8 + (grp + 1) * 16],
                                                                  op=ALU.mult), reads=[Bcur, Bv], writes=[Bcur])
                            P.op("dve", lambda e: e.tensor_tensor(out=PT[i2][:, 0:16], in0=cur[:, PAD:PAD + 16],
                                                                  in1=V_[i2][:, PAD:PAD + 16], op=ALU.subtract),
                                 reads=[Bcur, BV[i2]], writes=[BPT[i2]])
                            if g % 2 == 1:
                                for oc in range(2):
                                    go = grp * 2 + oc
                                    for tq in range(4):
                                        pb = oc * 4 + tq

                                        def mg(e):
                                            r = None
                                            for ic in range(2):
                                                r = e.matmul(psum[pb][:], lhsT=wgr[:, grp, ic, oc * 128:(oc + 1) * 128],
                                                             rhs=PT[ic][:, tq * 512:(tq + 1) * 512],
                                                             start=(ic == 0), stop=(ic == 1))
                                            return r
                                        P.op("pe", mg, reads=[BPT[0], BPT[1], Bv], writes=[Bps[pb]])
                                        P.op("act", lambda e: e.activation(
                                            out=yT[:, go, tq * 512:(tq + 1) * 512], in_=psum[pb][:], func=AF.Copy,
                                            scale=vec[:, go:go + 1]), reads=[Bps[pb], Bv], writes=[ByT[go]])
                        P.barrier()
                out_proj(l, yT, ByT)


        def sb_layer(l):
            w_qkv, negm_d = L[l, "w_qkv"], L[l, "negm"]
            NH = 16
            with ExitStack() as ls:
                LC = Ctx(nc, ls)
                QT = LC.sb("QT", [128, NDC, S], BF16)
                KT = LC.sb("KT", [128, NDC, S], BF16)
                BQ = [[B(f"QT{hd}_{qg}") for qg in range(4)] for hd in range(NH)]
                BK = [B(f"KT{g}") for g in range(NDC)]
                Bvd = [B(f"vst{c}") for c in range(NCH)]
                with ExitStack() as us:
                    UC = Ctx(nc, us)
                    uT = UC.sb("uT", [128, NDC, S], BF16)
                    BuT = [B(f"uTq{i}") for i in range(4)]
                    norm_to_uT(l, uT, BuT)
                    with ExitStack() as ms:
                        MC = Ctx(nc, ms)
                        wq = [MC.sb(f"wq{i}", [128, NDC, 128], BF16) for i in range(2)]
                        wv = MC.sb("wv", [128, NDC, 512], BF16)
                        vst = [MC.sb(f"vst{i}", [128, 512], BF16) for i in range(2)]
                        Bwq = [B(f"wq{i}") for i in range(2)]
                        Bwv = B("wv")
                        Bvs = [B(f"vstage{i}") for i in range(2)]

                        def load_wq(j):
                            i2 = j % 2
                            P.dma("pool", lambda e: e.dma_start(
                                out=wq[i2][:], in_=w_qkv[:, j * 128:(j + 1) * 128].rearrange("(c p) n -> p c n", p=128)),
                                writes=[Bwq[i2]])
                        load_wq(0)
                        for j in range(16):
                            i2 = j % 2
                            if j + 1 < 16:
                                load_wq(j + 1)
                            g = j % 8
                            for tq in range(4):
                                pb = (j % 2) * 4 + tq

                                def mm(e):
                                    r = None
                                    for dc in range(NDC):
                                        r = e.matmul(psum[pb][:], lhsT=wq[i2][:, dc, :],
                                                     rhs=uT[:, dc, tq * 512:(tq + 1) * 512],
                                                     start=(dc == 0), stop=(dc == NDC - 1))
                                    return r
                                P.op("pe", mm, reads=[Bwq[i2], BuT[tq]], writes=[Bps[pb]])
                                if j < 8:
                                    P.op("act", lambda e: e.activation(out=QT[:, g, tq * 512:(tq + 1) * 512],
                                                                       in_=psum[pb][:], func=AF.Copy, scale=0.125),
                                         reads=[Bps[pb]], writes=[BQ[2 * g][tq], BQ[2 * g + 1][tq]])
                                else:
                                    P.op("dve", lambda e: e.tensor_copy(out=KT[:, g, tq * 512:(tq + 1) * 512],
                                                                        in_=psum[pb][:]),
                                         reads=[Bps[pb]], writes=[BK[g]])
                        for half in range(2):
                            P.dma("pool", lambda e: e.dma_start(
                                out=wv[:], in_=w_qkv[:, 2 * D + half * 512:2 * D + (half + 1) * 512].rearrange(
                                    "(c p) n -> p c n", p=128)), writes=[Bwv])
                            for c in range(NCH):
                                pb = c % 8
                                i2 = c % 2

                                def mv(e):
                                    r = None
                                    for dc in range(NDC):
                                        r = e.matmul(psum[pb][:], lhsT=uT[:, dc, c * 128:(c + 1) * 128],
                                                     rhs=wv[:, dc, :], start=(dc == 0), stop=(dc == NDC - 1))
                                    return r
                                P.op("pe", mv, reads=[Bwv, BuT[c // 4]], writes=[Bps[pb]])
                                if c % 2 == 0:
                                    P.op("act", lambda e: e.copy(out=vst[i2][:], in_=psum[pb][:]),
                                         reads=[Bps[pb]], writes=[Bvs[i2]])
                                else:
                                    P.op("dve", lambda e: e.tensor_copy(out=vst[i2][:], in_=psum[pb][:]),
                                         reads=[Bps[pb]], writes=[Bvs[i2]])
                                P.dma("sp", lambda e: e.dma_start(
                                    out=ust_d[c * 128:(c + 1) * 128, half * 512:(half + 1) * 512], in_=vst[i2][:]),
                                    reads=[Bvs[i2]], writes=[Bvd[c]])
                        P.barrier()
                with ExitStack() as ms:
                    MC = Ctx(nc, ms)
                    Vt = MC.sb("Vt", [128, NCH, D], BF16)
                    BV = B("Vt")
                    for c in range(NCH):
                        P.dma("sp", lambda e: e.dma_start(out=Vt[:, c, :], in_=ust_d[c * 128:(c + 1) * 128, :]),
                              reads=[Bvd[c]], writes=[BV])
                    negm = MC.sb("negm", [128, 4, 512], BF16)
                    ntri = MC.sb("ntri", [128, 128], BF16)
                    nones = MC.sb("nones", [128, 128], BF16)
                    identb = MC.sb("identb2", [128, 128], BF16)
                    Bc2 = B("sb_consts")
                    P.dma("pool", lambda e: e.dma_start(out=negm[:].rearrange("p a b -> p (a b)"), in_=negm_d),
                          writes=[Bc2])
                    P.op("dve", lambda e: e.tensor_scalar(out=ntri[:], in0=cst[:, 384:512], scalar1=-1.0, scalar2=None,
                                                          op0=ALU.mult), reads=[Bcst], writes=[Bc2])
                    P.op("dve", lambda e: e.tensor_scalar(out=nones[:], in0=ones, scalar1=-1.0, scalar2=None,
                                                          op0=ALU.mult), reads=[Bcst], writes=[Bc2])
                    P.op("dve", lambda e: e.tensor_copy(out=identb[:], in_=ident), reads=[Bcst], writes=[Bc2])
                    NR = 3
                    Lb = [MC.sb(f"Lb{i}", [128, 1024], BF16) for i in range(NR)]
                    AT = [MC.sb(f"AT{i}", [128, 1024], BF16) for i in range(NR)]
                    Et = [MC.sb(f"Et{i}", [128, 1024], F32) for i in range(2)]
                    Lacc = MC.sb("Lacc", [128, 512], F32)
                    Laccb = [MC.sb(f"Laccb{i}", [128, 512], BF16) for i in range(2)]
                    BLb = [B(f"Lb{i}") for i in range(NR)]
                    BAT = [B(f"AT{i}") for i in range(NR)]
                    BEt = [B(f"Et{i}") for i in range(2)]
                    BLacc = B("Lacc")
                    BLaccb = [B(f"Laccb{i}") for i in range(2)]
                    pairs = []
                    for hd in range(NH):
                        for qg in range(4):
                            nkb = 4 * qg + 4
                            for kb in range(nkb - 1, 0, -2):
                                pairs.append(dict(hd=hd, qg=qg, kb=kb, first=(kb == nkb - 1), last=(kb == 1),
                                                  grp=hd * 4 + qg))
                    nacc = [0]

                    def xpair(i):
                        x0 = (i % 3) * 2
                        return x0, psum_all[:, x0 * 512:(x0 + 2) * 512]

                    def stage_a(i, b):
                        hd, qg, kb = b["hd"], b["qg"], b["kb"]
                        g, pbase = hd // 2, (hd % 2) * 64
                        x0, xp = xpair(i)

                        def mz(e):
                            r = None
                            for t in range(2):
                                k_ = kb - t
                                r = e.matmul(psum[x0 + t][:], lhsT=KT[pbase:pbase + 64, g, k_ * 128:(k_ + 1) * 128],
                                             rhs=QT[pbase:pbase + 64, g, qg * 512:(qg + 1) * 512], start=True,
                                             stop=True, skip_group_check=True)
                                if k_ >= 4 * qg:
                                    r = e.matmul(psum[x0 + t][:], lhsT=identb[:], rhs=negm[:, k_ - 4 * qg, :],
                                                 start=False, stop=True, skip_group_check=True)
                            return r
                        P.op("pe", mz, reads=[BK[g], BQ[hd][qg], Bc2], writes=[Bps[x0], Bps[x0 + 1]])
                        P.op("act", lambda e: e.activation(out=Et[i % 2][:], in_=xp, func=AF.Exp),
                             reads=[Bps[x0], Bps[x0 + 1]], writes=[BEt[i % 2]])
                        P.op("act", lambda e: e.activation(out=Lb[i % NR][:], in_=Et[i % 2][:], func=AF.Ln,
                                                           bias=1.0, scale=1.0),
                             reads=[BEt[i % 2]], writes=[BLb[i % NR]])

                    def stage_b(i, b):
                        hd, qg, kb = b["hd"], b["qg"], b["kb"]
                        x0, xp = xpair(i)
                        first, last = b["first"], b["last"]
                        la = nacc[0] % 2
                        L0, L1 = Lb[i % NR][:, 0:512], Lb[i % NR][:, 512:1024]

                        def m2(e):
                            r = e.matmul(psum[x0][:], lhsT=ntri[:], rhs=L0, start=False, stop=True,
                                         skip_group_check=True)
                            if not first:
                                r = e.matmul(psum[x0][:], lhsT=nones[:], rhs=Laccb[la][:], start=False, stop=True,
                                             skip_group_check=True)
                            r = e.matmul(psum[x0 + 1][:], lhsT=ntri[:], rhs=L1, start=False, stop=True,
                                         skip_group_check=True)
                            r = e.matmul(psum[x0 + 1][:], lhsT=nones[:], rhs=L0, start=False, stop=True,
                                         skip_group_check=True)
                            if not first:
                                r = e.matmul(psum[x0 + 1][:], lhsT=nones[:], rhs=Laccb[la][:], start=False, stop=True,
                                             skip_group_check=True)
                            return r
                        P.op("pe", m2, reads=[BLb[i % NR], Bc2] + ([] if first else [BLaccb[la]]),
                             writes=[Bps[x0], Bps[x0 + 1]])
                        P.op("act", lambda e: e.activation(out=AT[i % NR][:], in_=xp, func=AF.Exp),
                             reads=[Bps[x0], Bps[x0 + 1]], writes=[BAT[i % NR]])
                        if not last:
                            if first:
                                P.op("dve", lambda e: e.tensor_tensor(out=Lacc[:], in0=L0, in1=L1, op=ALU.add),
                                     reads=[BLb[i % NR]], writes=[BLacc])
                            else:
                                P.op("dve", lambda e: e.tensor_tensor(out=Lacc[:], in0=Lacc[:], in1=L0, op=ALU.add),
                                     reads=[BLb[i % NR], BLacc], writes=[BLacc])
                                P.op("dve", lambda e: e.tensor_tensor(out=Lacc[:], in0=Lacc[:], in1=L1, op=ALU.add),
                                     reads=[BLb[i % NR], BLacc], writes=[BLacc])
                            nacc[0] += 1
                            lb2 = nacc[0] % 2
                            P.op("dve", lambda e: e.tensor_copy(out=Laccb[lb2][:], in_=Lacc[:]),
                                 reads=[BLacc], writes=[BLaccb[lb2]])

                    def stage_c(i, b):
                        hd, qg, kb = b["hd"], b["qg"], b["kb"]
                        g, pbase = hd // 2, (hd % 2) * 64
                        ob = 6 + b["grp"] % 2
                        first, last = b["first"], b["last"]

                        def mo(e):
                            r = e.matmul(psum[ob][:], lhsT=Vt[:, kb, g * 128:(g + 1) * 128],
                                         rhs=AT[i % NR][:, 0:512], start=first, stop=False, skip_group_check=True)
                            r = e.matmul(psum[ob][:], lhsT=Vt[:, kb - 1, g * 128:(g + 1) * 128],
                                         rhs=AT[i % NR][:, 512:1024], start=False, stop=last, skip_group_check=True)
                            return r
                        P.op("pe", mo, reads=[BV, BAT[i % NR]], writes=[Bps[ob]])
                        if last:
                            P.op("act", lambda e: e.copy(out=QT[pbase:pbase + 64, g, qg * 512:(qg + 1) * 512],
                                                         in_=psum[ob][pbase:pbase + 64, :]),
                                 reads=[Bps[ob]], writes=[BQ[hd][qg]])

                    nb = len(pairs)
                    for i in range(nb + 2):
                        if i < nb:
                            stage_a(i, pairs[i])
                        if 1 <= i <= nb:
                            stage_b(i - 1, pairs[i - 1])
                        if i >= 2:
                            stage_c(i - 2, pairs[i - 2])
                    P.barrier()
                out_proj(l, QT, [BQ[hd][qg] for hd in range(NH) for qg in range(4)])


        for l in layers:
            if cfg["mix"][l]:
                [lru_layer, pool_layer, sb_layer][l % 3](l)
            if cfg["moe"][l]:
                for c in range(NCH):
                    P.dma("sp", lambda e: e.dma_start(out=hsp_d[c * 128:(c + 1) * 128, :], in_=h[:, c, :]),
                          reads=[Bh[c]], writes=[Bhsp[c]])
                moe_A(l)
                hst["stack"].close()
                moe_B(l)
                hst["stack"] = ExitStack()
                hst["n"] += 1
                h = hst["stack"].enter_context(nc.sbuf_tensor(f"h_res{hst['n']}", [128, NCH, D], F32))
                for c in range(NCH):
                    P.dma("sp", lambda e: e.dma_start(out=h[:, c, :], in_=hsp_d[c * 128:(c + 1) * 128, :]),
                          reads=[Bhsp[c]], writes=[Bh[c]])
                moe_C(l)

        if cfg.get("final", False):
            fcol = 8 * D
            with ExitStack() as ms:
                MC = Ctx(nc, ms)
                gam = MC.sb("gamf", [128, D], F32)
                junk = MC.sb("junkf", [128, D], F32)
                P.dma("sp", lambda e: e.dma_start(out=gam[:], in_=nrm_d[:, fcol:fcol + D]), writes=[B("gamf")])
                for c in range(NCH):
                    par = c % 2
                    Br = rms_rstd(c, par, junk, B("junkf"))
                    P.op("dve", lambda e: e.scalar_tensor_tensor(
                        out=h[:, c, :], in0=h[:, c, :], scalar=rstd[:, par:par + 1],
                        in1=gam[:], op0=ALU.mult, op1=ALU.mult),
                        reads=[Bh[c], Br, B("gamf")], writes=[Bh[c]])
                P.barrier()
        Bout = B("out_d")
        for c in range(NCH):
            P.dma("sp", lambda e: e.dma_start(out=out_d[c * 128:(c + 1) * 128, :], in_=h[:, c, :]),
                  reads=[Bh[c]], writes=[Bout])
        P.barrier()
        hst["stack"].close()
        print("instructions:", P.n_instr, {k: v for k, v in P.ecnt.items()})
    return nc


def host_consts():
    ident = np.eye(128, dtype=np.float32)
    tri = np.triu(np.ones((128, 128), np.float32), 1)
    ones = np.ones((128, 128), np.float32)
    lowi = np.tril(np.ones((128, 128), np.float32))
    cst = np.concatenate([ident, tri, ones, lowi], axis=1)
    ecap = np.tile((np.arange(NE, dtype=np.float32) * CAP)[None, None, :], (128, NCH, 1)).reshape(128, NCH * NE)
    return np.ascontiguousarray(cst), np.ascontiguousarray(ecap)


def layer_inputs(inputs, l):
    m = {}
    m[f"w_router{l}"] = np.ascontiguousarray(inputs["moe_w_router"][l])
    m[f"b_router{l}"] = np.ascontiguousarray(np.broadcast_to(inputs["moe_b_router"][l][None, :], (128, NE)))
    m[f"w_gu{l}"] = np.ascontiguousarray(inputs["moe_w_gate_up"][l])
    bgu = inputs["moe_b_gate_up"][l].reshape(NE, 16, 128).transpose(2, 0, 1).reshape(128, NE * 16)
    m[f"b_gu{l}"] = np.ascontiguousarray(bgu)
    m[f"w_dn{l}"] = np.ascontiguousarray(inputs["moe_w_down"][l])
    m[f"b_dn{l}"] = np.ascontiguousarray(inputs["moe_b_down"][l])
    return m


def mixer_inputs(inputs, l):
    kind, slot = l % 3, l // 3
    m = {}
    if kind == 0:
        m[f"lru_w_in{l}"] = np.ascontiguousarray(inputs["lru_w_in"][slot])
        rows = [inputs["lru_conv_w"][slot][k] for k in range(4)] + [
            inputs["lru_conv_b"][slot], inputs["lru_b_a"][slot], inputs["lru_b_x"][slot], inputs["lru_a_param"][slot]]
        v = np.stack(rows, axis=-1).reshape(8, 128, 8).transpose(1, 0, 2).reshape(128, 64)
        m[f"lru_vec{l}"] = np.ascontiguousarray(v.astype(np.float32))
        m[f"lru_w_a{l}"] = np.ascontiguousarray(inputs["lru_w_a"][slot])
        m[f"lru_w_x{l}"] = np.ascontiguousarray(inputs["lru_w_x"][slot])
        m[f"mix_w_out{l}"] = np.ascontiguousarray(inputs["lru_w_out"][slot])
    elif kind == 1:
        m[f"pool_w_in{l}"] = np.ascontiguousarray(inputs["pool_w_in"][slot])
        m[f"pool_w_grp{l}"] = np.ascontiguousarray(inputs["pool_w_group"][slot])
        sc = inputs["pool_scale"][slot].reshape(8, 128).T
        corr = np.ones((4, 16), np.float32)
        for gi, w in enumerate((2, 4, 8, 16)):
            t = np.arange(16)
            corr[gi] = 1.0 / np.minimum(t + 1, w)
        v = np.concatenate([sc, np.broadcast_to(corr.reshape(1, 64), (128, 64))], axis=1)
        m[f"pool_vec{l}"] = np.ascontiguousarray(v.astype(np.float32))
        m[f"mix_w_out{l}"] = np.ascontiguousarray(inputs["pool_w_out"][slot])
    else:
        m[f"sb_w_qkv{l}"] = np.ascontiguousarray(inputs["sb_w_qkv"][slot])
        m[f"mix_w_out{l}"] = np.ascontiguousarray(inputs["sb_w_out"][slot])
        nm = np.zeros((128, 4, 512), np.float32)
        sl = np.arange(128)[:, None]
        tl = np.arange(512)[None, :]
        for mi in range(4):
            nm[:, mi, :] = np.where(mi * 128 + sl < tl, 0.0, -30000.0)
        m[f"sb_negmask{l}"] = np.ascontiguousarray(nm.reshape(128, 2048))
    return m


def norms_input(inputs):
    rows = [inputs["mix_norm"][i] for i in range(4)] + [inputs["ffn_norm"][i] for i in range(4)] + [inputs["final_norm"]]
    v = np.concatenate(rows).astype(np.float32)
    return np.ascontiguousarray(np.broadcast_to(v[None, :], (128, 9 * D)))


N_CORES = 8


def kernel(**inputs):
    inputs = {k: np.asarray(v) for k, v in inputs.items()}
    cfg = dict(layers=[0, 1, 2, 3], moe=[True] * 4, mix=[True] * 4, final=True)
    nc = bass.Bass("TRN2", target_bir_lowering=False)
    build(nc, cfg)
    cst, ecap = host_consts()
    shared = {"cst": cst, "ecap": ecap, "norms": norms_input(inputs)}
    for l in range(4):
        shared.update(layer_inputs(inputs, l))
        shared.update(mixer_inputs(inputs, l))
    x = np.ascontiguousarray(inputs["x"].astype(np.float32))
    in_maps = []
    for c in range(N_CORES):
        m = dict(shared)
        m["x"] = x[c]
        in_maps.append(m)
    res = run_bass_kernel_spmd(nc, in_maps, core_ids=list(range(N_CORES)))
    out = np.stack([np.asarray(r["out"]) for r in res.results], axis=0)
    return out.astype(np.float32)
```

```python
import numpy as np
from contextlib import ExitStack
import concourse.bass as bass
import concourse.mybir as mybir
from concourse.bass_utils import run_bass_kernel_spmd

F32 = mybir.dt.float32
BF16 = mybir.dt.bfloat16
I32 = mybir.dt.int32
U32 = mybir.dt.uint32
AF = mybir.ActivationFunctionType
ALU = mybir.AluOpType
AX = mybir.AxisListType

D = 1024
S = 2048
NCH = S // 128
NDC = D // 128
NE = 32
TOPK = 4
CAP = 384
NSLOT = NE * CAP
RMS_EPS = 1e-6
SAME_ENG_SYNC = True


class Buf:
    __slots__ = ("name", "w", "r")

    def __init__(self, name):
        self.name = name
        self.w = None
        self.r = {}


class Prog:
    def __init__(self, nc, stack, n_dsem=(20, 8, 20)):
        self.nc = nc
        self.eng = {"pe": nc.tensor, "dve": nc.vector, "act": nc.scalar,
                    "pool": nc.gpsimd, "sp": nc.sync}
        self.esem = {k: stack.enter_context(nc.semaphore("es_" + k)) for k in self.eng}
        self.ecnt = {k: 0 for k in self.eng}
        self.waited = {k: {} for k in self.eng}
        self.dq = {}
        for q, n in zip(("sp", "act", "pool"), n_dsem):
            sems = [stack.enter_context(nc.semaphore(f"ds_{q}{i}")) for i in range(n)]
            self.dq[q] = {"sems": sems, "cnt": [0] * n, "next": 0}
        self.n_instr = 0

    def _wait(self, e, evs):
        for ev in evs:
            if ev is None:
                continue
            key, sem, val = ev
            if not SAME_ENG_SYNC and key == e:
                continue
            if key == e and e == "pe":
                continue
            if self.waited[e].get(key, 0) >= val:
                continue
            self.eng[e].wait_ge(sem, val)
            self.waited[e][key] = val

    def _deps(self, reads, writes):
        evs = []
        for b in reads:
            evs.append(b.w)
        for b in writes:
            evs.append(b.w)
            evs.extend(b.r.values())
        return evs

    def _commit(self, ev, reads, writes):
        for b in reads:
            old = b.r.get(ev[0])
            if old is None or old[2] < ev[2]:
                b.r[ev[0]] = ev
        for b in writes:
            b.w = ev
            b.r = {}

    def op(self, e, fn, reads=(), writes=()):
        self._wait(e, self._deps(reads, writes))
        ins = fn(self.eng[e])
        self.ecnt[e] += 1
        ins.then_inc(self.esem[e], 1)
        ev = (e, self.esem[e], self.ecnt[e])
        self._commit(ev, reads, writes)
        self.n_instr += 1
        return ev

    def dma(self, q, fn, reads=(), writes=()):
        d = self.dq[q]
        i = d["next"]
        d["next"] = (i + 1) % len(d["sems"])
        key = f"d_{q}{i}"
        evs = self._deps(reads, writes)
        if d["cnt"][i] > 0:
            evs.append((key, d["sems"][i], d["cnt"][i]))
        self._wait(q, evs)
        ins = fn(self.eng[q])
        d["cnt"][i] += 16
        ins.then_inc(d["sems"][i], 16)
        ev = (key, d["sems"][i], d["cnt"][i])
        self._commit(ev, reads, writes)
        self.n_instr += 1
        return ev

    def barrier(self):
        evs = [(k, self.esem[k], self.ecnt[k]) for k in self.eng if self.ecnt[k] > 0]
        for q, d in self.dq.items():
            for i, sm_ in enumerate(d["sems"]):
                if d["cnt"][i] > 0:
                    evs.append((f"d_{q}{i}", sm_, d["cnt"][i]))
        for e in self.eng:
            self._wait(e, [ev for ev in evs if ev[0] != e])

    def wait_all(self, e, bufs):
        evs = []
        for b in bufs:
            evs.append(b.w)
            evs.extend(b.r.values())
        self._wait(e, evs)


class Ctx:
    def __init__(self, nc, stack):
        self.nc = nc
        self.stack = stack
        self.bufs = {}

    _uid = [0]

    def sb(self, name, shape, dt):
        Ctx._uid[0] += 1
        t = self.stack.enter_context(self.nc.sbuf_tensor(f"sb_{name}_{Ctx._uid[0]}", list(shape), dt))
        return t

    def ps(self, name, shape, dt=F32):
        Ctx._uid[0] += 1
        t = self.stack.enter_context(self.nc.psum_tensor(f"pp_{name}_{Ctx._uid[0]}", list(shape), dt))
        return t

    def B(self, name):
        if name not in self.bufs:
            self.bufs[name] = Buf(name)
        return self.bufs[name]


def build(nc, cfg):
    layers = cfg["layers"]
    stack = ExitStack()
    with stack:
        P = Prog(nc, stack)
        C = Ctx(nc, stack)
        B = C.B

        def din(name, shape, dt=F32):
            return nc.dram_tensor(name, list(shape), dt, kind="ExternalInput").ap()

        x_d = din("x", [S, D])
        out_d = nc.dram_tensor("out", [S, D], F32, kind="ExternalOutput").ap()
        cst_d = din("cst", [128, 4 * 128])
        ecap_d = din("ecap", [128, NCH * NE])
        slotc_d = din("slotc", [128, 2 * NE * 3])
        nrm_d = din("norms", [128, 9 * D])
        L = {}
        for l in layers:
            if cfg["moe"][l]:
                L[l, "w_router"] = din(f"w_router{l}", [D, NE])
                L[l, "b_router"] = din(f"b_router{l}", [128, NE])
                L[l, "w_gu"] = din(f"w_gu{l}", [NE, D, 2 * D])
                L[l, "b_gu"] = din(f"b_gu{l}", [128, NE * 16])
                L[l, "w_dn"] = din(f"w_dn{l}", [NE, D, D])
                L[l, "b_dn"] = din(f"b_dn{l}", [NE, D])

        for l in layers:
            if cfg["mix"][l]:
                kind = l % 3
                if kind == 0:
                    L[l, "w_in"] = din(f"lru_w_in{l}", [D, 2 * D])
                    L[l, "vec"] = din(f"lru_vec{l}", [128, 8 * 8])
                    L[l, "w_a"] = din(f"lru_w_a{l}", [8, 128, 128])
                    L[l, "w_x"] = din(f"lru_w_x{l}", [8, 128, 128])
                    L[l, "w_out"] = din(f"mix_w_out{l}", [D, D])
                elif kind == 1:
                    L[l, "w_in"] = din(f"pool_w_in{l}", [D, D])
                    L[l, "w_grp"] = din(f"pool_w_grp{l}", [4, 256, 256])
                    L[l, "vec"] = din(f"pool_vec{l}", [128, 8 + 64])
                    L[l, "w_out"] = din(f"mix_w_out{l}", [D, D])
                else:
                    L[l, "w_qkv"] = din(f"sb_w_qkv{l}", [D, 3 * D])
                    L[l, "w_out"] = din(f"mix_w_out{l}", [D, D])
                    L[l, "negm"] = din(f"sb_negmask{l}", [128, 4 * 512])
        dbg = cfg.get("debug", False)
        skind = "ExternalOutput" if dbg else "Internal"
        xs_d = nc.dram_tensor("xs_scr", [NSLOT, D], BF16, kind=skind).ap()
        ys_d = nc.dram_tensor("ys_scr", [NSLOT, D], F32, kind=skind).ap()
        ust_d = nc.dram_tensor("ust_scr", [S, D], BF16, kind=skind).ap()
        if dbg:
            dbg_d = nc.dram_tensor("dbg", [128, 8 * 512], F32, kind="ExternalOutput").ap()
            dbgx_d = nc.dram_tensor("dbgx", [128, NDC * CAP], BF16, kind="ExternalOutput").ap()
            dbgw_d = nc.dram_tensor("dbgw", [128, NDC * 512], BF16, kind="ExternalOutput").ap()
            dbgh_d = nc.dram_tensor("dbgh", [128, NDC * CAP], BF16, kind="ExternalOutput").ap()
        B_xs, B_ys = B("xs_d"), B("ys_d")

        cst = C.sb("cst", [128, 4 * 128], F32)
        ident = cst[:, 0:128]
        tri = cst[:, 128:256]
        ones = cst[:, 256:384]
        ecap = C.sb("ecap", [128, NCH * NE], F32)
        ssq = C.sb("ssq", [128, 2], F32)
        rstd = C.sb("rstd", [128, 2], F32)
        Bcst = B("cst")
        gat = C.sb("gat", [128, NCH, NE], F32)
        dki = C.sb("dki", [128, NCH * TOPK], I32)
        gk = C.sb("gk", [128, NCH * TOPK], F32)
        bgu = C.sb("bgu", [128, NE * 16], F32)
        bdn = C.sb("bdn", [NE, D], F32)
        slotc = C.sb("slotc", [128, 2, NE, 3], F32)
        yidx = C.sb("yidx", [128, 2, NE * 3], I32)
        carry3 = C.sb("carry3", [128, NE, 1], F32)
        hsp_d = nc.dram_tensor("hsp_scr", [S, D], F32, kind="Internal").ap()
        Bhsp = [B(f"hsp{c}") for c in range(NCH)]

        psum_all = C.ps("psall", [128, 8 * 512])
        psum = [psum_all[:, i * 512:(i + 1) * 512] for i in range(8)]
        Bps = [B(f"ps{i}") for i in range(8)]

        bound_reg = nc.gpsimd.to_reg(NSLOT - 1)
        hst = {"stack": ExitStack(), "n": 0}
        h = hst["stack"].enter_context(nc.sbuf_tensor("h_res0", [128, NCH, D], F32))
        Bh = [B(f"h{c}") for c in range(NCH)]

        P.dma("sp", lambda e: e.dma_start(out=cst[:], in_=cst_d), writes=[Bcst])
        P.dma("pool", lambda e: e.dma_start(out=ecap[:], in_=ecap_d), writes=[Bcst])
        P.dma("sp", lambda e: e.dma_start(out=slotc[:].rearrange("p a e s -> p (a e s)"), in_=slotc_d), writes=[Bcst])
        bound_reg2 = nc.gpsimd.to_reg(2 * NSLOT - 1)
        ys2_d = ys_d.rearrange("n (h c) -> (n h) c", h=2)
        for c in range(NCH):
            P.dma("sp", lambda e: e.dma_start(out=h[:, c, :], in_=x_d[c * 128:(c + 1) * 128, :]),
                  writes=[Bh[c]])

        def rms_rstd(c, par, junk, Bj):
            Bs, Br = B(f"ssq{par}"), B(f"rstd{par}")
            P.op("act", lambda e: e.activation(out=junk[:], in_=h[:, c, :], func=AF.Square,
                                               accum_out=ssq[:, par:par + 1]),
                 reads=[Bh[c]], writes=[Bj, Bs])
            P.op("dve", lambda e: e.tensor_scalar(out=rstd[:, par:par + 1], in0=ssq[:, par:par + 1],
                                                  scalar1=1.0 / D, scalar2=RMS_EPS, op0=ALU.mult, op1=ALU.add),
                 reads=[Bs], writes=[Br])
            P.op("act", lambda e: e.activation(out=rstd[:, par:par + 1], in_=rstd[:, par:par + 1], func=AF.Sqrt),
                 reads=[Br], writes=[Br])
            P.op("dve", lambda e: e.reciprocal(out=rstd[:, par:par + 1], in_=rstd[:, par:par + 1]),
                 reads=[Br], writes=[Br])
            return Br

        MS = {}
        RB = []

        def moe_common(l):
            Bw = B("moe_small")
            Ball = B("route")
            RB[:] = [B(f"route{i}") for i in range(4)]
            return (4 + l) * D, L[l, "w_router"], L[l, "b_router"], L[l, "w_gu"], L[l, "b_gu"], L[l, "w_dn"], L[l, "b_dn"], Bw, Ball

        def moe_A(l):
            gcol, w_router, b_router, w_gu, b_gu, w_dn, b_dn, Bw, Ball = moe_common(l)
            P.dma("sp", lambda e: e.dma_start(out=bgu[:], in_=b_gu), writes=[Bw])
            P.dma("sp", lambda e: e.dma_start(out=bdn[:], in_=b_dn), writes=[Bw])
            with ExitStack() as ms:
                MC = Ctx(nc, ms)
                wr = MC.sb("wr", [128, NDC, NE], F32)
                br = MC.sb("br", [128, NE], F32)
                gam = MC.sb("gam", [128, D], F32)
                junk = MC.sb("junk", [128, D], F32)
                Bj = B("junk")
                P.dma("sp", lambda e: e.dma_start(out=wr[:], in_=w_router.rearrange("(c p) n -> p c n", p=128)),
                      writes=[Bw])
                P.dma("sp", lambda e: e.dma_start(out=br[:], in_=b_router), writes=[Bw])
                P.dma("sp", lambda e: e.dma_start(out=gam[:], in_=nrm_d[:, gcol:gcol + D]), writes=[Bw])
                NUF = 3
                uf = [MC.sb(f"uf{i}", [128, D], F32) for i in range(NUF)]
                ubig = MC.sb("ubig", [128, NCH, D], BF16)
                uT = [MC.sb(f"uT{i}", [128, NDC, 128], F32) for i in range(2)]
                ssq16 = MC.sb("ssq16", [128, NCH], F32)
                rs16 = MC.sb("rs16", [128, NCH], F32)
                logit = MC.sb("logit", [128, NCH, NE], F32)
                top8 = MC.sb("top8", [128, NCH, 8], F32)
                mask = MC.sb("mask", [128, NCH, NE], F32)
                sm = MC.sb("sm", [128, NCH, 2], F32)
                dest = MC.sb("dest", [128, NCH, NE], F32)
                tmp32 = MC.sb("tmp32", [128, NCH, NE], F32)
                off = MC.sb("off", [128, NCH, NE], F32)
                dk = MC.sb("dk", [128, NCH, TOPK], F32)
                Blog = [B(f"logit{c}") for c in range(NCH)]
                Bub = [B(f"ubig{c}") for c in range(NCH)]
                Bss = B("ssq16")
                for c in range(NCH):
                    P.op("act", lambda e: e.activation(out=junk[:], in_=h[:, c, :], func=AF.Square,
                                                       accum_out=ssq16[:, c:c + 1]),
                         reads=[Bh[c]], writes=[Bj, Bss])
                P.op("dve", lambda e: e.tensor_scalar(out=rs16[:], in0=ssq16[:], scalar1=1.0 / D, scalar2=RMS_EPS,
                                                      op0=ALU.mult, op1=ALU.add), reads=[Bss], writes=[Bss])
                P.op("act", lambda e: e.activation(out=rs16[:], in_=rs16[:], func=AF.Sqrt), reads=[Bss],
                     writes=[Bss])
                P.op("dve", lambda e: e.reciprocal(out=rs16[:], in_=rs16[:]), reads=[Bss], writes=[Bss])

                def s1(c):
                    i3 = c % NUF
                    P.op("dve", lambda e: e.scalar_tensor_tensor(
                        out=uf[i3][:], in0=h[:, c, :], scalar=rs16[:, c:c + 1], in1=gam[:],
                        op0=ALU.mult, op1=ALU.mult), reads=[Bh[c], Bss, Bw], writes=[B(f"uf{i3}")])
                    P.op("act", lambda e: e.copy(out=ubig[:, c, :], in_=uf[i3][:]), reads=[B(f"uf{i3}")],
                         writes=[Bub[c]])

                def s2(c):
                    i3, par = c % NUF, c % 2
                    for g in range(2):
                        pb = 2 * par + g

                        def tr(e):
                            r = None
                            for j in range(4):
                                dc = g * 4 + j
                                r = e.transpose(out=psum[pb][:, j * 128:(j + 1) * 128],
                                                in_=uf[i3][:, dc * 128:(dc + 1) * 128], identity=ident)
                            return r
                        P.op("pe", tr, reads=[B(f"uf{i3}"), Bcst], writes=[Bps[pb]])
                        src = psum[pb][:].rearrange("p (a b) -> p a b", a=4)
                        if g == 0:
                            P.op("act", lambda e: e.copy(out=uT[par][:, 0:4, :], in_=src),
                                 reads=[Bps[pb]], writes=[B(f"uT{par}")])
                        else:
                            P.op("dve", lambda e: e.tensor_copy(out=uT[par][:, 4:8, :], in_=src),
                                 reads=[Bps[pb]], writes=[B(f"uT{par}")])

                def s3(c):
                    par = c % 2
                    pl = 4 + par

                    def rt(e):
                        r = None
                        for dc in range(NDC):
                            r = e.matmul(psum[pl][:, 0:NE], lhsT=uT[par][:, dc, :], rhs=wr[:, dc, :],
                                         start=(dc == 0), stop=(dc == NDC - 1))
                        return r
                    P.op("pe", rt, reads=[B(f"uT{par}"), Bw], writes=[Bps[pl]])
                    P.op("dve", lambda e: e.tensor_tensor(out=logit[:, c, :], in0=psum[pl][:, 0:NE], in1=br[:],
                                                          op=ALU.add),
                         reads=[Bps[pl], Bw], writes=[Blog[c]])
                    P.op("dve", lambda e: e.max(out=top8[:, c, :], in_=logit[:, c, :]), reads=[Blog[c]],
                         writes=[Blog[c]])

                carry = carry3[:, :, 0]
                xs_bufs = []
                MS['xs_bufs'] = xs_bufs
                NRG = 4
                GC = NCH // NRG

                def route_batch(c0, c1, hi):
                    nch = c1 - c0
                    Bl = Blog[c0:c1]
                    Br_ = RB[hi]
                    L3 = [128, nch, NE]
                    sl3 = lambda t: t[:, c0:c1, :]
                    fl = lambda t: t[:, c0:c1, :].rearrange("p c e -> p (c e)")
                    P.op("dve", lambda e: e.tensor_tensor(out=sl3(mask), in0=sl3(logit),
                                                          in1=top8[:, c0:c1, 3:4].broadcast_to(L3), op=ALU.is_ge),
                         reads=Bl, writes=[Br_])
                    P.op("dve", lambda e: e.tensor_tensor(out=sl3(gat), in0=sl3(logit),
                                                          in1=top8[:, c0:c1, 0:1].broadcast_to(L3), op=ALU.subtract),
                         reads=Bl, writes=[Br_])
                    P.op("act", lambda e: e.activation(out=sl3(gat), in_=sl3(gat), func=AF.Exp), reads=[Br_],
                         writes=[Br_])
                    P.op("dve", lambda e: e.tensor_tensor(out=sl3(gat), in0=sl3(gat), in1=sl3(mask), op=ALU.mult),
                         reads=[Br_], writes=[Br_])
                    P.op("dve", lambda e: e.tensor_reduce(out=sm[:, c0:c1, 0], in_=sl3(gat), axis=AX.X, op=ALU.add),
                         reads=[Br_], writes=[Br_])
                    P.op("dve", lambda e: e.reciprocal(out=sm[:, c0:c1, 1], in_=sm[:, c0:c1, 0]), reads=[Br_],
                         writes=[Br_])
                    P.op("dve", lambda e: e.tensor_tensor(out=sl3(gat), in0=sl3(gat),
                                                          in1=sm[:, c0:c1, 1:2].broadcast_to(L3), op=ALU.mult),
                         reads=[Br_], writes=[Br_])
                    W = nch * NE
                    P.op("pe", lambda e: e.matmul(psum[6][:, 0:W], lhsT=tri, rhs=fl(mask), start=True, stop=True),
                         reads=[Br_, Bcst], writes=[Bps[6]])
                    P.op("pe", lambda e: e.matmul(psum[7][:, 0:W], lhsT=ones, rhs=fl(mask), start=True, stop=True),
                         reads=[Br_, Bcst], writes=[Bps[7]])
                    P.op("dve", lambda e: e.tensor_copy(out=fl(tmp32), in_=psum[7][:, 0:W]),
                         reads=[Bps[7]], writes=[Br_])
                    if c0 == 0:
                        P.op("dve", lambda e: e.memset(off[:, c0, :], 0.0), writes=[Br_])
                    else:
                        P.op("dve", lambda e: e.tensor_copy(out=off[:, c0, :], in_=carry),
                             reads=[B("carry")], writes=[Br_])
                    for c in range(c0 + 1, c1):
                        P.op("dve", lambda e: e.tensor_tensor(out=off[:, c, :], in0=off[:, c - 1, :],
                                                              in1=tmp32[:, c - 1, :], op=ALU.add),
                             reads=[Br_], writes=[Br_])
                    if True:
                        P.op("dve", lambda e: e.tensor_tensor(out=carry, in0=off[:, c1 - 1, :],
                                                              in1=tmp32[:, c1 - 1, :], op=ALU.add),
                             reads=[Br_], writes=[B("carry")])
                    P.op("dve", lambda e: e.tensor_tensor(out=fl(off), in0=fl(off), in1=psum[6][:, 0:W], op=ALU.add),
                         reads=[Br_, Bps[6]], writes=[Br_])
                    P.op("dve", lambda e: e.tensor_scalar(out=fl(tmp32), in0=fl(off), scalar1=float(CAP), scalar2=None,
                                                          op0=ALU.is_lt), reads=[Br_], writes=[Br_])
                    P.op("dve", lambda e: e.tensor_tensor(out=fl(gat), in0=fl(gat), in1=fl(tmp32), op=ALU.mult),
                         reads=[Br_], writes=[Br_])
                    P.op("dve", lambda e: e.tensor_tensor(out=fl(dest), in0=fl(off), in1=ecap[:, c0 * NE:c1 * NE],
                                                          op=ALU.add), reads=[Br_, Bcst], writes=[Br_])
                    P.op("dve", lambda e: e.tensor_scalar(out=fl(tmp32), in0=fl(tmp32), scalar1=-1.0e6, scalar2=1.0e6,
                                                          op0=ALU.mult, op1=ALU.add), reads=[Br_], writes=[Br_])
                    P.op("dve", lambda e: e.tensor_tensor(out=fl(dest), in0=fl(dest), in1=fl(tmp32), op=ALU.add),
                         reads=[Br_], writes=[Br_])
                    gk3 = gk[:].rearrange("p (c k) -> p c k", k=TOPK)
                    for k in range(TOPK):
                        P.op("dve", lambda e: e.tensor_tensor(out=sl3(mask), in0=sl3(logit),
                                                              in1=top8[:, c0:c1, k:k + 1].broadcast_to(L3),
                                                              op=ALU.is_equal), reads=[Br_], writes=[Br_])
                        P.op("dve", lambda e: e.tensor_tensor(out=sl3(tmp32), in0=sl3(mask), in1=sl3(dest),
                                                              op=ALU.mult), reads=[Br_], writes=[Br_])
                        P.op("dve", lambda e: e.tensor_reduce(out=dk[:, c0:c1, k], in_=sl3(tmp32), axis=AX.X,
                                                              op=ALU.add), reads=[Br_], writes=[Br_])
                        P.op("dve", lambda e: e.tensor_tensor(out=sl3(tmp32), in0=sl3(mask), in1=sl3(gat),
                                                              op=ALU.mult), reads=[Br_], writes=[Br_])
                        P.op("dve", lambda e: e.tensor_reduce(out=gk3[:, c0:c1, k], in_=sl3(tmp32), axis=AX.X,
                                                              op=ALU.add), reads=[Br_], writes=[Br_])
                    P.op("dve", lambda e: e.tensor_copy(out=dki[:, c0 * TOPK:c1 * TOPK],
                                                        in_=dk[:, c0:c1, :].rearrange("p c k -> p (c k)")),
                         reads=[Br_], writes=[Br_])
                    for c in range(c0, c1):
                        for k in range(TOPK):
                            col = c * TOPK + k
                            bx = Buf(f"xs{col}")
                            xs_bufs.append(bx)
                            P.dma("pool", lambda e: e.indirect_dma_start(
                                out=xs_d, out_offset=bass.IndirectOffsetOnAxis(ap=dki[:, col:col + 1], axis=0),
                                in_=ubig[:, c, :], in_offset=None, bounds_check=bound_reg, oob_is_err=False),
                                reads=[Bub[c], Br_], writes=[bx])

                for it in range(NCH + 2):
                    if it < NCH:
                        s1(it)
                    if 1 <= it <= NCH:
                        s2(it - 1)
                    if it >= 2:
                        s3(it - 2)
                    for gi in range(NRG - 1):
                        if it == (gi + 1) * GC + 1:
                            route_batch(gi * GC, (gi + 1) * GC, gi)
                route_batch((NRG - 1) * GC, NCH, NRG - 1)
                vtmp = MC.sb("vtmp", [128, NE, 3], F32)
                vtm2 = MC.sb("vtm2", [128, NE, 3], F32)
                Byi = B("yidx")
                P.op("dve", lambda e: e.tensor_tensor(out=vtmp[:], in0=slotc[:, 1, :, :],
                                                      in1=carry3[:].broadcast_to([128, NE, 3]), op=ALU.is_lt),
                     reads=[B("carry"), Bcst], writes=[Byi])
                P.op("dve", lambda e: e.tensor_scalar(out=vtmp[:], in0=vtmp[:], scalar1=-1.0e6, scalar2=1.0e6,
                                                      op0=ALU.mult, op1=ALU.add), reads=[Byi], writes=[Byi])
                P.op("dve", lambda e: e.tensor_tensor(out=vtmp[:], in0=vtmp[:], in1=slotc[:, 0, :, :], op=ALU.add),
                     reads=[Byi, Bcst], writes=[Byi])
                P.op("dve", lambda e: e.tensor_scalar(out=vtm2[:], in0=vtmp[:], scalar1=1.0, scalar2=None,
                                                      op0=ALU.add), reads=[Byi], writes=[Byi])
                P.op("dve", lambda e: e.tensor_copy(out=yidx[:, 0, :], in_=vtmp[:].rearrange("p e s -> p (e s)")),
                     reads=[Byi], writes=[Byi])
                P.op("dve", lambda e: e.tensor_copy(out=yidx[:, 1, :], in_=vtm2[:].rearrange("p e s -> p (e s)")),
                     reads=[Byi], writes=[Byi])
                P.barrier()


        def moe_B(l):
            gcol, w_router, b_router, w_gu, b_gu, w_dn, b_dn, Bw, Ball = moe_common(l)
            xs_bufs = MS["xs_bufs"]
            with ExitStack() as ms:
                MC = Ctx(nc, ms)
                PW = 256
                NWF, NWG, NWD = 6, 16, 8
                wf = [MC.sb(f"wf{i}", [128, NDC, PW], F32) for i in range(NWF)]
                wg = [MC.sb(f"wg{i}", [128, NDC, PW], BF16) for i in range(NWG)]
                wd = [MC.sb(f"wd{i}", [128, NDC, PW], BF16) for i in range(NWD)]
                Bwf = [B(f"wf{i}") for i in range(NWF)]
                Bwg = [B(f"wg{i}") for i in range(NWG)]
                Bwd = [B(f"wd{i}") for i in range(NWD)]
                xT = [MC.sb(f"xT{i}", [128, NDC, CAP], BF16) for i in range(2)]
                hT = [MC.sb(f"hT{i}", [128, NDC, CAP], BF16) for i in range(2)]
                ysb = [MC.sb(f"ysb{i}", [128, 512], F32) for i in range(4)]
                t_g = [MC.sb(f"t_g{i}", [128, CAP], F32) for i in range(2)]
                t_s = [MC.sb(f"t_s{i}", [128, CAP], F32) for i in range(2)]
                t_u = [MC.sb(f"t_u{i}", [128, CAP], F32) for i in range(2)]
                BxT = [B(f"xT{i}") for i in range(2)]
                BhT = [B(f"hT{i}") for i in range(2)]
                Bys = [B(f"ysb{i}") for i in range(4)]
                Btg = [B(f"t_g{i}") for i in range(2)]
                Bts = [B(f"t_s{i}") for i in range(2)]
                Btu = [B(f"t_u{i}") for i in range(2)]
                NPC = 12
                NG = NE * NPC

                def piece_src(G):
                    e_, p = G // NPC, G % NPC
                    if p < 8:
                        q, is_up = p // 2, p % 2
                        c0 = is_up * D + q * PW
                        return w_gu[e_, :, c0:c0 + PW].rearrange("(c p) n -> p c n", p=128)
                    q = p - 8
                    return w_dn[e_, :, q * PW:(q + 1) * PW].rearrange("(c p) n -> p c n", p=128)

                def piece_dst(G):
                    e_, p = G // NPC, G % NPC
                    if p < 8:
                        i = (e_ * 8 + p) % NWG
                        return wg[i], Bwg[i]
                    i = (e_ * 4 + (p - 8)) % NWD
                    return wd[i], Bwd[i]

                def issue_dma(G):
                    if G >= NG:
                        return
                    i = G % NWF
                    P.dma("sp", lambda e: e.dma_start(out=wf[i][:], in_=piece_src(G)), writes=[Bwf[i]])

                def issue_cast(G):
                    if G >= NG:
                        return
                    i = G % NWF
                    dst, Bdst = piece_dst(G)
                    if (G % NPC) < 8:
                        P.op("act", lambda e: e.copy(out=dst[:], in_=wf[i][:]), reads=[Bwf[i]], writes=[Bdst])
                    else:
                        P.op("dve", lambda e: e.tensor_copy(out=dst[:], in_=wf[i][:]), reads=[Bwf[i]], writes=[Bdst])

                def tick(G):
                    issue_cast(G)
                    issue_dma(G + NWF)

                xr = MC.sb("xr", [128, CAP // 128, D], BF16)
                identb = MC.sb("identb3", [128, 128], BF16)
                Bxr = B("xr")
                P.op("dve", lambda e: e.tensor_copy(out=identb[:], in_=ident), reads=[Bcst], writes=[B("identb3")])

                def load_x(e_):
                    if e_ >= NE:
                        return
                    P.dma("sp", lambda e: e.dma_start(
                        out=xr[:], in_=xs_d[e_ * CAP:(e_ + 1) * CAP, :].rearrange("(c p) n -> p c n", p=128)),
                        reads=xs_bufs, writes=[Bxr])

                def transpose_x(e_):
                    if e_ >= NE:
                        return
                    i2 = e_ % 2
                    pv = psum[7][:].bitcast(BF16)
                    for sc in range(CAP // 128):
                        def tr(e):
                            r = None
                            for dc in range(NDC):
                                r = e.transpose(out=pv[:, dc * 128:(dc + 1) * 128],
                                                in_=xr[:, sc, dc * 128:(dc + 1) * 128], identity=identb[:])
                            return r
                        P.op("pe", tr, reads=[Bxr, B("identb3")], writes=[Bps[7]])
                        P.op("dve", lambda e: e.tensor_copy(out=xT[i2][:, :, sc * 128:(sc + 1) * 128],
                                                            in_=pv.rearrange("p (a b) -> p a b", a=NDC)),
                             reads=[Bps[7]], writes=[BxT[i2]])

                def compute_expert(e_):
                    i2 = e_ % 2
                    Gn = (e_ + 1) * NPC
                    transpose_x(e_ + 1)
                    load_x(e_ + 2)
                    for fcp in range(8):
                        q, j = fcp // 2, fcp % 2
                        sg = (e_ * 8 + 2 * q) % NWG
                        su = (e_ * 8 + 2 * q + 1) % NWG
                        pg, pu = (fcp % 2) * 2, (fcp % 2) * 2 + 1
                        ti = fcp % 2

                        def mm(e, slot, pb):
                            r = None
                            for dc in range(NDC):
                                r = e.matmul(psum[pb][:, 0:CAP], lhsT=wg[slot][:, dc, j * 128:(j + 1) * 128],
                                             rhs=xT[i2][:, dc, :], start=(dc == 0), stop=(dc == NDC - 1))
                            return r
                        P.op("pe", lambda e: mm(e, sg, pg), reads=[Bwg[sg], BxT[i2]], writes=[Bps[pg]])
                        P.op("pe", lambda e: mm(e, su, pu), reads=[Bwg[su], BxT[i2]], writes=[Bps[pu]])
                        bg = bgu[:, e_ * 16 + fcp:e_ * 16 + fcp + 1]
                        bu = bgu[:, e_ * 16 + 8 + fcp:e_ * 16 + 8 + fcp + 1]
                        P.op("dve", lambda e: e.tensor_scalar(out=t_g[ti][:], in0=psum[pg][:, 0:CAP], scalar1=bg,
                                                              scalar2=7.0, op0=ALU.add, op1=ALU.min),
                             reads=[Bps[pg], Bw], writes=[Btg[ti]])
                        P.op("act", lambda e: e.activation(out=t_s[ti][:], in_=t_g[ti][:], func=AF.Sigmoid,
                                                           scale=1.702),
                             reads=[Btg[ti]], writes=[Bts[ti]])
                        P.op("dve", lambda e: e.tensor_scalar(out=t_u[ti][:], in0=psum[pu][:, 0:CAP], scalar1=bu,
                                                              scalar2=7.0, op0=ALU.add, op1=ALU.min),
                             reads=[Bps[pu], Bw], writes=[Btu[ti]])
                        P.op("dve", lambda e: e.tensor_scalar(out=t_u[ti][:], in0=t_u[ti][:], scalar1=-7.0,
                                                               scalar2=1.0, op0=ALU.max, op1=ALU.add),
                             reads=[Btu[ti]], writes=[Btu[ti]])
                        P.op("dve", lambda e: e.tensor_tensor(out=t_g[ti][:], in0=t_g[ti][:], in1=t_s[ti][:],
                                                               op=ALU.mult),
                             reads=[Btg[ti], Bts[ti]], writes=[Btg[ti]])
                        P.op("dve", lambda e: e.tensor_tensor(out=hT[i2][:, fcp, :], in0=t_u[ti][:],
                                                              in1=t_g[ti][:], op=ALU.mult),
                             reads=[Btu[ti], Btg[ti]], writes=[BhT[i2]])
                        tick(Gn + fcp)
                    chunks = [(s0, min(128, CAP - s0)) for s0 in range(0, CAP, 128)]
                    step = 0
                    for half in range(2):
                        for (s0, sn) in chunks:
                            idx = step
                            pb = 4 + idx % 3
                            yi = idx % 4

                            def md(e):
                                r = None
                                for qq in range(2):
                                    sd = (e_ * 4 + half * 2 + qq) % NWD
                                    for fc in range(NDC):
                                        r = e.matmul(psum[pb][0:sn, qq * PW:(qq + 1) * PW],
                                                     lhsT=hT[i2][:, fc, s0:s0 + sn], rhs=wd[sd][:, fc, :],
                                                     start=(fc == 0), stop=(fc == NDC - 1))
                                return r
                            sds = [(e_ * 4 + half * 2 + qq) % NWD for qq in range(2)]
                            P.op("pe", md, reads=[BhT[i2]] + [Bwd[x] for x in sds], writes=[Bps[pb]])
                            P.op("act", lambda e: e.copy(out=ysb[yi][0:sn, :], in_=psum[pb][0:sn, :]),
                                 reads=[Bps[pb]], writes=[Bys[yi]])
                            col = e_ * 3 + s0 // 128
                            P.dma("pool", lambda e: e.indirect_dma_start(
                                out=ys2_d, out_offset=bass.IndirectOffsetOnAxis(ap=yidx[:, half, col:col + 1], axis=0),
                                in_=ysb[yi][:], in_offset=None, bounds_check=bound_reg2, oob_is_err=False),
                                reads=[Bys[yi], B("yidx")], writes=[Buf("ys")])
                            if step < 4:
                                tick(Gn + 8 + step)
                            step += 1
                    for st in range(step, 4):
                        tick(Gn + 8 + st)

                for G in range(NWF):
                    issue_dma(G)
                load_x(0)
                transpose_x(0)
                load_x(1)
                for G in range(NPC):
                    tick(G)
                for e_ in range(NE):
                    compute_expert(e_)
                P.barrier()

        def moe_C(l):
            gcol, w_router, b_router, w_gu, b_gu, w_dn, b_dn, Bw, Ball = moe_common(l)
            with ExitStack() as ms:
                MC = Ctx(nc, ms)
                yg = [MC.sb(f"yg{i}", [128, D], F32) for i in range(8)]
                Byg = [B(f"yg{i}") for i in range(8)]
                gT = MC.sb("gT", [NE, 128], F32)
                for i in range(8):
                    P.op("dve", lambda e: e.memset(yg[i][:], 0.0), writes=[Byg[i]])
                for c in range(NCH):
                    P.op("pe", lambda e: e.transpose(out=psum[0][0:NE, 0:128], in_=gat[:, c, :], identity=ident),
                         reads=RB + [Bcst], writes=[Bps[0]])
                    P.op("act", lambda e: e.copy(out=gT[:], in_=psum[0][0:NE, 0:128]), reads=[Bps[0]],
                         writes=[B("gT")])
                    for half in range(2):
                        P.op("pe", lambda e: e.matmul(psum[1 + half][:], lhsT=gT[:],
                                                      rhs=bdn[:, half * 512:(half + 1) * 512],
                                                      start=True, stop=True),
                             reads=[B("gT"), Bw], writes=[Bps[1 + half]])
                        P.op("dve", lambda e: e.tensor_tensor(out=h[:, c, half * 512:(half + 1) * 512],
                                                              in0=h[:, c, half * 512:(half + 1) * 512],
                                                              in1=psum[1 + half][:], op=ALU.add),
                             reads=[Bps[1 + half], Bh[c]], writes=[Bh[c]])
                    for k in range(TOPK):
                        col = c * TOPK + k
                        yi = (c % 2) * 4 + k
                        P.dma("pool", lambda e: e.indirect_dma_start(
                            out=yg[yi][:], out_offset=None, in_=ys_d,
                            in_offset=bass.IndirectOffsetOnAxis(ap=dki[:, col:col + 1], axis=0),
                            bounds_check=bound_reg, oob_is_err=False),
                            reads=list(RB), writes=[Byg[yi]])
                        P.op("dve", lambda e: e.scalar_tensor_tensor(
                            out=h[:, c, :], in0=yg[yi][:], scalar=gk[:, col:col + 1], in1=h[:, c, :],
                            op0=ALU.mult, op1=ALU.add),
                            reads=[Byg[yi], Bh[c]] + RB, writes=[Bh[c]])
                P.barrier()


        def norm_to_uT(l, uT, BuT):
            gcol = l * D
            with ExitStack() as ms:
                MC = Ctx(nc, ms)
                gam = MC.sb("gam", [128, D], F32)
                junk = MC.sb("junk", [128, D], F32)
                NU = 3
                ubf = [MC.sb(f"ubf{i}", [128, D], BF16) for i in range(NU)]
                identb = MC.sb("identb", [128, 128], BF16)
                ssq16 = MC.sb("nssq16", [128, NCH], F32)
                rs16 = MC.sb("nrs16", [128, NCH], F32)
                Bg, Bj, Bss = B("gam_m"), B("junk_m"), B("nssq16")
                P.dma("sp", lambda e: e.dma_start(out=gam[:], in_=nrm_d[:, gcol:gcol + D]), writes=[Bg])
                P.op("dve", lambda e: e.tensor_copy(out=identb[:], in_=ident), reads=[Bcst], writes=[Bg])
                for c in range(NCH):
                    P.op("act", lambda e: e.activation(out=junk[:], in_=h[:, c, :], func=AF.Square,
                                                       accum_out=ssq16[:, c:c + 1]),
                         reads=[Bh[c]], writes=[Bj, Bss])
                P.op("dve", lambda e: e.tensor_scalar(out=rs16[:], in0=ssq16[:], scalar1=1.0 / D, scalar2=RMS_EPS,
                                                      op0=ALU.mult, op1=ALU.add), reads=[Bss], writes=[Bss])
                P.op("act", lambda e: e.activation(out=rs16[:], in_=rs16[:], func=AF.Sqrt), reads=[Bss], writes=[Bss])
                P.op("dve", lambda e: e.reciprocal(out=rs16[:], in_=rs16[:]), reads=[Bss], writes=[Bss])

                def s1(c):
                    i3 = c % NU
                    P.op("dve", lambda e: e.scalar_tensor_tensor(
                        out=ubf[i3][:], in0=h[:, c, :], scalar=rs16[:, c:c + 1], in1=gam[:],
                        op0=ALU.mult, op1=ALU.mult), reads=[Bh[c], Bss, Bg], writes=[B(f"ubf{i3}")])

                def s2(c):
                    i3, pb = c % NU, c % 2
                    pv = psum[pb][:].bitcast(BF16)

                    def tr(e):
                        r = None
                        for dc in range(NDC):
                            r = e.transpose(out=pv[:, dc * 128:(dc + 1) * 128],
                                            in_=ubf[i3][:, dc * 128:(dc + 1) * 128], identity=identb[:])
                        return r
                    P.op("pe", tr, reads=[B(f"ubf{i3}"), Bg], writes=[Bps[pb]])
                    src = pv.rearrange("p (a b) -> p a b", a=NDC)
                    P.op("act", lambda e: e.copy(out=uT[:, :, c * 128:(c + 1) * 128], in_=src),
                         reads=[Bps[pb]], writes=[BuT[c // 4]])

                for it in range(NCH + 1):
                    if it < NCH:
                        s1(it)
                    if it >= 1:
                        s2(it - 1)
                P.barrier()

        def out_proj(l, yT, ByT):
            w_out = L[l, "w_out"]
            with ExitStack() as ms:
                MC = Ctx(nc, ms)
                wo = MC.sb("wo", [128, NDC, D], BF16)
                Bwo = B("wo")
                for half in range(2):
                    P.dma("pool", lambda e: e.dma_start(
                        out=wo[:, :, half * 512:(half + 1) * 512],
                        in_=w_out[:, half * 512:(half + 1) * 512].rearrange("(c p) n -> p c n", p=128)),
                        writes=[Bwo])
                for c in range(NCH):
                    for half in range(2):
                        pb = (c * 2 + half) % 8

                        def mm(e):
                            r = None
                            for g in range(NDC):
                                r = e.matmul(psum[pb][:], lhsT=yT[:, g, c * 128:(c + 1) * 128],
                                             rhs=wo[:, g, half * 512:(half + 1) * 512],
                                             start=(g == 0), stop=(g == NDC - 1))
                            return r
                        P.op("pe", mm, reads=list(ByT) + [Bwo], writes=[Bps[pb]])
                        P.op("dve", lambda e: e.tensor_tensor(out=h[:, c, half * 512:(half + 1) * 512],
                                                              in0=h[:, c, half * 512:(half + 1) * 512],
                                                              in1=psum[pb][:], op=ALU.add),
                             reads=[Bps[pb], Bh[c]], writes=[Bh[c]])
                P.barrier()

        def lru_layer(l):
            w_in, vec_d, w_a, w_x = L[l, "w_in"], L[l, "vec"], L[l, "w_a"], L[l, "w_x"]
            PAD = 4
            with ExitStack() as ls:
                LC = Ctx(nc, ls)
                yT = LC.sb("yT", [128, NDC, S], BF16)
                ByT = [B(f"yT{g}") for g in range(NDC)]
                with ExitStack() as us:
                    UC = Ctx(nc, us)
                    uT = UC.sb("uT", [128, NDC, S], BF16)
                    BuT = [B(f"uTq{i}") for i in range(4)]
                    norm_to_uT(l, uT, BuT)
                    with ExitStack() as ms:
                        MC = Ctx(nc, ms)
                        vec = MC.sb("vec", [128, 8, 8], F32)
                        sc = MC.sb("sc", [128, 8, 2], F32)
                        wa = MC.sb("wa", [128, 8, 128], BF16)
                        wx = MC.sb("wx", [128, 8, 128], BF16)
                        wig = [MC.sb(f"wig{i}", [128, NDC, 128], BF16) for i in range(2)]
                        wix = [MC.sb(f"wix{i}", [128, NDC, 128], BF16) for i in range(2)]
                        XB = MC.sb("XB", [128, PAD + S], F32)
                        XC = MC.sb("XC", [128, S], F32)
                        XCB = MC.sb("XCB", [128, S], BF16)
                        R = MC.sb("R", [128, S], F32)
                        I_ = MC.sb("I", [128, S], F32)
                        A = MC.sb("A", [128, S], F32)
                        Bv = B("lru_small")
                        BXB, BXC, BXCB, BR, BI, BA = B("XB"), B("XC"), B("XCB"), B("R"), B("I"), B("A")
                        Bwig = [B(f"wig{i}") for i in range(2)]
                        Bwix = [B(f"wix{i}") for i in range(2)]
                        P.dma("sp", lambda e: e.dma_start(out=vec[:].rearrange("p g k -> p (g k)"), in_=vec_d),
                              writes=[Bv])
                        P.dma("pool", lambda e: e.dma_start(out=wa[:], in_=w_a.rearrange("g i o -> i g o")),
                              writes=[Bv])
                        P.dma("pool", lambda e: e.dma_start(out=wx[:], in_=w_x.rearrange("g i o -> i g o")),
                              writes=[Bv])
                        P.op("act", lambda e: e.activation(out=sc[:, :, 0], in_=vec[:, :, 7], func=AF.Exp, scale=-1.0),
                             reads=[Bv], writes=[Bv])
                        P.op("act", lambda e: e.activation(out=sc[:, :, 0], in_=sc[:, :, 0], func=AF.Ln, bias=1.0,
                                                           scale=1.0), reads=[Bv], writes=[Bv])
                        P.op("dve", lambda e: e.tensor_scalar(out=sc[:, :, 1], in0=sc[:, :, 0], scalar1=-16.0,
                                                              scalar2=None, op0=ALU.mult), reads=[Bv], writes=[Bv])
                        P.op("dve", lambda e: e.tensor_scalar(out=sc[:, :, 0], in0=sc[:, :, 0], scalar1=-8.0,
                                                              scalar2=None, op0=ALU.mult), reads=[Bv], writes=[Bv])
                        P.op("dve", lambda e: e.memset(XB[:, 0:PAD], 0.0), writes=[BXB])

                        def load_w(g):
                            i2 = g % 2
                            P.dma("pool", lambda e: e.dma_start(
                                out=wig[i2][:], in_=w_in[:, g * 128:(g + 1) * 128].rearrange("(c p) n -> p c n", p=128)),
                                writes=[Bwig[i2]])
                            P.dma("pool", lambda e: e.dma_start(
                                out=wix[i2][:],
                                in_=w_in[:, D + g * 128:D + (g + 1) * 128].rearrange("(c p) n -> p c n", p=128)),
                                writes=[Bwix[i2]])

                        def proj(wt, Bwt, tq, pb):
                            def mm(e):
                                r = None
                                for dc in range(NDC):
                                    r = e.matmul(psum[pb][:], lhsT=wt[:, dc, :], rhs=uT[:, dc, tq * 512:(tq + 1) * 512],
                                                 start=(dc == 0), stop=(dc == NDC - 1))
                                return r
                            P.op("pe", mm, reads=[Bwt, BuT[tq]], writes=[Bps[pb]])

                        T_ = MC.sb("T", [128, S], F32)
                        BT = [B(f"lruT{i}") for i in range(4)]
                        load_w(0)
                        for g in range(NDC):
                            i2 = g % 2
                            if g + 1 < NDC:
                                load_w(g + 1)
                            for tq in range(4):
                                proj(wix[i2], Bwix[i2], tq, tq)
                                P.op("act", lambda e: e.copy(out=XB[:, PAD + tq * 512:PAD + (tq + 1) * 512],
                                                             in_=psum[tq][:]), reads=[Bps[tq]], writes=[BXB])
                            for tq in range(4):
                                proj(wig[i2], Bwig[i2], tq, 4 + tq)
                            P.op("dve", lambda e: e.tensor_scalar(out=XC[:], in0=XB[:, PAD:PAD + S],
                                                                  scalar1=vec[:, g, 3:4], scalar2=vec[:, g, 4:5],
                                                                  op0=ALU.mult, op1=ALU.add),
                                 reads=[BXB, Bv], writes=[BXC])
                            for tq in range(4):
                                sl = slice(tq * 512, (tq + 1) * 512)
                                P.op("act", lambda e: e.activation(out=T_[:, sl], in_=psum[4 + tq][:], func=AF.Square,
                                                                   scale=0.21145921592589454),
                                     reads=[Bps[4 + tq]], writes=[BT[tq]])
                            for j in range(1, 4):
                                P.op("dve", lambda e: e.scalar_tensor_tensor(
                                    out=XC[:], in0=XB[:, PAD - j:PAD - j + S], scalar=vec[:, g, 3 - j:4 - j], in1=XC[:],
                                    op0=ALU.mult, op1=ALU.add), reads=[BXB, Bv, BXC], writes=[BXC])
                            P.op("act", lambda e: e.copy(out=XCB[:], in_=XC[:]), reads=[BXC], writes=[BXCB])
                            for tq in range(4):
                                sl = slice(tq * 512, (tq + 1) * 512)
                                P.op("dve", lambda e: e.scalar_tensor_tensor(
                                    out=T_[:, sl], in0=T_[:, sl], scalar=1.0, in1=psum[4 + tq][:],
                                    op0=ALU.add, op1=ALU.mult), reads=[BT[tq], Bps[4 + tq]], writes=[BT[tq]])
                            for tq in range(4):
                                P.op("pe", lambda e: e.matmul(psum[tq][:], lhsT=wa[:, g, :],
                                                              rhs=XCB[:, tq * 512:(tq + 1) * 512], start=True, stop=True),
                                     reads=[BXCB, Bv], writes=[Bps[tq]])
                                P.op("act", lambda e: e.activation(out=R[:, tq * 512:(tq + 1) * 512], in_=psum[tq][:],
                                                                   func=AF.Sigmoid, bias=vec[:, g, 5:6], scale=1.0),
                                     reads=[Bps[tq], Bv], writes=[BR])
                            for tq in range(4):
                                P.op("pe", lambda e: e.matmul(psum[tq][:], lhsT=wx[:, g, :],
                                                              rhs=XCB[:, tq * 512:(tq + 1) * 512], start=True, stop=True),
                                     reads=[BXCB, Bv], writes=[Bps[tq]])
                                P.op("act", lambda e: e.activation(out=I_[:, tq * 512:(tq + 1) * 512],
                                                                   in_=psum[tq][:], func=AF.Sigmoid,
                                                                   bias=vec[:, g, 6:7], scale=1.0),
                                     reads=[Bps[tq], Bv], writes=[BI])
                            for tq in range(4):
                                sl = slice(tq * 512, (tq + 1) * 512)
                                P.op("act", lambda e: e.activation(out=T_[:, sl], in_=T_[:, sl], func=AF.Sigmoid,
                                                                   scale=1.5957691216057308), reads=[BT[tq]],
                                     writes=[BT[tq]])
                            P.op("act", lambda e: e.activation(out=A[:], in_=R[:], func=AF.Exp, scale=sc[:, g, 0:1]),
                                 reads=[BR, Bv], writes=[BA])
                            P.op("act", lambda e: e.activation(out=R[:], in_=R[:], func=AF.Exp, scale=sc[:, g, 1:2]),
                                 reads=[BR, Bv], writes=[BR])
                            for tq in range(4):
                                sl = slice(tq * 512, (tq + 1) * 512)
                                P.op("dve", lambda e: e.tensor_tensor(out=T_[:, sl], in0=T_[:, sl], in1=psum[4 + tq][:],
                                                                      op=ALU.mult), reads=[BT[tq], Bps[4 + tq]],
                                     writes=[BT[tq]])
                            P.op("dve", lambda e: e.tensor_tensor(out=I_[:], in0=I_[:], in1=XC[:], op=ALU.mult),
                                 reads=[BI, BXC], writes=[BI])
                            P.op("act", lambda e: e.activation(out=R[:], in_=R[:], func=AF.Sqrt, scale=-1.0, bias=1.0),
                                 reads=[BR], writes=[BR])
                            P.op("dve", lambda e: e.tensor_tensor(out=I_[:], in0=I_[:], in1=R[:], op=ALU.mult),
                                 reads=[BI, BR], writes=[BI])
                            Y = XB[:, PAD:PAD + S]
                            P.op("dve", lambda e: e.tensor_tensor_scan(out=Y, data0=A[:], data1=I_[:], initial=0.0,
                                                                       op0=ALU.mult, op1=ALU.add),
                                 reads=[BA, BI], writes=[BXB])
                            for tq in range(4):
                                sl = slice(tq * 512, (tq + 1) * 512)
                                P.op("dve", lambda e: e.tensor_tensor(out=yT[:, g, sl], in0=T_[:, sl], in1=Y[:, sl],
                                                                      op=ALU.mult), reads=[BT[tq], BXB], writes=[ByT[g]])
                        P.barrier()
                out_proj(l, yT, ByT)

        def pool_layer(l):
            w_in, w_grp, vec_d = L[l, "w_in"], L[l, "w_grp"], L[l, "vec"]
            PAD = 16
            WINS = (2, 4, 8, 16)
            with ExitStack() as ls:
                LC = Ctx(nc, ls)
                yT = LC.sb("yT", [128, NDC, S], BF16)
                ByT = [B(f"yT{g}") for g in range(NDC)]
                with ExitStack() as us:
                    UC = Ctx(nc, us)
                    uT = UC.sb("uT", [128, NDC, S], BF16)
                    BuT = [B(f"uTq{i}") for i in range(4)]
                    norm_to_uT(l, uT, BuT)
                    with ExitStack() as ms:
                        MC = Ctx(nc, ms)
                        vec = MC.sb("pvec", [128, 8 + 64], F32)
                        wgr = MC.sb("wgr", [128, 4, 2, 256], BF16)
                        wi = [MC.sb(f"wi{i}", [128, NDC, 128], BF16) for i in range(2)]
                        V_ = [MC.sb(f"V{i}", [128, PAD + S], F32) for i in range(2)]
                        S1 = [MC.sb(f"S1{i}", [128, PAD + S], F32) for i in range(2)]
                        S2 = [MC.sb(f"S2{i}", [128, PAD + S], F32) for i in range(2)]
                        PT = [MC.sb(f"PT{i}", [128, S], BF16) for i in range(2)]
                        Bv = B("pool_small")
                        Bwi = [B(f"pwi{i}") for i in range(2)]
                        BV = [B(f"pV{i}") for i in range(2)]
                        BS1 = [B(f"pS1{i}") for i in range(2)]
                        BS2 = [B(f"pS2{i}") for i in range(2)]
                        BPT = [B(f"pPT{i}") for i in range(2)]
                        P.dma("sp", lambda e: e.dma_start(out=vec[:], in_=vec_d), writes=[Bv])
                        P.dma("pool", lambda e: e.dma_start(
                            out=wgr[:].rearrange("p a b o -> p (a b) o"),
                            in_=w_grp.rearrange("a (b p) o -> p (a b) o", p=128)), writes=[Bv])
                        for i in range(2):
                            P.op("dve", lambda e: e.memset(V_[i][:, 0:PAD], 0.0), writes=[BV[i]])
                            P.op("dve", lambda e: e.memset(S1[i][:, 0:PAD], 0.0), writes=[BS1[i]])
                            P.op("dve", lambda e: e.memset(S2[i][:, 0:PAD], 0.0), writes=[BS2[i]])

                        def load_w(g):
                            i2 = g % 2
                            P.dma("pool", lambda e: e.dma_start(
                                out=wi[i2][:], in_=w_in[:, g * 128:(g + 1) * 128].rearrange("(c p) n -> p c n", p=128)),
                                writes=[Bwi[i2]])

                        load_w(0)
                        for g in range(NDC):
                            i2 = g % 2
                            grp = g // 2
                            win = WINS[grp]
                            if g + 1 < NDC:
                                load_w(g + 1)
                            for tq in range(4):
                                pb = (g % 2) * 4 + tq

                                def mm(e):
                                    r = None
                                    for dc in range(NDC):
                                        r = e.matmul(psum[pb][:], lhsT=wi[i2][:, dc, :],
                                                     rhs=uT[:, dc, tq * 512:(tq + 1) * 512],
                                                     start=(dc == 0), stop=(dc == NDC - 1))
                                    return r
                                P.op("pe", mm, reads=[Bwi[i2], BuT[tq]], writes=[Bps[pb]])
                                P.op("act", lambda e: e.copy(out=V_[i2][:, PAD + tq * 512:PAD + (tq + 1) * 512],
                                                             in_=psum[pb][:]), reads=[Bps[pb]], writes=[BV[i2]])
                            cur, Bcur = V_[i2], BV[i2]
                            w = 1
                            nxt = [(S1[i2], BS1[i2]), (S2[i2], BS2[i2])]
                            k = 0
                            while w < win:
                                dst, Bdst = nxt[k % 2]
                                P.op("dve", lambda e: e.tensor_tensor(out=dst[:, PAD:PAD + S], in0=cur[:, PAD:PAD + S],
                                                                      in1=cur[:, PAD - w:PAD - w + S], op=ALU.add),
                                     reads=[Bcur], writes=[Bdst])
                                cur, Bcur = dst, Bdst
                                w *= 2
                                k += 1
                            P.op("dve", lambda e: e.scalar_tensor_tensor(
                                out=PT[i2][:], in0=cur[:, PAD:PAD + S], scalar=1.0 / win, in1=V_[i2][:, PAD:PAD + S],
                                op0=ALU.mult, op1=ALU.subtract), reads=[Bcur, BV[i2]], writes=[BPT[i2]])
                            P.op("dve", lambda e: e.tensor_tensor(out=cur[:, PAD:PAD + 16], in0=cur[:, PAD:PAD + 16],
                                                                  in1=vec[:, 8 + grp * 16:8 + (grp + 1) * 16],
                                                                  op=ALU.mult), reads=[Bcur, Bv], writes=[Bcur])
                            P.op("dve", lambda e: e.tensor_tensor(out=PT[i2][:, 0:16], in0=cur[:, PAD:PAD + 16],
                                                                  in1=V_[i2][:, PAD:PAD + 16], op=ALU.subtract),
                                 reads=[Bcur, BV[i2]], writes=[BPT[i2]])
                            if g % 2 == 1:
                                for oc in range(2):
                                    go = grp * 2 + oc
                                    for tq in range(4):
                                        pb = oc * 4 + tq

                                        def mg(e):
                                            r = None
                                            for ic in range(2):
                                                r = e.matmul(psum[pb][:], lhsT=wgr[:, grp, ic, oc * 128:(oc + 1) * 128],
                                                             rhs=PT[ic][:, tq * 512:(tq + 1) * 512],
                                                             start=(ic == 0), stop=(ic == 1))
                                            return r
                                        P.op("pe", mg, reads=[BPT[0], BPT[1], Bv], writes=[Bps[pb]])
                                        P.op("act", lambda e: e.activation(
                                            out=yT[:, go, tq * 512:(tq + 1) * 512], in_=psum[pb][:], func=AF.Copy,
                                            scale=vec[:, go:go + 1]), reads=[Bps[pb], Bv], writes=[ByT[go]])
                        P.barrier()
                out_proj(l, yT, ByT)


        def sb_layer(l):
            w_qkv, negm_d = L[l, "w_qkv"], L[l, "negm"]
            NH = 16
            with ExitStack() as ls:
                LC = Ctx(nc, ls)
                QT = LC.sb("QT", [128, NDC, S], BF16)
                KT = LC.sb("KT", [128, NDC, S], BF16)
                BQ = [[B(f"QT{hd}_{qg}") for qg in range(4)] for hd in range(NH)]
                BK = [B(f"KT{g}") for g in range(NDC)]
                Bvd = [B(f"vst{c}") for c in range(NCH)]
                with ExitStack() as us:
                    UC = Ctx(nc, us)
                    uT = UC.sb("uT", [128, NDC, S], BF16)
                    BuT = [B(f"uTq{i}") for i in range(4)]
                    norm_to_uT(l, uT, BuT)
                    with ExitStack() as ms:
                        MC = Ctx(nc, ms)
                        wq = [MC.sb(f"wq{i}", [128, NDC, 128], BF16) for i in range(2)]
                        wv = MC.sb("wv", [128, NDC, 512], BF16)
                        vst = [MC.sb(f"vst{i}", [128, 512], BF16) for i in range(2)]
                        Bwq = [B(f"wq{i}") for i in range(2)]
                        Bwv = B("wv")
                        Bvs = [B(f"vstage{i}") for i in range(2)]

                        def load_wq(j):
                            i2 = j % 2
                            P.dma("pool", lambda e: e.dma_start(
                                out=wq[i2][:], in_=w_qkv[:, j * 128:(j + 1) * 128].rearrange("(c p) n -> p c n", p=128)),
                                writes=[Bwq[i2]])
                        load_wq(0)
                        for j in range(16):
                            i2 = j % 2
                            if j + 1 < 16:
                                load_wq(j + 1)
                            g = j % 8
                            for tq in range(4):
                                pb = (j % 2) * 4 + tq

                                def mm(e):
                                    r = None
                                    for dc in range(NDC):
                                        r = e.matmul(psum[pb][:], lhsT=wq[i2][:, dc, :],
                                                     rhs=uT[:, dc, tq * 512:(tq + 1) * 512],
                                                     start=(dc == 0), stop=(dc == NDC - 1))
                                    return r
                                P.op("pe", mm, reads=[Bwq[i2], BuT[tq]], writes=[Bps[pb]])
                                if j < 8:
                                    P.op("act", lambda e: e.activation(out=QT[:, g, tq * 512:(tq + 1) * 512],
                                                                       in_=psum[pb][:], func=AF.Copy, scale=0.125),
                                         reads=[Bps[pb]], writes=[BQ[2 * g][tq], BQ[2 * g + 1][tq]])
                                else:
                                    P.op("dve", lambda e: e.tensor_copy(out=KT[:, g, tq * 512:(tq + 1) * 512],
                                                                        in_=psum[pb][:]),
                                         reads=[Bps[pb]], writes=[BK[g]])
                        for half in range(2):
                            P.dma("pool", lambda e: e.dma_start(
                                out=wv[:], in_=w_qkv[:, 2 * D + half * 512:2 * D + (half + 1) * 512].rearrange(
                                    "(c p) n -> p c n", p=128)), writes=[Bwv])
                            for c in range(NCH):
                                pb = c % 8
                                i2 = c % 2

                                def mv(e):
                                    r = None
                                    for dc in range(NDC):
                                        r = e.matmul(psum[pb][:], lhsT=uT[:, dc, c * 128:(c + 1) * 128],
                                                     rhs=wv[:, dc, :], start=(dc == 0), stop=(dc == NDC - 1))
                                    return r
                                P.op("pe", mv, reads=[Bwv, BuT[c // 4]], writes=[Bps[pb]])
                                if c % 2 == 0:
                                    P.op("act", lambda e: e.copy(out=vst[i2][:], in_=psum[pb][:]),
                                         reads=[Bps[pb]], writes=[Bvs[i2]])
                                else:
                                    P.op("dve", lambda e: e.tensor_copy(out=vst[i2][:], in_=psum[pb][:]),
                                         reads=[Bps[pb]], writes=[Bvs[i2]])
                                P.dma("sp", lambda e: e.dma_start(
                                    out=ust_d[c * 128:(c + 1) * 128, half * 512:(half + 1) * 512], in_=vst[i2][:]),
                                    reads=[Bvs[i2]], writes=[Bvd[c]])
                        P.barrier()
                with ExitStack() as ms:
                    MC = Ctx(nc, ms)
                    Vt = MC.sb("Vt", [128, NCH, D], BF16)
                    BV = B("Vt")
                    for c in range(NCH):
                        P.dma("sp", lambda e: e.dma_start(out=Vt[:, c, :], in_=ust_d[c * 128:(c + 1) * 128, :]),
                              reads=[Bvd[c]], writes=[BV])
                    negm = MC.sb("negm", [128, 4, 512], BF16)
                    ntri = MC.sb("ntri", [128, 128], BF16)
                    nones = MC.sb("nones", [128, 128], BF16)
                    identb = MC.sb("identb2", [128, 128], BF16)
                    Bc2 = B("sb_consts")
                    P.dma("pool", lambda e: e.dma_start(out=negm[:].rearrange("p a b -> p (a b)"), in_=negm_d),
                          writes=[Bc2])
                    P.op("dve", lambda e: e.tensor_scalar(out=ntri[:], in0=cst[:, 384:512], scalar1=-1.0, scalar2=None,
                                                          op0=ALU.mult), reads=[Bcst], writes=[Bc2])
                    P.op("dve", lambda e: e.tensor_scalar(out=nones[:], in0=ones, scalar1=-1.0, scalar2=None,
                                                          op0=ALU.mult), reads=[Bcst], writes=[Bc2])
                    P.op("dve", lambda e: e.tensor_copy(out=identb[:], in_=ident), reads=[Bcst], writes=[Bc2])
                    NR = 3
                    Lb = [MC.sb(f"Lb{i}", [128, 1024], BF16) for i in range(NR)]
                    AT = [MC.sb(f"AT{i}", [128, 1024], BF16) for i in range(NR)]
                    Et = [MC.sb(f"Et{i}", [128, 1024], F32) for i in range(2)]
                    Lacc = MC.sb("Lacc", [128, 512], F32)
                    Laccb = [MC.sb(f"Laccb{i}", [128, 512], BF16) for i in range(2)]
                    BLb = [B(f"Lb{i}") for i in range(NR)]
                    BAT = [B(f"AT{i}") for i in range(NR)]
                    BEt = [B(f"Et{i}") for i in range(2)]
                    BLacc = B("Lacc")
                    BLaccb = [B(f"Laccb{i}") for i in range(2)]
                    pairs = []
                    for hd in range(NH):
                        for qg in range(4):
                            nkb = 4 * qg + 4
                            for kb in range(nkb - 1, 0, -2):
                                pairs.append(dict(hd=hd, qg=qg, kb=kb, first=(kb == nkb - 1), last=(kb == 1),
                                                  grp=hd * 4 + qg))
                    nacc = [0]

                    def xpair(i):
                        x0 = (i % 3) * 2
                        return x0, psum_all[:, x0 * 512:(x0 + 2) * 512]

                    def stage_a(i, b):
                        hd, qg, kb = b["hd"], b["qg"], b["kb"]
                        g, pbase = hd // 2, (hd % 2) * 64
                        x0, xp = xpair(i)

                        def mz(e):
                            r = None
                            for t in range(2):
                                k_ = kb - t
                                r = e.matmul(psum[x0 + t][:], lhsT=KT[pbase:pbase + 64, g, k_ * 128:(k_ + 1) * 128],
                                             rhs=QT[pbase:pbase + 64, g, qg * 512:(qg + 1) * 512], start=True,
                                             stop=True, skip_group_check=True)
                                if k_ >= 4 * qg:
                                    r = e.matmul(psum[x0 + t][:], lhsT=identb[:], rhs=negm[:, k_ - 4 * qg, :],
                                                 start=False, stop=True, skip_group_check=True)
                            return r
                        P.op("pe", mz, reads=[BK[g], BQ[hd][qg], Bc2], writes=[Bps[x0], Bps[x0 + 1]])
                        P.op("act", lambda e: e.activation(out=Et[i % 2][:], in_=xp, func=AF.Exp),
                             reads=[Bps[x0], Bps[x0 + 1]], writes=[BEt[i % 2]])
                        P.op("act", lambda e: e.activation(out=Lb[i % NR][:], in_=Et[i % 2][:], func=AF.Ln,
                                                           bias=1.0, scale=1.0),
                             reads=[BEt[i % 2]], writes=[BLb[i % NR]])

                    def stage_b(i, b):
                        hd, qg, kb = b["hd"], b["qg"], b["kb"]
                        x0, xp = xpair(i)
                        first, last = b["first"], b["last"]
                        la = nacc[0] % 2
                        L0, L1 = Lb[i % NR][:, 0:512], Lb[i % NR][:, 512:1024]

                        def m2(e):
                            r = e.matmul(psum[x0][:], lhsT=ntri[:], rhs=L0, start=False, stop=True,
                                         skip_group_check=True)
                            if not first:
                                r = e.matmul(psum[x0][:], lhsT=nones[:], rhs=Laccb[la][:], start=False, stop=True,
                                             skip_group_check=True)
                            r = e.matmul(psum[x0 + 1][:], lhsT=ntri[:], rhs=L1, start=False, stop=True,
                                         skip_group_check=True)
                            r = e.matmul(psum[x0 + 1][:], lhsT=nones[:], rhs=L0, start=False, stop=True,
                                         skip_group_check=True)
                            if not first:
                                r = e.matmul(psum[x0 + 1][:], lhsT=nones[:], rhs=Laccb[la][:], start=False, stop=True,
                                             skip_group_check=True)
                            return r
                        P.op("pe", m2, reads=[BLb[i % NR], Bc2] + ([] if first else [BLaccb[la]]),
                             writes=[Bps[x0], Bps[x0 + 1]])
                        P.op("act", lambda e: e.activation(out=AT[i % NR][:], in_=xp, func=AF.Exp),
                             reads=[Bps[x0], Bps[x0 + 1]], writes=[BAT[i % NR]])
                        if not last:
                            if first:
                                P.op("dve", lambda e: e.tensor_tensor(out=Lacc[:], in0=L0, in1=L1, op=ALU.add),
                                     reads=[BLb[i % NR]], writes=[BLacc])
                            else:
                                P.op("dve", lambda e: e.tensor_tensor(out=Lacc[:], in0=Lacc[:], in1=L0, op=ALU.add),
                                     reads=[BLb[i % NR], BLacc], writes=[BLacc])
                                P.op("dve", lambda e: e.tensor_tensor(out=Lacc[:], in0=Lacc[:], in1=L1, op=ALU.add),
                                     reads=[BLb[i % NR], BLacc], writes=[BLacc])
                            nacc[0] += 1
                            lb2 = nacc[0] % 2
                            P.op("dve", lambda e: e.tensor_copy(out=Laccb[lb2][:], in_=Lacc[:]),
                                 reads=[BLacc], writes=[BLaccb[lb2]])

                    def stage_c(i, b):
                        hd, qg, kb = b["hd"], b["qg"], b["kb"]
                        g, pbase = hd // 2, (hd % 2) * 64
                        ob = 6 + b["grp"] % 2
                        first, last = b["first"], b["last"]

                        def mo(e):
                            r = e.matmul(psum[ob][:], lhsT=Vt[:, kb, g * 128:(g + 1) * 128],
                                         rhs=AT[i % NR][:, 0:512], start=first, stop=False, skip_group_check=True)
                            r = e.matmul(psum[ob][:], lhsT=Vt[:, kb - 1, g * 128:(g + 1) * 128],
                                         rhs=AT[i % NR][:, 512:1024], start=False, stop=last, skip_group_check=True)
                            return r
                        P.op("pe", mo, reads=[BV, BAT[i % NR]], writes=[Bps[ob]])
                        if last:
                            P.op("act", lambda e: e.copy(out=QT[pbase:pbase + 64, g, qg * 512:(qg + 1) * 512],
                                                         in_=psum[ob][pbase:pbase + 64, :]),
                                 reads=[Bps[ob]], writes=[BQ[hd][qg]])

                    nb = len(pairs)
                    for i in range(nb + 2):
                        if i < nb:
                            stage_a(i, pairs[i])
                        if 1 <= i <= nb:
                            stage_b(i - 1, pairs[i - 1])
                        if i >= 2:
                            stage_c(i - 2, pairs[i - 2])
                    P.barrier()
                out_proj(l, QT, [BQ[hd][qg] for hd in range(NH) for qg in range(4)])


        for l in layers:
            if cfg["mix"][l]:
                [lru_layer, pool_layer, sb_layer][l % 3](l)
            if cfg["moe"][l]:
                for c in range(NCH):
                    P.dma("sp", lambda e: e.dma_start(out=hsp_d[c * 128:(c + 1) * 128, :], in_=h[:, c, :]),
                          reads=[Bh[c]], writes=[Bhsp[c]])
                moe_A(l)
                hst["stack"].close()
                moe_B(l)
                hst["stack"] = ExitStack()
                hst["n"] += 1
                h = hst["stack"].enter_context(nc.sbuf_tensor(f"h_res{hst['n']}", [128, NCH, D], F32))
                for c in range(NCH):
                    P.dma("sp", lambda e: e.dma_start(out=h[:, c, :], in_=hsp_d[c * 128:(c + 1) * 128, :]),
                          reads=[Bhsp[c]], writes=[Bh[c]])
                moe_C(l)

        if cfg.get("final", False):
            fcol = 8 * D
            with ExitStack() as ms:
                MC = Ctx(nc, ms)
                gam = MC.sb("gamf", [128, D], F32)
                junk = MC.sb("junkf", [128, D], F32)
                P.dma("sp", lambda e: e.dma_start(out=gam[:], in_=nrm_d[:, fcol:fcol + D]), writes=[B("gamf")])
                for c in range(NCH):
                    par = c % 2
                    Br = rms_rstd(c, par, junk, B("junkf"))
                    P.op("dve", lambda e: e.scalar_tensor_tensor(
                        out=h[:, c, :], in0=h[:, c, :], scalar=rstd[:, par:par + 1],
                        in1=gam[:], op0=ALU.mult, op1=ALU.mult),
                        reads=[Bh[c], Br, B("gamf")], writes=[Bh[c]])
                P.barrier()
        Bout = B("out_d")
        for c in range(NCH):
            P.dma("sp", lambda e: e.dma_start(out=out_d[c * 128:(c + 1) * 128, :], in_=h[:, c, :]),
                  reads=[Bh[c]], writes=[Bout])
        P.barrier()
        hst["stack"].close()
        print("instructions:", P.n_instr, {k: v for k, v in P.ecnt.items()})
    return nc


def host_consts():
    ident = np.eye(128, dtype=np.float32)
    tri = np.triu(np.ones((128, 128), np.float32), 1)
    ones = np.ones((128, 128), np.float32)
    lowi = np.tril(np.ones((128, 128), np.float32))
    cst = np.concatenate([ident, tri, ones, lowi], axis=1)
    ecap = np.tile((np.arange(NE, dtype=np.float32) * CAP)[None, None, :], (128, NCH, 1)).reshape(128, NCH * NE)
    return np.ascontiguousarray(cst), np.ascontiguousarray(ecap)


def host_slotc():
    p = np.arange(128, dtype=np.float32)[:, None, None]
    e = np.arange(NE, dtype=np.float32)[None, :, None]
    sc = np.arange(3, dtype=np.float32)[None, None, :]
    rowb = 2.0 * (e * CAP + sc * 128 + p)
    spos = np.broadcast_to(sc * 128 + p, (128, NE, 3))
    return np.ascontiguousarray(np.stack([rowb, spos], axis=1).reshape(128, 2 * NE * 3).astype(np.float32))


def layer_inputs(inputs, l):
    m = {}
    m[f"w_router{l}"] = np.ascontiguousarray(inputs["moe_w_router"][l])
    m[f"b_router{l}"] = np.ascontiguousarray(np.broadcast_to(inputs["moe_b_router"][l][None, :], (128, NE)))
    m[f"w_gu{l}"] = np.ascontiguousarray(inputs["moe_w_gate_up"][l])
    bgu = inputs["moe_b_gate_up"][l].reshape(NE, 16, 128).transpose(2, 0, 1).reshape(128, NE * 16)
    m[f"b_gu{l}"] = np.ascontiguousarray(bgu)
    m[f"w_dn{l}"] = np.ascontiguousarray(inputs["moe_w_down"][l])
    m[f"b_dn{l}"] = np.ascontiguousarray(inputs["moe_b_down"][l])
    return m


def mixer_inputs(inputs, l):
    kind, slot = l % 3, l // 3
    m = {}
    if kind == 0:
        m[f"lru_w_in{l}"] = np.ascontiguousarray(inputs["lru_w_in"][slot])
        rows = [inputs["lru_conv_w"][slot][k] for k in range(4)] + [
            inputs["lru_conv_b"][slot], inputs["lru_b_a"][slot], inputs["lru_b_x"][slot], inputs["lru_a_param"][slot]]
        v = np.stack(rows, axis=-1).reshape(8, 128, 8).transpose(1, 0, 2).reshape(128, 64)
        m[f"lru_vec{l}"] = np.ascontiguousarray(v.astype(np.float32))
        m[f"lru_w_a{l}"] = np.ascontiguousarray(inputs["lru_w_a"][slot])
        m[f"lru_w_x{l}"] = np.ascontiguousarray(inputs["lru_w_x"][slot])
        m[f"mix_w_out{l}"] = np.ascontiguousarray(inputs["lru_w_out"][slot])
    elif kind == 1:
        m[f"pool_w_in{l}"] = np.ascontiguousarray(inputs["pool_w_in"][slot])
        m[f"pool_w_grp{l}"] = np.ascontiguousarray(inputs["pool_w_group"][slot])
        sc = inputs["pool_scale"][slot].reshape(8, 128).T
        corr = np.ones((4, 16), np.float32)
        for gi, w in enumerate((2, 4, 8, 16)):
            t = np.arange(16)
            corr[gi] = 1.0 / np.minimum(t + 1, w)
        v = np.concatenate([sc, np.broadcast_to(corr.reshape(1, 64), (128, 64))], axis=1)
        m[f"pool_vec{l}"] = np.ascontiguousarray(v.astype(np.float32))
        m[f"mix_w_out{l}"] = np.ascontiguousarray(inputs["pool_w_out"][slot])
    else:
        m[f"sb_w_qkv{l}"] = np.ascontiguousarray(inputs["sb_w_qkv"][slot])
        m[f"mix_w_out{l}"] = np.ascontiguousarray(inputs["sb_w_out"][slot])
        nm = np.zeros((128, 4, 512), np.float32)
        sl = np.arange(128)[:, None]
        tl = np.arange(512)[None, :]
        for mi in range(4):
            nm[:, mi, :] = np.where(mi * 128 + sl < tl, 0.0, -30000.0)
        m[f"sb_negmask{l}"] = np.ascontiguousarray(nm.reshape(128, 2048))
    return m


def norms_input(inputs):
    rows = [inputs["mix_norm"][i] for i in range(4)] + [inputs["ffn_norm"][i] for i in range(4)] + [inputs["final_norm"]]
    v = np.concatenate(rows).astype(np.float32)
    return np.ascontiguousarray(np.broadcast_to(v[None, :], (128, 9 * D)))


N_CORES = 8


def kernel(**inputs):
    inputs = {k: np.asarray(v) for k, v in inputs.items()}
    cfg = dict(layers=[0, 1, 2, 3], moe=[True] * 4, mix=[True] * 4, final=True)
    nc = bass.Bass("TRN2", target_bir_lowering=False)
    build(nc, cfg)
    cst, ecap = host_consts()
    shared = {"cst": cst, "ecap": ecap, "slotc": host_slotc(), "norms": norms_input(inputs)}
    for l in range(4):
        shared.update(layer_inputs(inputs, l))
        shared.update(mixer_inputs(inputs, l))
    x = np.ascontiguousarray(inputs["x"].astype(np.float32))
    in_maps = []
    for c in range(N_CORES):
        m = dict(shared)
        m["x"] = x[c]
        in_maps.append(m)
    res = run_bass_kernel_spmd(nc, in_maps, core_ids=list(range(N_CORES)))
    out = np.stack([np.asarray(r["out"]) for r in res.results], axis=0)
    return out.astype(np.float32)
```
